# Optimizing a Trainium2 kernel written in Bass

```python
import math
import jax
import jax.numpy as jnp
from jax import lax
import numpy as np

D_MODEL = 1024
BATCH = 4
SEQ = 4096
DEPTH = 4

GRID_W = 64
CTX_LEN = 256
N_BRANCH = 4
BRANCH_W = D_MODEL // 2
HG_HEADS = 4
HG_DIM = BRANCH_W // HG_HEADS
HG_CHUNK = 64
DA_HEADS = 4
DA_QK_DIM = BRANCH_W // (2 * DA_HEADS)
DA_V_DIM = BRANCH_W // DA_HEADS
Q_BLOCK = 128
ROPE_BASE = 10000.0
ROPE_AXIS_DIM = DA_QK_DIM // 2
CV_WIDTH = 31
SC_WIDTH = 3
MOE_GROUPS = 4
MOE_EXPERTS_PER_GROUP = 8
MOE_EXPERTS = MOE_GROUPS * MOE_EXPERTS_PER_GROUP
MOE_TOP_K = 2
MOE_FF = D_MODEL // 2
MOE_ROW_BLOCK = 128
ADA_CHUNKS = 6
EPS = 1e-6
IN_SPLIT_WIDTHS = (BRANCH_W,) * 8 + (2 * BRANCH_W,) + (BRANCH_W,) * 3 + (D_MODEL,) * N_BRANCH
IN_WIDTH = 13 * BRANCH_W + N_BRANCH * D_MODEL

kernel_name = "hybrid_flow_backbone"


def rmsnorm(x, g):
    xf = x.astype(jnp.float32)
    y = xf * lax.rsqrt(jnp.mean(xf * xf, axis=-1, keepdims=True) + EPS)
    return (y * g.astype(jnp.float32)).astype(x.dtype)


def layernorm(x, g, b):
    xf = x.astype(jnp.float32)
    xc = xf - jnp.mean(xf, axis=-1, keepdims=True)
    y = xc * lax.rsqrt(jnp.mean(xc * xc, axis=-1, keepdims=True) + EPS)
    return (y * g.astype(jnp.float32) + b.astype(jnp.float32)).astype(x.dtype)


def depthwise_conv(x, w):
    k = w.shape[0]
    return lax.conv_general_dilated(
        x, w[:, None, :].astype(x.dtype), window_strides=(1,),
        padding=[((k - 1) // 2, (k - 1) // 2)],
        dimension_numbers=("NWC", "WIO", "NWC"), feature_group_count=x.shape[-1])


def hgrn_lower_bounds(logits):
    p = jax.nn.softmax(logits.astype(jnp.float32), axis=0)
    cum = jnp.cumsum(p, axis=0)
    return cum - cum[0:1]


def split_heads(a):
    b, t, _ = a.shape
    return a.astype(jnp.float32).reshape(b, t, HG_HEADS, HG_DIM).transpose(0, 2, 1, 3)


def hgrn_forget(f_pre, lb):
    log_f = jnp.logaddexp(jnp.log(lb), jnp.log1p(-lb) + jax.nn.log_sigmoid(f_pre))
    key = (1.0 - lb) * jax.nn.sigmoid(-f_pre)
    return log_f, key


def gla_chunk_scan(q, k, v, log_f, s0):
    bsz, heads, t_len, _ = q.shape
    dv = v.shape[-1]
    n_chunks = t_len // HG_CHUNK

    def to_chunks(a):
        return jnp.moveaxis(a.reshape(bsz, heads, n_chunks, HG_CHUNK, a.shape[-1]), 2, 0)

    lower = jnp.tril(jnp.ones((HG_CHUNK, HG_CHUNK), dtype=bool))[:, :, None]

    def step(state, chunk):
        qc, kc, vc, lf = chunk
        b = jnp.cumsum(lf, axis=-2)
        o = jnp.einsum("bhtd,bhde->bhte", qc * jnp.exp(b), state)
        rel = jnp.where(lower, b[..., :, None, :] - b[..., None, :, :], -jnp.inf)
        scores = jnp.einsum("bhtd,bhsd,bhtsd->bhts", qc, kc, jnp.exp(rel))
        o = o + jnp.einsum("bhts,bhse->bhte", scores, vc)
        b_end = b[..., -1:, :]
        state = (jnp.exp(b_end)[..., 0, :, None] * state
                 + jnp.einsum("bhsd,bhse->bhde", kc * jnp.exp(b_end - b), vc))
        return state, o

    state, o = lax.scan(step, s0, (to_chunks(q), to_chunks(k), to_chunks(v), to_chunks(log_f)))
    o = jnp.moveaxis(o, 0, 2).reshape(bsz, heads, t_len, dv)
    return o, state


def hgrn2_branch(parts_c, parts_l, lb, norm_g, ctx_out):
    q_c, i_c = split_heads(parts_c[0]), split_heads(parts_c[1])
    q_l, i_l = split_heads(parts_l[0]), split_heads(parts_l[1])
    s0 = jnp.zeros((q_l.shape[0], HG_HEADS, HG_DIM, HG_DIM), jnp.float32)
    outs_c, outs_l = [], []
    for d in range(2):
        lb_d = lb[d].reshape(HG_HEADS, 1, HG_DIM)
        lf_c, k_c = hgrn_forget(split_heads(parts_c[2 + d]), lb_d)
        lf_l, k_l = hgrn_forget(split_heads(parts_l[2 + d]), lb_d)
        seq_c = (q_c, k_c, i_c, lf_c)
        seq_l = (q_l, k_l, i_l, lf_l)
        if d == 1:
            seq_c = tuple(jnp.flip(a, axis=2) for a in seq_c)
            seq_l = tuple(jnp.flip(a, axis=2) for a in seq_l)
        o_c, s_ctx = gla_chunk_scan(*seq_c, s0)
        o_l, _ = gla_chunk_scan(*seq_l, s_ctx)
        if d == 1:
            o_c, o_l = jnp.flip(o_c, axis=2), jnp.flip(o_l, axis=2)
        outs_c.append(o_c)
        outs_l.append(o_l)

    def readout(o, g_pre):
        b, _, t, _ = o.shape
        y = rmsnorm(o, norm_g.reshape(HG_HEADS, 1, HG_DIM)).transpose(0, 2, 1, 3).reshape(b, t, BRANCH_W)
        return (y * jax.nn.sigmoid(g_pre.astype(jnp.float32))).astype(g_pre.dtype)

    y_l = readout(outs_l[0] + outs_l[1], parts_l[4])
    y_c = readout(outs_c[0] + outs_c[1], parts_c[4]) if ctx_out else None
    return y_c, y_l


def axial_rope_tables(n_tokens, dtype):
    rows = n_tokens // GRID_W
    row = jnp.repeat(jnp.arange(rows, dtype=jnp.float32), GRID_W)
    col = jnp.tile(jnp.arange(GRID_W, dtype=jnp.float32), rows)
    inv_freq = ROPE_BASE ** (-jnp.arange(0, ROPE_AXIS_DIM, 2, dtype=jnp.float32) / ROPE_AXIS_DIM)
    ang_r = row[:, None] * inv_freq
    ang_c = col[:, None] * inv_freq
    ang = jnp.concatenate([ang_r, ang_r, ang_c, ang_c], axis=-1)
    return jnp.cos(ang).astype(dtype), jnp.sin(ang).astype(dtype)


def apply_axial_rope(x, cos, sin):
    r1, r2, c1, c2 = jnp.split(x, 4, axis=-1)
    rot = jnp.concatenate([-r2, r1, -c2, c1], axis=-1)
    return x * cos[None, :, None, None, :] + rot * sin[None, :, None, None, :]


def diff_attn_branch(parts_c, parts_l, lam_vec, norm_g, lambda_init, ctx_out):
    bsz, n_lat, _ = parts_l[0].shape

    def qk_heads(a):
        return a.reshape(a.shape[0], a.shape[1], DA_HEADS, 2, DA_QK_DIM)

    def v_heads(a):
        return a.reshape(a.shape[0], a.shape[1], DA_HEADS, DA_V_DIM)

    cos, sin = axial_rope_tables(n_lat, parts_l[0].dtype)
    q_l = apply_axial_rope(qk_heads(parts_l[0]), cos, sin)
    k_l = apply_axial_rope(qk_heads(parts_l[1]), cos, sin)
    k_c, v_c = qk_heads(parts_c[1]), v_heads(parts_c[2])
    k_all = jnp.concatenate([k_c, k_l], axis=1)
    v_all = jnp.concatenate([v_c, v_heads(parts_l[2])], axis=1)

    lv = lam_vec.astype(jnp.float32)
    lam = jnp.exp(jnp.sum(lv[0] * lv[1])) - jnp.exp(jnp.sum(lv[2] * lv[3])) + lambda_init
    scale = DA_QK_DIM ** -0.5

    def attend(q, k, v):
        s = jnp.einsum("bqhcd,bkhcd->bhcqk", q, k, preferred_element_type=jnp.float32) * scale
        p = jax.nn.softmax(s, axis=-1)
        a = p[:, :, 0] - lam * p[:, :, 1]
        return jnp.einsum("bhqk,bkhe->bqhe", a.astype(v.dtype), v)

    def head_out(o):
        y = rmsnorm(o, norm_g) * (1.0 - lambda_init)
        return y.reshape(o.shape[0], o.shape[1], BRANCH_W)

    n_blocks = n_lat // Q_BLOCK
    q_blocks = jnp.moveaxis(q_l.reshape(bsz, n_blocks, Q_BLOCK, DA_HEADS, 2, DA_QK_DIM), 1, 0)
    o_l = lax.map(lambda qb: attend(qb, k_all, v_all), q_blocks)
    o_l = jnp.moveaxis(o_l, 0, 1).reshape(bsz, n_lat, DA_HEADS, DA_V_DIM)
    y_l = head_out(o_l)
    y_c = head_out(attend(qk_heads(parts_c[0]), k_c, v_c)) if ctx_out else None
    return y_c, y_l


def conformer_conv_branch(u, w_dw, b_dw, ln_g, ln_b):
    a, gate = jnp.split(u, 2, axis=-1)
    v = a * jax.nn.sigmoid(gate)
    v = depthwise_conv(v, w_dw) + b_dw.astype(v.dtype)
    return jax.nn.silu(layernorm(v, ln_g, ln_b))


def short_conv_branch(b_gate, c_gate, v, w):
    return b_gate * depthwise_conv(c_gate * v, w)


def merge_branches(ys, gate_pres, w_branch_l, w_out_l):
    m = jax.nn.sigmoid(gate_pres[0]) * (ys[0] @ w_branch_l[0])
    for k in range(1, N_BRANCH):
        m = m + jax.nn.sigmoid(gate_pres[k]) * (ys[k] @ w_branch_l[k])
    return m @ w_out_l


def token_mixer(h_l, h_c, layer, ctx_out, w_in_l, w_branch_l, w_out_l, lb_l, hg_g, da_lam, da_g,
                cv_w, cv_b, cv_lg, cv_lb, sc_w_l):
    points = np.cumsum(IN_SPLIT_WIDTHS)[:-1].tolist()
    p_l = jnp.split(h_l @ w_in_l, points, axis=-1)
    p_c = jnp.split(h_c @ w_in_l, points, axis=-1)
    lambda_init = 0.8 - 0.6 * math.exp(-0.3 * layer)
    hg_c, hg_l = hgrn2_branch(p_c[0:5], p_l[0:5], lb_l, hg_g, ctx_out)
    da_c, da_l = diff_attn_branch(p_c[5:8], p_l[5:8], da_lam, da_g, lambda_init, ctx_out)

    def local_branches(p):
        return (conformer_conv_branch(p[8], cv_w, cv_b, cv_lg, cv_lb),
                short_conv_branch(p[9], p[10], p[11], sc_w_l))

    cv_l, sc_l = local_branches(p_l)
    out_l = merge_branches((hg_l, da_l, cv_l, sc_l), p_l[12:16], w_branch_l, w_out_l)
    if not ctx_out:
        return None, out_l
    cv_c, sc_c = local_branches(p_c)
    out_c = merge_branches((hg_c, da_c, cv_c, sc_c), p_c[12:16], w_branch_l, w_out_l)
    return out_c, out_l


def moe_ffn(t, w_grp, b_grp, w_exp, b_exp, w_gate, w_up, w_down):
    n, d = t.shape
    grp_prob = jax.nn.softmax(jnp.matmul(t, w_grp, preferred_element_type=jnp.float32)
                              + b_grp.astype(jnp.float32), axis=-1)
    p_grp, grp = lax.top_k(grp_prob, 1)
    exp_logits = (jnp.matmul(t, w_exp, preferred_element_type=jnp.float32)
                  + b_exp.astype(jnp.float32)).reshape(n, MOE_GROUPS, MOE_EXPERTS_PER_GROUP)
    in_grp = exp_logits[jnp.arange(n), grp[:, 0]]
    top_logit, top_idx = lax.top_k(in_grp, MOE_TOP_K)
    w_top = jax.nn.softmax(top_logit, axis=-1) * p_grp
    expert = grp * MOE_EXPERTS_PER_GROUP + top_idx

    n_assign = n * MOE_TOP_K
    flat_e = expert.reshape(-1)
    flat_w = w_top.reshape(-1)
    flat_tok = jnp.repeat(jnp.arange(n, dtype=jnp.int32), MOE_TOP_K)
    counts = jax.ops.segment_sum(jnp.ones_like(flat_e), flat_e, num_segments=MOE_EXPERTS)
    padded = (counts + MOE_ROW_BLOCK - 1) // MOE_ROW_BLOCK * MOE_ROW_BLOCK
    pad_end = jnp.cumsum(padded)
    pad_start = pad_end - padded
    raw_start = jnp.cumsum(counts) - counts
    order = jnp.argsort(flat_e)
    e_sorted = flat_e[order]
    dest = pad_start[e_sorted] + jnp.arange(n_assign, dtype=jnp.int32) - raw_start[e_sorted]
    n_blocks = -(-n_assign // MOE_ROW_BLOCK) + MOE_EXPERTS
    n_rows = n_blocks * MOE_ROW_BLOCK
    row_tok = jnp.full((n_rows,), n, jnp.int32).at[dest].set(flat_tok[order])
    row_w = jnp.zeros((n_rows,), jnp.float32).at[dest].set(flat_w[order])
    blk_e = jnp.minimum(jnp.searchsorted(pad_end, jnp.arange(n_blocks, dtype=jnp.int32) * MOE_ROW_BLOCK,
                                         side="right"), MOE_EXPERTS - 1)
    t_pad = jnp.concatenate([t, jnp.zeros((1, d), t.dtype)], axis=0)

    def expert_block(args):
        tok, e = args
        xb = t_pad[tok]
        hb = jax.nn.silu(xb @ w_gate[e]) * (xb @ w_up[e])
        return hb @ w_down[e]

    y = lax.map(expert_block, (row_tok.reshape(n_blocks, MOE_ROW_BLOCK), blk_e))
    y = y.reshape(n_rows, d).astype(jnp.float32) * row_w[:, None]
    out = jnp.zeros((n + 1, d), jnp.float32).at[row_tok].add(y)[:n]
    return out.astype(t.dtype)


def setup_inputs(seed: int = 0) -> dict:
    key = jax.random.key(seed)
    ks = jax.random.split(key, 32)
    D, W = D_MODEL, BRANCH_W

    def nrm(i, shape, scale):
        return jax.random.normal(ks[i], shape, jnp.float32) * scale

    return {
        "x": nrm(0, (BATCH, SEQ, D), 1.0),
        "c": nrm(1, (BATCH, D), 1.0),
        "ctx": nrm(2, (BATCH, CTX_LEN, D), 1.0),
        "c_ctx": nrm(3, (D,), 1.0),
        "ada_w": nrm(4, (DEPTH, D, ADA_CHUNKS * D), 0.5 * D ** -0.5),
        "ada_b": nrm(5, (DEPTH, ADA_CHUNKS * D), 0.02),
        "norm1_g": 1.0 + nrm(6, (DEPTH, D), 0.05),
        "norm2_g": 1.0 + nrm(7, (DEPTH, D), 0.05),
        "w_in": nrm(8, (DEPTH, D, IN_WIDTH), D ** -0.5),
        "w_branch": nrm(9, (DEPTH, N_BRANCH, W, D), W ** -0.5),
        "w_out": nrm(10, (DEPTH, D, D), D ** -0.5),
        "hg_lb_logits": nrm(11, (DEPTH, 2, W), 0.5),
        "hg_norm_g": 1.0 + nrm(12, (DEPTH, W), 0.05),
        "da_lambda": nrm(13, (DEPTH, 4, DA_QK_DIM), 0.1),
        "da_norm_g": 1.0 + nrm(14, (DEPTH, DA_V_DIM), 0.05),
        "cv_dw_w": nrm(15, (DEPTH, CV_WIDTH, W), CV_WIDTH ** -0.5),
        "cv_dw_b": nrm(16, (DEPTH, W), 0.02),
        "cv_ln_g": 1.0 + nrm(17, (DEPTH, W), 0.05),
        "cv_ln_b": nrm(18, (DEPTH, W), 0.02),
        "sc_w": nrm(19, (DEPTH, SC_WIDTH, W), SC_WIDTH ** -0.5),
        "moe_w_grp": nrm(20, (DEPTH, D, MOE_GROUPS), D ** -0.5),
        "moe_b_grp": nrm(21, (DEPTH, MOE_GROUPS), 0.01),
        "moe_w_exp": nrm(22, (DEPTH, D, MOE_EXPERTS), D ** -0.5),
        "moe_b_exp": nrm(23, (DEPTH, MOE_EXPERTS), 0.01),
        "moe_w_gate": nrm(24, (DEPTH, MOE_EXPERTS, D, MOE_FF), D ** -0.5),
        "moe_w_up": nrm(25, (DEPTH, MOE_EXPERTS, D, MOE_FF), D ** -0.5),
        "moe_w_down": nrm(26, (DEPTH, MOE_EXPERTS, MOE_FF, D), MOE_FF ** -0.5),
        "final_g": 1.0 + nrm(27, (D,), 0.05),
    }


def reference(x, c, ctx, c_ctx, ada_w, ada_b, norm1_g, norm2_g, w_in, w_branch, w_out,
              hg_lb_logits, hg_norm_g, da_lambda, da_norm_g, cv_dw_w, cv_dw_b, cv_ln_g, cv_ln_b,
              sc_w, moe_w_grp, moe_b_grp, moe_w_exp, moe_b_exp, moe_w_gate, moe_w_up, moe_w_down,
              final_g):
    lower_bounds = hgrn_lower_bounds(hg_lb_logits)
    s_lat = jax.nn.silu(c)[:, None, :]
    s_ctx = jax.nn.silu(c_ctx)
    bsz, n_lat, d = x.shape
    xc = ctx
    for layer in range(DEPTH):
        ctx_out = layer < DEPTH - 1
        mod_l = jnp.split(s_lat @ ada_w[layer] + ada_b[layer], ADA_CHUNKS, axis=-1)
        mod_c = jnp.split(s_ctx @ ada_w[layer] + ada_b[layer], ADA_CHUNKS, axis=-1)
        h_l = rmsnorm(x, norm1_g[layer]) * (1.0 + mod_l[1]) + mod_l[0]
        h_c = rmsnorm(xc, norm1_g[layer]) * (1.0 + mod_c[1]) + mod_c[0]
        y_c, y_l = token_mixer(h_l, h_c, layer, ctx_out, w_in[layer], w_branch[layer], w_out[layer],
                               lower_bounds[layer], hg_norm_g[layer], da_lambda[layer], da_norm_g[layer],
                               cv_dw_w[layer], cv_dw_b[layer], cv_ln_g[layer], cv_ln_b[layer], sc_w[layer])
        x = x + mod_l[2] * y_l
        h_l = rmsnorm(x, norm2_g[layer]) * (1.0 + mod_l[4]) + mod_l[3]
        moe_args = (moe_w_grp[layer], moe_b_grp[layer], moe_w_exp[layer], moe_b_exp[layer],
                    moe_w_gate[layer], moe_w_up[layer], moe_w_down[layer])
        if ctx_out:
            xc = xc + mod_c[2] * y_c
            h_c = rmsnorm(xc, norm2_g[layer]) * (1.0 + mod_c[4]) + mod_c[3]
            f = moe_ffn(jnp.concatenate([h_l.reshape(-1, d), h_c.reshape(-1, d)], axis=0), *moe_args)
            x = x + mod_l[5] * f[: bsz * n_lat].reshape(x.shape)
            xc = xc + mod_c[5] * f[bsz * n_lat:].reshape(xc.shape)
        else:
            x = x + mod_l[5] * moe_ffn(h_l.reshape(-1, d), *moe_args).reshape(x.shape)
    return rmsnorm(x, final_g)
```

```python
import contextlib
import math
import numpy as np
import concourse.bass as bass
import concourse.mybir as mybir
from concourse.bass_utils import run_bass_kernel_spmd

F32 = mybir.dt.float32
BF = mybir.dt.bfloat16
U32 = mybir.dt.uint32
AF = mybir.ActivationFunctionType
ALU = mybir.AluOpType
AX = mybir.AxisListType

D = 1024
NCTX = 256
NLAT = 4096
T = NCTX + NLAT
NCH = T // 128
NB = 2 * T // 128 + 32
SPARSE = True
DEPTH = 4
W = 512
INW = 10752
EPS = 1e-6
TILES = [(0, 256)] + [(256 + 512 * i, 512) for i in range(8)]
C_HQ, C_HI, C_HFF, C_HFB, C_HG = 0, 512, 1024, 1536, 2048
C_DQ, C_DK, C_DV = 2560, 3072, 3584
C_CVA, C_CVG = 4096, 4608
C_SB, C_SC, C_SX = 5120, 5632, 6144
C_GATE = 6656


class Trk:
    __slots__ = ("w", "r")

    def __init__(self):
        self.w = None
        self.r = {}


class Tl:
    def __init__(self, h, trk=None):
        self.h = h
        self.t = trk or Trk()

    def __getitem__(self, k):
        return self.h[k]


class Stream:
    def __init__(self, name):
        self.name = name
        self.ops = []
        self.seen = {}
        self.sem = None
        self.cnt = 0
        self.dslots = []
        self.dnext = 0


class Sch:
    SEM_MAX = 30000

    def __init__(self, nc, es):
        self.nc = nc
        self.es = es
        self.st = {k: Stream(k) for k in ("pe", "act", "dve", "pool", "sp")}
        self.nsem = 0
        for k, s in self.st.items():
            self._newsem(s)
        for k, n in (("sp", 24), ("pool", 12), ("act", 6)):
            s = self.st[k]
            for i in range(n):
                s.dslots.append([self._sem(), 0])

    def _sem(self):
        self.nsem += 1
        return self.es.enter_context(self.nc.semaphore("s%d" % self.nsem))

    def _newsem(self, s):
        s.sem = self._sem()
        s.cnt = 0

    def _need(self, s, tok, waits):
        if tok is None:
            return
        sem, val, src = tok
        if src == "pe" and s.name == "pe":
            return
        if s.seen.get(id(sem), 0) >= val:
            return
        k = id(sem)
        if k not in waits or waits[k][1] < val:
            waits[k] = (sem, val)

    def _deps(self, s, reads, writes):
        waits = {}
        for b in reads:
            self._need(s, b.t.w, waits)
        for b in writes:
            self._need(s, b.t.w, waits)
            for tok in b.t.r.values():
                self._need(s, tok, waits)
        for k, (sem, val) in waits.items():
            s.seen[k] = val
        return list(waits.values())

    def _mark(self, tok, reads, writes):
        for b in reads:
            b.t.r[id(tok[0])] = tok
        for b in writes:
            b.t.w = tok
            b.t.r = {}

    def op(self, eng, fn, reads=(), writes=()):
        s = self.st[eng]
        if s.cnt >= self.SEM_MAX:
            self._newsem(s)
        waits = self._deps(s, reads, writes)
        s.cnt += 1
        tok = (s.sem, s.cnt, eng)
        s.ops.append((waits, fn, (s.sem, 1)))
        self._mark(tok, reads, writes)
        return tok

    def dma(self, q, out, in_, reads=(), writes=(), **kw):
        return self.dma_fn(q, (lambda e: e.dma_start(out=out, in_=in_, **kw)), reads, writes)

    def dma_fn(self, q, fn, reads=(), writes=()):
        s = self.st[q]
        slot = s.dslots[s.dnext % len(s.dslots)]
        s.dnext += 1
        waits = self._deps(s, reads, writes)
        if slot[1] > 0 and s.seen.get(id(slot[0]), 0) < slot[1]:
            waits.append((slot[0], slot[1]))
            s.seen[id(slot[0])] = slot[1]
        slot[1] += 16
        tok = (slot[0], slot[1], "dma")
        s.ops.append((waits, fn, (slot[0], 16)))
        self._mark(tok, reads, writes)
        return tok

    def barrier(self):
        toks = []
        for s in self.st.values():
            if s.cnt > 0:
                toks.append((s.sem, s.cnt))
            for sl in s.dslots:
                if sl[1] > 0:
                    toks.append((sl[0], sl[1]))
        for s in self.st.values():
            waits = []
            for sem, val in toks:
                if sem is s.sem:
                    continue
                if s.seen.get(id(sem), 0) < val:
                    waits.append((sem, val))
                    s.seen[id(sem)] = val
            if waits:
                s.ops.append((waits, None, None))

    def emit(self):
        nc = self.nc
        self.barrier()
        with nc.Block() as block:
            def run(s):
                def f(e):
                    for waits, fn, inc in s.ops:
                        for sem, val in waits:
                            e.wait_ge(sem, val)
                        if fn is not None:
                            fn(e).then_inc(inc[0], inc[1])
                return f
            block.tensor(run(self.st["pe"]))
            block.scalar(run(self.st["act"]))
            block.vector(run(self.st["dve"]))
            block.gpsimd(run(self.st["pool"]))
            block.sync(run(self.st["sp"]))


class Prog:
    def __init__(self, nc, n_layers=DEPTH, dbg=None, nexp=32):
        self.nc = nc
        self.nexp = nexp
        self.n_layers = n_layers
        self.dbg = dbg or {}

    def sb(self, es, name, shape, dt):
        self.uid = getattr(self, "uid", 0) + 1
        return Tl(es.enter_context(self.nc.sbuf_tensor("%s_u%d" % (name, self.uid), list(shape), dt)))

    def dram(self, name, shape, dt, kind="Internal"):
        return self.nc.dram_tensor(name, list(shape), dt, kind=kind).ap()

    def mm(self, out, lhsT, rhs, start, stop, r, w):
        self.S.op("pe", lambda e: e.matmul(out, lhsT=lhsT, rhs=rhs, start=start, stop=stop), r, w)

    def tr(self, out, in_, ident, r, w):
        self.S.op("pe", lambda e: e.transpose(out=out, in_=in_, identity=ident), r, w)

    def act(self, out, in_, func, r, w, **kw):
        self.S.op("act", lambda e: e.activation(out=out, in_=in_, func=func, **kw), r, w)

    def V(self, eng, name, r, w, *a, **kw):
        self.S.op(eng, lambda e: getattr(e, name)(*a, **kw), r, w)

    def psb(self):
        b = self.banks[self.bi % 8]
        self.bi += 1
        return b

    def build(self):
        nc = self.nc
        L = self.n_layers
        i = {}
        def inp(name, shape, dt=F32):
            i[name] = self.dram(name, shape, dt, kind="ExternalInput")
        inp("x", [NLAT, D]); inp("c", [1, D]); inp("ctx", [NCTX, D]); inp("c_ctx", [1, D])
        inp("ada_w", [L, D, 6 * D]); inp("ada_b", [DEPTH, 6 * D])
        inp("norm1_g", [DEPTH, D]); inp("norm2_g", [DEPTH, D])
        inp("w_in", [L, D, INW]); inp("w_branch", [L, 4, W, D]); inp("w_out", [L, D, D])
        inp("hg_lb", [DEPTH, 2, W]); inp("hg_norm_g", [DEPTH, W])
        inp("da_lambda", [DEPTH, 256]); inp("da_norm_g", [DEPTH, 128])
        inp("cv_dw_w", [DEPTH, 31, W]); inp("cv_dw_b", [DEPTH, W]); inp("cv_ln_g", [DEPTH, W]); inp("cv_ln_b", [DEPTH, W])
        inp("sc_w", [DEPTH, 3, W])
        inp("moe_w_r", [DEPTH, D, 36]); inp("moe_b_r", [DEPTH, 36])
        inp("moe_w_gate", [L, self.nexp, D, W]); inp("moe_w_up", [L, self.nexp, D, W]); inp("moe_w_down", [L, self.nexp, W, D])
        inp("final_g", [1, D]); inp("ltri", [128, 128])
        inp("cst", [128, 1056]); inp("rope", [2, 128, T])
        self.i = i
        self.out = self.dram("out", [NLAT, D], F32, kind="ExternalOutput")
        self.X = self.dram("Xs", [T, D], F32)
        self.HT = self.dram("HTs", [8, 128, T], BF)
        self.Y = self.dram("Ys", [4, 4, 128, T], BF)
        self.MT = self.dram("MTs", [8, 128, T], BF)
        self.MODR = self.dram("MODs", [2, 128, 6 * D], F32)
        self.H2 = self.dram("H2s", [T + 1, D], BF)
        self.ROWS = self.dram("ROWSs", [NB * 128, 4], F32)
        self.ACC2 = self.dram("ACC2s", [2 * T, D], F32)
        self.H2t = Trk(); self.ROWSt = Trk(); self.ACC2t = Trk()
        self.Xt = [Trk() for _ in range(NCH)]
        self.HTt = [Trk() for _ in range(9)]
        self.Yt = [[Trk() for _ in range(9)] for _ in range(4)]
        self.MTt = [Trk() for _ in range(9)]
        self.MODt = Trk()
        self.outt = Trk()
        dbg_out = {}
        for k, shp in self.dbg.items():
            if not isinstance(shp, tuple):
                continue
            dbg_out[k] = self.dram("dbg_" + k, shp[0], shp[1], kind="ExternalOutput")
        self.dbg_out = dbg_out

        with contextlib.ExitStack() as es:
            self.S = S = Sch(nc, es)
            self.banks = [Tl(es.enter_context(nc.psum_tensor("pb%d" % k, [128, 512], F32))) for k in range(8)]
            self.bi = 0
            self.cst = self.sb(es, "cst_sb", [128, 1056], F32)
            S.dma("sp", self.cst[:], i["cst"], [], [self.cst])
            self.identb = self.sb(es, "identb", [128, 128], BF)
            self.onesb = self.sb(es, "onesb", [128, 128], BF)
            self.Rb = self.sb(es, "Rb", [128, 128], BF)
            self.identf = Tl(self.cst.h, self.cst.t)
            self.V("dve", "tensor_copy", [self.cst], [self.identb], out=self.identb[:], in_=self.cst[:, 0:128])
            self.V("dve", "tensor_copy", [self.cst], [self.onesb], out=self.onesb[:], in_=self.cst[:, 128:256])
            self.V("dve", "tensor_copy", [self.cst], [self.Rb], out=self.Rb[:], in_=self.cst[:, 256:384])
            self.sT = []
            for which, src in enumerate((i["c"], i["c_ctx"])):
                cT = self.sb(es, "cT%d" % which, [128, 8], F32)
                S.dma("sp", cT[:], src.rearrange("o (kc p) -> p (o kc)", p=128), [], [cT], allow_slow_non_contiguous=True)
                sg = self.sb(es, "cS%d" % which, [128, 8], F32)
                self.act(sg[:], cT[:], AF.Silu, [cT], [sg])
                rep = self.sb(es, "sT%d" % which, [128, 8, 128], F32)
                self.V("dve", "tensor_copy", [sg], [rep], out=rep[:], in_=sg[:].unsqueeze(2).to_broadcast([128, 8, 128]))
                self.sT.append(rep)
            self.P = self.sb(es, "Pparams", [128, 600], F32)
            self.RW = self.sb(es, "RW", [128, NCH, 32], F32)
            self.RK = self.sb(es, "RK", [128, NCH, 32], F32)
            self.Asum = self.sb(es, "Asum", [128, 32], F32)
            self.ltri = self.sb(es, "ltri_sb", [128, 128], F32)
            S.dma("sp", self.ltri[:], i["ltri"], [], [self.ltri])
            zr = self.sb(es, "zrow", [1, D], BF)
            self.V("dve", "memset", [], [zr], zr[:], 0.0)
            S.dma("sp", self.H2[T:T + 1, :], zr[:], [zr], [Tl(None, self.H2t)])
            self.LB = self.sb(es, "LB", [128, DEPTH, 8], F32)
            self.OML = self.sb(es, "OML", [128, DEPTH, 8], F32)
            lbe = self.sb(es, "lbe", [128, DEPTH, 8], F32)
            lbt = self.sb(es, "lbt", [128, 16], F32)
            for l_ in range(DEPTH):
                for d_ in range(2):
                    for j_ in range(4):
                        S.dma("sp", lbe[:, l_, d_ * 4 + j_:d_ * 4 + j_ + 1], i["hg_lb"][l_, d_:d_ + 1, j_ * 128:(j_ + 1) * 128].rearrange("o p -> p o"),
                              [], [lbe], allow_slow_non_contiguous=True)
            self.act(lbe[:], lbe[:], AF.Exp, [lbe], [lbe])
            self.V("dve", "tensor_tensor", [lbe], [lbt], out=lbt[:, 0:8], in0=lbe[:, 0, :], in1=lbe[:, 1, :], op=ALU.add)
            self.V("dve", "tensor_tensor", [lbe, lbt], [lbt], out=lbt[:, 0:8], in0=lbt[:, 0:8], in1=lbe[:, 2, :], op=ALU.add)
            self.V("dve", "tensor_tensor", [lbe, lbt], [lbt], out=lbt[:, 0:8], in0=lbt[:, 0:8], in1=lbe[:, 3, :], op=ALU.add)
            self.V("dve", "reciprocal", [lbt], [lbt], out=lbt[:, 8:16], in_=lbt[:, 0:8])
            self.V("dve", "memset", [], [self.LB], self.LB[:], 0.0)
            for l_ in range(1, DEPTH):
                self.V("dve", "tensor_tensor", [lbe, lbt], [lbe], out=lbe[:, l_, :], in0=lbe[:, l_, :], in1=lbt[:, 8:16], op=ALU.mult)
                self.V("dve", "tensor_tensor", [lbe, self.LB], [self.LB], out=self.LB[:, l_, :], in0=self.LB[:, l_ - 1, :], in1=lbe[:, l_, :], op=ALU.add)
            self.V("dve", "tensor_scalar", [self.LB], [self.OML], out=self.OML[:], in0=self.LB[:], scalar1=-1.0, scalar2=1.0, op0=ALU.mult, op1=ALU.add)
            self.seg = self.sb(es, "seg", [128, 512], F32)
            self.V("dve", "memset", [], [self.seg], self.seg[:], 1.0)
            self.V("dve", "memset", [self.seg], [self.seg], self.seg[:].rearrange("p (c s) -> p c s", s=64)[:, :, 0:1], 0.0)
            S.barrier()
            S.dma("sp", self.X[0:NCTX, :], i["ctx"], [], self._xt(0, 2))
            for q in range(4):
                S.dma("sp", self.X[NCTX + q * 1024: NCTX + (q + 1) * 1024, :], i["x"][q * 1024:(q + 1) * 1024, :], [],
                      self._xt(2 + q * 8, 8))
            for l in range(L):
                self.layer(l)
            self.final_norm()
            S.emit()

    def _xt(self, c0, n):
        return [Tl(None, t) for t in self.Xt[c0:c0 + n]]

    def layer(self, l):
        stop = self.dbg.get("stop")
        self.phase_mod(l)
        self.load_params(l)
        self.phase_norm(l, 1)
        if stop == "norm1":
            return
        self.phase_sconv(l)
        self.phase_conv(l)
        if stop == "convs":
            return self.dump_dbg()
        self.phase_attn(l)
        if stop == "attn":
            return self.dump_dbg()
        self.phase_hgrn(l)
        if stop == "hgrn":
            return self.dump_dbg()
        self.phase_merge(l)
        if stop == "merge":
            return self.dump_dbg()
        self.phase_norm(l, 2)
        if SPARSE:
            self.phase_moe_sparse(l)
        else:
            self.phase_moe(l)
        if stop == "moe":
            return self.dump_dbg()

    def dump_dbg(self):
        S = self.S
        if "y" in self.dbg_out:
            S.dma("sp", self.dbg_out["y"], self.Y, [Tl(None, t) for k in range(4) for t in self.Yt[k]], [Tl(None, Trk())])
        if "x" in self.dbg_out:
            S.dma("sp", self.dbg_out["x"], self.X, [Tl(None, t) for t in self.Xt], [Tl(None, Trk())])

    def load_w(self, es, name, l, c0, ncols):
        w = self.sb(es, name, [128, 8, ncols], BF)
        self.S.dma("pool", w[:], self.i["w_in"][l, :, c0:c0 + ncols].rearrange("(kc p) n -> p kc n", p=128), [], [w])
        return w

    def load_ht(self, buf, ti):
        t0, N = TILES[ti]
        self.S.dma("sp", buf[:, :, 0:N], self.HT[:, :, t0:t0 + N].rearrange("k p n -> p k n"), [Tl(None, self.HTt[ti])], [buf])

    def proj(self, ps, w, j0, ht, N, width=128):
        for kc in range(8):
            self.mm(ps[0:width, 0:N], w[:, kc, j0:j0 + width], ht[:, kc, 0:N], kc == 0, kc == 7, [w, ht], [ps])

    def rstd_from(self, src_ap, scale, out_t, tmp_t, N, r):
        self.V("dve", "tensor_scalar", r, [tmp_t], out=tmp_t[:, 0:N], in0=src_ap, scalar1=scale, scalar2=EPS, op0=ALU.mult, op1=ALU.add)
        self.V("dve", "reciprocal", [tmp_t], [tmp_t], out=tmp_t[:, 0:N], in_=tmp_t[:, 0:N])
        self.act(out_t[:, 0:N], tmp_t[:, 0:N], AF.Sqrt, [tmp_t], [out_t])

    def load_params(self, l):
        S = self.S
        i = self.i
        P = self.P
        nc = True
        def ld(dst, src):
            S.dma("sp", dst, src, [], [P], allow_slow_non_contiguous=True)
        for j in range(4):
            sl = slice(j * 128, (j + 1) * 128)
            ld(P[:, j * 3:j * 3 + 3], i["sc_w"][l][:, sl].rearrange("k p -> p k"))
            ld(P[:, 12 + j * 31:12 + (j + 1) * 31], i["cv_dw_w"][l][:, sl].rearrange("k p -> p k"))
            ld(P[:, 136 + j:137 + j], i["cv_dw_b"][l:l + 1, sl].rearrange("o p -> p o"))
            ld(P[:, 140 + j:141 + j], i["cv_ln_g"][l:l + 1, sl].rearrange("o p -> p o"))
            ld(P[:, 144 + j:145 + j], i["cv_ln_b"][l:l + 1, sl].rearrange("o p -> p o"))
            ld(P[:, 148 + j:149 + j], i["hg_norm_g"][l:l + 1, sl].rearrange("o p -> p o"))
        ld(P[:, 152:153], i["da_norm_g"][l:l + 1, :].rearrange("o p -> p o"))
        S.dma("sp", P[:, 160:416], i["da_lambda"][l:l + 1, :].partition_broadcast(128), [], [P])
        S.dma("sp", P[:, 416:452], i["moe_b_r"][l:l + 1, :].partition_broadcast(128), [], [P])
        lam_init = 0.8 - 0.6 * math.exp(-0.3 * l)
        self.V("dve", "tensor_tensor", [P], [P], out=P[:, 460:524], in0=P[:, 160:224], in1=P[:, 224:288], op=ALU.mult)
        self.V("dve", "tensor_tensor", [P], [P], out=P[:, 524:588], in0=P[:, 288:352], in1=P[:, 352:416], op=ALU.mult)
        self.V("dve", "tensor_reduce", [P], [P], out=P[:, 155:157], in_=P[:, 460:588].rearrange("p (a b) -> p a b", a=2), axis=AX.X, op=ALU.add)
        self.act(P[:, 157:159], P[:, 155:157], AF.Exp, [P], [P])
        self.V("dve", "tensor_tensor", [P], [P], out=P[:, 159:160], in0=P[:, 158:159], in1=P[:, 157:158], op=ALU.subtract)
        self.V("dve", "tensor_scalar", [P], [P], out=P[:, 153:154], in0=P[:, 159:160], scalar1=-lam_init, scalar2=1.0, op0=ALU.add, op1=ALU.mult)
        self.V("dve", "tensor_scalar", [P], [P], out=P[:, 154:155], in0=P[:, 152:153], scalar1=1.0 - lam_init, scalar2=0.0, op0=ALU.mult, op1=ALU.add)

    def phase_mod(self, l):
        S = self.S
        i = self.i
        modt = Tl(None, self.MODt)
        with contextlib.ExitStack() as es:
            wt = [self.sb(es, "adaw%d" % k, [128, 8, 512], F32) for k in range(2)]
            bt = [self.sb(es, "adab%d" % k, [1, 512], F32) for k in range(2)]
            ot = [self.sb(es, "adao%d" % k, [128, 512], F32) for k in range(2)]
            n = 0
            for blk in range(12):
                w = wt[blk % 2]
                b = bt[blk % 2]
                S.dma("sp", w[:], i["ada_w"][l, :, blk * 512:(blk + 1) * 512].rearrange("(kc p) n -> p kc n", p=128), [], [w])
                S.dma("sp", b[:], i["ada_b"][l:l + 1, blk * 512:(blk + 1) * 512], [], [b])
                for which in range(2):
                    ps = self.psb()
                    for kc in range(8):
                        self.mm(ps[:], self.sT[which][:, kc, :], w[:, kc, :], kc == 0, False, [self.sT[which], w], [ps])
                    self.mm(ps[:], self.cst[0:1, 128:256], b[0:1, :], False, True, [self.cst, b], [ps])
                    o = ot[n % 2]
                    n += 1
                    self.V("dve", "tensor_copy", [ps], [o], out=o[:], in_=ps[:])
                    S.dma("sp", self.MODR[which, :, blk * 512:(blk + 1) * 512], o[:], [o], [modt])
            S.barrier()

    def phase_norm(self, l, which):
        S = self.S
        i = self.i
        gsrc = i["norm1_g"] if which == 1 else i["norm2_g"]
        sh, sc = (0, 1) if which == 1 else (3, 4)
        modt = Tl(None, self.MODt)
        with contextlib.ExitStack() as es:
            A = [self.sb(es, "nA%d" % k, [128, D], F32) for k in range(2)]
            B = [self.sb(es, "nB%d" % k, [128, D], F32) for k in range(2)]
            g = self.sb(es, "ng", [128, D], F32)
            S.dma("sp", g[:], gsrc[l:l + 1, :].partition_broadcast(128), [], [g])
            for k in range(2):
                S.dma("sp", A[k][:], self.MODR[k, :, sc * D:(sc + 1) * D], [modt], [A[k]])
                S.dma("sp", B[k][:], self.MODR[k, :, sh * D:(sh + 1) * D], [modt], [B[k]])
                self.V("dve", "scalar_tensor_tensor", [A[k], g], [A[k]], out=A[k][:], in0=A[k][:], scalar=1.0, in1=g[:],
                       op0=ALU.add, op1=ALU.mult)
            xs = [self.sb(es, "nx%d" % k, [128, D], F32) for k in range(3)]
            sq = self.sb(es, "nsq", [128, D], F32)
            t1 = [self.sb(es, "nt%d" % k, [128, D], F32) for k in range(2)]
            hb = [self.sb(es, "nhb%d" % k, [128, D], BF) for k in range(2)]
            st = [self.sb(es, "nst%d" % k, [128, 4], F32) for k in range(2)]
            hT = [self.sb(es, "nhT%d" % k, [128, 8, 512], BF) for k in range(2)]
            if which == 2:
                wr = self.sb(es, "nwr", [128, 8, 36], F32)
                S.dma("sp", wr[:], i["moe_w_r"][l].rearrange("(kc p) n -> p kc n", p=128), [], [wr])
                h32 = [self.sb(es, "nh32%d" % k, [128, D], F32) for k in range(2)]
                h32T = [self.sb(es, "nh32T%d" % k, [128, 8, 128], F32) for k in range(2)]
                Rt = [self.sb(es, "nR%d" % k, [128, 160], F32) for k in range(2)]
            for ti, (t0, N) in enumerate(TILES):
                ht = hT[ti % 2]
                for cc in range(N // 128):
                    c = t0 // 128 + cc
                    lat = 0 if c >= 2 else 1
                    x = xs[c % 3]
                    xt = Tl(None, self.Xt[c])
                    S.dma("sp", x[:], self.X[c * 128:(c + 1) * 128, :], [xt], [x])
                    s_ = st[c % 2]
                    self.act(sq[:], x[:], AF.Square, [x], [sq, s_], accum_out=s_[:, 0:1])
                    self.V("dve", "tensor_scalar", [s_], [s_], out=s_[:, 1:2], in0=s_[:, 0:1], scalar1=1.0 / D, scalar2=EPS,
                           op0=ALU.mult, op1=ALU.add)
                    self.V("dve", "reciprocal", [s_], [s_], out=s_[:, 2:3], in_=s_[:, 1:2])
                    self.act(s_[:, 3:4], s_[:, 2:3], AF.Sqrt, [s_], [s_])
                    t = t1[c % 2]
                    self.V("dve", "scalar_tensor_tensor", [x, s_, A[lat]], [t], out=t[:], in0=x[:], scalar=s_[:, 3:4],
                           in1=A[lat][:], op0=ALU.mult, op1=ALU.mult)
                    h = hb[c % 2]
                    self.V("pool", "tensor_tensor", [t, B[lat]], [h], out=h[:], in0=t[:], in1=B[lat][:], op=ALU.add)
                    ps = self.psb()
                    pv = ps[:].bitcast(BF).rearrange("p (k n) -> p k n", k=8)
                    for k in range(8):
                        self.tr(pv[:, k, :], h[:, k * 128:(k + 1) * 128], self.identb[:], [h, self.identb], [ps])
                    self.act(ht[:, :, cc * 128:(cc + 1) * 128], pv, AF.Copy, [ps], [ht])
                    if which == 2:
                        self.route(c, t, B[lat], h32[c % 2], h32T[c % 2], wr, Rt[c % 2])
                        if SPARSE:
                            S.dma("sp", self.H2[c * 128:(c + 1) * 128, :], h[:], [h], [Tl(None, self.H2t)])
                            self.rank(c, Rt[c % 2])
                S.dma("sp", self.HT[:, :, t0:t0 + N].rearrange("k p n -> p k n"), ht[:, :, 0:N], [ht], [Tl(None, self.HTt[ti])])
            S.barrier()
        if "h1" in self.dbg_out and l == self.dbg.get("layer", 0) and which == 1:
            S.dma("sp", self.dbg_out["h1"], self.HT, [Tl(None, t) for t in self.HTt], [Tl(None, Trk())])


    def phase_sconv(self, l):
        S = self.S
        P = self.P
        with contextlib.ExitStack() as es:
            wb = self.load_w(es, "scwb", l, C_SB, 512)
            wc = self.load_w(es, "scwc", l, C_SC, 512)
            wx = self.load_w(es, "scwx", l, C_SX, 512)
            ub = self.sb(es, "scu", [128, T + 3], F32)
            bb = self.sb(es, "scb", [128, T], F32)
            hts = [self.sb(es, "scht%d" % k, [128, 8, 512], BF) for k in range(2)]
            cs = [self.sb(es, "sccs%d" % k, [128, 512], F32) for k in range(2)]
            acc = [self.sb(es, "scacc%d" % k, [128, 512], F32) for k in range(2)]
            ys = [self.sb(es, "scy%d" % k, [128, 512], BF) for k in range(2)]
            self.V("pool", "memset", [], [ub], ub[:], 0.0)
            n = 0
            for j in range(4):
                for ti, (t0, N) in enumerate(TILES):
                    ht = hts[n % 2]
                    c_ = cs[n % 2]
                    n += 1
                    self.load_ht(ht, ti)
                    base = t0 + 1 if ti == 0 else t0 + 2
                    pb, pc, px = self.psb(), self.psb(), self.psb()
                    self.proj(pb, wb, j * 128, ht, N)
                    self.proj(pc, wc, j * 128, ht, N)
                    self.proj(px, wx, j * 128, ht, N)
                    self.act(c_[:, 0:N], pc[:, 0:N], AF.Copy, [pc], [c_])
                    self.V("dve", "tensor_tensor", [c_, px], [ub], out=ub[:, base:base + N], in0=c_[:, 0:N], in1=px[:, 0:N], op=ALU.mult)
                    self.act(bb[:, t0:t0 + N], pb[:, 0:N], AF.Copy, [pb], [bb])
                for ti, (t0, N) in enumerate(TILES):
                    base = t0 + 1 if ti == 0 else t0 + 2
                    a = acc[ti % 2]
                    y = ys[ti % 2]
                    self.V("dve", "tensor_scalar", [ub, P], [a], out=a[:, 0:N], in0=ub[:, base - 1:base - 1 + N], scalar1=P[:, j * 3:j * 3 + 1],
                           scalar2=0.0, op0=ALU.mult, op1=ALU.add)
                    for k in (1, 2):
                        self.V("dve", "scalar_tensor_tensor", [ub, P, a], [a], out=a[:, 0:N], in0=ub[:, base - 1 + k:base - 1 + k + N],
                               scalar=P[:, j * 3 + k:j * 3 + k + 1], in1=a[:, 0:N], op0=ALU.mult, op1=ALU.add)
                    self.V("pool", "tensor_tensor", [a, bb], [y], out=y[:, 0:N], in0=a[:, 0:N], in1=bb[:, t0:t0 + N], op=ALU.mult)
                    S.dma("sp", self.Y[3, j, :, t0:t0 + N], y[:, 0:N], [y], [Tl(None, self.Yt[3][ti])])
            S.barrier()

    def phase_conv(self, l):
        S = self.S
        P = self.P
        onesf = self.cst[:, 128:256]
        with contextlib.ExitStack() as es:
            wa = self.load_w(es, "cvwa", l, C_CVA, 512)
            wg = self.load_w(es, "cvwg", l, C_CVG, 512)
            vb = self.sb(es, "cvv", [128, 4, T + 45], BF)
            hts = [self.sb(es, "cvht%d" % k, [128, 8, 512], BF) for k in range(2)]
            sg = [self.sb(es, "cvsg%d" % k, [128, 512], F32) for k in range(2)]
            self.V("pool", "memset", [], [vb], vb[:], 0.0)
            n = 0
            for ti, (t0, N) in enumerate(TILES):
                ht = hts[ti % 2]
                self.load_ht(ht, ti)
                base = t0 + 15 if ti == 0 else t0 + 30
                for j in range(4):
                    pa, pg = self.psb(), self.psb()
                    self.proj(pa, wa, j * 128, ht, N)
                    self.proj(pg, wg, j * 128, ht, N)
                    s_ = sg[n % 2]
                    n += 1
                    self.act(s_[:, 0:N], pg[:, 0:N], AF.Sigmoid, [pg], [s_])
                    self.V("dve", "tensor_tensor", [s_, pa], [vb], out=vb[:, j, base:base + N], in0=pa[:, 0:N], in1=s_[:, 0:N], op=ALU.mult)
            ca = [self.sb(es, "cvca%d" % k, [128, 4, 512], F32) for k in range(2)]
            cp = self.sb(es, "cvcp", [128, 512], F32)
            sq = self.sb(es, "cvsq", [128, 4, 512], F32)
            mean = self.sb(es, "cvmean", [128, 512], F32)
            tmp = self.sb(es, "cvtmp", [128, 512], F32)
            rstd = self.sb(es, "cvrstd", [128, 512], F32)
            dd = [self.sb(es, "cvd%d" % k, [128, 512], F32) for k in range(2)]
            ys = [self.sb(es, "cvy%d" % k, [128, 4, 512], BF) for k in range(2)]
            for ti, (t0, N) in enumerate(TILES):
                base = t0 + 15 if ti == 0 else t0 + 30
                a = ca[ti % 2]
                for j in range(4):
                    wcol = lambda k: P[:, 12 + j * 31 + k:12 + j * 31 + k + 1]
                    src = lambda k: vb[:, j, base - 15 + k:base - 15 + k + N]
                    self.V("dve", "tensor_scalar", [vb, P], [a], out=a[:, j, 0:N], in0=src(0), scalar1=wcol(0), scalar2=P[:, 136 + j:137 + j],
                           op0=ALU.mult, op1=ALU.add)
                    for k in range(1, 31):
                        self.V("dve", "scalar_tensor_tensor", [vb, P, a], [a], out=a[:, j, 0:N], in0=src(k), scalar=wcol(k), in1=a[:, j, 0:N],
                               op0=ALU.mult, op1=ALU.add)
                    self.act(sq[:, j, 0:N], a[:, j, 0:N], AF.Square, [a], [sq])
                p1, p2 = self.psb(), self.psb()
                for j in range(4):
                    self.mm(p1[:, 0:N], onesf, a[:, j, 0:N], j == 0, j == 3, [self.cst, a], [p1])
                for j in range(4):
                    self.mm(p2[:, 0:N], onesf, sq[:, j, 0:N], j == 0, j == 3, [self.cst, sq], [p2])
                self.act(mean[:, 0:N], p1[:, 0:N], AF.Copy, [p1], [mean], scale=1.0 / W)
                self.V("pool", "tensor_tensor", [mean], [tmp], out=tmp[:, 0:N], in0=mean[:, 0:N], in1=mean[:, 0:N], op=ALU.mult)
                self.V("dve", "scalar_tensor_tensor", [p2, tmp], [tmp], out=tmp[:, 0:N], in0=p2[:, 0:N], scalar=1.0 / W, in1=tmp[:, 0:N],
                       op0=ALU.mult, op1=ALU.subtract)
                self.rstd_from(tmp[:, 0:N], 1.0, rstd, tmp, N, [tmp])
                y = ys[ti % 2]
                for j in range(4):
                    d = dd[j % 2]
                    self.V("dve", "tensor_tensor", [a, mean], [d], out=d[:, 0:N], in0=a[:, j, 0:N], in1=mean[:, 0:N], op=ALU.subtract)
                    self.V("pool", "tensor_tensor", [d, rstd], [d], out=d[:, 0:N], in0=d[:, 0:N], in1=rstd[:, 0:N], op=ALU.mult)
                    self.V("dve", "tensor_scalar", [d, P], [d], out=d[:, 0:N], in0=d[:, 0:N], scalar1=P[:, 140 + j:141 + j], scalar2=P[:, 144 + j:145 + j],
                           op0=ALU.mult, op1=ALU.add)
                    self.act(y[:, j, 0:N], d[:, 0:N], AF.Silu, [d], [y])
                S.dma("sp", self.Y[2, :, :, t0:t0 + N].rearrange("j p n -> p j n"), y[:, :, 0:N], [y], [Tl(None, self.Yt[2][ti])])
            S.barrier()

    def rope(self, ps, raw, cos, sin, t1, t2, dst_ap, dst_t, N, rot_ps):
        self.act(raw[:, 0:N], ps[:, 0:N], AF.Copy, [ps], [raw])
        self.mm(rot_ps[:, 0:N], self.Rb[:], raw[:, 0:N], True, True, [self.Rb, raw], [rot_ps])
        self.V("pool", "tensor_tensor", [raw, cos], [t1], out=t1[:, 0:N], in0=raw[:, 0:N], in1=cos[:, 0:N], op=ALU.mult)
        self.V("dve", "tensor_tensor", [rot_ps, sin], [t2], out=t2[:, 0:N], in0=rot_ps[:, 0:N], in1=sin[:, 0:N], op=ALU.mult)
        self.V("pool", "tensor_tensor", [t1, t2], [dst_t], out=dst_ap, in0=t1[:, 0:N], in1=t2[:, 0:N], op=ALU.add)

    def phase_attn(self, l):
        S = self.S
        P = self.P
        i = self.i
        onesf = self.cst[:, 128:256]
        B = self.banks
        with contextlib.ExitStack() as es:
            wq = self.load_w(es, "dawq", l, C_DQ, 512)
            wk = self.load_w(es, "dawk", l, C_DK, 512)
            wv = self.load_w(es, "dawv", l, C_DV, 512)
            KT = self.sb(es, "daKT", [128, 4, T], BF)
            Vt = self.sb(es, "daV", [128, NCH, 512], BF)
            hts = [self.sb(es, "daht%d" % k, [128, 8, 512], BF) for k in range(2)]
            cos = [self.sb(es, "dacos%d" % k, [128, 512], F32) for k in range(2)]
            sin = [self.sb(es, "dasin%d" % k, [128, 512], F32) for k in range(2)]
            raw = [self.sb(es, "daraw%d" % k, [128, 512], BF) for k in range(2)]
            t1 = [self.sb(es, "dat1%d" % k, [128, 512], F32) for k in range(2)]
            t2 = [self.sb(es, "dat2%d" % k, [128, 512], F32) for k in range(2)]
            n = 0
            for ti, (t0, N) in enumerate(TILES):
                ht = hts[ti % 2]
                self.load_ht(ht, ti)
                S.dma("sp", cos[ti % 2][:, 0:N], i["rope"][0, :, t0:t0 + N], [], [cos[ti % 2]])
                S.dma("sp", sin[ti % 2][:, 0:N], i["rope"][1, :, t0:t0 + N], [], [sin[ti % 2]])
                for h in range(4):
                    ps, rp = self.psb(), self.psb()
                    self.proj(ps, wk, h * 128, ht, N)
                    self.rope(ps, raw[n % 2], cos[ti % 2], sin[ti % 2], t1[n % 2], t2[n % 2], KT[:, h, t0:t0 + N], KT, N, rp)
                    n += 1
                for cc in range(N // 128):
                    ps = self.psb()
                    for kc in range(8):
                        self.mm(ps[:, :], ht[:, kc, cc * 128:(cc + 1) * 128], wv[:, kc, :], kc == 0, kc == 7, [ht, wv], [ps])
                    self.act(Vt[:, t0 // 128 + cc, :], ps[:, :], AF.Copy, [ps], [Vt])
            QT = [self.sb(es, "daQT%d" % k, [128, 512], BF) for k in range(2)]
            QZ = [[self.sb(es, "daQZ%d_%d" % (k, c), [128, 512], BF) for c in range(2)] for k in range(2)]
            for k in range(2):
                for c in range(2):
                    self.V("pool", "memset", [], [QZ[k][c]], QZ[k][c][:], 0.0)
            pT = [self.sb(es, "dapT%d" % k, [128, 512], BF) for k in range(4)]
            rd = [self.sb(es, "dard%d" % k, [128, 512], F32) for k in range(2)]
            rp_ = [self.sb(es, "darp%d" % k, [128, 512], F32) for k in range(2)]
            rinv = self.sb(es, "darinv", [128, 512], F32)
            Oc = [self.sb(es, "daOc%d" % k, [128, 512], F32) for k in range(2)]
            o = self.sb(es, "dao", [128, 512], F32)
            sq = self.sb(es, "dasq", [128, 512], F32)
            tmp = self.sb(es, "datmp", [128, 512], F32)
            rstd = self.sb(es, "darstd", [128, 512], F32)
            ys = [self.sb(es, "day%d" % k, [128, 4, 512], BF) for k in range(2)]
            it = 0
            for ti, (t0, N) in enumerate(TILES):
                ht = hts[ti % 2]
                self.load_ht(ht, ti)
                S.dma("sp", cos[ti % 2][:, 0:N], i["rope"][0, :, t0:t0 + N], [], [cos[ti % 2]])
                S.dma("sp", sin[ti % 2][:, 0:N], i["rope"][1, :, t0:t0 + N], [], [sin[ti % 2]])
                nk = 2 if ti == 0 else NCH
                y = ys[ti % 2]
                for h in range(4):
                    qt = QT[h % 2]
                    self.proj(B[7], wq, h * 128, ht, N)
                    self.rope(B[7], raw[n % 2], cos[ti % 2], sin[ti % 2], t1[n % 2], t2[n % 2], qt[:, 0:N], qt, N, B[2])
                    n += 1
                    qz = QZ[h % 2]
                    self.V("pool", "tensor_copy", [qt], [qz[0]], out=qz[0][0:64, 0:N], in_=qt[0:64, 0:N])
                    self.V("dve", "tensor_copy", [qt], [qz[1]], out=qz[1][64:128, 0:N], in_=qt[64:128, 0:N])
                    its = [(c, kc) for c in range(2) for kc in range(nk)]
                    slots = {}
                    def qk(j):
                        c, kc = its[j]
                        sps = B[2 + it_base[0] % 3]
                        p = pT[it_base[0] % 4]
                        it_base[0] += 1
                        slots[j] = (sps, p)
                        p0 = c * 64
                        self.mm(sps[:, 0:N], KT[:, h, kc * 128:(kc + 1) * 128], qz[c][:, 0:N], True, True, [KT, qz[c]], [sps])
                    it_base = [it]
                    qk(0)
                    if len(its) > 1:
                        qk(1)
                    for j, (c, kc) in enumerate(its):
                        sps, p = slots.pop(j)
                        oacc = B[c]
                        self.act(p[:, 0:N], sps[:, 0:N], AF.Exp, [sps], [p], scale=0.125)
                        if j + 2 < len(its):
                            qk(j + 2)
                        self.mm(oacc[:, 0:N], Vt[:, kc, h * 128:(h + 1) * 128], p[:, 0:N], kc == 0, kc == nk - 1, [Vt, p], [oacc])
                        rsb = B[5 + c]
                        self.mm(rsb[:, 0:N], self.onesb[:], p[:, 0:N], kc == 0, kc == nk - 1, [self.onesb, p], [rsb])
                        if kc == nk - 1:
                            self.V("dve", "reciprocal", [rsb], [rinv], out=rinv[:, 0:N], in_=rsb[:, 0:N])
                            self.V("dve", "tensor_tensor", [oacc, rinv], [Oc[c]], out=Oc[c][:, 0:N], in0=oacc[:, 0:N], in1=rinv[:, 0:N], op=ALU.mult)
                    it = it_base[0]
                    self.V("dve", "scalar_tensor_tensor", [Oc[0], Oc[1], P], [o], out=o[:, 0:N], in0=Oc[1][:, 0:N], scalar=P[:, 153:154], in1=Oc[0][:, 0:N],
                           op0=ALU.mult, op1=ALU.add)
                    self.act(sq[:, 0:N], o[:, 0:N], AF.Square, [o], [sq])
                    self.mm(B[7][:, 0:N], onesf, sq[:, 0:N], True, True, [self.cst, sq], [B[7]])
                    self.rstd_from(B[7][:, 0:N], 1.0 / 128, rstd, tmp, N, [B[7]])
                    self.V("dve", "scalar_tensor_tensor", [o, rstd, P], [y], out=y[:, h, 0:N], in0=o[:, 0:N], scalar=P[:, 154:155], in1=rstd[:, 0:N],
                           op0=ALU.mult, op1=ALU.mult)
                S.dma("sp", self.Y[1, :, :, t0:t0 + N].rearrange("j p n -> p j n"), y[:, :, 0:N], [y], [Tl(None, self.Yt[1][ti])])
            S.barrier()

    def phase_hgrn(self, l):
        S = self.S
        P = self.P
        onesf = self.cst[:, 128:256]
        B = self.banks
        with contextlib.ExitStack() as es:
            OF = self.sb(es, "hgOF", [128, T], F32)
            S32 = self.sb(es, "hgS", [128, 128], F32)
            Sbf = [self.sb(es, "hgSb%d" % k, [128, 128], BF) for k in range(2)]
            hts = [self.sb(es, "hght%d" % k, [128, 8, 512], BF) for k in range(2)]
            wts = [[self.sb(es, "hgw%d_%d" % (a, k), [128, 8, 128], BF) for k in range(4)] for a in range(2)]
            def f32(name, n=2):
                return [self.sb(es, "%s%d" % (name, k), [128, 512], F32) for k in range(n)]
            def b16(name, n=2):
                return [self.sb(es, "%s%d" % (name, k), [128, 512], BF) for k in range(n)]
            q32, ee, ff, lf, kk, pre, bb, d3, d2 = (f32("hgq"), f32("hge"), f32("hgf"), f32("hglf"), f32("hgk"), f32("hgpre"),
                                                    f32("hgb"), f32("hgd3"), f32("hgd2"))
            E1, E2, E3, E4 = f32("hgE1"), f32("hgE2"), f32("hgE3"), f32("hgE4")
            d3a, E3b, E4b = f32("hgd3a"), f32("hgE3b"), f32("hgE4b")
            qE1, qE3, kE4, kE2 = b16("hgqE1"), b16("hgqE3"), b16("hgkE4"), b16("hgkE2")
            qE3b, kE4b = b16("hgqE3b"), b16("hgkE4b")
            amt = [self.sb(es, "hgamt%d" % k, [128, 128], F32) for k in range(2)]
            amt2 = [self.sb(es, "hgamu%d" % k, [128, 128], F32) for k in range(2)]
            iT = [self.sb(es, "hgiT%d" % k, [128, 4, 128], BF) for k in range(2)]
            kT = [self.sb(es, "hgkT%d" % k, [128, 4, 128], BF) for k in range(2)]
            AM = [self.sb(es, "hgAM%d" % k, [128, 128], BF) for k in range(2)]
            osum = self.sb(es, "hgo", [128, 512], F32)
            sq = self.sb(es, "hgsq", [128, 512], F32)
            tmp = self.sb(es, "hgtmp", [128, 512], F32)
            rstd = self.sb(es, "hgrstd", [128, 512], F32)
            sgg = self.sb(es, "hgsg", [128, 512], F32)
            y1 = self.sb(es, "hgy1", [128, 512], F32)
            ys = [self.sb(es, "hgy%d" % k, [128, 512], BF) for k in range(2)]
            rot = [2]
            def rb():
                b = B[rot[0]]
                rot[0] = 2 + (rot[0] - 1) % 6
                return b
            n = 0
            am_i = 0
            for h in range(4):
                for d in range(2):
                    w = wts[(h * 2 + d) % 2]
                    cols = (C_HQ, C_HFF if d == 0 else C_HFB, C_HI, C_HG)
                    for k in range(4):
                        S.dma("pool", w[k][:], self.i["w_in"][l, :, cols[k] + h * 128:cols[k] + (h + 1) * 128].rearrange("(kc p) n -> p kc n", p=128), [], [w[k]])
                    self.V("pool", "memset", [], [S32], S32[:], 0.0)
                    self.V("pool", "memset", [], [Sbf[0]], Sbf[0][:], 0.0)
                    cur = 0
                    lbc = self.LB[:, l, d * 4 + h:d * 4 + h + 1]
                    omc = self.OML[:, l, d * 4 + h:d * 4 + h + 1]
                    order = list(range(9)) if d == 0 else [0] + list(range(8, 0, -1))
                    mask = self.cst[:, 384:512] if d == 0 else self.cst[:, 512:640]
                    masko = self.cst[:, 640:768] if d == 0 else self.cst[:, 768:896]
                    for ti in order:
                        t0, N = TILES[ti]
                        nb = N // 128
                        nch = N // 64
                        z = n % 2
                        n += 1
                        ht = hts[z]
                        self.load_ht(ht, ti)
                        pq, pf = rb(), rb()
                        self.proj(pq, w[0], 0, ht, N)
                        self.proj(pf, w[1], 0, ht, N)
                        self.act(q32[z][:, 0:N], pq[:, 0:N], AF.Copy, [pq], [q32[z]])
                        self.act(ee[z][:, 0:N], pf[:, 0:N], AF.Exp, [pf], [ee[z]], scale=-1.0)
                        self.V("dve", "tensor_scalar", [ee[z]], [ee[z]], out=ee[z][:, 0:N], in0=ee[z][:, 0:N], scalar1=1.0, scalar2=1.0, op0=ALU.add, op1=ALU.mult)
                        self.V("dve", "reciprocal", [ee[z]], [ee[z]], out=ee[z][:, 0:N], in_=ee[z][:, 0:N])
                        self.V("dve", "tensor_scalar", [ee[z], self.LB, self.OML], [ff[z]], out=ff[z][:, 0:N], in0=ee[z][:, 0:N], scalar1=omc, scalar2=lbc,
                               op0=ALU.mult, op1=ALU.add)
                        self.act(lf[z][:, 0:N], ff[z][:, 0:N], AF.Ln, [ff[z]], [lf[z]])
                        self.V("pool", "tensor_scalar", [ff[z]], [kk[z]], out=kk[z][:, 0:N], in0=ff[z][:, 0:N], scalar1=-1.0, scalar2=1.0, op0=ALU.mult, op1=ALU.add)
                        self.V("dve", "tensor_tensor_scan", [self.seg, lf[z]], [pre[z]], out=pre[z][:, 0:N], data0=self.seg[:, 0:N], data1=lf[z][:, 0:N],
                               initial=0.0, op0=ALU.mult, op1=ALU.add)
                        v3 = lambda t_: t_[:, 0:N].rearrange("p (c s) -> p c s", s=64)
                        bc = lambda t_, col: v3(t_)[:, :, col:col + 1].to_broadcast([128, nch, 64])
                        if d == 0:
                            b_ = pre[z]
                            cend = 63
                        else:
                            b_ = bb[z]
                            cend = 0
                            self.V("dve", "tensor_tensor", [lf[z], pre[z]], [bb[z]], out=bb[z][:, 0:N], in0=lf[z][:, 0:N], in1=pre[z][:, 0:N], op=ALU.subtract)
                            self.V("dve", "tensor_tensor", [bb[z], pre[z]], [bb[z]], out=v3(bb[z]), in0=v3(bb[z]), in1=bc(pre[z], 63), op=ALU.add)
                        self.V("dve", "tensor_tensor", [b_], [d3[z]], out=v3(d3[z]), in0=v3(b_), in1=bc(b_, 32), op=ALU.subtract)
                        self.V("pool", "tensor_tensor", [b_], [d2[z]], out=v3(d2[z]), in0=v3(b_), in1=bc(b_, cend), op=ALU.subtract)
                        self.act(E1[z][:, 0:N], b_[:, 0:N], AF.Exp, [b_], [E1[z]])
                        self.act(E2[z][:, 0:N], d2[z][:, 0:N], AF.Exp, [d2[z]], [E2[z]], scale=-1.0)
                        v32 = lambda t_: t_[:, 0:N].rearrange("p (c s) -> p c s", s=32)
                        self.V("dve", "tensor_tensor", [b_], [d3a[z]], out=v32(d3a[z]), in0=v32(b_), in1=v32(b_)[:, :, 16:17].to_broadcast([128, 2 * nch, 32]), op=ALU.subtract)
                        self.act(E3[z][:, 0:N], d3a[z][:, 0:N], AF.Exp, [d3a[z]], [E3[z]])
                        self.act(E4[z][:, 0:N], d3a[z][:, 0:N], AF.Exp, [d3a[z]], [E4[z]], scale=-1.0)
                        self.act(E3b[z][:, 0:N], d3[z][:, 0:N], AF.Exp, [d3[z]], [E3b[z]])
                        self.act(E4b[z][:, 0:N], d3[z][:, 0:N], AF.Exp, [d3[z]], [E4b[z]], scale=-1.0)
                        self.V("dve", "tensor_tensor", [q32[z], E3b[z]], [qE3b[z]], out=qE3b[z][:, 0:N], in0=q32[z][:, 0:N], in1=E3b[z][:, 0:N], op=ALU.mult)
                        self.V("pool", "tensor_tensor", [kk[z], E4b[z]], [kE4b[z]], out=kE4b[z][:, 0:N], in0=kk[z][:, 0:N], in1=E4b[z][:, 0:N], op=ALU.mult)
                        qz, kz = (slice(0, 32), slice(32, 64)) if d == 0 else (slice(32, 64), slice(0, 32))
                        self.V("dve", "memset", [qE3b[z]], [qE3b[z]], v3(qE3b[z])[:, :, qz], 0.0)
                        self.V("pool", "memset", [kE4b[z]], [kE4b[z]], v3(kE4b[z])[:, :, kz], 0.0)
                        self.V("dve", "tensor_tensor", [q32[z], E1[z]], [qE1[z]], out=qE1[z][:, 0:N], in0=q32[z][:, 0:N], in1=E1[z][:, 0:N], op=ALU.mult)
                        self.V("pool", "tensor_tensor", [q32[z], E3[z]], [qE3[z]], out=qE3[z][:, 0:N], in0=q32[z][:, 0:N], in1=E3[z][:, 0:N], op=ALU.mult)
                        self.V("dve", "tensor_tensor", [kk[z], E4[z]], [kE4[z]], out=kE4[z][:, 0:N], in0=kk[z][:, 0:N], in1=E4[z][:, 0:N], op=ALU.mult)
                        self.V("pool", "tensor_tensor", [kk[z], E2[z]], [kE2[z]], out=kE2[z][:, 0:N], in0=kk[z][:, 0:N], in1=E2[z][:, 0:N], op=ALU.mult)
                        for cc in range(nb):
                            pi = rb()
                            for kc in range(8):
                                self.mm(pi[:, 0:128], ht[:, kc, cc * 128:(cc + 1) * 128], w[2][:, kc, :], kc == 0, kc == 7, [ht, w[2]], [pi])
                            self.act(iT[z][:, cc, :], pi[:, 0:128], AF.Copy, [pi], [iT[z]])
                            pt = rb()
                            ptv = pt[:].bitcast(BF)
                            self.tr(ptv[:, 0:128], kE2[z][:, cc * 128:(cc + 1) * 128], self.identb[:], [kE2[z], self.identb], [pt])
                            self.act(kT[z][:, cc, :], ptv[:, 0:128], AF.Copy, [pt], [kT[z]])
                        ops = B[z]
                        blks = list(range(nb)) if d == 0 else list(range(nb - 1, -1, -1))
                        for blk in blks:
                            pa = rb()
                            bs = slice(blk * 128, (blk + 1) * 128)
                            self.mm(pa[:, 0:128], kE4[z][:, bs], qE3[z][:, bs], True, True, [kE4[z], qE3[z]], [pa])
                            pb_ = rb()
                            self.mm(pb_[:, 0:128], kE4b[z][:, bs], qE3b[z][:, bs], True, True, [kE4b[z], qE3b[z]], [pb_])
                            am = AM[am_i % 2]
                            a1 = amt[am_i % 2]
                            a2 = amt2[am_i % 2]
                            am_i += 1
                            self.V("dve", "tensor_tensor", [pa, self.cst], [a1], out=a1[:], in0=pa[:, 0:128], in1=mask, op=ALU.mult)
                            self.V("dve", "tensor_tensor", [pb_, self.cst], [a2], out=a2[:], in0=pb_[:, 0:128], in1=masko, op=ALU.mult)
                            self.V("pool", "tensor_tensor", [a1, a2], [am], out=am[:], in0=a1[:], in1=a2[:], op=ALU.add)
                            for ch in ((0, 1) if d == 0 else (1, 0)):
                                p0 = ch * 64
                                c0 = blk * 128 + p0
                                self.mm(ops[:, c0:c0 + 64], Sbf[cur][:], qE1[z][:, c0:c0 + 64], True, False, [Sbf[cur], qE1[z]], [ops])
                                self.mm(ops[:, c0:c0 + 64], iT[z][p0:p0 + 64, blk, :], am[p0:p0 + 64, p0:p0 + 64], False, True, [iT[z], am], [ops])
                                pS = rb()
                                self.mm(pS[:, 0:128], kT[z][p0:p0 + 64, blk, :], iT[z][p0:p0 + 64, blk, :], True, True, [kT[z], iT[z]], [pS])
                                ce = c0 + cend
                                self.V("dve", "scalar_tensor_tensor", [S32, E1[z], pS], [S32], out=S32[:], in0=S32[:], scalar=E1[z][:, ce:ce + 1], in1=pS[:, 0:128],
                                       op0=ALU.mult, op1=ALU.add)
                                cur = 1 - cur
                                self.act(Sbf[cur][:], S32[:], AF.Copy, [S32], [Sbf[cur]])
                        if d == 0:
                            self.act(OF[:, t0:t0 + N], ops[:, 0:N], AF.Copy, [ops], [OF])
                        else:
                            self.V("dve", "tensor_tensor", [OF, ops], [osum], out=osum[:, 0:N], in0=OF[:, t0:t0 + N], in1=ops[:, 0:N], op=ALU.add)
                            self.act(sq[:, 0:N], osum[:, 0:N], AF.Square, [osum], [sq])
                            pss = rb()
                            self.mm(pss[:, 0:N], onesf, sq[:, 0:N], True, True, [self.cst, sq], [pss])
                            self.rstd_from(pss[:, 0:N], 1.0 / 128, rstd, tmp, N, [pss])
                            pg = rb()
                            self.proj(pg, w[3], 0, ht, N)
                            self.act(sgg[:, 0:N], pg[:, 0:N], AF.Sigmoid, [pg], [sgg])
                            self.V("dve", "scalar_tensor_tensor", [osum, rstd, P], [y1], out=y1[:, 0:N], in0=osum[:, 0:N], scalar=P[:, 148 + h:149 + h], in1=rstd[:, 0:N],
                                   op0=ALU.mult, op1=ALU.mult)
                            y = ys[n % 2]
                            self.V("pool", "tensor_tensor", [y1, sgg], [y], out=y[:, 0:N], in0=y1[:, 0:N], in1=sgg[:, 0:N], op=ALU.mult)
                            S.dma("sp", self.Y[0, h, :, t0:t0 + N], y[:, 0:N], [y], [Tl(None, self.Yt[0][ti])])
            S.barrier()

    def phase_merge(self, l):
        S = self.S
        i = self.i
        with contextlib.ExitStack() as es:
            wg = self.sb(es, "mgwg", [128, 8, 4096], BF)
            for k in range(4):
                S.dma("pool", wg[:, :, k * 1024:(k + 1) * 1024], i["w_in"][l, :, C_GATE + k * 1024:C_GATE + (k + 1) * 1024].rearrange("(kc p) n -> p kc n", p=128), [], [wg])
            wb = self.sb(es, "mgwb", [128, 4, 4, 1024], BF)
            for k in range(4):
                S.dma("pool", wb[:, k, :, :], i["w_branch"][l, k].rearrange("(cc p) n -> p cc n", p=128), [], [wb])
            ht = self.sb(es, "mght", [128, 8, 512], BF)
            Yk = [self.sb(es, "mgY%d" % k, [128, 4, 512], BF) for k in range(4)]
            sg = [self.sb(es, "mgsg%d" % k, [128, 512], F32) for k in range(2)]
            tmp = [self.sb(es, "mgtmp%d" % k, [128, 512], F32) for k in range(2)]
            macc = [self.sb(es, "mgacc%d" % k, [128, 512], F32) for k in range(2)]
            mT = [self.sb(es, "mgmT%d" % k, [128, 8, 512], BF) for k in range(2)]
            n = 0
            for ti, (t0, N) in enumerate(TILES):
                self.load_ht(ht, ti)
                for k in range(4):
                    S.dma("sp", Yk[k][:, :, 0:N], self.Y[k, :, :, t0:t0 + N].rearrange("j p n -> p j n"), [Tl(None, self.Yt[k][ti])], [Yk[k]])
                m = mT[ti % 2]
                for nch in range(8):
                    ma = macc[nch % 2]
                    for k in range(4):
                        pg, pp = self.psb(), self.psb()
                        self.proj(pg, wg, k * 1024 + nch * 128, ht, N)
                        for cc in range(4):
                            self.mm(pp[:, 0:N], wb[:, k, cc, nch * 128:(nch + 1) * 128], Yk[k][:, cc, 0:N], cc == 0, cc == 3, [wb, Yk[k]], [pp])
                        s_ = sg[n % 2]
                        t_ = tmp[n % 2]
                        n += 1
                        self.act(s_[:, 0:N], pg[:, 0:N], AF.Sigmoid, [pg], [s_])
                        if k == 0:
                            self.V("dve", "tensor_tensor", [pp, s_], [ma], out=ma[:, 0:N], in0=pp[:, 0:N], in1=s_[:, 0:N], op=ALU.mult)
                        else:
                            self.V("dve", "tensor_tensor", [pp, s_], [t_], out=t_[:, 0:N], in0=pp[:, 0:N], in1=s_[:, 0:N], op=ALU.mult)
                            self.V("pool", "tensor_tensor", [ma, t_], [ma], out=ma[:, 0:N], in0=ma[:, 0:N], in1=t_[:, 0:N], op=ALU.add)
                    self.V("pool", "tensor_copy", [ma], [m], out=m[:, nch, 0:N], in_=ma[:, 0:N])
                S.dma("sp", self.MT[:, :, t0:t0 + N].rearrange("k p n -> p k n"), m[:, :, 0:N], [m], [Tl(None, self.MTt[ti])])
            S.barrier()
        with contextlib.ExitStack() as es:
            wo = self.sb(es, "mgwo", [128, 8, 1024], BF)
            S.dma("pool", wo[:], i["w_out"][l].rearrange("(kc p) n -> p kc n", p=128), [], [wo])
            mts = [self.sb(es, "mgmt%d" % k, [128, 8, 512], BF) for k in range(2)]
            self.residual_setup(es, 2)
            for ti, (t0, N) in enumerate(TILES):
                mt = mts[ti % 2]
                S.dma("sp", mt[:, :, 0:N], self.MT[:, :, t0:t0 + N].rearrange("k p n -> p k n"), [Tl(None, self.MTt[ti])], [mt])
                for cc in range(N // 128):
                    c = t0 // 128 + cc
                    halves = []
                    for half in range(2):
                        po = self.psb()
                        for kc in range(8):
                            self.mm(po[:, :], mt[:, kc, cc * 128:(cc + 1) * 128], wo[:, kc, half * 512:(half + 1) * 512], kc == 0, kc == 7, [mt, wo], [po])
                        halves.append(po)
                    self.residual(c, lambda half: halves[half][:, :], halves)
            S.barrier()

    def residual_setup(self, es, modidx):
        S = self.S
        self.rmod = [self.sb(es, "rsmod%d" % k, [128, D], F32) for k in range(2)]
        for k in range(2):
            S.dma("sp", self.rmod[k][:], self.MODR[k, :, modidx * D:(modidx + 1) * D], [Tl(None, self.MODt)], [self.rmod[k]])
        self.rx = [self.sb(es, "rsx%d" % k, [128, D], F32) for k in range(2)]
        self.rtmp = [self.sb(es, "rstmp%d" % k, [128, D], F32) for k in range(2)]

    def residual(self, c, delta_ap, delta_tiles):
        S = self.S
        lat = 0 if c >= 2 else 1
        x = self.rx[c % 2]
        t = self.rtmp[c % 2]
        xt = Tl(None, self.Xt[c])
        S.dma("sp", x[:], self.X[c * 128:(c + 1) * 128, :], [xt], [x])
        for half in range(2):
            hs = slice(half * 512, (half + 1) * 512)
            self.V("dve", "tensor_tensor", [delta_tiles[half], self.rmod[lat]], [t], out=t[:, hs], in0=delta_ap(half), in1=self.rmod[lat][:, hs], op=ALU.mult)
        self.V("pool", "tensor_tensor", [x, t], [x], out=x[:], in0=x[:], in1=t[:], op=ALU.add)
        S.dma("sp", self.X[c * 128:(c + 1) * 128, :], x[:], [x], [xt])

    def route(self, c, t, Bm, h32, h32T, wr, R):
        P = self.P
        self.V("pool", "tensor_tensor", [t, Bm], [h32], out=h32[:], in0=t[:], in1=Bm[:], op=ALU.add)
        for g in range(2):
            ps = self.psb()
            for k in range(4):
                kk = g * 4 + k
                self.tr(ps[:, k * 128:(k + 1) * 128], h32[:, kk * 128:(kk + 1) * 128], self.cst[:, 0:128], [h32, self.cst], [ps])
            self.act(h32T[:, g * 4:(g + 1) * 4, :], ps[:].rearrange("p (k n) -> p k n", k=4), AF.Copy, [ps], [h32T])
        pl = self.psb()
        for kc in range(8):
            self.mm(pl[:, 0:36], h32T[:, kc, :], wr[:, kc, :], kc == 0, kc == 7, [h32T, wr], [pl])
        dv = lambda name, w_, **kw: self.V("dve", name, [R, P] + w_[1:], [w_[0]], **kw)
        self.V("dve", "tensor_tensor", [pl, P], [R], out=R[:, 0:36], in0=pl[:, 0:36], in1=P[:, 416:452], op=ALU.add)
        RR = [R]
        dv("tensor_reduce", RR, out=R[:, 36:37], in_=R[:, 0:4], axis=AX.X, op=ALU.max)
        dv("tensor_scalar", RR, out=R[:, 37:38], in0=R[:, 36:37], scalar1=-1.0, scalar2=0.0, op0=ALU.mult, op1=ALU.add)
        self.act(R[:, 44:48], R[:, 0:4], AF.Exp, [R], [R], bias=R[:, 37:38], accum_out=R[:, 38:39])
        dv("reciprocal", RR, out=R[:, 39:40], in_=R[:, 38:39])
        dv("tensor_scalar", RR, out=R[:, 40:44], in0=R[:, 0:4], scalar1=R[:, 36:37], scalar2=1.0, op0=ALU.is_equal, op1=ALU.mult)
        dv("tensor_tensor", RR, out=R[:, 48:80].rearrange("p (g e) -> p g e", g=4), in0=R[:, 4:36].rearrange("p (g e) -> p g e", g=4),
           in1=R[:, 40:44].unsqueeze(2).to_broadcast([128, 4, 8]), op=ALU.mult)
        dv("tensor_reduce", RR, out=R[:, 80:88], in_=R[:, 48:80].rearrange("p (g e) -> p e g", g=4), axis=AX.X, op=ALU.add)
        dv("tensor_reduce", RR, out=R[:, 88:89], in_=R[:, 80:88], axis=AX.X, op=ALU.max)
        dv("tensor_scalar", RR, out=R[:, 89:97], in0=R[:, 80:88], scalar1=R[:, 88:89], scalar2=1.0, op0=ALU.is_equal, op1=ALU.mult)
        dv("scalar_tensor_tensor", RR, out=R[:, 97:105], in0=R[:, 89:97], scalar=-1e30, in1=R[:, 80:88], op0=ALU.mult, op1=ALU.add)
        dv("tensor_reduce", RR, out=R[:, 105:106], in_=R[:, 97:105], axis=AX.X, op=ALU.max)
        dv("tensor_scalar", RR, out=R[:, 106:114], in0=R[:, 97:105], scalar1=R[:, 105:106], scalar2=1.0, op0=ALU.is_equal, op1=ALU.mult)
        dv("tensor_tensor", RR, out=R[:, 114:115], in0=R[:, 105:106], in1=R[:, 88:89], op=ALU.subtract)
        self.act(R[:, 115:116], R[:, 114:115], AF.Exp, [R], [R])
        dv("tensor_scalar", RR, out=R[:, 116:117], in0=R[:, 115:116], scalar1=1.0, scalar2=1.0, op0=ALU.add, op1=ALU.mult)
        dv("reciprocal", RR, out=R[:, 116:117], in_=R[:, 116:117])
        dv("tensor_tensor", RR, out=R[:, 117:118], in0=R[:, 116:117], in1=R[:, 39:40], op=ALU.mult)
        dv("tensor_tensor", RR, out=R[:, 118:119], in0=R[:, 39:40], in1=R[:, 117:118], op=ALU.subtract)
        dv("tensor_scalar", RR, out=R[:, 119:127], in0=R[:, 89:97], scalar1=R[:, 117:118], scalar2=0.0, op0=ALU.mult, op1=ALU.add)
        dv("scalar_tensor_tensor", RR, out=R[:, 119:127], in0=R[:, 106:114], scalar=R[:, 118:119], in1=R[:, 119:127], op0=ALU.mult, op1=ALU.add)
        self.V("dve", "tensor_tensor", [R], [self.RW], out=self.RW[:, c, :].rearrange("p (g e) -> p g e", g=4),
               in0=R[:, 40:44].unsqueeze(2).to_broadcast([128, 4, 8]), in1=R[:, 119:127].unsqueeze(1).to_broadcast([128, 4, 8]), op=ALU.mult)

    def phase_moe(self, l):
        S = self.S
        i = self.i
        with contextlib.ExitStack() as es:
            acc = self.sb(es, "moacc", [128, 10, D], F32)
            hTb = self.sb(es, "mohT", [128, 8, 1280], BF)
            wts = [(self.sb(es, "mowg%d" % k, [128, 8, 512], BF), self.sb(es, "mowu%d" % k, [128, 8, 512], BF),
                    self.sb(es, "mowd%d" % k, [128, 4, D], BF)) for k in range(2)]
            sG = [self.sb(es, "mosg%d" % k, [128, 512], F32) for k in range(2)]
            Hh = [self.sb(es, "moHh%d" % k, [128, 4, 512], BF) for k in range(2)]
            self.residual_setup(es, 5)
            n = 0
            m = 0
            for blk in ((0, 1, 2), (3, 4), (5, 6), (7, 8)):
                col = 0
                tcs = []
                for ti in blk:
                    t0, N = TILES[ti]
                    S.dma("sp", hTb[:, :, col:col + N], self.HT[:, :, t0:t0 + N].rearrange("k p n -> p k n"), [Tl(None, self.HTt[ti])], [hTb])
                    tcs.append((col, N, t0))
                    col += N
                for e in range(self.nexp):
                    wg, wu, wd = wts[e % 2]
                    S.dma("pool", wg[:], i["moe_w_gate"][l, e].rearrange("(kc p) f -> p kc f", p=128), [], [wg])
                    S.dma("pool", wu[:], i["moe_w_up"][l, e].rearrange("(kc p) f -> p kc f", p=128), [], [wu])
                    S.dma("pool", wd[:], i["moe_w_down"][l, e].rearrange("(fc p) n -> p fc n", p=128), [], [wd])
                    for (col, N, t0) in tcs:
                        hh = Hh[m % 2]
                        m += 1
                        for fc in range(4):
                            pG, pU = self.psb(), self.psb()
                            for kc in range(8):
                                self.mm(pG[:, 0:N], wg[:, kc, fc * 128:(fc + 1) * 128], hTb[:, kc, col:col + N], kc == 0, kc == 7, [wg, hTb], [pG])
                            for kc in range(8):
                                self.mm(pU[:, 0:N], wu[:, kc, fc * 128:(fc + 1) * 128], hTb[:, kc, col:col + N], kc == 0, kc == 7, [wu, hTb], [pU])
                            sg = sG[n % 2]
                            n += 1
                            self.act(sg[:, 0:N], pG[:, 0:N], AF.Silu, [pG], [sg])
                            self.V("dve", "tensor_tensor", [sg, pU], [hh], out=hh[:, fc, 0:N], in0=sg[:, 0:N], in1=pU[:, 0:N], op=ALU.mult)
                        for cc in range(N // 128):
                            ci = col // 128 + cc
                            c = t0 // 128 + cc
                            for half in range(2):
                                hs = slice(half * 512, (half + 1) * 512)
                                pD = self.psb()
                                for fc in range(4):
                                    self.mm(pD[:, :], hh[:, fc, cc * 128:(cc + 1) * 128], wd[:, fc, hs], fc == 0, fc == 3, [hh, wd], [pD])
                                if e == 0:
                                    self.V("dve", "tensor_scalar", [pD, self.RW], [acc], out=acc[:, ci, hs], in0=pD[:, :], scalar1=self.RW[:, c, e:e + 1], scalar2=0.0,
                                           op0=ALU.mult, op1=ALU.add)
                                else:
                                    self.V("dve", "scalar_tensor_tensor", [pD, self.RW, acc], [acc], out=acc[:, ci, hs], in0=pD[:, :], scalar=self.RW[:, c, e:e + 1],
                                           in1=acc[:, ci, hs], op0=ALU.mult, op1=ALU.add)
                for (col, N, t0) in tcs:
                    for cc in range(N // 128):
                        ci = col // 128 + cc
                        c = t0 // 128 + cc
                        self.residual(c, lambda half, ci=ci: acc[:, ci, half * 512:(half + 1) * 512], [acc, acc])
            S.barrier()

    def rank(self, c, R):
        A = R[:, 128:160]
        self.V("dve", "tensor_scalar", [self.RW], [R], out=A, in0=self.RW[:, c, :], scalar1=0.0, scalar2=1.0, op0=ALU.is_gt, op1=ALU.mult)
        if c == 0:
            self.V("dve", "memset", [], [self.Asum], self.Asum[:], 0.0)
        ps = self.psb()
        self.mm(ps[:, 0:32], self.ltri[:], A, True, False, [self.ltri, R], [ps])
        self.mm(ps[:, 0:32], self.cst[:, 128:256], self.Asum[:], False, True, [self.cst, self.Asum], [ps])
        self.act(self.RK[:, c, :], ps[:, 0:32], AF.Copy, [ps], [self.RK])
        self.V("dve", "tensor_tensor", [self.Asum, R], [self.Asum], out=self.Asum[:], in0=self.Asum[:], in1=A, op=ALU.add)

    def phase_moe_sparse(self, l):
        S = self.S
        i = self.i
        onesf = self.cst[:, 128:256]
        rows_t = Tl(None, self.ROWSt)
        acc_t = Tl(None, self.ACC2t)
        h2_t = Tl(None, self.H2t)
        with contextlib.ExitStack() as es:
            G = self.sb(es, "spG", [128, 1024], F32)
            widx = self.sb(es, "spwidx", [128, 2, NB], U32)
            init = self.sb(es, "spinit", [128, 128, 4], F32)
            self.V("pool", "memset", [], [init], init[:], 0.0)
            self.V("pool", "memset", [init], [init], init[:, :, 0:1], float(T))
            self.V("pool", "memset", [init], [init], init[:, :, 2:4], 1.0e6)
            S.dma("sp", self.ROWS.rearrange("(j p) c -> j (p c)", p=128), init[0:NB, :, :].rearrange("j p c -> j (p c)"), [init], [rows_t])
            ps = self.psb()
            self.mm(ps[:, 0:32], onesf, self.Asum[:], True, True, [self.cst, self.Asum], [ps])
            cnt, pad, pend, pst = G[:, 0:32], G[:, 32:64], G[:, 64:96], G[:, 96:128]
            cmp = self.sb(es, "spcmp", [128, NB, 32], F32)
            self.V("dve", "tensor_copy", [ps], [G], out=cnt, in_=ps[:, 0:32])
            cmp2 = cmp[:].rearrange("p a b -> p (a b)")[:, 0:32 * 68].rearrange("p (e m) -> p e m", m=68)
            self.V("dve", "tensor_tensor", [G, self.cst], [cmp], out=cmp2, in0=cnt.unsqueeze(2).to_broadcast([128, 32, 68]),
                   in1=self.cst[:, 896:896 + 68].unsqueeze(1).to_broadcast([128, 32, 68]), op=ALU.is_gt)
            self.V("dve", "tensor_reduce", [cmp], [G], out=pad, in_=cmp2, axis=AX.X, op=ALU.add)
            self.V("dve", "tensor_scalar", [G], [G], out=pad, in0=pad, scalar1=128.0, scalar2=0.0, op0=ALU.mult, op1=ALU.add)
            self.V("dve", "tensor_tensor_scan", [G, self.cst], [G], out=pend, data0=onesf[:, 0:32], data1=pad, initial=0.0, op0=ALU.mult, op1=ALU.add)
            self.V("dve", "tensor_tensor", [G], [G], out=pst, in0=pend, in1=pad, op=ALU.subtract)
            self.V("dve", "tensor_tensor", [G, self.cst], [cmp], out=cmp[:], in0=pend.unsqueeze(1).to_broadcast([128, NB, 32]),
                   in1=self.cst[:, 896:896 + NB].unsqueeze(2).to_broadcast([128, NB, 32]), op=ALU.is_le)
            be = G[:, 128:128 + NB]
            self.V("dve", "tensor_reduce", [cmp], [G], out=be, in_=cmp[:], axis=AX.X, op=ALU.add)
            self.V("dve", "tensor_scalar", [G], [G], out=be, in0=be, scalar1=31.0, scalar2=128.0, op0=ALU.min, op1=ALU.mult)
            same = G[:, 384:384 + NB]
            self.V("dve", "memset", [G], [G], same, 0.0)
            self.V("dve", "tensor_tensor", [G], [G], out=G[:, 385:384 + NB], in0=G[:, 129:128 + NB], in1=G[:, 128:127 + NB], op=ALU.is_equal)
            wf = G[:, 256:256 + NB]
            self.V("dve", "tensor_scalar", [G, self.cst], [G], out=wf, in0=be, scalar1=self.cst[:, 1024:1025], scalar2=2.0, op0=ALU.add, op1=ALU.mult)
            self.V("dve", "tensor_scalar", [G], [G], out=wf, in0=wf, scalar1=float(l * 8192), scalar2=1.0, op0=ALU.add, op1=ALU.mult)
            self.V("dve", "scalar_tensor_tensor", [G], [G], out=wf, in0=same, scalar=1.0e8, in1=wf, op0=ALU.mult, op1=ALU.add)
            self.V("dve", "tensor_copy", [G], [widx], out=widx[:, 0, :], in_=wf)
            self.V("dve", "tensor_scalar", [G], [G], out=wf, in0=wf, scalar1=1.0, scalar2=1.0, op0=ALU.add, op1=ALU.mult)
            self.V("dve", "tensor_copy", [G], [widx], out=widx[:, 1, :], in_=wf)
            Q = [self.sb(es, "spQ%d" % k, [128, 160], F32) for k in range(2)]
            rec = [self.sb(es, "sprec%d" % k, [128, 2, 4], F32) for k in range(2)]
            didx = [self.sb(es, "spdidx%d" % k, [128, 2], U32) for k in range(2)]
            for c in range(NCH):
                q = Q[c % 2]
                r_ = rec[c % 2]
                di = didx[c % 2]
                A, dst, d1, m1 = q[:, 0:32], q[:, 32:64], q[:, 64:96], q[:, 96:128]
                rd = [self.RW, self.RK, G, q]
                self.V("dve", "tensor_scalar", rd, [q], out=A, in0=self.RW[:, c, :], scalar1=0.0, scalar2=1.0, op0=ALU.is_gt, op1=ALU.mult)
                self.V("dve", "tensor_tensor", rd, [q], out=dst, in0=self.RK[:, c, :], in1=pst, op=ALU.add)
                self.V("dve", "scalar_tensor_tensor", rd, [q], out=d1, in0=dst, scalar=1.0, in1=A, op0=ALU.add, op1=ALU.mult)
                self.V("dve", "tensor_reduce", rd, [q], out=q[:, 128:129], in_=d1, axis=AX.X, op=ALU.max)
                self.V("dve", "tensor_scalar", rd, [q], out=m1, in0=d1, scalar1=q[:, 128:129], scalar2=1.0, op0=ALU.is_equal, op1=ALU.mult)
                self.V("dve", "tensor_tensor", rd, [q], out=m1, in0=m1, in1=self.RW[:, c, :], op=ALU.mult)
                self.V("dve", "tensor_reduce", rd, [q], out=q[:, 129:130], in_=m1, axis=AX.X, op=ALU.add)
                self.V("dve", "tensor_reduce", rd, [q], out=q[:, 130:131], in_=self.RW[:, c, :], axis=AX.X, op=ALU.add)
                self.V("dve", "tensor_scalar", rd, [q], out=m1, in0=A, scalar1=-1.0e9, scalar2=1.0e9, op0=ALU.mult, op1=ALU.add)
                self.V("dve", "tensor_tensor", rd, [q], out=m1, in0=m1, in1=dst, op=ALU.add)
                self.V("dve", "tensor_reduce", rd, [q], out=q[:, 131:132], in_=m1, axis=AX.X, op=ALU.min)
                self.V("dve", "tensor_scalar", rd, [q], out=q[:, 132:133], in0=q[:, 128:129], scalar1=-1.0, scalar2=1.0, op0=ALU.add, op1=ALU.mult)
                self.V("dve", "memset", [], [r_], r_[:], 0.0)
                for k in range(2):
                    self.V("dve", "tensor_scalar", [self.cst, r_], [r_], out=r_[:, k, 0:1], in0=self.cst[:, 1024:1025], scalar1=float(c * 128), scalar2=1.0,
                           op0=ALU.add, op1=ALU.mult)
                    self.V("dve", "tensor_scalar", [self.cst, r_], [r_], out=r_[:, k, 2:3], in0=self.cst[:, 1024:1025], scalar1=float(c * 128 + k * T), scalar2=2.0,
                           op0=ALU.add, op1=ALU.mult)
                    self.V("dve", "tensor_scalar", [r_], [r_], out=r_[:, k, 3:4], in0=r_[:, k, 2:3], scalar1=1.0, scalar2=1.0, op0=ALU.add, op1=ALU.mult)
                self.V("dve", "tensor_tensor", [q, r_], [r_], out=r_[:, 0, 1:2], in0=q[:, 130:131], in1=q[:, 129:130], op=ALU.subtract)
                self.V("dve", "tensor_copy", [q, r_], [r_], out=r_[:, 1, 1:2], in_=q[:, 129:130])
                self.V("dve", "tensor_copy", [q], [di], out=di[:, 0:1], in_=q[:, 131:132])
                self.V("dve", "tensor_copy", [q], [di], out=di[:, 1:2], in_=q[:, 132:133])
                for k in range(2):
                    S.dma_fn("pool", (lambda e, r_=r_, di=di, k=k: e.indirect_dma_start(out=self.ROWS, out_offset=bass.IndirectOffsetOnAxis(ap=di[:, k:k + 1], axis=0),
                                                                                      in_=r_[:, k, :], in_offset=None)), [r_, di], [rows_t])
            wgv = i["moe_w_gate"].rearrange("l e (p j) f -> (l e p) (j f)", j=8).rearrange("r (h x) -> (r h) x", h=2)
            wuv = i["moe_w_up"].rearrange("l e (p j) f -> (l e p) (j f)", j=8).rearrange("r (h x) -> (r h) x", h=2)
            wdv = i["moe_w_down"].rearrange("l e (p j) n -> (l e p) (j n)", j=4).rearrange("r (h x) -> (r h) x", h=2)
            wts = [(self.sb(es, "spwg%d" % k, [128, 8, 512], BF), self.sb(es, "spwu%d" % k, [128, 8, 512], BF),
                    self.sb(es, "spwd%d" % k, [128, 4, D], BF)) for k in range(1)]
            recs = [self.sb(es, "sprc%d" % k, [128, 4], F32) for k in range(2)]
            recu = [self.sb(es, "spru%d" % k, [128, 4], U32) for k in range(2)]
            hbs = [self.sb(es, "sphb%d" % k, [128, D], BF) for k in range(2)]
            hTs = [self.sb(es, "sphT%d" % k, [128, 8, 128], BF) for k in range(2)]
            sGs = [self.sb(es, "spsg%d" % k, [128, 512], F32) for k in range(2)]
            Hhs = [self.sb(es, "spHh%d" % k, [128, 512], BF) for k in range(2)]
            HhTs = [self.sb(es, "spHhT%d" % k, [128, 4, 128], BF) for k in range(2)]
            ys = [self.sb(es, "spy%d" % k, [128, D], F32) for k in range(2)]
            def gather(dst_ap, src, idx_ap, r, w, skip=False):
                if skip:
                    S.dma_fn("pool", (lambda e: e.indirect_dma_start(out=dst_ap, out_offset=None, in_=src, in_offset=bass.IndirectOffsetOnAxis(ap=idx_ap, axis=0),
                                                                     bounds_check=self._wbound_reg(e), oob_is_err=False)), r, w)
                else:
                    S.dma_fn("pool", (lambda e: e.indirect_dma_start(out=dst_ap, out_offset=None, in_=src, in_offset=bass.IndirectOffsetOnAxis(ap=idx_ap, axis=0))), r, w)
            for j in range(NB):
                z = j % 2
                rc, ru, hb, hT, sg, Hh, HhT, y = recs[z], recu[z], hbs[z], hTs[z], sGs[z], Hhs[z], HhTs[z], ys[z]
                wg, wu, wd = wts[0]
                S.dma("sp", rc[:], self.ROWS[j * 128:(j + 1) * 128, :], [rows_t], [rc])
                self.V("dve", "tensor_copy", [rc], [ru], out=ru[:], in_=rc[:])
                gather(hb[:], self.H2, ru[:, 0:1], [ru, h2_t], [hb])
                for h_ in range(2):
                    gather(wg[:, h_ * 4:(h_ + 1) * 4, :].rearrange("p j f -> p (j f)"), wgv, widx[:, h_, j:j + 1], [widx], [wg], skip=True)
                    gather(wu[:, h_ * 4:(h_ + 1) * 4, :].rearrange("p j f -> p (j f)"), wuv, widx[:, h_, j:j + 1], [widx], [wu], skip=True)
                    gather(wd[:, h_ * 2:(h_ + 1) * 2, :].rearrange("p j f -> p (j f)"), wdv, widx[:, h_, j:j + 1], [widx], [wd], skip=True)
                pt = self.psb()
                ptv = pt[:].bitcast(BF).rearrange("p (k n) -> p k n", k=8)
                hbv = hb[:].rearrange("t (p j) -> t p j", j=8)
                for jx in range(8):
                    self.tr(ptv[:, jx, :], hbv[:, :, jx], self.identb[:], [hb, self.identb], [pt])
                self.act(hT[:], ptv, AF.Copy, [pt], [hT])
                pG, pU = self.psb(), self.psb()
                for jx in range(8):
                    self.mm(pG[:, :], hT[:, jx, :], wg[:, jx, :], jx == 0, jx == 7, [hT, wg], [pG])
                for jx in range(8):
                    self.mm(pU[:, :], hT[:, jx, :], wu[:, jx, :], jx == 0, jx == 7, [hT, wu], [pU])
                self.act(sg[:], pG[:, :], AF.Silu, [pG], [sg])
                self.V("dve", "scalar_tensor_tensor", [pU, rc, sg], [Hh], out=Hh[:], in0=pU[:, :], scalar=rc[:, 1:2], in1=sg[:], op0=ALU.mult, op1=ALU.mult)
                pt2 = self.psb()
                pt2v = pt2[:].bitcast(BF)[:, 0:512].rearrange("p (k n) -> p k n", k=4)
                Hhv = Hh[:].rearrange("t (p j) -> t p j", j=4)
                for jx in range(4):
                    self.tr(pt2v[:, jx, :], Hhv[:, :, jx], self.identb[:], [Hh, self.identb], [pt2])
                self.act(HhT[:], pt2v, AF.Copy, [pt2], [HhT])
                for half in range(2):
                    pD = self.psb()
                    for jx in range(4):
                        self.mm(pD[:, :], HhT[:, jx, :], wd[:, jx, half * 512:(half + 1) * 512], jx == 0, jx == 3, [HhT, wd], [pD])
                    if half == 0:
                        self.act(y[:, 0:512], pD[:, :], AF.Copy, [pD], [y])
                    else:
                        self.V("dve", "tensor_copy", [pD], [y], out=y[:, 512:1024], in_=pD[:, :])
                for h_ in range(2):
                    S.dma_fn("pool", (lambda e, y=y, ru=ru, h_=h_: e.indirect_dma_start(out=self.ACC2.rearrange("r (h x) -> (r h) x", h=2),
                                                                                        out_offset=bass.IndirectOffsetOnAxis(ap=ru[:, 2 + h_:3 + h_], axis=0),
                                                                                        in_=y[:, h_ * 512:(h_ + 1) * 512], in_offset=None,
                                                                                        bounds_check=self._bound_reg(e), oob_is_err=False)), [y, ru], [acc_t])
            self.residual_setup(es, 5)
            a0 = [self.sb(es, "spa0%d" % k, [128, D], F32) for k in range(2)]
            a1 = [self.sb(es, "spa1%d" % k, [128, D], F32) for k in range(2)]
            for c in range(NCH):
                u0, u1 = a0[c % 2], a1[c % 2]
                S.dma("sp", u0[:], self.ACC2[c * 128:(c + 1) * 128, :], [acc_t], [u0])
                S.dma("sp", u1[:], self.ACC2[T + c * 128:T + (c + 1) * 128, :], [acc_t], [u1])
                self.V("pool", "tensor_tensor", [u0, u1], [u0], out=u0[:], in0=u0[:], in1=u1[:], op=ALU.add)
                self.residual(c, lambda half, u0=u0: u0[:, half * 512:(half + 1) * 512], [u0, u0])
            S.barrier()

    def _wbound_reg(self, e):
        if getattr(self, "_wbreg", None) is None:
            self._wbreg = e.to_reg(self.n_layers * 32 * 128 * 2 - 1)
        return self._wbreg

    def _bound_reg(self, e):
        if getattr(self, "_breg", None) is None:
            self._breg = e.to_reg(4 * T - 1)
        return self._breg

    def final_norm(self):
        S = self.S
        with contextlib.ExitStack() as es:
            g = self.sb(es, "fng", [128, D], F32)
            S.dma("sp", g[:], self.i["final_g"].partition_broadcast(128), [], [g])
            xs = [self.sb(es, "fnx%d" % k, [128, D], F32) for k in range(3)]
            sq = self.sb(es, "fnsq", [128, D], F32)
            st = [self.sb(es, "fnst%d" % k, [128, 4], F32) for k in range(2)]
            ot = [self.sb(es, "fno%d" % k, [128, D], F32) for k in range(2)]
            for c in range(2, NCH):
                x = xs[c % 3]
                S.dma("sp", x[:], self.X[c * 128:(c + 1) * 128, :], [Tl(None, self.Xt[c])], [x])
                s_ = st[c % 2]
                self.act(sq[:], x[:], AF.Square, [x], [sq, s_], accum_out=s_[:, 0:1])
                self.V("dve", "tensor_scalar", [s_], [s_], out=s_[:, 1:2], in0=s_[:, 0:1], scalar1=1.0 / D, scalar2=EPS, op0=ALU.mult, op1=ALU.add)
                self.V("dve", "reciprocal", [s_], [s_], out=s_[:, 2:3], in_=s_[:, 1:2])
                self.act(s_[:, 3:4], s_[:, 2:3], AF.Sqrt, [s_], [s_])
                o = ot[c % 2]
                self.V("dve", "scalar_tensor_tensor", [x, s_, g], [o], out=o[:], in0=x[:], scalar=s_[:, 3:4], in1=g[:], op0=ALU.mult, op1=ALU.mult)
                S.dma("sp", self.out[(c - 2) * 128:(c - 1) * 128, :], o[:], [o], [Tl(None, self.outt)])


def make_consts():
    cst = np.zeros((128, 1056), np.float32)
    cst[:, 0:128] = np.eye(128, dtype=np.float32)
    cst[:, 128:256] = 1.0
    R = np.zeros((128, 128), np.float32)
    for blk in range(2):
        o = blk * 64
        for q in range(16):
            R[o + 16 + q, o + q] = -1.0
            R[o + q, o + 16 + q] = 1.0
            R[o + 48 + q, o + 32 + q] = -1.0
            R[o + 32 + q, o + 48 + q] = 1.0
    cst[:, 256:384] = R
    s = np.arange(128)[:, None]
    t = np.arange(128)[None, :]
    same = (s // 64) == (t // 64)
    same32 = (s // 32) == (t // 32)
    cst[:, 384:512] = (same32 & (t >= s)).astype(np.float32)
    cst[:, 512:640] = (same32 & (t <= s)).astype(np.float32)
    cst[:, 640:768] = (same & (s % 64 < 32) & (t % 64 >= 32)).astype(np.float32)
    cst[:, 768:896] = (same & (s % 64 >= 32) & (t % 64 < 32)).astype(np.float32)
    cst[:, 896:1024] = 128.0 * np.arange(128, dtype=np.float32)[None, :]
    cst[:, 1024] = np.arange(128, dtype=np.float32)
    cst[:, 1025:1057 - 1 + 0] = 0.0
    inv_freq = (10000.0 ** (-np.arange(0, 32, 2, dtype=np.float32) / 32)).astype(np.float32)
    pos = np.arange(NLAT)
    row = (pos // 64).astype(np.float32)
    col = (pos % 64).astype(np.float32)
    ang_r = row[:, None] * inv_freq
    ang_c = col[:, None] * inv_freq
    ang = np.concatenate([ang_r, ang_r, ang_c, ang_c], axis=-1).astype(np.float32)
    rope = np.zeros((2, 128, T), np.float32)
    rope[0, :, :NCTX] = 1.0
    rope[0, 0:64, NCTX:] = np.cos(ang).T
    rope[0, 64:128, NCTX:] = np.cos(ang).T
    rope[1, 0:64, NCTX:] = np.sin(ang).T
    rope[1, 64:128, NCTX:] = np.sin(ang).T
    return cst, rope


def make_in_maps(inputs, cores, L=DEPTH, nexp=32):
    f = lambda a: np.ascontiguousarray(np.asarray(a, dtype=np.float32))
    cst, rope = make_consts()
    shared = {
        "c_ctx": f(inputs["c_ctx"]).reshape(1, D),
        "ada_w": f(inputs["ada_w"][:L]), "ada_b": f(inputs["ada_b"]),
        "norm1_g": f(inputs["norm1_g"]), "norm2_g": f(inputs["norm2_g"]),
        "w_in": f(inputs["w_in"][:L]), "w_branch": f(inputs["w_branch"][:L]), "w_out": f(inputs["w_out"][:L]),
        "hg_lb": f(inputs["hg_lb_logits"]), "hg_norm_g": f(inputs["hg_norm_g"]),
        "da_lambda": f(inputs["da_lambda"]).reshape(DEPTH, 256), "da_norm_g": f(inputs["da_norm_g"]),
        "cv_dw_w": f(inputs["cv_dw_w"]), "cv_dw_b": f(inputs["cv_dw_b"]),
        "cv_ln_g": f(inputs["cv_ln_g"]), "cv_ln_b": f(inputs["cv_ln_b"]),
        "sc_w": f(inputs["sc_w"]),
        "moe_w_r": np.ascontiguousarray(np.concatenate([f(inputs["moe_w_grp"]), f(inputs["moe_w_exp"])], axis=-1)),
        "moe_b_r": np.ascontiguousarray(np.concatenate([f(inputs["moe_b_grp"]), f(inputs["moe_b_exp"])], axis=-1)),
        "moe_w_gate": f(inputs["moe_w_gate"][:L, :nexp]), "moe_w_up": f(inputs["moe_w_up"][:L, :nexp]), "moe_w_down": f(inputs["moe_w_down"][:L, :nexp]),
        "final_g": f(inputs["final_g"]).reshape(1, D),
        "cst": cst, "rope": rope, "ltri": np.triu(np.ones((128, 128), np.float32), 1),
    }
    maps = []
    for cid in cores:
        b = cid % 4
        m = dict(shared)
        m["x"] = f(inputs["x"][b])
        m["c"] = f(inputs["c"][b]).reshape(1, D)
        m["ctx"] = f(inputs["ctx"][b])
        maps.append(m)
    return maps


def kernel(**inputs):
    nc = bass.Bass("TRN2", target_bir_lowering=False)
    Prog(nc).build()
    maps = make_in_maps(inputs, list(range(4)))
    res = run_bass_kernel_spmd(nc, maps, core_ids=list(range(4)))
    return np.stack([np.asarray(res.results[b]["out"], dtype=np.float32) for b in range(4)], axis=0)
```

```python
import contextlib
import math
import numpy as np
import concourse.bass as bass
import concourse.mybir as mybir
from concourse.bass_utils import run_bass_kernel_spmd

F32 = mybir.dt.float32
BF = mybir.dt.bfloat16
U32 = mybir.dt.uint32
AF = mybir.ActivationFunctionType
ALU = mybir.AluOpType
AX = mybir.AxisListType

D = 1024
NCTX = 256
NLAT = 4096
T = NCTX + NLAT
NCH = T // 128
NB = 2 * T // 128 + 32
SPARSE = True
DEPTH = 4
W = 512
INW = 10752
EPS = 1e-6
TILES = [(0, 256)] + [(256 + 512 * i, 512) for i in range(8)]
C_HQ, C_HI, C_HFF, C_HFB, C_HG = 0, 512, 1024, 1536, 2048
C_DQ, C_DK, C_DV = 2560, 3072, 3584
C_CVA, C_CVG = 4096, 4608
C_SB, C_SC, C_SX = 5120, 5632, 6144
C_GATE = 6656


class Trk:
    __slots__ = ("w", "r")

    def __init__(self):
        self.w = None
        self.r = {}


class Tl:
    def __init__(self, h, trk=None):
        self.h = h
        self.t = trk or Trk()

    def __getitem__(self, k):
        return self.h[k]


class Stream:
    def __init__(self, name):
        self.name = name
        self.ops = []
        self.seen = {}
        self.sem = None
        self.cnt = 0
        self.dslots = []
        self.dnext = 0


class Sch:
    SEM_MAX = 30000

    def __init__(self, nc, es):
        self.nc = nc
        self.es = es
        self.st = {k: Stream(k) for k in ("pe", "act", "dve", "pool", "sp")}
        self.nsem = 0
        for k, s in self.st.items():
            self._newsem(s)
        for k, n in (("sp", 24), ("pool", 12), ("act", 6)):
            s = self.st[k]
            for i in range(n):
                s.dslots.append([self._sem(), 0])

    def _sem(self):
        self.nsem += 1
        return self.es.enter_context(self.nc.semaphore("s%d" % self.nsem))

    def _newsem(self, s):
        s.sem = self._sem()
        s.cnt = 0

    def _need(self, s, tok, waits):
        if tok is None:
            return
        sem, val, src = tok
        if src == "pe" and s.name == "pe":
            return
        if s.seen.get(id(sem), 0) >= val:
            return
        k = id(sem)
        if k not in waits or waits[k][1] < val:
            waits[k] = (sem, val)

    def _deps(self, s, reads, writes):
        waits = {}
        for b in reads:
            self._need(s, b.t.w, waits)
        for b in writes:
            self._need(s, b.t.w, waits)
            for tok in b.t.r.values():
                self._need(s, tok, waits)
        for k, (sem, val) in waits.items():
            s.seen[k] = val
        return list(waits.values())

    def _mark(self, tok, reads, writes):
        for b in reads:
            b.t.r[id(tok[0])] = tok
        for b in writes:
            b.t.w = tok
            b.t.r = {}

    def op(self, eng, fn, reads=(), writes=()):
        s = self.st[eng]
        if s.cnt >= self.SEM_MAX:
            self._newsem(s)
        waits = self._deps(s, reads, writes)
        s.cnt += 1
        tok = (s.sem, s.cnt, eng)
        s.ops.append((waits, fn, (s.sem, 1)))
        self._mark(tok, reads, writes)
        return tok

    def dma(self, q, out, in_, reads=(), writes=(), **kw):
        return self.dma_fn(q, (lambda e: e.dma_start(out=out, in_=in_, **kw)), reads, writes)

    def dma_fn(self, q, fn, reads=(), writes=()):
        s = self.st[q]
        slot = s.dslots[s.dnext % len(s.dslots)]
        s.dnext += 1
        waits = self._deps(s, reads, writes)
        if slot[1] > 0 and s.seen.get(id(slot[0]), 0) < slot[1]:
            waits.append((slot[0], slot[1]))
            s.seen[id(slot[0])] = slot[1]
        slot[1] += 16
        tok = (slot[0], slot[1], "dma")
        s.ops.append((waits, fn, (slot[0], 16)))
        self._mark(tok, reads, writes)
        return tok

    def barrier(self):
        toks = []
        for s in self.st.values():
            if s.cnt > 0:
                toks.append((s.sem, s.cnt))
            for sl in s.dslots:
                if sl[1] > 0:
                    toks.append((sl[0], sl[1]))
        for s in self.st.values():
            waits = []
            for sem, val in toks:
                if sem is s.sem:
                    continue
                if s.seen.get(id(sem), 0) < val:
                    waits.append((sem, val))
                    s.seen[id(sem)] = val
            if waits:
                s.ops.append((waits, None, None))

    def emit(self):
        nc = self.nc
        self.barrier()
        with nc.Block() as block:
            def run(s):
                def f(e):
                    for waits, fn, inc in s.ops:
                        for sem, val in waits:
                            e.wait_ge(sem, val)
                        if fn is not None:
                            fn(e).then_inc(inc[0], inc[1])
                return f
            block.tensor(run(self.st["pe"]))
            block.scalar(run(self.st["act"]))
            block.vector(run(self.st["dve"]))
            block.gpsimd(run(self.st["pool"]))
            block.sync(run(self.st["sp"]))


class Prog:
    def __init__(self, nc, n_layers=DEPTH, dbg=None, nexp=32):
        self.nc = nc
        self.nexp = nexp
        self.n_layers = n_layers
        self.dbg = dbg or {}

    def sb(self, es, name, shape, dt):
        self.uid = getattr(self, "uid", 0) + 1
        return Tl(es.enter_context(self.nc.sbuf_tensor("%s_u%d" % (name, self.uid), list(shape), dt)))

    def dram(self, name, shape, dt, kind="Internal"):
        return self.nc.dram_tensor(name, list(shape), dt, kind=kind).ap()

    def mm(self, out, lhsT, rhs, start, stop, r, w):
        self.S.op("pe", lambda e: e.matmul(out, lhsT=lhsT, rhs=rhs, start=start, stop=stop), r, w)

    def tr(self, out, in_, ident, r, w):
        self.S.op("pe", lambda e: e.transpose(out=out, in_=in_, identity=ident), r, w)

    def act(self, out, in_, func, r, w, **kw):
        self.S.op("act", lambda e: e.activation(out=out, in_=in_, func=func, **kw), r, w)

    def V(self, eng, name, r, w, *a, **kw):
        self.S.op(eng, lambda e: getattr(e, name)(*a, **kw), r, w)

    def psb(self):
        b = self.banks[self.bi % 8]
        self.bi += 1
        return b

    def build(self):
        nc = self.nc
        L = self.n_layers
        i = {}
        def inp(name, shape, dt=F32):
            i[name] = self.dram(name, shape, dt, kind="ExternalInput")
        inp("x", [NLAT, D]); inp("c", [1, D]); inp("ctx", [NCTX, D]); inp("c_ctx", [1, D])
        inp("ada_w", [L, D, 6 * D]); inp("ada_b", [DEPTH, 6 * D])
        inp("norm1_g", [DEPTH, D]); inp("norm2_g", [DEPTH, D])
        inp("w_in", [L, D, INW]); inp("w_branch", [L, 4, W, D]); inp("w_out", [L, D, D])
        inp("hg_lb", [DEPTH, 2, W]); inp("hg_norm_g", [DEPTH, W])
        inp("da_lambda", [DEPTH, 256]); inp("da_norm_g", [DEPTH, 128])
        inp("cv_dw_w", [DEPTH, 31, W]); inp("cv_dw_b", [DEPTH, W]); inp("cv_ln_g", [DEPTH, W]); inp("cv_ln_b", [DEPTH, W])
        inp("sc_w", [DEPTH, 3, W])
        inp("moe_w_r", [DEPTH, D, 36]); inp("moe_b_r", [DEPTH, 36])
        inp("moe_w_gate", [L, self.nexp, D, W]); inp("moe_w_up", [L, self.nexp, D, W]); inp("moe_w_down", [L, self.nexp, W, D])
        inp("final_g", [1, D]); inp("ltri", [128, 128])
        inp("cst", [128, 1056]); inp("rope", [2, 128, T])
        self.i = i
        self.out = self.dram("out", [NLAT, D], F32, kind="ExternalOutput")
        self.X = self.dram("Xs", [T, D], F32)
        self.HT = self.dram("HTs", [8, 128, T], BF)
        self.Y = self.dram("Ys", [4, 4, 128, T], BF)
        self.MT = self.dram("MTs", [8, 128, T], BF)
        self.MODR = self.dram("MODs", [2, 128, 6 * D], F32)
        self.H2 = self.dram("H2s", [T + 1, D], BF)
        self.ROWS = self.dram("ROWSs", [NB * 128, 4], F32)
        self.ACC2 = self.dram("ACC2s", [2 * T, D], F32)
        self.H2t = Trk(); self.ROWSt = Trk(); self.ACC2t = Trk()
        self.Xt = [Trk() for _ in range(NCH)]
        self.HTt = [Trk() for _ in range(9)]
        self.Yt = [[Trk() for _ in range(9)] for _ in range(4)]
        self.MTt = [Trk() for _ in range(9)]
        self.MODt = Trk()
        self.outt = Trk()
        dbg_out = {}
        for k, shp in self.dbg.items():
            if not isinstance(shp, tuple):
                continue
            dbg_out[k] = self.dram("dbg_" + k, shp[0], shp[1], kind="ExternalOutput")
        self.dbg_out = dbg_out

        with contextlib.ExitStack() as es:
            self.S = S = Sch(nc, es)
            self.banks = [Tl(es.enter_context(nc.psum_tensor("pb%d" % k, [128, 512], F32))) for k in range(8)]
            self.bi = 0
            self.cst = self.sb(es, "cst_sb", [128, 1056], F32)
            S.dma("sp", self.cst[:], i["cst"], [], [self.cst])
            self.identb = self.sb(es, "identb", [128, 128], BF)
            self.onesb = self.sb(es, "onesb", [128, 128], BF)
            self.Rb = self.sb(es, "Rb", [128, 128], BF)
            self.identf = Tl(self.cst.h, self.cst.t)
            self.V("dve", "tensor_copy", [self.cst], [self.identb], out=self.identb[:], in_=self.cst[:, 0:128])
            self.V("dve", "tensor_copy", [self.cst], [self.onesb], out=self.onesb[:], in_=self.cst[:, 128:256])
            self.V("dve", "tensor_copy", [self.cst], [self.Rb], out=self.Rb[:], in_=self.cst[:, 256:384])
            self.sT = []
            for which, src in enumerate((i["c"], i["c_ctx"])):
                cT = self.sb(es, "cT%d" % which, [128, 8], F32)
                S.dma("sp", cT[:], src.rearrange("o (kc p) -> p (o kc)", p=128), [], [cT], allow_slow_non_contiguous=True)
                sg = self.sb(es, "cS%d" % which, [128, 8], F32)
                self.act(sg[:], cT[:], AF.Silu, [cT], [sg])
                rep = self.sb(es, "sT%d" % which, [128, 8, 128], F32)
                self.V("dve", "tensor_copy", [sg], [rep], out=rep[:], in_=sg[:].unsqueeze(2).to_broadcast([128, 8, 128]))
                self.sT.append(rep)
            self.P = self.sb(es, "Pparams", [128, 600], F32)
            self.RW = self.sb(es, "RW", [128, NCH, 32], F32)
            self.RK = self.sb(es, "RK", [128, NCH, 32], F32)
            self.Asum = self.sb(es, "Asum", [128, 32], F32)
            self.ltri = self.sb(es, "ltri_sb", [128, 128], F32)
            S.dma("sp", self.ltri[:], i["ltri"], [], [self.ltri])
            zr = self.sb(es, "zrow", [1, D], BF)
            self.V("dve", "memset", [], [zr], zr[:], 0.0)
            S.dma("sp", self.H2[T:T + 1, :], zr[:], [zr], [Tl(None, self.H2t)])
            self.LB = self.sb(es, "LB", [128, DEPTH, 8], F32)
            self.OML = self.sb(es, "OML", [128, DEPTH, 8], F32)
            lbe = self.sb(es, "lbe", [128, DEPTH, 8], F32)
            lbt = self.sb(es, "lbt", [128, 16], F32)
            for l_ in range(DEPTH):
                for d_ in range(2):
                    for j_ in range(4):
                        S.dma("sp", lbe[:, l_, d_ * 4 + j_:d_ * 4 + j_ + 1], i["hg_lb"][l_, d_:d_ + 1, j_ * 128:(j_ + 1) * 128].rearrange("o p -> p o"),
                              [], [lbe], allow_slow_non_contiguous=True)
            self.act(lbe[:], lbe[:], AF.Exp, [lbe], [lbe])
            self.V("dve", "tensor_tensor", [lbe], [lbt], out=lbt[:, 0:8], in0=lbe[:, 0, :], in1=lbe[:, 1, :], op=ALU.add)
            self.V("dve", "tensor_tensor", [lbe, lbt], [lbt], out=lbt[:, 0:8], in0=lbt[:, 0:8], in1=lbe[:, 2, :], op=ALU.add)
            self.V("dve", "tensor_tensor", [lbe, lbt], [lbt], out=lbt[:, 0:8], in0=lbt[:, 0:8], in1=lbe[:, 3, :], op=ALU.add)
            self.V("dve", "reciprocal", [lbt], [lbt], out=lbt[:, 8:16], in_=lbt[:, 0:8])
            self.V("dve", "memset", [], [self.LB], self.LB[:], 0.0)
            for l_ in range(1, DEPTH):
                self.V("dve", "tensor_tensor", [lbe, lbt], [lbe], out=lbe[:, l_, :], in0=lbe[:, l_, :], in1=lbt[:, 8:16], op=ALU.mult)
                self.V("dve", "tensor_tensor", [lbe, self.LB], [self.LB], out=self.LB[:, l_, :], in0=self.LB[:, l_ - 1, :], in1=lbe[:, l_, :], op=ALU.add)
            self.V("dve", "tensor_scalar", [self.LB], [self.OML], out=self.OML[:], in0=self.LB[:], scalar1=-1.0, scalar2=1.0, op0=ALU.mult, op1=ALU.add)
            self.seg = self.sb(es, "seg", [128, 512], F32)
            self.V("dve", "memset", [], [self.seg], self.seg[:], 1.0)
            self.V("dve", "memset", [self.seg], [self.seg], self.seg[:].rearrange("p (c s) -> p c s", s=64)[:, :, 0:1], 0.0)
            S.barrier()
            S.dma("sp", self.X[0:NCTX, :], i["ctx"], [], self._xt(0, 2))
            for q in range(4):
                S.dma("sp", self.X[NCTX + q * 1024: NCTX + (q + 1) * 1024, :], i["x"][q * 1024:(q + 1) * 1024, :], [],
                      self._xt(2 + q * 8, 8))
            for l in range(L):
                self.layer(l)
            self.final_norm()
            S.emit()

    def _xt(self, c0, n):
        return [Tl(None, t) for t in self.Xt[c0:c0 + n]]

    def layer(self, l):
        stop = self.dbg.get("stop")
        self.phase_mod(l)
        self.load_params(l)
        self.phase_norm(l, 1)
        if stop == "norm1":
            return
        self.phase_sconv(l)
        self.phase_conv(l)
        if stop == "convs":
            return self.dump_dbg()
        self.phase_attn(l)
        if stop == "attn":
            return self.dump_dbg()
        self.phase_hgrn(l)
        if stop == "hgrn":
            return self.dump_dbg()
        self.phase_merge(l)
        if stop == "merge":
            return self.dump_dbg()
        self.phase_norm(l, 2)
        if SPARSE:
            self.phase_moe_sparse(l)
        else:
            self.phase_moe(l)
        if stop == "moe":
            return self.dump_dbg()

    def dump_dbg(self):
        S = self.S
        if "y" in self.dbg_out:
            S.dma("sp", self.dbg_out["y"], self.Y, [Tl(None, t) for k in range(4) for t in self.Yt[k]], [Tl(None, Trk())])
        if "x" in self.dbg_out:
            S.dma("sp", self.dbg_out["x"], self.X, [Tl(None, t) for t in self.Xt], [Tl(None, Trk())])

    def load_w(self, es, name, l, c0, ncols):
        w = self.sb(es, name, [128, 8, ncols], BF)
        self.S.dma("pool", w[:], self.i["w_in"][l, :, c0:c0 + ncols].rearrange("(kc p) n -> p kc n", p=128), [], [w])
        return w

    def load_ht(self, buf, ti):
        t0, N = TILES[ti]
        self.S.dma("sp", buf[:, :, 0:N], self.HT[:, :, t0:t0 + N].rearrange("k p n -> p k n"), [Tl(None, self.HTt[ti])], [buf])

    def proj(self, ps, w, j0, ht, N, width=128):
        for kc in range(8):
            self.mm(ps[0:width, 0:N], w[:, kc, j0:j0 + width], ht[:, kc, 0:N], kc == 0, kc == 7, [w, ht], [ps])

    def rstd_from(self, src_ap, scale, out_t, tmp_t, N, r):
        self.V("dve", "tensor_scalar", r, [tmp_t], out=tmp_t[:, 0:N], in0=src_ap, scalar1=scale, scalar2=EPS, op0=ALU.mult, op1=ALU.add)
        self.V("dve", "reciprocal", [tmp_t], [tmp_t], out=tmp_t[:, 0:N], in_=tmp_t[:, 0:N])
        self.act(out_t[:, 0:N], tmp_t[:, 0:N], AF.Sqrt, [tmp_t], [out_t])

    def load_params(self, l):
        S = self.S
        i = self.i
        P = self.P
        nc = True
        def ld(dst, src):
            S.dma("sp", dst, src, [], [P], allow_slow_non_contiguous=True)
        for j in range(4):
            sl = slice(j * 128, (j + 1) * 128)
            ld(P[:, j * 3:j * 3 + 3], i["sc_w"][l][:, sl].rearrange("k p -> p k"))
            ld(P[:, 12 + j * 31:12 + (j + 1) * 31], i["cv_dw_w"][l][:, sl].rearrange("k p -> p k"))
            ld(P[:, 136 + j:137 + j], i["cv_dw_b"][l:l + 1, sl].rearrange("o p -> p o"))
            ld(P[:, 140 + j:141 + j], i["cv_ln_g"][l:l + 1, sl].rearrange("o p -> p o"))
            ld(P[:, 144 + j:145 + j], i["cv_ln_b"][l:l + 1, sl].rearrange("o p -> p o"))
            ld(P[:, 148 + j:149 + j], i["hg_norm_g"][l:l + 1, sl].rearrange("o p -> p o"))
        ld(P[:, 152:153], i["da_norm_g"][l:l + 1, :].rearrange("o p -> p o"))
        S.dma("sp", P[:, 160:416], i["da_lambda"][l:l + 1, :].partition_broadcast(128), [], [P])
        S.dma("sp", P[:, 416:452], i["moe_b_r"][l:l + 1, :].partition_broadcast(128), [], [P])
        lam_init = 0.8 - 0.6 * math.exp(-0.3 * l)
        self.V("dve", "tensor_tensor", [P], [P], out=P[:, 460:524], in0=P[:, 160:224], in1=P[:, 224:288], op=ALU.mult)
        self.V("dve", "tensor_tensor", [P], [P], out=P[:, 524:588], in0=P[:, 288:352], in1=P[:, 352:416], op=ALU.mult)
        self.V("dve", "tensor_reduce", [P], [P], out=P[:, 155:157], in_=P[:, 460:588].rearrange("p (a b) -> p a b", a=2), axis=AX.X, op=ALU.add)
        self.act(P[:, 157:159], P[:, 155:157], AF.Exp, [P], [P])
        self.V("dve", "tensor_tensor", [P], [P], out=P[:, 159:160], in0=P[:, 158:159], in1=P[:, 157:158], op=ALU.subtract)
        self.V("dve", "tensor_scalar", [P], [P], out=P[:, 153:154], in0=P[:, 159:160], scalar1=-lam_init, scalar2=1.0, op0=ALU.add, op1=ALU.mult)
        self.V("dve", "tensor_scalar", [P], [P], out=P[:, 154:155], in0=P[:, 152:153], scalar1=1.0 - lam_init, scalar2=0.0, op0=ALU.mult, op1=ALU.add)

    def phase_mod(self, l):
        S = self.S
        i = self.i
        modt = Tl(None, self.MODt)
        with contextlib.ExitStack() as es:
            wt = [self.sb(es, "adaw%d" % k, [128, 8, 512], F32) for k in range(2)]
            bt = [self.sb(es, "adab%d" % k, [1, 512], F32) for k in range(2)]
            ot = [self.sb(es, "adao%d" % k, [128, 512], F32) for k in range(2)]
            n = 0
            for blk in range(12):
                w = wt[blk % 2]
                b = bt[blk % 2]
                S.dma("sp", w[:], i["ada_w"][l, :, blk * 512:(blk + 1) * 512].rearrange("(kc p) n -> p kc n", p=128), [], [w])
                S.dma("sp", b[:], i["ada_b"][l:l + 1, blk * 512:(blk + 1) * 512], [], [b])
                for which in range(2):
                    ps = self.psb()
                    for kc in range(8):
                        self.mm(ps[:], self.sT[which][:, kc, :], w[:, kc, :], kc == 0, False, [self.sT[which], w], [ps])
                    self.mm(ps[:], self.cst[0:1, 128:256], b[0:1, :], False, True, [self.cst, b], [ps])
                    o = ot[n % 2]
                    n += 1
                    self.V("dve", "tensor_copy", [ps], [o], out=o[:], in_=ps[:])
                    S.dma("sp", self.MODR[which, :, blk * 512:(blk + 1) * 512], o[:], [o], [modt])
            S.barrier()

    def phase_norm(self, l, which):
        S = self.S
        i = self.i
        gsrc = i["norm1_g"] if which == 1 else i["norm2_g"]
        sh, sc = (0, 1) if which == 1 else (3, 4)
        modt = Tl(None, self.MODt)
        with contextlib.ExitStack() as es:
            A = [self.sb(es, "nA%d" % k, [128, D], F32) for k in range(2)]
            B = [self.sb(es, "nB%d" % k, [128, D], F32) for k in range(2)]
            g = self.sb(es, "ng", [128, D], F32)
            S.dma("sp", g[:], gsrc[l:l + 1, :].partition_broadcast(128), [], [g])
            for k in range(2):
                S.dma("sp", A[k][:], self.MODR[k, :, sc * D:(sc + 1) * D], [modt], [A[k]])
                S.dma("sp", B[k][:], self.MODR[k, :, sh * D:(sh + 1) * D], [modt], [B[k]])
                self.V("dve", "scalar_tensor_tensor", [A[k], g], [A[k]], out=A[k][:], in0=A[k][:], scalar=1.0, in1=g[:],
                       op0=ALU.add, op1=ALU.mult)
            xs = [self.sb(es, "nx%d" % k, [128, D], F32) for k in range(3)]
            sq = self.sb(es, "nsq", [128, D], F32)
            t1 = [self.sb(es, "nt%d" % k, [128, D], F32) for k in range(2)]
            hb = [self.sb(es, "nhb%d" % k, [128, D], BF) for k in range(2)]
            st = [self.sb(es, "nst%d" % k, [128, 4], F32) for k in range(2)]
            hT = [self.sb(es, "nhT%d" % k, [128, 8, 512], BF) for k in range(2)]
            if which == 2:
                wr = self.sb(es, "nwr", [128, 8, 36], F32)
                S.dma("sp", wr[:], i["moe_w_r"][l].rearrange("(kc p) n -> p kc n", p=128), [], [wr])
                h32 = [self.sb(es, "nh32%d" % k, [128, D], F32) for k in range(2)]
                h32T = [self.sb(es, "nh32T%d" % k, [128, 8, 128], F32) for k in range(2)]
                Rt = [self.sb(es, "nR%d" % k, [128, 160], F32) for k in range(2)]
            for ti, (t0, N) in enumerate(TILES):
                ht = hT[ti % 2]
                for cc in range(N // 128):
                    c = t0 // 128 + cc
                    lat = 0 if c >= 2 else 1
                    x = xs[c % 3]
                    xt = Tl(None, self.Xt[c])
                    S.dma("sp", x[:], self.X[c * 128:(c + 1) * 128, :], [xt], [x])
                    s_ = st[c % 2]
                    self.act(sq[:], x[:], AF.Square, [x], [sq, s_], accum_out=s_[:, 0:1])
                    self.V("dve", "tensor_scalar", [s_], [s_], out=s_[:, 1:2], in0=s_[:, 0:1], scalar1=1.0 / D, scalar2=EPS,
                           op0=ALU.mult, op1=ALU.add)
                    self.V("dve", "reciprocal", [s_], [s_], out=s_[:, 2:3], in_=s_[:, 1:2])
                    self.act(s_[:, 3:4], s_[:, 2:3], AF.Sqrt, [s_], [s_])
                    t = t1[c % 2]
                    self.V("dve", "scalar_tensor_tensor", [x, s_, A[lat]], [t], out=t[:], in0=x[:], scalar=s_[:, 3:4],
                           in1=A[lat][:], op0=ALU.mult, op1=ALU.mult)
                    h = hb[c % 2]
                    self.V("pool", "tensor_tensor", [t, B[lat]], [h], out=h[:], in0=t[:], in1=B[lat][:], op=ALU.add)
                    ps = self.psb()
                    pv = ps[:].bitcast(BF).rearrange("p (k n) -> p k n", k=8)
                    for k in range(8):
                        self.tr(pv[:, k, :], h[:, k * 128:(k + 1) * 128], self.identb[:], [h, self.identb], [ps])
                    self.act(ht[:, :, cc * 128:(cc + 1) * 128], pv, AF.Copy, [ps], [ht])
                    if which == 2:
                        self.route(c, t, B[lat], h32[c % 2], h32T[c % 2], wr, Rt[c % 2])
                        if SPARSE:
                            S.dma("sp", self.H2[c * 128:(c + 1) * 128, :], h[:], [h], [Tl(None, self.H2t)])
                            self.rank(c, Rt[c % 2])
                S.dma("sp", self.HT[:, :, t0:t0 + N].rearrange("k p n -> p k n"), ht[:, :, 0:N], [ht], [Tl(None, self.HTt[ti])])
            S.barrier()
        if "h1" in self.dbg_out and l == self.dbg.get("layer", 0) and which == 1:
            S.dma("sp", self.dbg_out["h1"], self.HT, [Tl(None, t) for t in self.HTt], [Tl(None, Trk())])


    def phase_sconv(self, l):
        S = self.S
        P = self.P
        with contextlib.ExitStack() as es:
            wb = self.load_w(es, "scwb", l, C_SB, 512)
            wc = self.load_w(es, "scwc", l, C_SC, 512)
            wx = self.load_w(es, "scwx", l, C_SX, 512)
            ub = self.sb(es, "scu", [128, T + 3], F32)
            bb = self.sb(es, "scb", [128, T], F32)
            hts = [self.sb(es, "scht%d" % k, [128, 8, 512], BF) for k in range(2)]
            cs = [self.sb(es, "sccs%d" % k, [128, 512], F32) for k in range(2)]
            acc = [self.sb(es, "scacc%d" % k, [128, 512], F32) for k in range(2)]
            ys = [self.sb(es, "scy%d" % k, [128, 512], BF) for k in range(2)]
            self.V("pool", "memset", [], [ub], ub[:], 0.0)
            n = 0
            for j in range(4):
                for ti, (t0, N) in enumerate(TILES):
                    ht = hts[n % 2]
                    c_ = cs[n % 2]
                    n += 1
                    self.load_ht(ht, ti)
                    base = t0 + 1 if ti == 0 else t0 + 2
                    pb, pc, px = self.psb(), self.psb(), self.psb()
                    self.proj(pb, wb, j * 128, ht, N)
                    self.proj(pc, wc, j * 128, ht, N)
                    self.proj(px, wx, j * 128, ht, N)
                    self.act(c_[:, 0:N], pc[:, 0:N], AF.Copy, [pc], [c_])
                    self.V("dve", "tensor_tensor", [c_, px], [ub], out=ub[:, base:base + N], in0=c_[:, 0:N], in1=px[:, 0:N], op=ALU.mult)
                    self.act(bb[:, t0:t0 + N], pb[:, 0:N], AF.Copy, [pb], [bb])
                for ti, (t0, N) in enumerate(TILES):
                    base = t0 + 1 if ti == 0 else t0 + 2
                    a = acc[ti % 2]
                    y = ys[ti % 2]
                    self.V("dve", "tensor_scalar", [ub, P], [a], out=a[:, 0:N], in0=ub[:, base - 1:base - 1 + N], scalar1=P[:, j * 3:j * 3 + 1],
                           scalar2=0.0, op0=ALU.mult, op1=ALU.add)
                    for k in (1, 2):
                        self.V("dve", "scalar_tensor_tensor", [ub, P, a], [a], out=a[:, 0:N], in0=ub[:, base - 1 + k:base - 1 + k + N],
                               scalar=P[:, j * 3 + k:j * 3 + k + 1], in1=a[:, 0:N], op0=ALU.mult, op1=ALU.add)
                    self.V("pool", "tensor_tensor", [a, bb], [y], out=y[:, 0:N], in0=a[:, 0:N], in1=bb[:, t0:t0 + N], op=ALU.mult)
                    S.dma("sp", self.Y[3, j, :, t0:t0 + N], y[:, 0:N], [y], [Tl(None, self.Yt[3][ti])])
            S.barrier()

    def phase_conv(self, l):
        S = self.S
        P = self.P
        onesf = self.cst[:, 128:256]
        with contextlib.ExitStack() as es:
            wa = self.load_w(es, "cvwa", l, C_CVA, 512)
            wg = self.load_w(es, "cvwg", l, C_CVG, 512)
            vb = self.sb(es, "cvv", [128, 4, T + 45], BF)
            hts = [self.sb(es, "cvht%d" % k, [128, 8, 512], BF) for k in range(2)]
            sg = [self.sb(es, "cvsg%d" % k, [128, 512], F32) for k in range(2)]
            self.V("pool", "memset", [], [vb], vb[:], 0.0)
            n = 0
            for ti, (t0, N) in enumerate(TILES):
                ht = hts[ti % 2]
                self.load_ht(ht, ti)
                base = t0 + 15 if ti == 0 else t0 + 30
                for j in range(4):
                    pa, pg = self.psb(), self.psb()
                    self.proj(pa, wa, j * 128, ht, N)
                    self.proj(pg, wg, j * 128, ht, N)
                    s_ = sg[n % 2]
                    n += 1
                    self.act(s_[:, 0:N], pg[:, 0:N], AF.Sigmoid, [pg], [s_])
                    self.V("dve", "tensor_tensor", [s_, pa], [vb], out=vb[:, j, base:base + N], in0=pa[:, 0:N], in1=s_[:, 0:N], op=ALU.mult)
            ca = [self.sb(es, "cvca%d" % k, [128, 4, 512], F32) for k in range(2)]
            cp = self.sb(es, "cvcp", [128, 512], F32)
            sq = self.sb(es, "cvsq", [128, 4, 512], F32)
            mean = self.sb(es, "cvmean", [128, 512], F32)
            tmp = self.sb(es, "cvtmp", [128, 512], F32)
            rstd = self.sb(es, "cvrstd", [128, 512], F32)
            dd = [self.sb(es, "cvd%d" % k, [128, 512], F32) for k in range(2)]
            ys = [self.sb(es, "cvy%d" % k, [128, 4, 512], BF) for k in range(2)]
            for ti, (t0, N) in enumerate(TILES):
                base = t0 + 15 if ti == 0 else t0 + 30
                a = ca[ti % 2]
                for j in range(4):
                    wcol = lambda k: P[:, 12 + j * 31 + k:12 + j * 31 + k + 1]
                    src = lambda k: vb[:, j, base - 15 + k:base - 15 + k + N]
                    self.V("dve", "tensor_scalar", [vb, P], [a], out=a[:, j, 0:N], in0=src(0), scalar1=wcol(0), scalar2=P[:, 136 + j:137 + j],
                           op0=ALU.mult, op1=ALU.add)
                    for k in range(1, 31):
                        self.V("dve", "scalar_tensor_tensor", [vb, P, a], [a], out=a[:, j, 0:N], in0=src(k), scalar=wcol(k), in1=a[:, j, 0:N],
                               op0=ALU.mult, op1=ALU.add)
                    self.act(sq[:, j, 0:N], a[:, j, 0:N], AF.Square, [a], [sq])
                p1, p2 = self.psb(), self.psb()
                for j in range(4):
                    self.mm(p1[:, 0:N], onesf, a[:, j, 0:N], j == 0, j == 3, [self.cst, a], [p1])
                for j in range(4):
                    self.mm(p2[:, 0:N], onesf, sq[:, j, 0:N], j == 0, j == 3, [self.cst, sq], [p2])
                self.act(mean[:, 0:N], p1[:, 0:N], AF.Copy, [p1], [mean], scale=1.0 / W)
                self.V("pool", "tensor_tensor", [mean], [tmp], out=tmp[:, 0:N], in0=mean[:, 0:N], in1=mean[:, 0:N], op=ALU.mult)
                self.V("dve", "scalar_tensor_tensor", [p2, tmp], [tmp], out=tmp[:, 0:N], in0=p2[:, 0:N], scalar=1.0 / W, in1=tmp[:, 0:N],
                       op0=ALU.mult, op1=ALU.subtract)
                self.rstd_from(tmp[:, 0:N], 1.0, rstd, tmp, N, [tmp])
                y = ys[ti % 2]
                for j in range(4):
                    d = dd[j % 2]
                    self.V("dve", "tensor_tensor", [a, mean], [d], out=d[:, 0:N], in0=a[:, j, 0:N], in1=mean[:, 0:N], op=ALU.subtract)
                    self.V("pool", "tensor_tensor", [d, rstd], [d], out=d[:, 0:N], in0=d[:, 0:N], in1=rstd[:, 0:N], op=ALU.mult)
                    self.V("dve", "tensor_scalar", [d, P], [d], out=d[:, 0:N], in0=d[:, 0:N], scalar1=P[:, 140 + j:141 + j], scalar2=P[:, 144 + j:145 + j],
                           op0=ALU.mult, op1=ALU.add)
                    self.act(y[:, j, 0:N], d[:, 0:N], AF.Silu, [d], [y])
                S.dma("sp", self.Y[2, :, :, t0:t0 + N].rearrange("j p n -> p j n"), y[:, :, 0:N], [y], [Tl(None, self.Yt[2][ti])])
            S.barrier()

    def rope(self, ps, raw, cos, sin, t1, t2, dst_ap, dst_t, N, rot_ps):
        self.act(raw[:, 0:N], ps[:, 0:N], AF.Copy, [ps], [raw])
        self.mm(rot_ps[:, 0:N], self.Rb[:], raw[:, 0:N], True, True, [self.Rb, raw], [rot_ps])
        self.V("pool", "tensor_tensor", [raw, cos], [t1], out=t1[:, 0:N], in0=raw[:, 0:N], in1=cos[:, 0:N], op=ALU.mult)
        self.V("dve", "tensor_tensor", [rot_ps, sin], [t2], out=t2[:, 0:N], in0=rot_ps[:, 0:N], in1=sin[:, 0:N], op=ALU.mult)
        self.V("pool", "tensor_tensor", [t1, t2], [dst_t], out=dst_ap, in0=t1[:, 0:N], in1=t2[:, 0:N], op=ALU.add)

    def phase_attn(self, l):
        S = self.S
        P = self.P
        i = self.i
        onesf = self.cst[:, 128:256]
        B = self.banks
        with contextlib.ExitStack() as es:
            wq = self.load_w(es, "dawq", l, C_DQ, 512)
            wk = self.load_w(es, "dawk", l, C_DK, 512)
            wv = self.load_w(es, "dawv", l, C_DV, 512)
            KT = self.sb(es, "daKT", [128, 4, T], BF)
            Vt = self.sb(es, "daV", [128, NCH, 512], BF)
            hts = [self.sb(es, "daht%d" % k, [128, 8, 512], BF) for k in range(2)]
            cos = [self.sb(es, "dacos%d" % k, [128, 512], F32) for k in range(2)]
            sin = [self.sb(es, "dasin%d" % k, [128, 512], F32) for k in range(2)]
            raw = [self.sb(es, "daraw%d" % k, [128, 512], BF) for k in range(2)]
            t1 = [self.sb(es, "dat1%d" % k, [128, 512], F32) for k in range(2)]
            t2 = [self.sb(es, "dat2%d" % k, [128, 512], F32) for k in range(2)]
            n = 0
            for ti, (t0, N) in enumerate(TILES):
                ht = hts[ti % 2]
                self.load_ht(ht, ti)
                S.dma("sp", cos[ti % 2][:, 0:N], i["rope"][0, :, t0:t0 + N], [], [cos[ti % 2]])
                S.dma("sp", sin[ti % 2][:, 0:N], i["rope"][1, :, t0:t0 + N], [], [sin[ti % 2]])
                for h in range(4):
                    ps, rp = self.psb(), self.psb()
                    self.proj(ps, wk, h * 128, ht, N)
                    self.rope(ps, raw[n % 2], cos[ti % 2], sin[ti % 2], t1[n % 2], t2[n % 2], KT[:, h, t0:t0 + N], KT, N, rp)
                    n += 1
                for cc in range(N // 128):
                    ps = self.psb()
                    for kc in range(8):
                        self.mm(ps[:, :], ht[:, kc, cc * 128:(cc + 1) * 128], wv[:, kc, :], kc == 0, kc == 7, [ht, wv], [ps])
                    self.act(Vt[:, t0 // 128 + cc, :], ps[:, :], AF.Copy, [ps], [Vt])
            QT = [self.sb(es, "daQT%d" % k, [128, 512], BF) for k in range(2)]
            QZ = [[self.sb(es, "daQZ%d_%d" % (k, c), [128, 512], BF) for c in range(2)] for k in range(2)]
            for k in range(2):
                for c in range(2):
                    self.V("pool", "memset", [], [QZ[k][c]], QZ[k][c][:], 0.0)
            pT = [self.sb(es, "dapT%d" % k, [128, 512], BF) for k in range(4)]
            rd = [self.sb(es, "dard%d" % k, [128, 512], F32) for k in range(2)]
            rp_ = [self.sb(es, "darp%d" % k, [128, 512], F32) for k in range(2)]
            rinv = self.sb(es, "darinv", [128, 512], F32)
            Oc = [self.sb(es, "daOc%d" % k, [128, 512], F32) for k in range(2)]
            o = self.sb(es, "dao", [128, 512], F32)
            sq = self.sb(es, "dasq", [128, 512], F32)
            tmp = self.sb(es, "datmp", [128, 512], F32)
            rstd = self.sb(es, "darstd", [128, 512], F32)
            ys = [self.sb(es, "day%d" % k, [128, 4, 512], BF) for k in range(2)]
            it = 0
            for ti, (t0, N) in enumerate(TILES):
                ht = hts[ti % 2]
                self.load_ht(ht, ti)
                S.dma("sp", cos[ti % 2][:, 0:N], i["rope"][0, :, t0:t0 + N], [], [cos[ti % 2]])
                S.dma("sp", sin[ti % 2][:, 0:N], i["rope"][1, :, t0:t0 + N], [], [sin[ti % 2]])
                nk = 2 if ti == 0 else NCH
                y = ys[ti % 2]
                for h in range(4):
                    qt = QT[h % 2]
                    self.proj(B[7], wq, h * 128, ht, N)
                    self.rope(B[7], raw[n % 2], cos[ti % 2], sin[ti % 2], t1[n % 2], t2[n % 2], qt[:, 0:N], qt, N, B[2])
                    n += 1
                    qz = QZ[h % 2]
                    self.V("pool", "tensor_copy", [qt], [qz[0]], out=qz[0][0:64, 0:N], in_=qt[0:64, 0:N])
                    self.V("dve", "tensor_copy", [qt], [qz[1]], out=qz[1][64:128, 0:N], in_=qt[64:128, 0:N])
                    its = [(c, kc) for c in range(2) for kc in range(nk)]
                    slots = {}
                    def qk(j):
                        c, kc = its[j]
                        sps = B[2 + it_base[0] % 3]
                        p = pT[it_base[0] % 4]
                        it_base[0] += 1
                        slots[j] = (sps, p)
                        p0 = c * 64
                        self.mm(sps[:, 0:N], KT[:, h, kc * 128:(kc + 1) * 128], qz[c][:, 0:N], True, True, [KT, qz[c]], [sps])
                    it_base = [it]
                    qk(0)
                    if len(its) > 1:
                        qk(1)
                    for j, (c, kc) in enumerate(its):
                        sps, p = slots.pop(j)
                        oacc = B[c]
                        self.act(p[:, 0:N], sps[:, 0:N], AF.Exp, [sps], [p], scale=0.125)
                        if j + 2 < len(its):
                            qk(j + 2)
                        self.mm(oacc[:, 0:N], Vt[:, kc, h * 128:(h + 1) * 128], p[:, 0:N], kc == 0, kc == nk - 1, [Vt, p], [oacc])
                        rsb = B[5 + c]
                        self.mm(rsb[:, 0:N], self.onesb[:], p[:, 0:N], kc == 0, kc == nk - 1, [self.onesb, p], [rsb])
                        if kc == nk - 1:
                            self.V("dve", "reciprocal", [rsb], [rinv], out=rinv[:, 0:N], in_=rsb[:, 0:N])
                            self.V("dve", "tensor_tensor", [oacc, rinv], [Oc[c]], out=Oc[c][:, 0:N], in0=oacc[:, 0:N], in1=rinv[:, 0:N], op=ALU.mult)
                    it = it_base[0]
                    self.V("dve", "scalar_tensor_tensor", [Oc[0], Oc[1], P], [o], out=o[:, 0:N], in0=Oc[1][:, 0:N], scalar=P[:, 153:154], in1=Oc[0][:, 0:N],
                           op0=ALU.mult, op1=ALU.add)
                    self.act(sq[:, 0:N], o[:, 0:N], AF.Square, [o], [sq])
                    self.mm(B[7][:, 0:N], onesf, sq[:, 0:N], True, True, [self.cst, sq], [B[7]])
                    self.rstd_from(B[7][:, 0:N], 1.0 / 128, rstd, tmp, N, [B[7]])
                    self.V("dve", "scalar_tensor_tensor", [o, rstd, P], [y], out=y[:, h, 0:N], in0=o[:, 0:N], scalar=P[:, 154:155], in1=rstd[:, 0:N],
                           op0=ALU.mult, op1=ALU.mult)
                S.dma("sp", self.Y[1, :, :, t0:t0 + N].rearrange("j p n -> p j n"), y[:, :, 0:N], [y], [Tl(None, self.Yt[1][ti])])
            S.barrier()

    def phase_hgrn(self, l):
        S = self.S
        P = self.P
        onesf = self.cst[:, 128:256]
        B = self.banks
        with contextlib.ExitStack() as es:
            OF = self.sb(es, "hgOF", [128, T], F32)
            S32 = self.sb(es, "hgS", [128, 128], F32)
            Sbf = [self.sb(es, "hgSb%d" % k, [128, 128], BF) for k in range(2)]
            hts = [self.sb(es, "hght%d" % k, [128, 8, 512], BF) for k in range(2)]
            wts = [[self.sb(es, "hgw%d_%d" % (a, k), [128, 8, 128], BF) for k in range(4)] for a in range(2)]
            def f32(name, n=2):
                return [self.sb(es, "%s%d" % (name, k), [128, 512], F32) for k in range(n)]
            def b16(name, n=2):
                return [self.sb(es, "%s%d" % (name, k), [128, 512], BF) for k in range(n)]
            q32, ee, ff, lf, kk, pre, bb, d3, d2 = (f32("hgq"), f32("hge"), f32("hgf"), f32("hglf"), f32("hgk"), f32("hgpre"),
                                                    f32("hgb"), f32("hgd3"), f32("hgd2"))
            E1, E2, E3, E4 = f32("hgE1"), f32("hgE2"), f32("hgE3"), f32("hgE4")
            d3a, E3b, E4b = f32("hgd3a"), f32("hgE3b"), f32("hgE4b")
            qE1, qE3, kE4, kE2 = b16("hgqE1"), b16("hgqE3"), b16("hgkE4"), b16("hgkE2")
            qE3b, kE4b = b16("hgqE3b"), b16("hgkE4b")
            amt = [self.sb(es, "hgamt%d" % k, [128, 128], F32) for k in range(2)]
            amt2 = [self.sb(es, "hgamu%d" % k, [128, 128], F32) for k in range(2)]
            iT = [self.sb(es, "hgiT%d" % k, [128, 4, 128], BF) for k in range(2)]
            kT = [self.sb(es, "hgkT%d" % k, [128, 4, 128], BF) for k in range(2)]
            AM = [self.sb(es, "hgAM%d" % k, [128, 128], BF) for k in range(2)]
            osum = self.sb(es, "hgo", [128, 512], F32)
            sq = self.sb(es, "hgsq", [128, 512], F32)
            tmp = self.sb(es, "hgtmp", [128, 512], F32)
            rstd = self.sb(es, "hgrstd", [128, 512], F32)
            sgg = self.sb(es, "hgsg", [128, 512], F32)
            y1 = self.sb(es, "hgy1", [128, 512], F32)
            ys = [self.sb(es, "hgy%d" % k, [128, 512], BF) for k in range(2)]
            rot = [2]
            def rb():
                b = B[rot[0]]
                rot[0] = 2 + (rot[0] - 1) % 6
                return b
            n = 0
            am_i = 0
            for h in range(4):
                for d in range(2):
                    w = wts[(h * 2 + d) % 2]
                    cols = (C_HQ, C_HFF if d == 0 else C_HFB, C_HI, C_HG)
                    for k in range(4):
                        S.dma("pool", w[k][:], self.i["w_in"][l, :, cols[k] + h * 128:cols[k] + (h + 1) * 128].rearrange("(kc p) n -> p kc n", p=128), [], [w[k]])
                    self.V("pool", "memset", [], [S32], S32[:], 0.0)
                    self.V("pool", "memset", [], [Sbf[0]], Sbf[0][:], 0.0)
                    cur = 0
                    lbc = self.LB[:, l, d * 4 + h:d * 4 + h + 1]
                    omc = self.OML[:, l, d * 4 + h:d * 4 + h + 1]
                    order = list(range(9)) if d == 0 else [0] + list(range(8, 0, -1))
                    mask = self.cst[:, 384:512] if d == 0 else self.cst[:, 512:640]
                    masko = self.cst[:, 640:768] if d == 0 else self.cst[:, 768:896]
                    for ti in order:
                        t0, N = TILES[ti]
                        nb = N // 128
                        nch = N // 64
                        z = n % 2
                        n += 1
                        ht = hts[z]
                        self.load_ht(ht, ti)
                        pq, pf = rb(), rb()
                        self.proj(pq, w[0], 0, ht, N)
                        self.proj(pf, w[1], 0, ht, N)
                        self.act(q32[z][:, 0:N], pq[:, 0:N], AF.Copy, [pq], [q32[z]])
                        self.act(ee[z][:, 0:N], pf[:, 0:N], AF.Exp, [pf], [ee[z]], scale=-1.0)
                        self.V("dve", "tensor_scalar", [ee[z]], [ee[z]], out=ee[z][:, 0:N], in0=ee[z][:, 0:N], scalar1=1.0, scalar2=1.0, op0=ALU.add, op1=ALU.mult)
                        self.V("dve", "reciprocal", [ee[z]], [ee[z]], out=ee[z][:, 0:N], in_=ee[z][:, 0:N])
                        self.V("dve", "tensor_scalar", [ee[z], self.LB, self.OML], [ff[z]], out=ff[z][:, 0:N], in0=ee[z][:, 0:N], scalar1=omc, scalar2=lbc,
                               op0=ALU.mult, op1=ALU.add)
                        self.act(lf[z][:, 0:N], ff[z][:, 0:N], AF.Ln, [ff[z]], [lf[z]])
                        self.V("pool", "tensor_scalar", [ff[z]], [kk[z]], out=kk[z][:, 0:N], in0=ff[z][:, 0:N], scalar1=-1.0, scalar2=1.0, op0=ALU.mult, op1=ALU.add)
                        self.V("dve", "tensor_tensor_scan", [self.seg, lf[z]], [pre[z]], out=pre[z][:, 0:N], data0=self.seg[:, 0:N], data1=lf[z][:, 0:N],
                               initial=0.0, op0=ALU.mult, op1=ALU.add)
                        v3 = lambda t_: t_[:, 0:N].rearrange("p (c s) -> p c s", s=64)
                        bc = lambda t_, col: v3(t_)[:, :, col:col + 1].to_broadcast([128, nch, 64])
                        if d == 0:
                            b_ = pre[z]
                            cend = 63
                        else:
                            b_ = bb[z]
                            cend = 0
                            self.V("dve", "tensor_tensor", [lf[z], pre[z]], [bb[z]], out=bb[z][:, 0:N], in0=lf[z][:, 0:N], in1=pre[z][:, 0:N], op=ALU.subtract)
                            self.V("dve", "tensor_tensor", [bb[z], pre[z]], [bb[z]], out=v3(bb[z]), in0=v3(bb[z]), in1=bc(pre[z], 63), op=ALU.add)
                        self.V("dve", "tensor_tensor", [b_], [d3[z]], out=v3(d3[z]), in0=v3(b_), in1=bc(b_, 32), op=ALU.subtract)
                        self.V("pool", "tensor_tensor", [b_], [d2[z]], out=v3(d2[z]), in0=v3(b_), in1=bc(b_, cend), op=ALU.subtract)
                        self.act(E1[z][:, 0:N], b_[:, 0:N], AF.Exp, [b_], [E1[z]])
                        self.act(E2[z][:, 0:N], d2[z][:, 0:N], AF.Exp, [d2[z]], [E2[z]], scale=-1.0)
                        v32 = lambda t_: t_[:, 0:N].rearrange("p (c s) -> p c s", s=32)
                        self.V("dve", "tensor_tensor", [b_], [d3a[z]], out=v32(d3a[z]), in0=v32(b_), in1=v32(b_)[:, :, 16:17].to_broadcast([128, 2 * nch, 32]), op=ALU.subtract)
                        self.act(E3[z][:, 0:N], d3a[z][:, 0:N], AF.Exp, [d3a[z]], [E3[z]])
                        self.act(E4[z][:, 0:N], d3a[z][:, 0:N], AF.Exp, [d3a[z]], [E4[z]], scale=-1.0)
                        self.act(E3b[z][:, 0:N], d3[z][:, 0:N], AF.Exp, [d3[z]], [E3b[z]])
                        self.act(E4b[z][:, 0:N], d3[z][:, 0:N], AF.Exp, [d3[z]], [E4b[z]], scale=-1.0)
                        self.V("dve", "tensor_tensor", [q32[z], E3b[z]], [qE3b[z]], out=qE3b[z][:, 0:N], in0=q32[z][:, 0:N], in1=E3b[z][:, 0:N], op=ALU.mult)
                        self.V("pool", "tensor_tensor", [kk[z], E4b[z]], [kE4b[z]], out=kE4b[z][:, 0:N], in0=kk[z][:, 0:N], in1=E4b[z][:, 0:N], op=ALU.mult)
                        qz, kz = (slice(0, 32), slice(32, 64)) if d == 0 else (slice(32, 64), slice(0, 32))
                        self.V("dve", "memset", [qE3b[z]], [qE3b[z]], v3(qE3b[z])[:, :, qz], 0.0)
                        self.V("pool", "memset", [kE4b[z]], [kE4b[z]], v3(kE4b[z])[:, :, kz], 0.0)
                        self.V("dve", "tensor_tensor", [q32[z], E1[z]], [qE1[z]], out=qE1[z][:, 0:N], in0=q32[z][:, 0:N], in1=E1[z][:, 0:N], op=ALU.mult)
                        self.V("pool", "tensor_tensor", [q32[z], E3[z]], [qE3[z]], out=qE3[z][:, 0:N], in0=q32[z][:, 0:N], in1=E3[z][:, 0:N], op=ALU.mult)
                        self.V("dve", "tensor_tensor", [kk[z], E4[z]], [kE4[z]], out=kE4[z][:, 0:N], in0=kk[z][:, 0:N], in1=E4[z][:, 0:N], op=ALU.mult)
                        self.V("pool", "tensor_tensor", [kk[z], E2[z]], [kE2[z]], out=kE2[z][:, 0:N], in0=kk[z][:, 0:N], in1=E2[z][:, 0:N], op=ALU.mult)
                        for cc in range(nb):
                            pi = rb()
                            for kc in range(8):
                                self.mm(pi[:, 0:128], ht[:, kc, cc * 128:(cc + 1) * 128], w[2][:, kc, :], kc == 0, kc == 7, [ht, w[2]], [pi])
                            self.act(iT[z][:, cc, :], pi[:, 0:128], AF.Copy, [pi], [iT[z]])
                            pt = rb()
                            ptv = pt[:].bitcast(BF)
                            self.tr(ptv[:, 0:128], kE2[z][:, cc * 128:(cc + 1) * 128], self.identb[:], [kE2[z], self.identb], [pt])
                            self.act(kT[z][:, cc, :], ptv[:, 0:128], AF.Copy, [pt], [kT[z]])
                        ops = B[z]
                        blks = list(range(nb)) if d == 0 else list(range(nb - 1, -1, -1))
                        for blk in blks:
                            pa = rb()
                            bs = slice(blk * 128, (blk + 1) * 128)
                            self.mm(pa[:, 0:128], kE4[z][:, bs], qE3[z][:, bs], True, True, [kE4[z], qE3[z]], [pa])
                            pb_ = rb()
                            self.mm(pb_[:, 0:128], kE4b[z][:, bs], qE3b[z][:, bs], True, True, [kE4b[z], qE3b[z]], [pb_])
                            am = AM[am_i % 2]
                            a1 = amt[am_i % 2]
                            a2 = amt2[am_i % 2]
                            am_i += 1
                            self.V("dve", "tensor_tensor", [pa, self.cst], [a1], out=a1[:], in0=pa[:, 0:128], in1=mask, op=ALU.mult)
                            self.V("dve", "tensor_tensor", [pb_, self.cst], [a2], out=a2[:], in0=pb_[:, 0:128], in1=masko, op=ALU.mult)
                            self.V("pool", "tensor_tensor", [a1, a2], [am], out=am[:], in0=a1[:], in1=a2[:], op=ALU.add)
                            for ch in ((0, 1) if d == 0 else (1, 0)):
                                p0 = ch * 64
                                c0 = blk * 128 + p0
                                self.mm(ops[:, c0:c0 + 64], Sbf[cur][:], qE1[z][:, c0:c0 + 64], True, False, [Sbf[cur], qE1[z]], [ops])
                                self.mm(ops[:, c0:c0 + 64], iT[z][p0:p0 + 64, blk, :], am[p0:p0 + 64, p0:p0 + 64], False, True, [iT[z], am], [ops])
                                pS = rb()
                                self.mm(pS[:, 0:128], kT[z][p0:p0 + 64, blk, :], iT[z][p0:p0 + 64, blk, :], True, True, [kT[z], iT[z]], [pS])
                                ce = c0 + cend
                                self.V("dve", "scalar_tensor_tensor", [S32, E1[z], pS], [S32], out=S32[:], in0=S32[:], scalar=E1[z][:, ce:ce + 1], in1=pS[:, 0:128],
                                       op0=ALU.mult, op1=ALU.add)
                                cur = 1 - cur
                                self.act(Sbf[cur][:], S32[:], AF.Copy, [S32], [Sbf[cur]])
                        if d == 0:
                            self.act(OF[:, t0:t0 + N], ops[:, 0:N], AF.Copy, [ops], [OF])
                        else:
                            self.V("dve", "tensor_tensor", [OF, ops], [osum], out=osum[:, 0:N], in0=OF[:, t0:t0 + N], in1=ops[:, 0:N], op=ALU.add)
                            self.act(sq[:, 0:N], osum[:, 0:N], AF.Square, [osum], [sq])
                            pss = rb()
                            self.mm(pss[:, 0:N], onesf, sq[:, 0:N], True, True, [self.cst, sq], [pss])
                            self.rstd_from(pss[:, 0:N], 1.0 / 128, rstd, tmp, N, [pss])
                            pg = rb()
                            self.proj(pg, w[3], 0, ht, N)
                            self.act(sgg[:, 0:N], pg[:, 0:N], AF.Sigmoid, [pg], [sgg])
                            self.V("dve", "scalar_tensor_tensor", [osum, rstd, P], [y1], out=y1[:, 0:N], in0=osum[:, 0:N], scalar=P[:, 148 + h:149 + h], in1=rstd[:, 0:N],
                                   op0=ALU.mult, op1=ALU.mult)
                            y = ys[n % 2]
                            self.V("pool", "tensor_tensor", [y1, sgg], [y], out=y[:, 0:N], in0=y1[:, 0:N], in1=sgg[:, 0:N], op=ALU.mult)
                            S.dma("sp", self.Y[0, h, :, t0:t0 + N], y[:, 0:N], [y], [Tl(None, self.Yt[0][ti])])
            S.barrier()

    def phase_merge(self, l):
        S = self.S
        i = self.i
        with contextlib.ExitStack() as es:
            wg = self.sb(es, "mgwg", [128, 8, 4096], BF)
            for k in range(4):
                S.dma("pool", wg[:, :, k * 1024:(k + 1) * 1024], i["w_in"][l, :, C_GATE + k * 1024:C_GATE + (k + 1) * 1024].rearrange("(kc p) n -> p kc n", p=128), [], [wg])
            wb = self.sb(es, "mgwb", [128, 4, 4, 1024], BF)
            for k in range(4):
                S.dma("pool", wb[:, k, :, :], i["w_branch"][l, k].rearrange("(cc p) n -> p cc n", p=128), [], [wb])
            ht = self.sb(es, "mght", [128, 8, 512], BF)
            Yk = [self.sb(es, "mgY%d" % k, [128, 4, 512], BF) for k in range(4)]
            sg = [self.sb(es, "mgsg%d" % k, [128, 512], F32) for k in range(2)]
            tmp = [self.sb(es, "mgtmp%d" % k, [128, 512], F32) for k in range(2)]
            macc = [self.sb(es, "mgacc%d" % k, [128, 512], F32) for k in range(2)]
            mT = [self.sb(es, "mgmT%d" % k, [128, 8, 512], BF) for k in range(2)]
            n = 0
            for ti, (t0, N) in enumerate(TILES):
                self.load_ht(ht, ti)
                for k in range(4):
                    S.dma("sp", Yk[k][:, :, 0:N], self.Y[k, :, :, t0:t0 + N].rearrange("j p n -> p j n"), [Tl(None, self.Yt[k][ti])], [Yk[k]])
                m = mT[ti % 2]
                for nch in range(8):
                    ma = macc[nch % 2]
                    for k in range(4):
                        pg, pp = self.psb(), self.psb()
                        self.proj(pg, wg, k * 1024 + nch * 128, ht, N)
                        for cc in range(4):
                            self.mm(pp[:, 0:N], wb[:, k, cc, nch * 128:(nch + 1) * 128], Yk[k][:, cc, 0:N], cc == 0, cc == 3, [wb, Yk[k]], [pp])
                        s_ = sg[n % 2]
                        t_ = tmp[n % 2]
                        n += 1
                        self.act(s_[:, 0:N], pg[:, 0:N], AF.Sigmoid, [pg], [s_])
                        if k == 0:
                            self.V("dve", "tensor_tensor", [pp, s_], [ma], out=ma[:, 0:N], in0=pp[:, 0:N], in1=s_[:, 0:N], op=ALU.mult)
                        else:
                            self.V("dve", "tensor_tensor", [pp, s_], [t_], out=t_[:, 0:N], in0=pp[:, 0:N], in1=s_[:, 0:N], op=ALU.mult)
                            self.V("pool", "tensor_tensor", [ma, t_], [ma], out=ma[:, 0:N], in0=ma[:, 0:N], in1=t_[:, 0:N], op=ALU.add)
                    self.V("pool", "tensor_copy", [ma], [m], out=m[:, nch, 0:N], in_=ma[:, 0:N])
                S.dma("sp", self.MT[:, :, t0:t0 + N].rearrange("k p n -> p k n"), m[:, :, 0:N], [m], [Tl(None, self.MTt[ti])])
            S.barrier()
        with contextlib.ExitStack() as es:
            wo = self.sb(es, "mgwo", [128, 8, 1024], BF)
            S.dma("pool", wo[:], i["w_out"][l].rearrange("(kc p) n -> p kc n", p=128), [], [wo])
            mts = [self.sb(es, "mgmt%d" % k, [128, 8, 512], BF) for k in range(2)]
            self.residual_setup(es, 2)
            for ti, (t0, N) in enumerate(TILES):
                mt = mts[ti % 2]
                S.dma("sp", mt[:, :, 0:N], self.MT[:, :, t0:t0 + N].rearrange("k p n -> p k n"), [Tl(None, self.MTt[ti])], [mt])
                for cc in range(N // 128):
                    c = t0 // 128 + cc
                    halves = []
                    for half in range(2):
                        po = self.psb()
                        for kc in range(8):
                            self.mm(po[:, :], mt[:, kc, cc * 128:(cc + 1) * 128], wo[:, kc, half * 512:(half + 1) * 512], kc == 0, kc == 7, [mt, wo], [po])
                        halves.append(po)
                    self.residual(c, lambda half: halves[half][:, :], halves)
            S.barrier()

    def residual_setup(self, es, modidx):
        S = self.S
        self.rmod = [self.sb(es, "rsmod%d" % k, [128, D], F32) for k in range(2)]
        for k in range(2):
            S.dma("sp", self.rmod[k][:], self.MODR[k, :, modidx * D:(modidx + 1) * D], [Tl(None, self.MODt)], [self.rmod[k]])
        self.rx = [self.sb(es, "rsx%d" % k, [128, D], F32) for k in range(2)]
        self.rtmp = [self.sb(es, "rstmp%d" % k, [128, D], F32) for k in range(2)]

    def residual(self, c, delta_ap, delta_tiles):
        S = self.S
        lat = 0 if c >= 2 else 1
        x = self.rx[c % 2]
        t = self.rtmp[c % 2]
        xt = Tl(None, self.Xt[c])
        S.dma("sp", x[:], self.X[c * 128:(c + 1) * 128, :], [xt], [x])
        for half in range(2):
            hs = slice(half * 512, (half + 1) * 512)
            self.V("dve", "tensor_tensor", [delta_tiles[half], self.rmod[lat]], [t], out=t[:, hs], in0=delta_ap(half), in1=self.rmod[lat][:, hs], op=ALU.mult)
        self.V("pool", "tensor_tensor", [x, t], [x], out=x[:], in0=x[:], in1=t[:], op=ALU.add)
        S.dma("sp", self.X[c * 128:(c + 1) * 128, :], x[:], [x], [xt])

    def route(self, c, t, Bm, h32, h32T, wr, R):
        P = self.P
        self.V("pool", "tensor_tensor", [t, Bm], [h32], out=h32[:], in0=t[:], in1=Bm[:], op=ALU.add)
        for g in range(2):
            ps = self.psb()
            for k in range(4):
                kk = g * 4 + k
                self.tr(ps[:, k * 128:(k + 1) * 128], h32[:, kk * 128:(kk + 1) * 128], self.cst[:, 0:128], [h32, self.cst], [ps])
            self.act(h32T[:, g * 4:(g + 1) * 4, :], ps[:].rearrange("p (k n) -> p k n", k=4), AF.Copy, [ps], [h32T])
        pl = self.psb()
        for kc in range(8):
            self.mm(pl[:, 0:36], h32T[:, kc, :], wr[:, kc, :], kc == 0, kc == 7, [h32T, wr], [pl])
        dv = lambda name, w_, **kw: self.V("dve", name, [R, P] + w_[1:], [w_[0]], **kw)
        self.V("dve", "tensor_tensor", [pl, P], [R], out=R[:, 0:36], in0=pl[:, 0:36], in1=P[:, 416:452], op=ALU.add)
        RR = [R]
        dv("tensor_reduce", RR, out=R[:, 36:37], in_=R[:, 0:4], axis=AX.X, op=ALU.max)
        dv("tensor_scalar", RR, out=R[:, 37:38], in0=R[:, 36:37], scalar1=-1.0, scalar2=0.0, op0=ALU.mult, op1=ALU.add)
        self.act(R[:, 44:48], R[:, 0:4], AF.Exp, [R], [R], bias=R[:, 37:38], accum_out=R[:, 38:39])
        dv("reciprocal", RR, out=R[:, 39:40], in_=R[:, 38:39])
        dv("tensor_scalar", RR, out=R[:, 40:44], in0=R[:, 0:4], scalar1=R[:, 36:37], scalar2=1.0, op0=ALU.is_equal, op1=ALU.mult)
        dv("tensor_tensor", RR, out=R[:, 48:80].rearrange("p (g e) -> p g e", g=4), in0=R[:, 4:36].rearrange("p (g e) -> p g e", g=4),
           in1=R[:, 40:44].unsqueeze(2).to_broadcast([128, 4, 8]), op=ALU.mult)
        dv("tensor_reduce", RR, out=R[:, 80:88], in_=R[:, 48:80].rearrange("p (g e) -> p e g", g=4), axis=AX.X, op=ALU.add)
        dv("tensor_reduce", RR, out=R[:, 88:89], in_=R[:, 80:88], axis=AX.X, op=ALU.max)
        dv("tensor_scalar", RR, out=R[:, 89:97], in0=R[:, 80:88], scalar1=R[:, 88:89], scalar2=1.0, op0=ALU.is_equal, op1=ALU.mult)
        dv("scalar_tensor_tensor", RR, out=R[:, 97:105], in0=R[:, 89:97], scalar=-1e30, in1=R[:, 80:88], op0=ALU.mult, op1=ALU.add)
        dv("tensor_reduce", RR, out=R[:, 105:106], in_=R[:, 97:105], axis=AX.X, op=ALU.max)
        dv("tensor_scalar", RR, out=R[:, 106:114], in0=R[:, 97:105], scalar1=R[:, 105:106], scalar2=1.0, op0=ALU.is_equal, op1=ALU.mult)
        dv("tensor_tensor", RR, out=R[:, 114:115], in0=R[:, 105:106], in1=R[:, 88:89], op=ALU.subtract)
        self.act(R[:, 115:116], R[:, 114:115], AF.Exp, [R], [R])
        dv("tensor_scalar", RR, out=R[:, 116:117], in0=R[:, 115:116], scalar1=1.0, scalar2=1.0, op0=ALU.add, op1=ALU.mult)
        dv("reciprocal", RR, out=R[:, 116:117], in_=R[:, 116:117])
        dv("tensor_tensor", RR, out=R[:, 117:118], in0=R[:, 116:117], in1=R[:, 39:40], op=ALU.mult)
        dv("tensor_tensor", RR, out=R[:, 118:119], in0=R[:, 39:40], in1=R[:, 117:118], op=ALU.subtract)
        dv("tensor_scalar", RR, out=R[:, 119:127], in0=R[:, 89:97], scalar1=R[:, 117:118], scalar2=0.0, op0=ALU.mult, op1=ALU.add)
        dv("scalar_tensor_tensor", RR, out=R[:, 119:127], in0=R[:, 106:114], scalar=R[:, 118:119], in1=R[:, 119:127], op0=ALU.mult, op1=ALU.add)
        self.V("dve", "tensor_tensor", [R], [self.RW], out=self.RW[:, c, :].rearrange("p (g e) -> p g e", g=4),
               in0=R[:, 40:44].unsqueeze(2).to_broadcast([128, 4, 8]), in1=R[:, 119:127].unsqueeze(1).to_broadcast([128, 4, 8]), op=ALU.mult)

    def phase_moe(self, l):
        S = self.S
        i = self.i
        with contextlib.ExitStack() as es:
            acc = self.sb(es, "moacc", [128, 10, D], F32)
            hTb = self.sb(es, "mohT", [128, 8, 1280], BF)
            wts = [(self.sb(es, "mowg%d" % k, [128, 8, 512], BF), self.sb(es, "mowu%d" % k, [128, 8, 512], BF),
                    self.sb(es, "mowd%d" % k, [128, 4, D], BF)) for k in range(2)]
            sG = [self.sb(es, "mosg%d" % k, [128, 512], F32) for k in range(2)]
            Hh = [self.sb(es, "moHh%d" % k, [128, 4, 512], BF) for k in range(2)]
            self.residual_setup(es, 5)
            n = 0
            m = 0
            for blk in ((0, 1, 2), (3, 4), (5, 6), (7, 8)):
                col = 0
                tcs = []
                for ti in blk:
                    t0, N = TILES[ti]
                    S.dma("sp", hTb[:, :, col:col + N], self.HT[:, :, t0:t0 + N].rearrange("k p n -> p k n"), [Tl(None, self.HTt[ti])], [hTb])
                    tcs.append((col, N, t0))
                    col += N
                for e in range(self.nexp):
                    wg, wu, wd = wts[e % 2]
                    S.dma("pool", wg[:], i["moe_w_gate"][l, e].rearrange("(kc p) f -> p kc f", p=128), [], [wg])
                    S.dma("pool", wu[:], i["moe_w_up"][l, e].rearrange("(kc p) f -> p kc f", p=128), [], [wu])
                    S.dma("pool", wd[:], i["moe_w_down"][l, e].rearrange("(fc p) n -> p fc n", p=128), [], [wd])
                    for (col, N, t0) in tcs:
                        hh = Hh[m % 2]
                        m += 1
                        for fc in range(4):
                            pG, pU = self.psb(), self.psb()
                            for kc in range(8):
                                self.mm(pG[:, 0:N], wg[:, kc, fc * 128:(fc + 1) * 128], hTb[:, kc, col:col + N], kc == 0, kc == 7, [wg, hTb], [pG])
                            for kc in range(8):
                                self.mm(pU[:, 0:N], wu[:, kc, fc * 128:(fc + 1) * 128], hTb[:, kc, col:col + N], kc == 0, kc == 7, [wu, hTb], [pU])
                            sg = sG[n % 2]
                            n += 1
                            self.act(sg[:, 0:N], pG[:, 0:N], AF.Silu, [pG], [sg])
                            self.V("dve", "tensor_tensor", [sg, pU], [hh], out=hh[:, fc, 0:N], in0=sg[:, 0:N], in1=pU[:, 0:N], op=ALU.mult)
                        for cc in range(N // 128):
                            ci = col // 128 + cc
                            c = t0 // 128 + cc
                            for half in range(2):
                                hs = slice(half * 512, (half + 1) * 512)
                                pD = self.psb()
                                for fc in range(4):
                                    self.mm(pD[:, :], hh[:, fc, cc * 128:(cc + 1) * 128], wd[:, fc, hs], fc == 0, fc == 3, [hh, wd], [pD])
                                if e == 0:
                                    self.V("dve", "tensor_scalar", [pD, self.RW], [acc], out=acc[:, ci, hs], in0=pD[:, :], scalar1=self.RW[:, c, e:e + 1], scalar2=0.0,
                                           op0=ALU.mult, op1=ALU.add)
                                else:
                                    self.V("dve", "scalar_tensor_tensor", [pD, self.RW, acc], [acc], out=acc[:, ci, hs], in0=pD[:, :], scalar=self.RW[:, c, e:e + 1],
                                           in1=acc[:, ci, hs], op0=ALU.mult, op1=ALU.add)
                for (col, N, t0) in tcs:
                    for cc in range(N // 128):
                        ci = col // 128 + cc
                        c = t0 // 128 + cc
                        self.residual(c, lambda half, ci=ci: acc[:, ci, half * 512:(half + 1) * 512], [acc, acc])
            S.barrier()

    def rank(self, c, R):
        A = R[:, 128:160]
        self.V("dve", "tensor_scalar", [self.RW], [R], out=A, in0=self.RW[:, c, :], scalar1=0.0, scalar2=1.0, op0=ALU.is_gt, op1=ALU.mult)
        if c == 0:
            self.V("dve", "memset", [], [self.Asum], self.Asum[:], 0.0)
        ps = self.psb()
        self.mm(ps[:, 0:32], self.ltri[:], A, True, False, [self.ltri, R], [ps])
        self.mm(ps[:, 0:32], self.cst[:, 128:256], self.Asum[:], False, True, [self.cst, self.Asum], [ps])
        self.act(self.RK[:, c, :], ps[:, 0:32], AF.Copy, [ps], [self.RK])
        self.V("dve", "tensor_tensor", [self.Asum, R], [self.Asum], out=self.Asum[:], in0=self.Asum[:], in1=A, op=ALU.add)

    def phase_moe_sparse(self, l):
        S = self.S
        i = self.i
        onesf = self.cst[:, 128:256]
        rows_t = Tl(None, self.ROWSt)
        acc_t = Tl(None, self.ACC2t)
        h2_t = Tl(None, self.H2t)
        with contextlib.ExitStack() as es:
            G = self.sb(es, "spG", [128, 1024], F32)
            widx = self.sb(es, "spwidx", [128, 2, NB], U32)
            init = self.sb(es, "spinit", [128, 128, 4], F32)
            self.V("pool", "memset", [], [init], init[:], 0.0)
            self.V("pool", "memset", [init], [init], init[:, :, 0:1], float(T))
            self.V("pool", "memset", [init], [init], init[:, :, 2:4], 1.0e6)
            S.dma("sp", self.ROWS.rearrange("(j p) c -> j (p c)", p=128), init[0:NB, :, :].rearrange("j p c -> j (p c)"), [init], [rows_t])
            ps = self.psb()
            self.mm(ps[:, 0:32], onesf, self.Asum[:], True, True, [self.cst, self.Asum], [ps])
            cnt, pad, pend, pst = G[:, 0:32], G[:, 32:64], G[:, 64:96], G[:, 96:128]
            cmp = self.sb(es, "spcmp", [128, NB, 32], F32)
            self.V("dve", "tensor_copy", [ps], [G], out=cnt, in_=ps[:, 0:32])
            cmp2 = cmp[:].rearrange("p a b -> p (a b)")[:, 0:32 * 68].rearrange("p (e m) -> p e m", m=68)
            self.V("dve", "tensor_tensor", [G, self.cst], [cmp], out=cmp2, in0=cnt.unsqueeze(2).to_broadcast([128, 32, 68]),
                   in1=self.cst[:, 896:896 + 68].unsqueeze(1).to_broadcast([128, 32, 68]), op=ALU.is_gt)
            self.V("dve", "tensor_reduce", [cmp], [G], out=pad, in_=cmp2, axis=AX.X, op=ALU.add)
            self.V("dve", "tensor_scalar", [G], [G], out=pad, in0=pad, scalar1=128.0, scalar2=0.0, op0=ALU.mult, op1=ALU.add)
            self.V("dve", "tensor_tensor_scan", [G, self.cst], [G], out=pend, data0=onesf[:, 0:32], data1=pad, initial=0.0, op0=ALU.mult, op1=ALU.add)
            self.V("dve", "tensor_tensor", [G], [G], out=pst, in0=pend, in1=pad, op=ALU.subtract)
            self.V("dve", "tensor_tensor", [G, self.cst], [cmp], out=cmp[:], in0=pend.unsqueeze(1).to_broadcast([128, NB, 32]),
                   in1=self.cst[:, 896:896 + NB].unsqueeze(2).to_broadcast([128, NB, 32]), op=ALU.is_le)
            be = G[:, 128:128 + NB]
            self.V("dve", "tensor_reduce", [cmp], [G], out=be, in_=cmp[:], axis=AX.X, op=ALU.add)
            self.V("dve", "tensor_scalar", [G], [G], out=be, in0=be, scalar1=31.0, scalar2=128.0, op0=ALU.min, op1=ALU.mult)
            same = G[:, 384:384 + NB]
            self.V("dve", "memset", [G], [G], same, 0.0)
            self.V("dve", "tensor_tensor", [G], [G], out=G[:, 386:384 + NB], in0=G[:, 130:128 + NB], in1=G[:, 128:126 + NB], op=ALU.is_equal)
            wf = G[:, 256:256 + NB]
            self.V("dve", "tensor_scalar", [G, self.cst], [G], out=wf, in0=be, scalar1=self.cst[:, 1024:1025], scalar2=2.0, op0=ALU.add, op1=ALU.mult)
            self.V("dve", "tensor_scalar", [G], [G], out=wf, in0=wf, scalar1=float(l * 8192), scalar2=1.0, op0=ALU.add, op1=ALU.mult)
            self.V("dve", "scalar_tensor_tensor", [G], [G], out=wf, in0=same, scalar=1.0e8, in1=wf, op0=ALU.mult, op1=ALU.add)
            self.V("dve", "tensor_copy", [G], [widx], out=widx[:, 0, :], in_=wf)
            self.V("dve", "tensor_scalar", [G], [G], out=wf, in0=wf, scalar1=1.0, scalar2=1.0, op0=ALU.add, op1=ALU.mult)
            self.V("dve", "tensor_copy", [G], [widx], out=widx[:, 1, :], in_=wf)
            Q = [self.sb(es, "spQ%d" % k, [128, 160], F32) for k in range(2)]
            rec = [self.sb(es, "sprec%d" % k, [128, 2, 4], F32) for k in range(2)]
            didx = [self.sb(es, "spdidx%d" % k, [128, 2], U32) for k in range(2)]
            for c in range(NCH):
                q = Q[c % 2]
                r_ = rec[c % 2]
                di = didx[c % 2]
                A, dst, d1, m1 = q[:, 0:32], q[:, 32:64], q[:, 64:96], q[:, 96:128]
                rd = [self.RW, self.RK, G, q]
                self.V("dve", "tensor_scalar", rd, [q], out=A, in0=self.RW[:, c, :], scalar1=0.0, scalar2=1.0, op0=ALU.is_gt, op1=ALU.mult)
                self.V("dve", "tensor_tensor", rd, [q], out=dst, in0=self.RK[:, c, :], in1=pst, op=ALU.add)
                self.V("dve", "scalar_tensor_tensor", rd, [q], out=d1, in0=dst, scalar=1.0, in1=A, op0=ALU.add, op1=ALU.mult)
                self.V("dve", "tensor_reduce", rd, [q], out=q[:, 128:129], in_=d1, axis=AX.X, op=ALU.max)
                self.V("dve", "tensor_scalar", rd, [q], out=m1, in0=d1, scalar1=q[:, 128:129], scalar2=1.0, op0=ALU.is_equal, op1=ALU.mult)
                self.V("dve", "tensor_tensor", rd, [q], out=m1, in0=m1, in1=self.RW[:, c, :], op=ALU.mult)
                self.V("dve", "tensor_reduce", rd, [q], out=q[:, 129:130], in_=m1, axis=AX.X, op=ALU.add)
                self.V("dve", "tensor_reduce", rd, [q], out=q[:, 130:131], in_=self.RW[:, c, :], axis=AX.X, op=ALU.add)
                self.V("dve", "tensor_scalar", rd, [q], out=m1, in0=A, scalar1=-1.0e9, scalar2=1.0e9, op0=ALU.mult, op1=ALU.add)
                self.V("dve", "tensor_tensor", rd, [q], out=m1, in0=m1, in1=dst, op=ALU.add)
                self.V("dve", "tensor_reduce", rd, [q], out=q[:, 131:132], in_=m1, axis=AX.X, op=ALU.min)
                self.V("dve", "tensor_scalar", rd, [q], out=q[:, 132:133], in0=q[:, 128:129], scalar1=-1.0, scalar2=1.0, op0=ALU.add, op1=ALU.mult)
                self.V("dve", "memset", [], [r_], r_[:], 0.0)
                for k in range(2):
                    self.V("dve", "tensor_scalar", [self.cst, r_], [r_], out=r_[:, k, 0:1], in0=self.cst[:, 1024:1025], scalar1=float(c * 128), scalar2=1.0,
                           op0=ALU.add, op1=ALU.mult)
                    self.V("dve", "tensor_scalar", [self.cst, r_], [r_], out=r_[:, k, 2:3], in0=self.cst[:, 1024:1025], scalar1=float(c * 128 + k * T), scalar2=2.0,
                           op0=ALU.add, op1=ALU.mult)
                    self.V("dve", "tensor_scalar", [r_], [r_], out=r_[:, k, 3:4], in0=r_[:, k, 2:3], scalar1=1.0, scalar2=1.0, op0=ALU.add, op1=ALU.mult)
                self.V("dve", "tensor_tensor", [q, r_], [r_], out=r_[:, 0, 1:2], in0=q[:, 130:131], in1=q[:, 129:130], op=ALU.subtract)
                self.V("dve", "tensor_copy", [q, r_], [r_], out=r_[:, 1, 1:2], in_=q[:, 129:130])
                self.V("dve", "tensor_copy", [q], [di], out=di[:, 0:1], in_=q[:, 131:132])
                self.V("dve", "tensor_copy", [q], [di], out=di[:, 1:2], in_=q[:, 132:133])
                for k in range(2):
                    S.dma_fn("pool", (lambda e, r_=r_, di=di, k=k: e.indirect_dma_start(out=self.ROWS, out_offset=bass.IndirectOffsetOnAxis(ap=di[:, k:k + 1], axis=0),
                                                                                      in_=r_[:, k, :], in_offset=None)), [r_, di], [rows_t])
            wgv = i["moe_w_gate"].rearrange("l e (p j) f -> (l e p) (j f)", j=8).rearrange("r (h x) -> (r h) x", h=2)
            wuv = i["moe_w_up"].rearrange("l e (p j) f -> (l e p) (j f)", j=8).rearrange("r (h x) -> (r h) x", h=2)
            wdv = i["moe_w_down"].rearrange("l e (p j) n -> (l e p) (j n)", j=4).rearrange("r (h x) -> (r h) x", h=2)
            wts = [(self.sb(es, "spwg%d" % k, [128, 8, 512], BF), self.sb(es, "spwu%d" % k, [128, 8, 512], BF),
                    self.sb(es, "spwd%d" % k, [128, 4, D], BF)) for k in range(2)]
            NQ = 4
            recs = [self.sb(es, "sprc%d" % k, [128, 4], F32) for k in range(NQ)]
            recu = [self.sb(es, "spru%d" % k, [128, 4], U32) for k in range(NQ)]
            hbs = [self.sb(es, "sphb%d" % k, [128, D], BF) for k in range(NQ)]
            hTs = [self.sb(es, "sphT%d" % k, [128, 8, 128], BF) for k in range(NQ)]
            sGs = [self.sb(es, "spsg%d" % k, [128, 512], F32) for k in range(2)]
            Hhs = [self.sb(es, "spHh%d" % k, [128, 512], BF) for k in range(2)]
            HhTs = [self.sb(es, "spHhT%d" % k, [128, 4, 128], BF) for k in range(2)]
            ys = [self.sb(es, "spy%d" % k, [128, D], F32) for k in range(2)]
            def gather(dst_ap, src, idx_ap, r, w, skip=False):
                if skip:
                    S.dma_fn("pool", (lambda e: e.indirect_dma_start(out=dst_ap, out_offset=None, in_=src, in_offset=bass.IndirectOffsetOnAxis(ap=idx_ap, axis=0),
                                                                     bounds_check=self._wbound_reg(e), oob_is_err=False)), r, w)
                else:
                    S.dma_fn("pool", (lambda e: e.indirect_dma_start(out=dst_ap, out_offset=None, in_=src, in_offset=bass.IndirectOffsetOnAxis(ap=idx_ap, axis=0))), r, w)

            def proA(j):
                rc, ru, hb = recs[j % NQ], recu[j % NQ], hbs[j % NQ]
                S.dma("sp", rc[:], self.ROWS[j * 128:(j + 1) * 128, :], [rows_t], [rc])
                self.V("dve", "tensor_copy", [rc], [ru], out=ru[:], in_=rc[:])
                gather(hb[:], self.H2, ru[:, 0:1], [ru, h2_t], [hb])

            def proB(j):
                hb, hT = hbs[j % NQ], hTs[j % NQ]
                pt = self.psb()
                ptv = pt[:].bitcast(BF).rearrange("p (k n) -> p k n", k=8)
                hbv = hb[:].rearrange("t (p j) -> t p j", j=8)
                for jx in range(8):
                    self.tr(ptv[:, jx, :], hbv[:, :, jx], self.identb[:], [hb, self.identb], [pt])
                self.act(hT[:], ptv, AF.Copy, [pt], [hT])

            def wload(j):
                wg, wu, wd = wts[j % 2]
                for h_ in range(2):
                    gather(wg[:, h_ * 4:(h_ + 1) * 4, :].rearrange("p j f -> p (j f)"), wgv, widx[:, h_, j:j + 1], [widx], [wg], skip=True)
                    gather(wu[:, h_ * 4:(h_ + 1) * 4, :].rearrange("p j f -> p (j f)"), wuv, widx[:, h_, j:j + 1], [widx], [wu], skip=True)
                    gather(wd[:, h_ * 2:(h_ + 1) * 2, :].rearrange("p j f -> p (j f)"), wdv, widx[:, h_, j:j + 1], [widx], [wd], skip=True)

            proA(0)
            proA(1)
            wload(0)
            proB(0)
            for j in range(NB):
                z = j % 2
                rc, ru, hT = recs[j % NQ], recu[j % NQ], hTs[j % NQ]
                sg, Hh, HhT, y = sGs[z], Hhs[z], HhTs[z], ys[z]
                wg, wu, wd = wts[z]
                if j + 2 < NB:
                    proA(j + 2)
                if j + 1 < NB:
                    wload(j + 1)
                pG, pU = self.psb(), self.psb()
                for jx in range(8):
                    self.mm(pG[:, :], hT[:, jx, :], wg[:, jx, :], jx == 0, jx == 7, [hT, wg], [pG])
                for jx in range(8):
                    self.mm(pU[:, :], hT[:, jx, :], wu[:, jx, :], jx == 0, jx == 7, [hT, wu], [pU])
                self.act(sg[:], pG[:, :], AF.Silu, [pG], [sg])
                self.V("dve", "scalar_tensor_tensor", [pU, rc, sg], [Hh], out=Hh[:], in0=pU[:, :], scalar=rc[:, 1:2], in1=sg[:], op0=ALU.mult, op1=ALU.mult)
                if j + 1 < NB:
                    proB(j + 1)
                pt2 = self.psb()
                pt2v = pt2[:].bitcast(BF)[:, 0:512].rearrange("p (k n) -> p k n", k=4)
                Hhv = Hh[:].rearrange("t (p j) -> t p j", j=4)
                for jx in range(4):
                    self.tr(pt2v[:, jx, :], Hhv[:, :, jx], self.identb[:], [Hh, self.identb], [pt2])
                self.act(HhT[:], pt2v, AF.Copy, [pt2], [HhT])
                for half in range(2):
                    pD = self.psb()
                    for jx in range(4):
                        self.mm(pD[:, :], HhT[:, jx, :], wd[:, jx, half * 512:(half + 1) * 512], jx == 0, jx == 3, [HhT, wd], [pD])
                    if half == 0:
                        self.act(y[:, 0:512], pD[:, :], AF.Copy, [pD], [y])
                    else:
                        self.V("dve", "tensor_copy", [pD], [y], out=y[:, 512:1024], in_=pD[:, :])
                for h_ in range(2):
                    S.dma_fn("pool", (lambda e, y=y, ru=ru, h_=h_: e.indirect_dma_start(out=self.ACC2.rearrange("r (h x) -> (r h) x", h=2),
                                                                                        out_offset=bass.IndirectOffsetOnAxis(ap=ru[:, 2 + h_:3 + h_], axis=0),
                                                                                        in_=y[:, h_ * 512:(h_ + 1) * 512], in_offset=None,
                                                                                        bounds_check=self._bound_reg(e), oob_is_err=False)), [y, ru], [acc_t])
            self.residual_setup(es, 5)
            a0 = [self.sb(es, "spa0%d" % k, [128, D], F32) for k in range(2)]
            a1 = [self.sb(es, "spa1%d" % k, [128, D], F32) for k in range(2)]
            for c in range(NCH):
                u0, u1 = a0[c % 2], a1[c % 2]
                S.dma("sp", u0[:], self.ACC2[c * 128:(c + 1) * 128, :], [acc_t], [u0])
                S.dma("sp", u1[:], self.ACC2[T + c * 128:T + (c + 1) * 128, :], [acc_t], [u1])
                self.V("pool", "tensor_tensor", [u0, u1], [u0], out=u0[:], in0=u0[:], in1=u1[:], op=ALU.add)
                self.residual(c, lambda half, u0=u0: u0[:, half * 512:(half + 1) * 512], [u0, u0])
            S.barrier()

    def _wbound_reg(self, e):
        if getattr(self, "_wbreg", None) is None:
            self._wbreg = e.to_reg(self.n_layers * 32 * 128 * 2 - 1)
        return self._wbreg

    def _bound_reg(self, e):
        if getattr(self, "_breg", None) is None:
            self._breg = e.to_reg(4 * T - 1)
        return self._breg

    def final_norm(self):
        S = self.S
        with contextlib.ExitStack() as es:
            g = self.sb(es, "fng", [128, D], F32)
            S.dma("sp", g[:], self.i["final_g"].partition_broadcast(128), [], [g])
            xs = [self.sb(es, "fnx%d" % k, [128, D], F32) for k in range(3)]
            sq = self.sb(es, "fnsq", [128, D], F32)
            st = [self.sb(es, "fnst%d" % k, [128, 4], F32) for k in range(2)]
            ot = [self.sb(es, "fno%d" % k, [128, D], F32) for k in range(2)]
            for c in range(2, NCH):
                x = xs[c % 3]
                S.dma("sp", x[:], self.X[c * 128:(c + 1) * 128, :], [Tl(None, self.Xt[c])], [x])
                s_ = st[c % 2]
                self.act(sq[:], x[:], AF.Square, [x], [sq, s_], accum_out=s_[:, 0:1])
                self.V("dve", "tensor_scalar", [s_], [s_], out=s_[:, 1:2], in0=s_[:, 0:1], scalar1=1.0 / D, scalar2=EPS, op0=ALU.mult, op1=ALU.add)
                self.V("dve", "reciprocal", [s_], [s_], out=s_[:, 2:3], in_=s_[:, 1:2])
                self.act(s_[:, 3:4], s_[:, 2:3], AF.Sqrt, [s_], [s_])
                o = ot[c % 2]
                self.V("dve", "scalar_tensor_tensor", [x, s_, g], [o], out=o[:], in0=x[:], scalar=s_[:, 3:4], in1=g[:], op0=ALU.mult, op1=ALU.mult)
                S.dma("sp", self.out[(c - 2) * 128:(c - 1) * 128, :], o[:], [o], [Tl(None, self.outt)])


def make_consts():
    cst = np.zeros((128, 1056), np.float32)
    cst[:, 0:128] = np.eye(128, dtype=np.float32)
    cst[:, 128:256] = 1.0
    R = np.zeros((128, 128), np.float32)
    for blk in range(2):
        o = blk * 64
        for q in range(16):
            R[o + 16 + q, o + q] = -1.0
            R[o + q, o + 16 + q] = 1.0
            R[o + 48 + q, o + 32 + q] = -1.0
            R[o + 32 + q, o + 48 + q] = 1.0
    cst[:, 256:384] = R
    s = np.arange(128)[:, None]
    t = np.arange(128)[None, :]
    same = (s // 64) == (t // 64)
    same32 = (s // 32) == (t // 32)
    cst[:, 384:512] = (same32 & (t >= s)).astype(np.float32)
    cst[:, 512:640] = (same32 & (t <= s)).astype(np.float32)
    cst[:, 640:768] = (same & (s % 64 < 32) & (t % 64 >= 32)).astype(np.float32)
    cst[:, 768:896] = (same & (s % 64 >= 32) & (t % 64 < 32)).astype(np.float32)
    cst[:, 896:1024] = 128.0 * np.arange(128, dtype=np.float32)[None, :]
    cst[:, 1024] = np.arange(128, dtype=np.float32)
    cst[:, 1025:1057 - 1 + 0] = 0.0
    inv_freq = (10000.0 ** (-np.arange(0, 32, 2, dtype=np.float32) / 32)).astype(np.float32)
    pos = np.arange(NLAT)
    row = (pos // 64).astype(np.float32)
    col = (pos % 64).astype(np.float32)
    ang_r = row[:, None] * inv_freq
    ang_c = col[:, None] * inv_freq
    ang = np.concatenate([ang_r, ang_r, ang_c, ang_c], axis=-1).astype(np.float32)
    rope = np.zeros((2, 128, T), np.float32)
    rope[0, :, :NCTX] = 1.0
    rope[0, 0:64, NCTX:] = np.cos(ang).T
    rope[0, 64:128, NCTX:] = np.cos(ang).T
    rope[1, 0:64, NCTX:] = np.sin(ang).T
    rope[1, 64:128, NCTX:] = np.sin(ang).T
    return cst, rope


def make_in_maps(inputs, cores, L=DEPTH, nexp=32):
    f = lambda a: np.ascontiguousarray(np.asarray(a, dtype=np.float32))
    cst, rope = make_consts()
    shared = {
        "c_ctx": f(inputs["c_ctx"]).reshape(1, D),
        "ada_w": f(inputs["ada_w"][:L]), "ada_b": f(inputs["ada_b"]),
        "norm1_g": f(inputs["norm1_g"]), "norm2_g": f(inputs["norm2_g"]),
        "w_in": f(inputs["w_in"][:L]), "w_branch": f(inputs["w_branch"][:L]), "w_out": f(inputs["w_out"][:L]),
        "hg_lb": f(inputs["hg_lb_logits"]), "hg_norm_g": f(inputs["hg_norm_g"]),
        "da_lambda": f(inputs["da_lambda"]).reshape(DEPTH, 256), "da_norm_g": f(inputs["da_norm_g"]),
        "cv_dw_w": f(inputs["cv_dw_w"]), "cv_dw_b": f(inputs["cv_dw_b"]),
        "cv_ln_g": f(inputs["cv_ln_g"]), "cv_ln_b": f(inputs["cv_ln_b"]),
        "sc_w": f(inputs["sc_w"]),
        "moe_w_r": np.ascontiguousarray(np.concatenate([f(inputs["moe_w_grp"]), f(inputs["moe_w_exp"])], axis=-1)),
        "moe_b_r": np.ascontiguousarray(np.concatenate([f(inputs["moe_b_grp"]), f(inputs["moe_b_exp"])], axis=-1)),
        "moe_w_gate": f(inputs["moe_w_gate"][:L, :nexp]), "moe_w_up": f(inputs["moe_w_up"][:L, :nexp]), "moe_w_down": f(inputs["moe_w_down"][:L, :nexp]),
        "final_g": f(inputs["final_g"]).reshape(1, D),
        "cst": cst, "rope": rope, "ltri": np.triu(np.ones((128, 128), np.float32), 1),
    }
    maps = []
    for cid in cores:
        b = cid % 4
        m = dict(shared)
        m["x"] = f(inputs["x"][b])
        m["c"] = f(inputs["c"][b]).reshape(1, D)
        m["ctx"] = f(inputs["ctx"][b])
        maps.append(m)
    return maps


def kernel(**inputs):
    nc = bass.Bass("TRN2", target_bir_lowering=False)
    Prog(nc).build()
    maps = make_in_maps(inputs, list(range(4)))
    res = run_bass_kernel_spmd(nc, maps, core_ids=list(range(4)))
    return np.stack([np.asarray(res.results[b]["out"], dtype=np.float32) for b in range(4)], axis=0)
```

```python
import contextlib
import math
import numpy as np
import concourse.bass as bass
import concourse.mybir as mybir
from concourse.bass_utils import run_bass_kernel_spmd

F32 = mybir.dt.float32
BF = mybir.dt.bfloat16
U32 = mybir.dt.uint32
AF = mybir.ActivationFunctionType
ALU = mybir.AluOpType
AX = mybir.AxisListType

D = 1024
NCTX = 256
NLAT = 4096
T = NCTX + NLAT
NCH = T // 128
NB = 2 * T // 128 + 32
SPARSE = True
DEPTH = 4
W = 512
INW = 10752
EPS = 1e-6
TILES = [(0, 256)] + [(256 + 512 * i, 512) for i in range(8)]
C_HQ, C_HI, C_HFF, C_HFB, C_HG = 0, 512, 1024, 1536, 2048
C_DQ, C_DK, C_DV = 2560, 3072, 3584
C_CVA, C_CVG = 4096, 4608
C_SB, C_SC, C_SX = 5120, 5632, 6144
C_GATE = 6656


class Trk:
    __slots__ = ("w", "r")

    def __init__(self):
        self.w = None
        self.r = {}


class Tl:
    def __init__(self, h, trk=None):
        self.h = h
        self.t = trk or Trk()

    def __getitem__(self, k):
        return self.h[k]


class Stream:
    def __init__(self, name):
        self.name = name
        self.ops = []
        self.seen = {}
        self.sem = None
        self.cnt = 0
        self.dslots = []
        self.dnext = 0


class Sch:
    SEM_MAX = 30000

    def __init__(self, nc, es):
        self.nc = nc
        self.es = es
        self.st = {k: Stream(k) for k in ("pe", "act", "dve", "pool", "sp")}
        self.nsem = 0
        for k, s in self.st.items():
            self._newsem(s)
        for k, n in (("sp", 24), ("pool", 12), ("act", 6)):
            s = self.st[k]
            for i in range(n):
                s.dslots.append([self._sem(), 0])

    def _sem(self):
        self.nsem += 1
        return self.es.enter_context(self.nc.semaphore("s%d" % self.nsem))

    def _newsem(self, s):
        s.sem = self._sem()
        s.cnt = 0

    def _need(self, s, tok, waits):
        if tok is None:
            return
        sem, val, src = tok
        if src == "pe" and s.name == "pe":
            return
        if s.seen.get(id(sem), 0) >= val:
            return
        k = id(sem)
        if k not in waits or waits[k][1] < val:
            waits[k] = (sem, val)

    def _deps(self, s, reads, writes):
        waits = {}
        for b in reads:
            self._need(s, b.t.w, waits)
        for b in writes:
            self._need(s, b.t.w, waits)
            for tok in b.t.r.values():
                self._need(s, tok, waits)
        for k, (sem, val) in waits.items():
            s.seen[k] = val
        return list(waits.values())

    def _mark(self, tok, reads, writes):
        for b in reads:
            b.t.r[id(tok[0])] = tok
        for b in writes:
            b.t.w = tok
            b.t.r = {}

    def op(self, eng, fn, reads=(), writes=()):
        s = self.st[eng]
        if s.cnt >= self.SEM_MAX:
            self._newsem(s)
        waits = self._deps(s, reads, writes)
        s.cnt += 1
        tok = (s.sem, s.cnt, eng)
        s.ops.append((waits, fn, (s.sem, 1)))
        self._mark(tok, reads, writes)
        return tok

    def dma(self, q, out, in_, reads=(), writes=(), **kw):
        return self.dma_fn(q, (lambda e: e.dma_start(out=out, in_=in_, **kw)), reads, writes)

    def dma_fn(self, q, fn, reads=(), writes=()):
        s = self.st[q]
        slot = s.dslots[s.dnext % len(s.dslots)]
        s.dnext += 1
        waits = self._deps(s, reads, writes)
        if slot[1] > 0 and s.seen.get(id(slot[0]), 0) < slot[1]:
            waits.append((slot[0], slot[1]))
            s.seen[id(slot[0])] = slot[1]
        slot[1] += 16
        tok = (slot[0], slot[1], "dma")
        s.ops.append((waits, fn, (slot[0], 16)))
        self._mark(tok, reads, writes)
        return tok

    def barrier(self):
        toks = []
        for s in self.st.values():
            if s.cnt > 0:
                toks.append((s.sem, s.cnt))
            for sl in s.dslots:
                if sl[1] > 0:
                    toks.append((sl[0], sl[1]))
        for s in self.st.values():
            waits = []
            for sem, val in toks:
                if sem is s.sem:
                    continue
                if s.seen.get(id(sem), 0) < val:
                    waits.append((sem, val))
                    s.seen[id(sem)] = val
            if waits:
                s.ops.append((waits, None, None))

    def emit(self):
        nc = self.nc
        self.barrier()
        with nc.Block() as block:
            def run(s):
                def f(e):
                    for waits, fn, inc in s.ops:
                        for sem, val in waits:
                            e.wait_ge(sem, val)
                        if fn is not None:
                            fn(e).then_inc(inc[0], inc[1])
                return f
            block.tensor(run(self.st["pe"]))
            block.scalar(run(self.st["act"]))
            block.vector(run(self.st["dve"]))
            block.gpsimd(run(self.st["pool"]))
            block.sync(run(self.st["sp"]))


class Prog:
    def __init__(self, nc, n_layers=DEPTH, dbg=None, nexp=32):
        self.nc = nc
        self.nexp = nexp
        self.n_layers = n_layers
        self.dbg = dbg or {}

    def sb(self, es, name, shape, dt):
        self.uid = getattr(self, "uid", 0) + 1
        return Tl(es.enter_context(self.nc.sbuf_tensor("%s_u%d" % (name, self.uid), list(shape), dt)))

    def dram(self, name, shape, dt, kind="Internal"):
        return self.nc.dram_tensor(name, list(shape), dt, kind=kind).ap()

    def mm(self, out, lhsT, rhs, start, stop, r, w):
        self.S.op("pe", lambda e: e.matmul(out, lhsT=lhsT, rhs=rhs, start=start, stop=stop), r, w)

    def tr(self, out, in_, ident, r, w):
        self.S.op("pe", lambda e: e.transpose(out=out, in_=in_, identity=ident), r, w)

    def act(self, out, in_, func, r, w, **kw):
        self.S.op("act", lambda e: e.activation(out=out, in_=in_, func=func, **kw), r, w)

    def V(self, eng, name, r, w, *a, **kw):
        self.S.op(eng, lambda e: getattr(e, name)(*a, **kw), r, w)

    def psb(self):
        b = self.banks[self.bi % 8]
        self.bi += 1
        return b

    def build(self):
        nc = self.nc
        L = self.n_layers
        i = {}
        def inp(name, shape, dt=F32):
            i[name] = self.dram(name, shape, dt, kind="ExternalInput")
        inp("x", [NLAT, D]); inp("c", [1, D]); inp("ctx", [NCTX, D]); inp("c_ctx", [1, D])
        inp("ada_w", [L, D, 6 * D]); inp("ada_b", [DEPTH, 6 * D])
        inp("norm1_g", [DEPTH, D]); inp("norm2_g", [DEPTH, D])
        inp("w_in", [L, D, INW]); inp("w_branch", [L, 4, W, D]); inp("w_out", [L, D, D])
        inp("hg_lb", [DEPTH, 2, W]); inp("hg_norm_g", [DEPTH, W])
        inp("da_lambda", [DEPTH, 256]); inp("da_norm_g", [DEPTH, 128])
        inp("cv_dw_w", [DEPTH, 31, W]); inp("cv_dw_b", [DEPTH, W]); inp("cv_ln_g", [DEPTH, W]); inp("cv_ln_b", [DEPTH, W])
        inp("sc_w", [DEPTH, 3, W])
        inp("moe_w_r", [DEPTH, D, 36]); inp("moe_b_r", [DEPTH, 36])
        inp("moe_w_gate", [L, self.nexp, D, W]); inp("moe_w_up", [L, self.nexp, D, W]); inp("moe_w_down", [L, self.nexp, W, D])
        inp("final_g", [1, D]); inp("ltri", [128, 128])
        inp("cst", [128, 1056]); inp("rope", [2, 128, T])
        self.i = i
        self.out = self.dram("out", [NLAT, D], F32, kind="ExternalOutput")
        self.X = self.dram("Xs", [T, D], F32)
        self.HT = self.dram("HTs", [8, 128, T], BF)
        self.Y = self.dram("Ys", [4, 4, 128, T], BF)
        self.MT = self.dram("MTs", [8, 128, T], BF)
        self.MODR = self.dram("MODs", [2, 128, 6 * D], F32)
        self.H2 = self.dram("H2s", [T + 1, D], BF)
        self.ROWS = self.dram("ROWSs", [NB * 128, 4], F32)
        self.ACC2 = self.dram("ACC2s", [2 * T, D], F32)
        self.H2t = Trk(); self.ROWSt = Trk(); self.ACC2t = Trk()
        self.Xt = [Trk() for _ in range(NCH)]
        self.HTt = [Trk() for _ in range(9)]
        self.Yt = [[Trk() for _ in range(9)] for _ in range(4)]
        self.MTt = [Trk() for _ in range(9)]
        self.MODt = Trk()
        self.outt = Trk()
        dbg_out = {}
        for k, shp in self.dbg.items():
            if not isinstance(shp, tuple):
                continue
            dbg_out[k] = self.dram("dbg_" + k, shp[0], shp[1], kind="ExternalOutput")
        self.dbg_out = dbg_out

        with contextlib.ExitStack() as es:
            self.S = S = Sch(nc, es)
            self.banks = [Tl(es.enter_context(nc.psum_tensor("pb%d" % k, [128, 512], F32))) for k in range(8)]
            self.bi = 0
            self.cst = self.sb(es, "cst_sb", [128, 1056], F32)
            S.dma("sp", self.cst[:], i["cst"], [], [self.cst])
            self.identb = self.sb(es, "identb", [128, 128], BF)
            self.onesb = self.sb(es, "onesb", [128, 128], BF)
            self.Rb = self.sb(es, "Rb", [128, 128], BF)
            self.identf = Tl(self.cst.h, self.cst.t)
            self.V("dve", "tensor_copy", [self.cst], [self.identb], out=self.identb[:], in_=self.cst[:, 0:128])
            self.V("dve", "tensor_copy", [self.cst], [self.onesb], out=self.onesb[:], in_=self.cst[:, 128:256])
            self.V("dve", "tensor_copy", [self.cst], [self.Rb], out=self.Rb[:], in_=self.cst[:, 256:384])
            self.sT = []
            for which, src in enumerate((i["c"], i["c_ctx"])):
                cT = self.sb(es, "cT%d" % which, [128, 8], F32)
                S.dma("sp", cT[:], src.rearrange("o (kc p) -> p (o kc)", p=128), [], [cT], allow_slow_non_contiguous=True)
                sg = self.sb(es, "cS%d" % which, [128, 8], F32)
                self.act(sg[:], cT[:], AF.Silu, [cT], [sg])
                rep = self.sb(es, "sT%d" % which, [128, 8, 128], F32)
                self.V("dve", "tensor_copy", [sg], [rep], out=rep[:], in_=sg[:].unsqueeze(2).to_broadcast([128, 8, 128]))
                self.sT.append(rep)
            self.P = self.sb(es, "Pparams", [128, 600], F32)
            self.RW = self.sb(es, "RW", [128, NCH, 32], F32)
            self.RK = self.sb(es, "RK", [128, NCH, 32], F32)
            self.Asum = self.sb(es, "Asum", [128, 32], F32)
            self.ltri = self.sb(es, "ltri_sb", [128, 128], F32)
            S.dma("sp", self.ltri[:], i["ltri"], [], [self.ltri])
            zr = self.sb(es, "zrow", [1, D], BF)
            self.V("dve", "memset", [], [zr], zr[:], 0.0)
            S.dma("sp", self.H2[T:T + 1, :], zr[:], [zr], [Tl(None, self.H2t)])
            self.LB = self.sb(es, "LB", [128, DEPTH, 8], F32)
            self.OML = self.sb(es, "OML", [128, DEPTH, 8], F32)
            lbe = self.sb(es, "lbe", [128, DEPTH, 8], F32)
            lbt = self.sb(es, "lbt", [128, 16], F32)
            for l_ in range(DEPTH):
                for d_ in range(2):
                    for j_ in range(4):
                        S.dma("sp", lbe[:, l_, d_ * 4 + j_:d_ * 4 + j_ + 1], i["hg_lb"][l_, d_:d_ + 1, j_ * 128:(j_ + 1) * 128].rearrange("o p -> p o"),
                              [], [lbe], allow_slow_non_contiguous=True)
            self.act(lbe[:], lbe[:], AF.Exp, [lbe], [lbe])
            self.V("dve", "tensor_tensor", [lbe], [lbt], out=lbt[:, 0:8], in0=lbe[:, 0, :], in1=lbe[:, 1, :], op=ALU.add)
            self.V("dve", "tensor_tensor", [lbe, lbt], [lbt], out=lbt[:, 0:8], in0=lbt[:, 0:8], in1=lbe[:, 2, :], op=ALU.add)
            self.V("dve", "tensor_tensor", [lbe, lbt], [lbt], out=lbt[:, 0:8], in0=lbt[:, 0:8], in1=lbe[:, 3, :], op=ALU.add)
            self.V("dve", "reciprocal", [lbt], [lbt], out=lbt[:, 8:16], in_=lbt[:, 0:8])
            self.V("dve", "memset", [], [self.LB], self.LB[:], 0.0)
            for l_ in range(1, DEPTH):
                self.V("dve", "tensor_tensor", [lbe, lbt], [lbe], out=lbe[:, l_, :], in0=lbe[:, l_, :], in1=lbt[:, 8:16], op=ALU.mult)
                self.V("dve", "tensor_tensor", [lbe, self.LB], [self.LB], out=self.LB[:, l_, :], in0=self.LB[:, l_ - 1, :], in1=lbe[:, l_, :], op=ALU.add)
            self.V("dve", "tensor_scalar", [self.LB], [self.OML], out=self.OML[:], in0=self.LB[:], scalar1=-1.0, scalar2=1.0, op0=ALU.mult, op1=ALU.add)
            self.seg = self.sb(es, "seg", [128, 512], F32)
            self.V("dve", "memset", [], [self.seg], self.seg[:], 1.0)
            self.V("dve", "memset", [self.seg], [self.seg], self.seg[:].rearrange("p (c s) -> p c s", s=64)[:, :, 0:1], 0.0)
            S.barrier()
            S.dma("sp", self.X[0:NCTX, :], i["ctx"], [], self._xt(0, 2))
            for q in range(4):
                S.dma("sp", self.X[NCTX + q * 1024: NCTX + (q + 1) * 1024, :], i["x"][q * 1024:(q + 1) * 1024, :], [],
                      self._xt(2 + q * 8, 8))
            for l in range(L):
                self.layer(l)
            self.final_norm()
            S.emit()

    def _xt(self, c0, n):
        return [Tl(None, t) for t in self.Xt[c0:c0 + n]]

    def layer(self, l):
        stop = self.dbg.get("stop")
        self.phase_mod(l)
        self.load_params(l)
        self.phase_norm(l, 1)
        if stop == "norm1":
            return
        self.phase_sconv(l)
        self.phase_conv(l)
        if stop == "convs":
            return self.dump_dbg()
        self.phase_attn(l)
        if stop == "attn":
            return self.dump_dbg()
        self.phase_hgrn(l)
        if stop == "hgrn":
            return self.dump_dbg()
        self.phase_merge(l)
        if stop == "merge":
            return self.dump_dbg()
        self.phase_norm(l, 2)
        if SPARSE:
            self.phase_moe_sparse(l)
        else:
            self.phase_moe(l)
        if stop == "moe":
            return self.dump_dbg()

    def dump_dbg(self):
        S = self.S
        if "y" in self.dbg_out:
            S.dma("sp", self.dbg_out["y"], self.Y, [Tl(None, t) for k in range(4) for t in self.Yt[k]], [Tl(None, Trk())])
        if "x" in self.dbg_out:
            S.dma("sp", self.dbg_out["x"], self.X, [Tl(None, t) for t in self.Xt], [Tl(None, Trk())])

    def load_w(self, es, name, l, c0, ncols):
        w = self.sb(es, name, [128, 8, ncols], BF)
        self.S.dma("pool", w[:], self.i["w_in"][l, :, c0:c0 + ncols].rearrange("(kc p) n -> p kc n", p=128), [], [w])
        return w

    def load_ht(self, buf, ti):
        t0, N = TILES[ti]
        self.S.dma("sp", buf[:, :, 0:N], self.HT[:, :, t0:t0 + N].rearrange("k p n -> p k n"), [Tl(None, self.HTt[ti])], [buf])

    def proj(self, ps, w, j0, ht, N, width=128):
        for kc in range(8):
            self.mm(ps[0:width, 0:N], w[:, kc, j0:j0 + width], ht[:, kc, 0:N], kc == 0, kc == 7, [w, ht], [ps])

    def rstd_from(self, src_ap, scale, out_t, tmp_t, N, r):
        self.V("dve", "tensor_scalar", r, [tmp_t], out=tmp_t[:, 0:N], in0=src_ap, scalar1=scale, scalar2=EPS, op0=ALU.mult, op1=ALU.add)
        self.V("dve", "reciprocal", [tmp_t], [tmp_t], out=tmp_t[:, 0:N], in_=tmp_t[:, 0:N])
        self.act(out_t[:, 0:N], tmp_t[:, 0:N], AF.Sqrt, [tmp_t], [out_t])

    def load_params(self, l):
        S = self.S
        i = self.i
        P = self.P
        nc = True
        def ld(dst, src):
            S.dma("sp", dst, src, [], [P], allow_slow_non_contiguous=True)
        for j in range(4):
            sl = slice(j * 128, (j + 1) * 128)
            ld(P[:, j * 3:j * 3 + 3], i["sc_w"][l][:, sl].rearrange("k p -> p k"))
            ld(P[:, 12 + j * 31:12 + (j + 1) * 31], i["cv_dw_w"][l][:, sl].rearrange("k p -> p k"))
            ld(P[:, 136 + j:137 + j], i["cv_dw_b"][l:l + 1, sl].rearrange("o p -> p o"))
            ld(P[:, 140 + j:141 + j], i["cv_ln_g"][l:l + 1, sl].rearrange("o p -> p o"))
            ld(P[:, 144 + j:145 + j], i["cv_ln_b"][l:l + 1, sl].rearrange("o p -> p o"))
            ld(P[:, 148 + j:149 + j], i["hg_norm_g"][l:l + 1, sl].rearrange("o p -> p o"))
        ld(P[:, 152:153], i["da_norm_g"][l:l + 1, :].rearrange("o p -> p o"))
        S.dma("sp", P[:, 160:416], i["da_lambda"][l:l + 1, :].partition_broadcast(128), [], [P])
        S.dma("sp", P[:, 416:452], i["moe_b_r"][l:l + 1, :].partition_broadcast(128), [], [P])
        lam_init = 0.8 - 0.6 * math.exp(-0.3 * l)
        self.V("dve", "tensor_tensor", [P], [P], out=P[:, 460:524], in0=P[:, 160:224], in1=P[:, 224:288], op=ALU.mult)
        self.V("dve", "tensor_tensor", [P], [P], out=P[:, 524:588], in0=P[:, 288:352], in1=P[:, 352:416], op=ALU.mult)
        self.V("dve", "tensor_reduce", [P], [P], out=P[:, 155:157], in_=P[:, 460:588].rearrange("p (a b) -> p a b", a=2), axis=AX.X, op=ALU.add)
        self.act(P[:, 157:159], P[:, 155:157], AF.Exp, [P], [P])
        self.V("dve", "tensor_tensor", [P], [P], out=P[:, 159:160], in0=P[:, 158:159], in1=P[:, 157:158], op=ALU.subtract)
        self.V("dve", "tensor_scalar", [P], [P], out=P[:, 153:154], in0=P[:, 159:160], scalar1=-lam_init, scalar2=1.0, op0=ALU.add, op1=ALU.mult)
        self.V("dve", "tensor_scalar", [P], [P], out=P[:, 154:155], in0=P[:, 152:153], scalar1=1.0 - lam_init, scalar2=0.0, op0=ALU.mult, op1=ALU.add)

    def phase_mod(self, l):
        S = self.S
        i = self.i
        modt = Tl(None, self.MODt)
        with contextlib.ExitStack() as es:
            wt = [self.sb(es, "adaw%d" % k, [128, 8, 512], F32) for k in range(2)]
            bt = [self.sb(es, "adab%d" % k, [1, 512], F32) for k in range(2)]
            ot = [self.sb(es, "adao%d" % k, [128, 512], F32) for k in range(2)]
            n = 0
            for blk in range(12):
                w = wt[blk % 2]
                b = bt[blk % 2]
                S.dma("sp", w[:], i["ada_w"][l, :, blk * 512:(blk + 1) * 512].rearrange("(kc p) n -> p kc n", p=128), [], [w])
                S.dma("sp", b[:], i["ada_b"][l:l + 1, blk * 512:(blk + 1) * 512], [], [b])
                for which in range(2):
                    ps = self.psb()
                    for kc in range(8):
                        self.mm(ps[:], self.sT[which][:, kc, :], w[:, kc, :], kc == 0, False, [self.sT[which], w], [ps])
                    self.mm(ps[:], self.cst[0:1, 128:256], b[0:1, :], False, True, [self.cst, b], [ps])
                    o = ot[n % 2]
                    n += 1
                    self.V("dve", "tensor_copy", [ps], [o], out=o[:], in_=ps[:])
                    S.dma("sp", self.MODR[which, :, blk * 512:(blk + 1) * 512], o[:], [o], [modt])
            S.barrier()

    def phase_norm(self, l, which):
        S = self.S
        i = self.i
        gsrc = i["norm1_g"] if which == 1 else i["norm2_g"]
        sh, sc = (0, 1) if which == 1 else (3, 4)
        modt = Tl(None, self.MODt)
        with contextlib.ExitStack() as es:
            A = [self.sb(es, "nA%d" % k, [128, D], F32) for k in range(2)]
            B = [self.sb(es, "nB%d" % k, [128, D], F32) for k in range(2)]
            g = self.sb(es, "ng", [128, D], F32)
            S.dma("sp", g[:], gsrc[l:l + 1, :].partition_broadcast(128), [], [g])
            for k in range(2):
                S.dma("sp", A[k][:], self.MODR[k, :, sc * D:(sc + 1) * D], [modt], [A[k]])
                S.dma("sp", B[k][:], self.MODR[k, :, sh * D:(sh + 1) * D], [modt], [B[k]])
                self.V("dve", "scalar_tensor_tensor", [A[k], g], [A[k]], out=A[k][:], in0=A[k][:], scalar=1.0, in1=g[:],
                       op0=ALU.add, op1=ALU.mult)
            xs = [self.sb(es, "nx%d" % k, [128, D], F32) for k in range(3)]
            sq = self.sb(es, "nsq", [128, D], F32)
            t1 = [self.sb(es, "nt%d" % k, [128, D], F32) for k in range(2)]
            hb = [self.sb(es, "nhb%d" % k, [128, D], BF) for k in range(2)]
            st = [self.sb(es, "nst%d" % k, [128, 4], F32) for k in range(2)]
            hT = [self.sb(es, "nhT%d" % k, [128, 8, 512], BF) for k in range(2)]
            if which == 2:
                wr = self.sb(es, "nwr", [128, 8, 36], F32)
                S.dma("sp", wr[:], i["moe_w_r"][l].rearrange("(kc p) n -> p kc n", p=128), [], [wr])
                h32 = [self.sb(es, "nh32%d" % k, [128, D], F32) for k in range(2)]
                h32T = [self.sb(es, "nh32T%d" % k, [128, 8, 128], F32) for k in range(2)]
                Rt = [self.sb(es, "nR%d" % k, [128, 160], F32) for k in range(2)]
            for ti, (t0, N) in enumerate(TILES):
                ht = hT[ti % 2]
                for cc in range(N // 128):
                    c = t0 // 128 + cc
                    lat = 0 if c >= 2 else 1
                    x = xs[c % 3]
                    xt = Tl(None, self.Xt[c])
                    S.dma("sp", x[:], self.X[c * 128:(c + 1) * 128, :], [xt], [x])
                    s_ = st[c % 2]
                    self.act(sq[:], x[:], AF.Square, [x], [sq, s_], accum_out=s_[:, 0:1])
                    self.V("dve", "tensor_scalar", [s_], [s_], out=s_[:, 1:2], in0=s_[:, 0:1], scalar1=1.0 / D, scalar2=EPS,
                           op0=ALU.mult, op1=ALU.add)
                    self.V("dve", "reciprocal", [s_], [s_], out=s_[:, 2:3], in_=s_[:, 1:2])
                    self.act(s_[:, 3:4], s_[:, 2:3], AF.Sqrt, [s_], [s_])
                    t = t1[c % 2]
                    self.V("dve", "scalar_tensor_tensor", [x, s_, A[lat]], [t], out=t[:], in0=x[:], scalar=s_[:, 3:4],
                           in1=A[lat][:], op0=ALU.mult, op1=ALU.mult)
                    h = hb[c % 2]
                    self.V("pool", "tensor_tensor", [t, B[lat]], [h], out=h[:], in0=t[:], in1=B[lat][:], op=ALU.add)
                    ps = self.psb()
                    pv = ps[:].bitcast(BF).rearrange("p (k n) -> p k n", k=8)
                    for k in range(8):
                        self.tr(pv[:, k, :], h[:, k * 128:(k + 1) * 128], self.identb[:], [h, self.identb], [ps])
                    self.act(ht[:, :, cc * 128:(cc + 1) * 128], pv, AF.Copy, [ps], [ht])
                    if which == 2:
                        self.route(c, t, B[lat], h32[c % 2], h32T[c % 2], wr, Rt[c % 2])
                        if SPARSE:
                            S.dma("sp", self.H2[c * 128:(c + 1) * 128, :], h[:], [h], [Tl(None, self.H2t)])
                            self.rank(c, Rt[c % 2])
                S.dma("sp", self.HT[:, :, t0:t0 + N].rearrange("k p n -> p k n"), ht[:, :, 0:N], [ht], [Tl(None, self.HTt[ti])])
            S.barrier()
        if "h1" in self.dbg_out and l == self.dbg.get("layer", 0) and which == 1:
            S.dma("sp", self.dbg_out["h1"], self.HT, [Tl(None, t) for t in self.HTt], [Tl(None, Trk())])


    def phase_sconv(self, l):
        S = self.S
        P = self.P
        with contextlib.ExitStack() as es:
            wb = self.load_w(es, "scwb", l, C_SB, 512)
            wc = self.load_w(es, "scwc", l, C_SC, 512)
            wx = self.load_w(es, "scwx", l, C_SX, 512)
            ub = self.sb(es, "scu", [128, T + 3], F32)
            bb = self.sb(es, "scb", [128, T], F32)
            hts = [self.sb(es, "scht%d" % k, [128, 8, 512], BF) for k in range(2)]
            cs = [self.sb(es, "sccs%d" % k, [128, 512], F32) for k in range(2)]
            acc = [self.sb(es, "scacc%d" % k, [128, 512], F32) for k in range(2)]
            ys = [self.sb(es, "scy%d" % k, [128, 512], BF) for k in range(2)]
            self.V("pool", "memset", [], [ub], ub[:], 0.0)
            n = 0
            for j in range(4):
                for ti, (t0, N) in enumerate(TILES):
                    ht = hts[n % 2]
                    c_ = cs[n % 2]
                    n += 1
                    self.load_ht(ht, ti)
                    base = t0 + 1 if ti == 0 else t0 + 2
                    pb, pc, px = self.psb(), self.psb(), self.psb()
                    self.proj(pb, wb, j * 128, ht, N)
                    self.proj(pc, wc, j * 128, ht, N)
                    self.proj(px, wx, j * 128, ht, N)
                    self.act(c_[:, 0:N], pc[:, 0:N], AF.Copy, [pc], [c_])
                    self.V("dve", "tensor_tensor", [c_, px], [ub], out=ub[:, base:base + N], in0=c_[:, 0:N], in1=px[:, 0:N], op=ALU.mult)
                    self.act(bb[:, t0:t0 + N], pb[:, 0:N], AF.Copy, [pb], [bb])
                for ti, (t0, N) in enumerate(TILES):
                    base = t0 + 1 if ti == 0 else t0 + 2
                    a = acc[ti % 2]
                    y = ys[ti % 2]
                    self.V("dve", "tensor_scalar", [ub, P], [a], out=a[:, 0:N], in0=ub[:, base - 1:base - 1 + N], scalar1=P[:, j * 3:j * 3 + 1],
                           scalar2=0.0, op0=ALU.mult, op1=ALU.add)
                    for k in (1, 2):
                        self.V("dve", "scalar_tensor_tensor", [ub, P, a], [a], out=a[:, 0:N], in0=ub[:, base - 1 + k:base - 1 + k + N],
                               scalar=P[:, j * 3 + k:j * 3 + k + 1], in1=a[:, 0:N], op0=ALU.mult, op1=ALU.add)
                    self.V("pool", "tensor_tensor", [a, bb], [y], out=y[:, 0:N], in0=a[:, 0:N], in1=bb[:, t0:t0 + N], op=ALU.mult)
                    S.dma("sp", self.Y[3, j, :, t0:t0 + N], y[:, 0:N], [y], [Tl(None, self.Yt[3][ti])])
            S.barrier()

    def phase_conv(self, l):
        S = self.S
        P = self.P
        onesf = self.cst[:, 128:256]
        with contextlib.ExitStack() as es:
            wa = self.load_w(es, "cvwa", l, C_CVA, 512)
            wg = self.load_w(es, "cvwg", l, C_CVG, 512)
            vb = self.sb(es, "cvv", [128, 4, T + 45], BF)
            hts = [self.sb(es, "cvht%d" % k, [128, 8, 512], BF) for k in range(2)]
            sg = [self.sb(es, "cvsg%d" % k, [128, 512], F32) for k in range(2)]
            self.V("pool", "memset", [], [vb], vb[:], 0.0)
            n = 0
            for ti, (t0, N) in enumerate(TILES):
                ht = hts[ti % 2]
                self.load_ht(ht, ti)
                base = t0 + 15 if ti == 0 else t0 + 30
                for j in range(4):
                    pa, pg = self.psb(), self.psb()
                    self.proj(pa, wa, j * 128, ht, N)
                    self.proj(pg, wg, j * 128, ht, N)
                    s_ = sg[n % 2]
                    n += 1
                    self.act(s_[:, 0:N], pg[:, 0:N], AF.Sigmoid, [pg], [s_])
                    self.V("dve", "tensor_tensor", [s_, pa], [vb], out=vb[:, j, base:base + N], in0=pa[:, 0:N], in1=s_[:, 0:N], op=ALU.mult)
            ca = [self.sb(es, "cvca%d" % k, [128, 4, 512], F32) for k in range(2)]
            cp = self.sb(es, "cvcp", [128, 512], F32)
            sq = self.sb(es, "cvsq", [128, 4, 512], F32)
            mean = self.sb(es, "cvmean", [128, 512], F32)
            tmp = self.sb(es, "cvtmp", [128, 512], F32)
            rstd = self.sb(es, "cvrstd", [128, 512], F32)
            dd = [self.sb(es, "cvd%d" % k, [128, 512], F32) for k in range(2)]
            ys = [self.sb(es, "cvy%d" % k, [128, 4, 512], BF) for k in range(2)]
            for ti, (t0, N) in enumerate(TILES):
                base = t0 + 15 if ti == 0 else t0 + 30
                a = ca[ti % 2]
                for j in range(4):
                    wcol = lambda k: P[:, 12 + j * 31 + k:12 + j * 31 + k + 1]
                    src = lambda k: vb[:, j, base - 15 + k:base - 15 + k + N]
                    self.V("dve", "tensor_scalar", [vb, P], [a], out=a[:, j, 0:N], in0=src(0), scalar1=wcol(0), scalar2=P[:, 136 + j:137 + j],
                           op0=ALU.mult, op1=ALU.add)
                    for k in range(1, 31):
                        self.V("dve", "scalar_tensor_tensor", [vb, P, a], [a], out=a[:, j, 0:N], in0=src(k), scalar=wcol(k), in1=a[:, j, 0:N],
                               op0=ALU.mult, op1=ALU.add)
                    self.act(sq[:, j, 0:N], a[:, j, 0:N], AF.Square, [a], [sq])
                p1, p2 = self.psb(), self.psb()
                for j in range(4):
                    self.mm(p1[:, 0:N], onesf, a[:, j, 0:N], j == 0, j == 3, [self.cst, a], [p1])
                for j in range(4):
                    self.mm(p2[:, 0:N], onesf, sq[:, j, 0:N], j == 0, j == 3, [self.cst, sq], [p2])
                self.act(mean[:, 0:N], p1[:, 0:N], AF.Copy, [p1], [mean], scale=1.0 / W)
                self.V("pool", "tensor_tensor", [mean], [tmp], out=tmp[:, 0:N], in0=mean[:, 0:N], in1=mean[:, 0:N], op=ALU.mult)
                self.V("dve", "scalar_tensor_tensor", [p2, tmp], [tmp], out=tmp[:, 0:N], in0=p2[:, 0:N], scalar=1.0 / W, in1=tmp[:, 0:N],
                       op0=ALU.mult, op1=ALU.subtract)
                self.rstd_from(tmp[:, 0:N], 1.0, rstd, tmp, N, [tmp])
                y = ys[ti % 2]
                for j in range(4):
                    d = dd[j % 2]
                    self.V("dve", "tensor_tensor", [a, mean], [d], out=d[:, 0:N], in0=a[:, j, 0:N], in1=mean[:, 0:N], op=ALU.subtract)
                    self.V("pool", "tensor_tensor", [d, rstd], [d], out=d[:, 0:N], in0=d[:, 0:N], in1=rstd[:, 0:N], op=ALU.mult)
                    self.V("dve", "tensor_scalar", [d, P], [d], out=d[:, 0:N], in0=d[:, 0:N], scalar1=P[:, 140 + j:141 + j], scalar2=P[:, 144 + j:145 + j],
                           op0=ALU.mult, op1=ALU.add)
                    self.act(y[:, j, 0:N], d[:, 0:N], AF.Silu, [d], [y])
                S.dma("sp", self.Y[2, :, :, t0:t0 + N].rearrange("j p n -> p j n"), y[:, :, 0:N], [y], [Tl(None, self.Yt[2][ti])])
            S.barrier()

    def rope(self, ps, raw, cos, sin, t1, t2, dst_ap, dst_t, N, rot_ps):
        self.act(raw[:, 0:N], ps[:, 0:N], AF.Copy, [ps], [raw])
        self.mm(rot_ps[:, 0:N], self.Rb[:], raw[:, 0:N], True, True, [self.Rb, raw], [rot_ps])
        self.V("pool", "tensor_tensor", [raw, cos], [t1], out=t1[:, 0:N], in0=raw[:, 0:N], in1=cos[:, 0:N], op=ALU.mult)
        self.V("dve", "tensor_tensor", [rot_ps, sin], [t2], out=t2[:, 0:N], in0=rot_ps[:, 0:N], in1=sin[:, 0:N], op=ALU.mult)
        self.V("pool", "tensor_tensor", [t1, t2], [dst_t], out=dst_ap, in0=t1[:, 0:N], in1=t2[:, 0:N], op=ALU.add)

    def phase_attn(self, l):
        S = self.S
        P = self.P
        i = self.i
        onesf = self.cst[:, 128:256]
        B = self.banks
        with contextlib.ExitStack() as es:
            wq = self.load_w(es, "dawq", l, C_DQ, 512)
            wk = self.load_w(es, "dawk", l, C_DK, 512)
            wv = self.load_w(es, "dawv", l, C_DV, 512)
            KT = self.sb(es, "daKT", [128, 4, T], BF)
            Vt = self.sb(es, "daV", [128, NCH, 512], BF)
            hts = [self.sb(es, "daht%d" % k, [128, 8, 512], BF) for k in range(2)]
            cos = [self.sb(es, "dacos%d" % k, [128, 512], F32) for k in range(2)]
            sin = [self.sb(es, "dasin%d" % k, [128, 512], F32) for k in range(2)]
            raw = [self.sb(es, "daraw%d" % k, [128, 512], BF) for k in range(2)]
            t1 = [self.sb(es, "dat1%d" % k, [128, 512], F32) for k in range(2)]
            t2 = [self.sb(es, "dat2%d" % k, [128, 512], F32) for k in range(2)]
            n = 0
            for ti, (t0, N) in enumerate(TILES):
                ht = hts[ti % 2]
                self.load_ht(ht, ti)
                S.dma("sp", cos[ti % 2][:, 0:N], i["rope"][0, :, t0:t0 + N], [], [cos[ti % 2]])
                S.dma("sp", sin[ti % 2][:, 0:N], i["rope"][1, :, t0:t0 + N], [], [sin[ti % 2]])
                for h in range(4):
                    ps, rp = self.psb(), self.psb()
                    self.proj(ps, wk, h * 128, ht, N)
                    self.rope(ps, raw[n % 2], cos[ti % 2], sin[ti % 2], t1[n % 2], t2[n % 2], KT[:, h, t0:t0 + N], KT, N, rp)
                    n += 1
                for cc in range(N // 128):
                    ps = self.psb()
                    for kc in range(8):
                        self.mm(ps[:, :], ht[:, kc, cc * 128:(cc + 1) * 128], wv[:, kc, :], kc == 0, kc == 7, [ht, wv], [ps])
                    self.act(Vt[:, t0 // 128 + cc, :], ps[:, :], AF.Copy, [ps], [Vt])
            QT = [self.sb(es, "daQT%d" % k, [128, 512], BF) for k in range(2)]
            QZ = [[self.sb(es, "daQZ%d_%d" % (k, c), [128, 512], BF) for c in range(2)] for k in range(2)]
            for k in range(2):
                for c in range(2):
                    self.V("pool", "memset", [], [QZ[k][c]], QZ[k][c][:], 0.0)
            pT = [self.sb(es, "dapT%d" % k, [128, 512], BF) for k in range(4)]
            rd = [self.sb(es, "dard%d" % k, [128, 512], F32) for k in range(2)]
            rp_ = [self.sb(es, "darp%d" % k, [128, 512], F32) for k in range(2)]
            rinv = self.sb(es, "darinv", [128, 512], F32)
            Oc = [self.sb(es, "daOc%d" % k, [128, 512], F32) for k in range(2)]
            o = self.sb(es, "dao", [128, 512], F32)
            sq = self.sb(es, "dasq", [128, 512], F32)
            tmp = self.sb(es, "datmp", [128, 512], F32)
            rstd = self.sb(es, "darstd", [128, 512], F32)
            ys = [self.sb(es, "day%d" % k, [128, 4, 512], BF) for k in range(2)]
            it = 0
            for ti, (t0, N) in enumerate(TILES):
                ht = hts[ti % 2]
                self.load_ht(ht, ti)
                S.dma("sp", cos[ti % 2][:, 0:N], i["rope"][0, :, t0:t0 + N], [], [cos[ti % 2]])
                S.dma("sp", sin[ti % 2][:, 0:N], i["rope"][1, :, t0:t0 + N], [], [sin[ti % 2]])
                nk = 2 if ti == 0 else NCH
                y = ys[ti % 2]
                for h in range(4):
                    qt = QT[h % 2]
                    self.proj(B[7], wq, h * 128, ht, N)
                    self.rope(B[7], raw[n % 2], cos[ti % 2], sin[ti % 2], t1[n % 2], t2[n % 2], qt[:, 0:N], qt, N, B[2])
                    n += 1
                    qz = QZ[h % 2]
                    self.V("pool", "tensor_copy", [qt], [qz[0]], out=qz[0][0:64, 0:N], in_=qt[0:64, 0:N])
                    self.V("dve", "tensor_copy", [qt], [qz[1]], out=qz[1][64:128, 0:N], in_=qt[64:128, 0:N])
                    its = [(c, kc) for c in range(2) for kc in range(nk)]
                    slots = {}
                    def qk(j):
                        c, kc = its[j]
                        sps = B[2 + it_base[0] % 3]
                        p = pT[it_base[0] % 4]
                        it_base[0] += 1
                        slots[j] = (sps, p)
                        p0 = c * 64
                        self.mm(sps[:, 0:N], KT[:, h, kc * 128:(kc + 1) * 128], qz[c][:, 0:N], True, True, [KT, qz[c]], [sps])
                    it_base = [it]
                    qk(0)
                    if len(its) > 1:
                        qk(1)
                    for j, (c, kc) in enumerate(its):
                        sps, p = slots.pop(j)
                        oacc = B[c]
                        self.act(p[:, 0:N], sps[:, 0:N], AF.Exp, [sps], [p], scale=0.125)
                        if j + 2 < len(its):
                            qk(j + 2)
                        self.mm(oacc[:, 0:N], Vt[:, kc, h * 128:(h + 1) * 128], p[:, 0:N], kc == 0, kc == nk - 1, [Vt, p], [oacc])
                        rsb = B[5 + c]
                        self.mm(rsb[:, 0:N], self.onesb[:], p[:, 0:N], kc == 0, kc == nk - 1, [self.onesb, p], [rsb])
                        if kc == nk - 1:
                            self.V("dve", "reciprocal", [rsb], [rinv], out=rinv[:, 0:N], in_=rsb[:, 0:N])
                            self.V("dve", "tensor_tensor", [oacc, rinv], [Oc[c]], out=Oc[c][:, 0:N], in0=oacc[:, 0:N], in1=rinv[:, 0:N], op=ALU.mult)
                    it = it_base[0]
                    self.V("dve", "scalar_tensor_tensor", [Oc[0], Oc[1], P], [o], out=o[:, 0:N], in0=Oc[1][:, 0:N], scalar=P[:, 153:154], in1=Oc[0][:, 0:N],
                           op0=ALU.mult, op1=ALU.add)
                    self.act(sq[:, 0:N], o[:, 0:N], AF.Square, [o], [sq])
                    self.mm(B[7][:, 0:N], onesf, sq[:, 0:N], True, True, [self.cst, sq], [B[7]])
                    self.rstd_from(B[7][:, 0:N], 1.0 / 128, rstd, tmp, N, [B[7]])
                    self.V("dve", "scalar_tensor_tensor", [o, rstd, P], [y], out=y[:, h, 0:N], in0=o[:, 0:N], scalar=P[:, 154:155], in1=rstd[:, 0:N],
                           op0=ALU.mult, op1=ALU.mult)
                S.dma("sp", self.Y[1, :, :, t0:t0 + N].rearrange("j p n -> p j n"), y[:, :, 0:N], [y], [Tl(None, self.Yt[1][ti])])
            S.barrier()

    def phase_hgrn(self, l):
        S = self.S
        B = self.banks
        with contextlib.ExitStack() as es:
            rot = [2]
            def rb():
                b = B[rot[0]]
                rot[0] = 2 + (rot[0] - 1) % 6
                return b
            sh = {k: self.sb(es, "hgs_" + k, [128, 512], F32) for k in ("osum", "sq", "tmp", "rstd", "sgg", "y1")}
            sh["ys"] = [self.sb(es, "hgys%d" % k, [128, 512], BF) for k in range(2)]
            sh["n"] = 0
            chains = []
            for k in range(2):
                c = {"k": k}
                c["OF"] = self.sb(es, "hgOF%d" % k, [128, T], F32)
                c["S32"] = self.sb(es, "hgS%d" % k, [128, 128], F32)
                c["Sbf"] = [self.sb(es, "hgSb%d_%d" % (k, j), [128, 128], BF) for j in range(2)]
                c["ht"] = self.sb(es, "hght%d" % k, [128, 8, 512], BF)
                c["w"] = [self.sb(es, "hgw%d_%d" % (k, j), [128, 8, 128], BF) for j in range(4)]
                for nm in ("q32", "ee", "ff", "lf", "kk", "pre", "bb", "d3", "d2", "d3a"):
                    c[nm] = self.sb(es, "hg%s%d" % (nm, k), [128, 512], F32)
                for nm in ("qE1", "qE3", "kE4", "kE2", "qE3b", "kE4b"):
                    c[nm] = self.sb(es, "hg%s%d" % (nm, k), [128, 512], BF)
                c["iT"] = self.sb(es, "hgiT%d" % k, [128, 4, 128], BF)
                c["kT"] = self.sb(es, "hgkT%d" % k, [128, 4, 128], BF)
                c["AM"] = [self.sb(es, "hgAM%d_%d" % (k, j), [128, 128], BF) for j in range(2)]
                c["a1"] = [self.sb(es, "hga1%d_%d" % (k, j), [128, 128], F32) for j in range(2)]
                c["a2"] = [self.sb(es, "hga2%d_%d" % (k, j), [128, 128], F32) for j in range(2)]
                c["ops"] = B[k]
                chains.append(c)
            for pair in ((0, 1), (2, 3)):
                for d in range(2):
                    gens = [self.hgrn_chain(l, h, d, chains[k], sh, rb) for k, h in enumerate(pair)]
                    live = list(gens)
                    while live:
                        for g in list(live):
                            try:
                                next(g)
                            except StopIteration:
                                live.remove(g)
            S.barrier()

    def hgrn_chain(self, l, h, d, c, sh, rb):
        S = self.S
        P = self.P
        onesf = self.cst[:, 128:256]
        w = c["w"]
        OF, S32, Sbf, ht, ops = c["OF"], c["S32"], c["Sbf"], c["ht"], c["ops"]
        q32, ee, ff, lf, kk, pre, bb, d3, d2, d3a = (c[n] for n in ("q32", "ee", "ff", "lf", "kk", "pre", "bb", "d3", "d2", "d3a"))
        qE1, qE3, kE4, kE2, qE3b, kE4b = (c[n] for n in ("qE1", "qE3", "kE4", "kE2", "qE3b", "kE4b"))
        iT, kT = c["iT"], c["kT"]
        E1, E3, E3b, E4b, E2, E4 = ee, ff, lf, d3, d2, d3a
        cols = (C_HQ, C_HFF if d == 0 else C_HFB, C_HI, C_HG)
        for k in range(4):
            S.dma("pool", w[k][:], self.i["w_in"][l, :, cols[k] + h * 128:cols[k] + (h + 1) * 128].rearrange("(kc p) n -> p kc n", p=128), [], [w[k]])
        self.V("pool", "memset", [], [S32], S32[:], 0.0)
        self.V("pool", "memset", [], [Sbf[0]], Sbf[0][:], 0.0)
        cur = 0
        am_i = 0
        lbc = self.LB[:, l, d * 4 + h:d * 4 + h + 1]
        omc = self.OML[:, l, d * 4 + h:d * 4 + h + 1]
        order = list(range(9)) if d == 0 else [0] + list(range(8, 0, -1))
        mask = self.cst[:, 384:512] if d == 0 else self.cst[:, 512:640]
        masko = self.cst[:, 640:768] if d == 0 else self.cst[:, 768:896]
        for ti in order:
            t0, N = TILES[ti]
            nb = N // 128
            nch = N // 64
            self.load_ht(ht, ti)
            pq, pf = rb(), rb()
            self.proj(pq, w[0], 0, ht, N)
            self.proj(pf, w[1], 0, ht, N)
            self.act(q32[:, 0:N], pq[:, 0:N], AF.Copy, [pq], [q32])
            self.act(ee[:, 0:N], pf[:, 0:N], AF.Exp, [pf], [ee], scale=-1.0)
            self.V("dve", "tensor_scalar", [ee], [ee], out=ee[:, 0:N], in0=ee[:, 0:N], scalar1=1.0, scalar2=1.0, op0=ALU.add, op1=ALU.mult)
            self.V("dve", "reciprocal", [ee], [ee], out=ee[:, 0:N], in_=ee[:, 0:N])
            self.V("dve", "tensor_scalar", [ee, self.LB, self.OML], [ff], out=ff[:, 0:N], in0=ee[:, 0:N], scalar1=omc, scalar2=lbc, op0=ALU.mult, op1=ALU.add)
            self.act(lf[:, 0:N], ff[:, 0:N], AF.Ln, [ff], [lf])
            self.V("pool", "tensor_scalar", [ff], [kk], out=kk[:, 0:N], in0=ff[:, 0:N], scalar1=-1.0, scalar2=1.0, op0=ALU.mult, op1=ALU.add)
            self.V("dve", "tensor_tensor_scan", [self.seg, lf], [pre], out=pre[:, 0:N], data0=self.seg[:, 0:N], data1=lf[:, 0:N], initial=0.0, op0=ALU.mult, op1=ALU.add)
            v3 = lambda t_: t_[:, 0:N].rearrange("p (c s) -> p c s", s=64)
            v32 = lambda t_: t_[:, 0:N].rearrange("p (c s) -> p c s", s=32)
            bc = lambda t_, col: v3(t_)[:, :, col:col + 1].to_broadcast([128, nch, 64])
            if d == 0:
                b_ = pre
                cend = 63
            else:
                b_ = bb
                cend = 0
                self.V("dve", "tensor_tensor", [lf, pre], [bb], out=bb[:, 0:N], in0=lf[:, 0:N], in1=pre[:, 0:N], op=ALU.subtract)
                self.V("dve", "tensor_tensor", [bb, pre], [bb], out=v3(bb), in0=v3(bb), in1=bc(pre, 63), op=ALU.add)
            yield
            self.V("dve", "tensor_tensor", [b_], [d3], out=v3(d3), in0=v3(b_), in1=bc(b_, 32), op=ALU.subtract)
            self.V("pool", "tensor_tensor", [b_], [d2], out=v3(d2), in0=v3(b_), in1=bc(b_, cend), op=ALU.subtract)
            self.V("dve", "tensor_tensor", [b_], [d3a], out=v32(d3a), in0=v32(b_), in1=v32(b_)[:, :, 16:17].to_broadcast([128, 2 * nch, 32]), op=ALU.subtract)
            self.act(E1[:, 0:N], b_[:, 0:N], AF.Exp, [b_], [E1])
            self.act(E2[:, 0:N], d2[:, 0:N], AF.Exp, [d2], [E2], scale=-1.0)
            self.act(E3[:, 0:N], d3a[:, 0:N], AF.Exp, [d3a], [E3])
            self.act(E4[:, 0:N], d3a[:, 0:N], AF.Exp, [d3a], [E4], scale=-1.0)
            self.act(E3b[:, 0:N], d3[:, 0:N], AF.Exp, [d3], [E3b])
            self.act(E4b[:, 0:N], d3[:, 0:N], AF.Exp, [d3], [E4b], scale=-1.0)
            self.V("dve", "tensor_tensor", [q32, E1], [qE1], out=qE1[:, 0:N], in0=q32[:, 0:N], in1=E1[:, 0:N], op=ALU.mult)
            self.V("pool", "tensor_tensor", [q32, E3], [qE3], out=qE3[:, 0:N], in0=q32[:, 0:N], in1=E3[:, 0:N], op=ALU.mult)
            self.V("dve", "tensor_tensor", [kk, E4], [kE4], out=kE4[:, 0:N], in0=kk[:, 0:N], in1=E4[:, 0:N], op=ALU.mult)
            self.V("pool", "tensor_tensor", [kk, E2], [kE2], out=kE2[:, 0:N], in0=kk[:, 0:N], in1=E2[:, 0:N], op=ALU.mult)
            self.V("dve", "tensor_tensor", [q32, E3b], [qE3b], out=qE3b[:, 0:N], in0=q32[:, 0:N], in1=E3b[:, 0:N], op=ALU.mult)
            self.V("pool", "tensor_tensor", [kk, E4b], [kE4b], out=kE4b[:, 0:N], in0=kk[:, 0:N], in1=E4b[:, 0:N], op=ALU.mult)
            qz, kz = (slice(0, 32), slice(32, 64)) if d == 0 else (slice(32, 64), slice(0, 32))
            self.V("dve", "memset", [qE3b], [qE3b], v3(qE3b)[:, :, qz], 0.0)
            self.V("pool", "memset", [kE4b], [kE4b], v3(kE4b)[:, :, kz], 0.0)
            yield
            for cc in range(nb):
                pi = rb()
                for kc in range(8):
                    self.mm(pi[:, 0:128], ht[:, kc, cc * 128:(cc + 1) * 128], w[2][:, kc, :], kc == 0, kc == 7, [ht, w[2]], [pi])
                self.act(iT[:, cc, :], pi[:, 0:128], AF.Copy, [pi], [iT])
                pt = rb()
                ptv = pt[:].bitcast(BF)
                self.tr(ptv[:, 0:128], kE2[:, cc * 128:(cc + 1) * 128], self.identb[:], [kE2, self.identb], [pt])
                self.act(kT[:, cc, :], ptv[:, 0:128], AF.Copy, [pt], [kT])
            yield
            blks = list(range(nb)) if d == 0 else list(range(nb - 1, -1, -1))
            for blk in blks:
                pa = rb()
                bs = slice(blk * 128, (blk + 1) * 128)
                self.mm(pa[:, 0:128], kE4[:, bs], qE3[:, bs], True, True, [kE4, qE3], [pa])
                pb_ = rb()
                self.mm(pb_[:, 0:128], kE4b[:, bs], qE3b[:, bs], True, True, [kE4b, qE3b], [pb_])
                am = c["AM"][am_i % 2]
                a1 = c["a1"][am_i % 2]
                a2 = c["a2"][am_i % 2]
                am_i += 1
                self.V("dve", "tensor_tensor", [pa, self.cst], [a1], out=a1[:], in0=pa[:, 0:128], in1=mask, op=ALU.mult)
                self.V("dve", "tensor_tensor", [pb_, self.cst], [a2], out=a2[:], in0=pb_[:, 0:128], in1=masko, op=ALU.mult)
                self.V("pool", "tensor_tensor", [a1, a2], [am], out=am[:], in0=a1[:], in1=a2[:], op=ALU.add)
                for ch in ((0, 1) if d == 0 else (1, 0)):
                    p0 = ch * 64
                    c0 = blk * 128 + p0
                    self.mm(ops[:, c0:c0 + 64], Sbf[cur][:], qE1[:, c0:c0 + 64], True, False, [Sbf[cur], qE1], [ops])
                    self.mm(ops[:, c0:c0 + 64], iT[p0:p0 + 64, blk, :], am[p0:p0 + 64, p0:p0 + 64], False, True, [iT, am], [ops])
                    pS = rb()
                    self.mm(pS[:, 0:128], kT[p0:p0 + 64, blk, :], iT[p0:p0 + 64, blk, :], True, True, [kT, iT], [pS])
                    ce = c0 + cend
                    self.V("dve", "scalar_tensor_tensor", [S32, E1, pS], [S32], out=S32[:], in0=S32[:], scalar=E1[:, ce:ce + 1], in1=pS[:, 0:128],
                           op0=ALU.mult, op1=ALU.add)
                    cur = 1 - cur
                    self.act(Sbf[cur][:], S32[:], AF.Copy, [S32], [Sbf[cur]])
                    yield
            if d == 0:
                self.act(OF[:, t0:t0 + N], ops[:, 0:N], AF.Copy, [ops], [OF])
            else:
                osum, sq, tmp, rstd, sgg, y1 = (sh[n_] for n_ in ("osum", "sq", "tmp", "rstd", "sgg", "y1"))
                self.V("dve", "tensor_tensor", [OF, ops], [osum], out=osum[:, 0:N], in0=OF[:, t0:t0 + N], in1=ops[:, 0:N], op=ALU.add)
                self.act(sq[:, 0:N], osum[:, 0:N], AF.Square, [osum], [sq])
                pss = rb()
                self.mm(pss[:, 0:N], onesf, sq[:, 0:N], True, True, [self.cst, sq], [pss])
                self.rstd_from(pss[:, 0:N], 1.0 / 128, rstd, tmp, N, [pss])
                pg = rb()
                self.proj(pg, w[3], 0, ht, N)
                self.act(sgg[:, 0:N], pg[:, 0:N], AF.Sigmoid, [pg], [sgg])
                self.V("dve", "scalar_tensor_tensor", [osum, rstd, P], [y1], out=y1[:, 0:N], in0=osum[:, 0:N], scalar=P[:, 148 + h:149 + h], in1=rstd[:, 0:N],
                       op0=ALU.mult, op1=ALU.mult)
                y = sh["ys"][sh["n"] % 2]
                sh["n"] += 1
                self.V("pool", "tensor_tensor", [y1, sgg], [y], out=y[:, 0:N], in0=y1[:, 0:N], in1=sgg[:, 0:N], op=ALU.mult)
                S.dma("sp", self.Y[0, h, :, t0:t0 + N], y[:, 0:N], [y], [Tl(None, self.Yt[0][ti])])
            yield

    def phase_merge(self, l):
        S = self.S
        i = self.i
        with contextlib.ExitStack() as es:
            wg = self.sb(es, "mgwg", [128, 8, 4096], BF)
            for k in range(4):
                S.dma("pool", wg[:, :, k * 1024:(k + 1) * 1024], i["w_in"][l, :, C_GATE + k * 1024:C_GATE + (k + 1) * 1024].rearrange("(kc p) n -> p kc n", p=128), [], [wg])
            wb = self.sb(es, "mgwb", [128, 4, 4, 1024], BF)
            for k in range(4):
                S.dma("pool", wb[:, k, :, :], i["w_branch"][l, k].rearrange("(cc p) n -> p cc n", p=128), [], [wb])
            ht = self.sb(es, "mght", [128, 8, 512], BF)
            Yk = [self.sb(es, "mgY%d" % k, [128, 4, 512], BF) for k in range(4)]
            sg = [self.sb(es, "mgsg%d" % k, [128, 512], F32) for k in range(2)]
            tmp = [self.sb(es, "mgtmp%d" % k, [128, 512], F32) for k in range(2)]
            macc = [self.sb(es, "mgacc%d" % k, [128, 512], F32) for k in range(2)]
            mT = [self.sb(es, "mgmT%d" % k, [128, 8, 512], BF) for k in range(2)]
            n = 0
            for ti, (t0, N) in enumerate(TILES):
                self.load_ht(ht, ti)
                for k in range(4):
                    S.dma("sp", Yk[k][:, :, 0:N], self.Y[k, :, :, t0:t0 + N].rearrange("j p n -> p j n"), [Tl(None, self.Yt[k][ti])], [Yk[k]])
                m = mT[ti % 2]
                for nch in range(8):
                    ma = macc[nch % 2]
                    for k in range(4):
                        pg, pp = self.psb(), self.psb()
                        self.proj(pg, wg, k * 1024 + nch * 128, ht, N)
                        for cc in range(4):
                            self.mm(pp[:, 0:N], wb[:, k, cc, nch * 128:(nch + 1) * 128], Yk[k][:, cc, 0:N], cc == 0, cc == 3, [wb, Yk[k]], [pp])
                        s_ = sg[n % 2]
                        t_ = tmp[n % 2]
                        n += 1
                        self.act(s_[:, 0:N], pg[:, 0:N], AF.Sigmoid, [pg], [s_])
                        if k == 0:
                            self.V("dve", "tensor_tensor", [pp, s_], [ma], out=ma[:, 0:N], in0=pp[:, 0:N], in1=s_[:, 0:N], op=ALU.mult)
                        else:
                            self.V("dve", "tensor_tensor", [pp, s_], [t_], out=t_[:, 0:N], in0=pp[:, 0:N], in1=s_[:, 0:N], op=ALU.mult)
                            self.V("pool", "tensor_tensor", [ma, t_], [ma], out=ma[:, 0:N], in0=ma[:, 0:N], in1=t_[:, 0:N], op=ALU.add)
                    self.V("pool", "tensor_copy", [ma], [m], out=m[:, nch, 0:N], in_=ma[:, 0:N])
                S.dma("sp", self.MT[:, :, t0:t0 + N].rearrange("k p n -> p k n"), m[:, :, 0:N], [m], [Tl(None, self.MTt[ti])])
            S.barrier()
        with contextlib.ExitStack() as es:
            wo = self.sb(es, "mgwo", [128, 8, 1024], BF)
            S.dma("pool", wo[:], i["w_out"][l].rearrange("(kc p) n -> p kc n", p=128), [], [wo])
            mts = [self.sb(es, "mgmt%d" % k, [128, 8, 512], BF) for k in range(2)]
            self.residual_setup(es, 2)
            for ti, (t0, N) in enumerate(TILES):
                mt = mts[ti % 2]
                S.dma("sp", mt[:, :, 0:N], self.MT[:, :, t0:t0 + N].rearrange("k p n -> p k n"), [Tl(None, self.MTt[ti])], [mt])
                for cc in range(N // 128):
                    c = t0 // 128 + cc
                    halves = []
                    for half in range(2):
                        po = self.psb()
                        for kc in range(8):
                            self.mm(po[:, :], mt[:, kc, cc * 128:(cc + 1) * 128], wo[:, kc, half * 512:(half + 1) * 512], kc == 0, kc == 7, [mt, wo], [po])
                        halves.append(po)
                    self.residual(c, lambda half: halves[half][:, :], halves)
            S.barrier()

    def residual_setup(self, es, modidx):
        S = self.S
        self.rmod = [self.sb(es, "rsmod%d" % k, [128, D], F32) for k in range(2)]
        for k in range(2):
            S.dma("sp", self.rmod[k][:], self.MODR[k, :, modidx * D:(modidx + 1) * D], [Tl(None, self.MODt)], [self.rmod[k]])
        self.rx = [self.sb(es, "rsx%d" % k, [128, D], F32) for k in range(2)]
        self.rtmp = [self.sb(es, "rstmp%d" % k, [128, D], F32) for k in range(2)]

    def residual(self, c, delta_ap, delta_tiles):
        S = self.S
        lat = 0 if c >= 2 else 1
        x = self.rx[c % 2]
        t = self.rtmp[c % 2]
        xt = Tl(None, self.Xt[c])
        S.dma("sp", x[:], self.X[c * 128:(c + 1) * 128, :], [xt], [x])
        for half in range(2):
            hs = slice(half * 512, (half + 1) * 512)
            self.V("dve", "tensor_tensor", [delta_tiles[half], self.rmod[lat]], [t], out=t[:, hs], in0=delta_ap(half), in1=self.rmod[lat][:, hs], op=ALU.mult)
        self.V("pool", "tensor_tensor", [x, t], [x], out=x[:], in0=x[:], in1=t[:], op=ALU.add)
        S.dma("sp", self.X[c * 128:(c + 1) * 128, :], x[:], [x], [xt])

    def route(self, c, t, Bm, h32, h32T, wr, R):
        P = self.P
        self.V("pool", "tensor_tensor", [t, Bm], [h32], out=h32[:], in0=t[:], in1=Bm[:], op=ALU.add)
        for g in range(2):
            ps = self.psb()
            for k in range(4):
                kk = g * 4 + k
                self.tr(ps[:, k * 128:(k + 1) * 128], h32[:, kk * 128:(kk + 1) * 128], self.cst[:, 0:128], [h32, self.cst], [ps])
            self.act(h32T[:, g * 4:(g + 1) * 4, :], ps[:].rearrange("p (k n) -> p k n", k=4), AF.Copy, [ps], [h32T])
        pl = self.psb()
        for kc in range(8):
            self.mm(pl[:, 0:36], h32T[:, kc, :], wr[:, kc, :], kc == 0, kc == 7, [h32T, wr], [pl])
        dv = lambda name, w_, **kw: self.V("dve", name, [R, P] + w_[1:], [w_[0]], **kw)
        self.V("dve", "tensor_tensor", [pl, P], [R], out=R[:, 0:36], in0=pl[:, 0:36], in1=P[:, 416:452], op=ALU.add)
        RR = [R]
        dv("tensor_reduce", RR, out=R[:, 36:37], in_=R[:, 0:4], axis=AX.X, op=ALU.max)
        dv("tensor_scalar", RR, out=R[:, 37:38], in0=R[:, 36:37], scalar1=-1.0, scalar2=0.0, op0=ALU.mult, op1=ALU.add)
        self.act(R[:, 44:48], R[:, 0:4], AF.Exp, [R], [R], bias=R[:, 37:38], accum_out=R[:, 38:39])
        dv("reciprocal", RR, out=R[:, 39:40], in_=R[:, 38:39])
        dv("tensor_scalar", RR, out=R[:, 40:44], in0=R[:, 0:4], scalar1=R[:, 36:37], scalar2=1.0, op0=ALU.is_equal, op1=ALU.mult)
        dv("tensor_tensor", RR, out=R[:, 48:80].rearrange("p (g e) -> p g e", g=4), in0=R[:, 4:36].rearrange("p (g e) -> p g e", g=4),
           in1=R[:, 40:44].unsqueeze(2).to_broadcast([128, 4, 8]), op=ALU.mult)
        dv("tensor_reduce", RR, out=R[:, 80:88], in_=R[:, 48:80].rearrange("p (g e) -> p e g", g=4), axis=AX.X, op=ALU.add)
        dv("tensor_reduce", RR, out=R[:, 88:89], in_=R[:, 80:88], axis=AX.X, op=ALU.max)
        dv("tensor_scalar", RR, out=R[:, 89:97], in0=R[:, 80:88], scalar1=R[:, 88:89], scalar2=1.0, op0=ALU.is_equal, op1=ALU.mult)
        dv("scalar_tensor_tensor", RR, out=R[:, 97:105], in0=R[:, 89:97], scalar=-1e30, in1=R[:, 80:88], op0=ALU.mult, op1=ALU.add)
        dv("tensor_reduce", RR, out=R[:, 105:106], in_=R[:, 97:105], axis=AX.X, op=ALU.max)
        dv("tensor_scalar", RR, out=R[:, 106:114], in0=R[:, 97:105], scalar1=R[:, 105:106], scalar2=1.0, op0=ALU.is_equal, op1=ALU.mult)
        dv("tensor_tensor", RR, out=R[:, 114:115], in0=R[:, 105:106], in1=R[:, 88:89], op=ALU.subtract)
        self.act(R[:, 115:116], R[:, 114:115], AF.Exp, [R], [R])
        dv("tensor_scalar", RR, out=R[:, 116:117], in0=R[:, 115:116], scalar1=1.0, scalar2=1.0, op0=ALU.add, op1=ALU.mult)
        dv("reciprocal", RR, out=R[:, 116:117], in_=R[:, 116:117])
        dv("tensor_tensor", RR, out=R[:, 117:118], in0=R[:, 116:117], in1=R[:, 39:40], op=ALU.mult)
        dv("tensor_tensor", RR, out=R[:, 118:119], in0=R[:, 39:40], in1=R[:, 117:118], op=ALU.subtract)
        dv("tensor_scalar", RR, out=R[:, 119:127], in0=R[:, 89:97], scalar1=R[:, 117:118], scalar2=0.0, op0=ALU.mult, op1=ALU.add)
        dv("scalar_tensor_tensor", RR, out=R[:, 119:127], in0=R[:, 106:114], scalar=R[:, 118:119], in1=R[:, 119:127], op0=ALU.mult, op1=ALU.add)
        self.V("dve", "tensor_tensor", [R], [self.RW], out=self.RW[:, c, :].rearrange("p (g e) -> p g e", g=4),
               in0=R[:, 40:44].unsqueeze(2).to_broadcast([128, 4, 8]), in1=R[:, 119:127].unsqueeze(1).to_broadcast([128, 4, 8]), op=ALU.mult)

    def phase_moe(self, l):
        S = self.S
        i = self.i
        with contextlib.ExitStack() as es:
            acc = self.sb(es, "moacc", [128, 10, D], F32)
            hTb = self.sb(es, "mohT", [128, 8, 1280], BF)
            wts = [(self.sb(es, "mowg%d" % k, [128, 8, 512], BF), self.sb(es, "mowu%d" % k, [128, 8, 512], BF),
                    self.sb(es, "mowd%d" % k, [128, 4, D], BF)) for k in range(2)]
            sG = [self.sb(es, "mosg%d" % k, [128, 512], F32) for k in range(2)]
            Hh = [self.sb(es, "moHh%d" % k, [128, 4, 512], BF) for k in range(2)]
            self.residual_setup(es, 5)
            n = 0
            m = 0
            for blk in ((0, 1, 2), (3, 4), (5, 6), (7, 8)):
                col = 0
                tcs = []
                for ti in blk:
                    t0, N = TILES[ti]
                    S.dma("sp", hTb[:, :, col:col + N], self.HT[:, :, t0:t0 + N].rearrange("k p n -> p k n"), [Tl(None, self.HTt[ti])], [hTb])
                    tcs.append((col, N, t0))
                    col += N
                for e in range(self.nexp):
                    wg, wu, wd = wts[e % 2]
                    S.dma("pool", wg[:], i["moe_w_gate"][l, e].rearrange("(kc p) f -> p kc f", p=128), [], [wg])
                    S.dma("pool", wu[:], i["moe_w_up"][l, e].rearrange("(kc p) f -> p kc f", p=128), [], [wu])
                    S.dma("pool", wd[:], i["moe_w_down"][l, e].rearrange("(fc p) n -> p fc n", p=128), [], [wd])
                    for (col, N, t0) in tcs:
                        hh = Hh[m % 2]
                        m += 1
                        for fc in range(4):
                            pG, pU = self.psb(), self.psb()
                            for kc in range(8):
                                self.mm(pG[:, 0:N], wg[:, kc, fc * 128:(fc + 1) * 128], hTb[:, kc, col:col + N], kc == 0, kc == 7, [wg, hTb], [pG])
                            for kc in range(8):
                                self.mm(pU[:, 0:N], wu[:, kc, fc * 128:(fc + 1) * 128], hTb[:, kc, col:col + N], kc == 0, kc == 7, [wu, hTb], [pU])
                            sg = sG[n % 2]
                            n += 1
                            self.act(sg[:, 0:N], pG[:, 0:N], AF.Silu, [pG], [sg])
                            self.V("dve", "tensor_tensor", [sg, pU], [hh], out=hh[:, fc, 0:N], in0=sg[:, 0:N], in1=pU[:, 0:N], op=ALU.mult)
                        for cc in range(N // 128):
                            ci = col // 128 + cc
                            c = t0 // 128 + cc
                            for half in range(2):
                                hs = slice(half * 512, (half + 1) * 512)
                                pD = self.psb()
                                for fc in range(4):
                                    self.mm(pD[:, :], hh[:, fc, cc * 128:(cc + 1) * 128], wd[:, fc, hs], fc == 0, fc == 3, [hh, wd], [pD])
                                if e == 0:
                                    self.V("dve", "tensor_scalar", [pD, self.RW], [acc], out=acc[:, ci, hs], in0=pD[:, :], scalar1=self.RW[:, c, e:e + 1], scalar2=0.0,
                                           op0=ALU.mult, op1=ALU.add)
                                else:
                                    self.V("dve", "scalar_tensor_tensor", [pD, self.RW, acc], [acc], out=acc[:, ci, hs], in0=pD[:, :], scalar=self.RW[:, c, e:e + 1],
                                           in1=acc[:, ci, hs], op0=ALU.mult, op1=ALU.add)
                for (col, N, t0) in tcs:
                    for cc in range(N // 128):
                        ci = col // 128 + cc
                        c = t0 // 128 + cc
                        self.residual(c, lambda half, ci=ci: acc[:, ci, half * 512:(half + 1) * 512], [acc, acc])
            S.barrier()

    def rank(self, c, R):
        A = R[:, 128:160]
        self.V("dve", "tensor_scalar", [self.RW], [R], out=A, in0=self.RW[:, c, :], scalar1=0.0, scalar2=1.0, op0=ALU.is_gt, op1=ALU.mult)
        if c == 0:
            self.V("dve", "memset", [], [self.Asum], self.Asum[:], 0.0)
        ps = self.psb()
        self.mm(ps[:, 0:32], self.ltri[:], A, True, False, [self.ltri, R], [ps])
        self.mm(ps[:, 0:32], self.cst[:, 128:256], self.Asum[:], False, True, [self.cst, self.Asum], [ps])
        self.act(self.RK[:, c, :], ps[:, 0:32], AF.Copy, [ps], [self.RK])
        self.V("dve", "tensor_tensor", [self.Asum, R], [self.Asum], out=self.Asum[:], in0=self.Asum[:], in1=A, op=ALU.add)

    def phase_moe_sparse(self, l):
        S = self.S
        i = self.i
        onesf = self.cst[:, 128:256]
        rows_t = Tl(None, self.ROWSt)
        acc_t = Tl(None, self.ACC2t)
        h2_t = Tl(None, self.H2t)
        with contextlib.ExitStack() as es:
            G = self.sb(es, "spG", [128, 1024], F32)
            widx = self.sb(es, "spwidx", [128, 2, NB], U32)
            init = self.sb(es, "spinit", [128, 128, 4], F32)
            self.V("pool", "memset", [], [init], init[:], 0.0)
            self.V("pool", "memset", [init], [init], init[:, :, 0:1], float(T))
            self.V("pool", "memset", [init], [init], init[:, :, 2:4], 1.0e6)
            S.dma("sp", self.ROWS.rearrange("(j p) c -> j (p c)", p=128), init[0:NB, :, :].rearrange("j p c -> j (p c)"), [init], [rows_t])
            ps = self.psb()
            self.mm(ps[:, 0:32], onesf, self.Asum[:], True, True, [self.cst, self.Asum], [ps])
            cnt, pad, pend, pst = G[:, 0:32], G[:, 32:64], G[:, 64:96], G[:, 96:128]
            cmp = self.sb(es, "spcmp", [128, NB, 32], F32)
            self.V("dve", "tensor_copy", [ps], [G], out=cnt, in_=ps[:, 0:32])
            cmp2 = cmp[:].rearrange("p a b -> p (a b)")[:, 0:32 * 68].rearrange("p (e m) -> p e m", m=68)
            self.V("dve", "tensor_tensor", [G, self.cst], [cmp], out=cmp2, in0=cnt.unsqueeze(2).to_broadcast([128, 32, 68]),
                   in1=self.cst[:, 896:896 + 68].unsqueeze(1).to_broadcast([128, 32, 68]), op=ALU.is_gt)
            self.V("dve", "tensor_reduce", [cmp], [G], out=pad, in_=cmp2, axis=AX.X, op=ALU.add)
            self.V("dve", "tensor_scalar", [G], [G], out=pad, in0=pad, scalar1=128.0, scalar2=0.0, op0=ALU.mult, op1=ALU.add)
            self.V("dve", "tensor_tensor_scan", [G, self.cst], [G], out=pend, data0=onesf[:, 0:32], data1=pad, initial=0.0, op0=ALU.mult, op1=ALU.add)
            self.V("dve", "tensor_tensor", [G], [G], out=pst, in0=pend, in1=pad, op=ALU.subtract)
            self.V("dve", "tensor_tensor", [G, self.cst], [cmp], out=cmp[:], in0=pend.unsqueeze(1).to_broadcast([128, NB, 32]),
                   in1=self.cst[:, 896:896 + NB].unsqueeze(2).to_broadcast([128, NB, 32]), op=ALU.is_le)
            be = G[:, 128:128 + NB]
            self.V("dve", "tensor_reduce", [cmp], [G], out=be, in_=cmp[:], axis=AX.X, op=ALU.add)
            self.V("dve", "tensor_scalar", [G], [G], out=be, in0=be, scalar1=31.0, scalar2=128.0, op0=ALU.min, op1=ALU.mult)
            same = G[:, 384:384 + NB]
            self.V("dve", "memset", [G], [G], same, 0.0)
            self.V("dve", "tensor_tensor", [G], [G], out=G[:, 386:384 + NB], in0=G[:, 130:128 + NB], in1=G[:, 128:126 + NB], op=ALU.is_equal)
            wf = G[:, 256:256 + NB]
            self.V("dve", "tensor_scalar", [G, self.cst], [G], out=wf, in0=be, scalar1=self.cst[:, 1024:1025], scalar2=2.0, op0=ALU.add, op1=ALU.mult)
            self.V("dve", "tensor_scalar", [G], [G], out=wf, in0=wf, scalar1=float(l * 8192), scalar2=1.0, op0=ALU.add, op1=ALU.mult)
            self.V("dve", "scalar_tensor_tensor", [G], [G], out=wf, in0=same, scalar=1.0e8, in1=wf, op0=ALU.mult, op1=ALU.add)
            self.V("dve", "tensor_copy", [G], [widx], out=widx[:, 0, :], in_=wf)
            self.V("dve", "tensor_scalar", [G], [G], out=wf, in0=wf, scalar1=1.0, scalar2=1.0, op0=ALU.add, op1=ALU.mult)
            self.V("dve", "tensor_copy", [G], [widx], out=widx[:, 1, :], in_=wf)
            Q = [self.sb(es, "spQ%d" % k, [128, 160], F32) for k in range(2)]
            rec = [self.sb(es, "sprec%d" % k, [128, 2, 4], F32) for k in range(2)]
            didx = [self.sb(es, "spdidx%d" % k, [128, 2], U32) for k in range(2)]
            for c in range(NCH):
                q = Q[c % 2]
                r_ = rec[c % 2]
                di = didx[c % 2]
                A, dst, d1, m1 = q[:, 0:32], q[:, 32:64], q[:, 64:96], q[:, 96:128]
                rd = [self.RW, self.RK, G, q]
                self.V("dve", "tensor_scalar", rd, [q], out=A, in0=self.RW[:, c, :], scalar1=0.0, scalar2=1.0, op0=ALU.is_gt, op1=ALU.mult)
                self.V("dve", "tensor_tensor", rd, [q], out=dst, in0=self.RK[:, c, :], in1=pst, op=ALU.add)
                self.V("dve", "scalar_tensor_tensor", rd, [q], out=d1, in0=dst, scalar=1.0, in1=A, op0=ALU.add, op1=ALU.mult)
                self.V("dve", "tensor_reduce", rd, [q], out=q[:, 128:129], in_=d1, axis=AX.X, op=ALU.max)
                self.V("dve", "tensor_scalar", rd, [q], out=m1, in0=d1, scalar1=q[:, 128:129], scalar2=1.0, op0=ALU.is_equal, op1=ALU.mult)
                self.V("dve", "tensor_tensor", rd, [q], out=m1, in0=m1, in1=self.RW[:, c, :], op=ALU.mult)
                self.V("dve", "tensor_reduce", rd, [q], out=q[:, 129:130], in_=m1, axis=AX.X, op=ALU.add)
                self.V("dve", "tensor_reduce", rd, [q], out=q[:, 130:131], in_=self.RW[:, c, :], axis=AX.X, op=ALU.add)
                self.V("dve", "tensor_scalar", rd, [q], out=m1, in0=A, scalar1=-1.0e9, scalar2=1.0e9, op0=ALU.mult, op1=ALU.add)
                self.V("dve", "tensor_tensor", rd, [q], out=m1, in0=m1, in1=dst, op=ALU.add)
                self.V("dve", "tensor_reduce", rd, [q], out=q[:, 131:132], in_=m1, axis=AX.X, op=ALU.min)
                self.V("dve", "tensor_scalar", rd, [q], out=q[:, 132:133], in0=q[:, 128:129], scalar1=-1.0, scalar2=1.0, op0=ALU.add, op1=ALU.mult)
                self.V("dve", "memset", [], [r_], r_[:], 0.0)
                for k in range(2):
                    self.V("dve", "tensor_scalar", [self.cst, r_], [r_], out=r_[:, k, 0:1], in0=self.cst[:, 1024:1025], scalar1=float(c * 128), scalar2=1.0,
                           op0=ALU.add, op1=ALU.mult)
                    self.V("dve", "tensor_scalar", [self.cst, r_], [r_], out=r_[:, k, 2:3], in0=self.cst[:, 1024:1025], scalar1=float(c * 128 + k * T), scalar2=2.0,
                           op0=ALU.add, op1=ALU.mult)
                    self.V("dve", "tensor_scalar", [r_], [r_], out=r_[:, k, 3:4], in0=r_[:, k, 2:3], scalar1=1.0, scalar2=1.0, op0=ALU.add, op1=ALU.mult)
                self.V("dve", "tensor_tensor", [q, r_], [r_], out=r_[:, 0, 1:2], in0=q[:, 130:131], in1=q[:, 129:130], op=ALU.subtract)
                self.V("dve", "tensor_copy", [q, r_], [r_], out=r_[:, 1, 1:2], in_=q[:, 129:130])
                self.V("dve", "tensor_copy", [q], [di], out=di[:, 0:1], in_=q[:, 131:132])
                self.V("dve", "tensor_copy", [q], [di], out=di[:, 1:2], in_=q[:, 132:133])
                for k in range(2):
                    S.dma_fn("pool", (lambda e, r_=r_, di=di, k=k: e.indirect_dma_start(out=self.ROWS, out_offset=bass.IndirectOffsetOnAxis(ap=di[:, k:k + 1], axis=0),
                                                                                      in_=r_[:, k, :], in_offset=None)), [r_, di], [rows_t])
            wgv = i["moe_w_gate"].rearrange("l e (p j) f -> (l e p) (j f)", j=8).rearrange("r (h x) -> (r h) x", h=2)
            wuv = i["moe_w_up"].rearrange("l e (p j) f -> (l e p) (j f)", j=8).rearrange("r (h x) -> (r h) x", h=2)
            wdv = i["moe_w_down"].rearrange("l e (p j) n -> (l e p) (j n)", j=4).rearrange("r (h x) -> (r h) x", h=2)
            wts = [(self.sb(es, "spwg%d" % k, [128, 8, 512], BF), self.sb(es, "spwu%d" % k, [128, 8, 512], BF),
                    self.sb(es, "spwd%d" % k, [128, 4, D], BF)) for k in range(2)]
            NQ = 4
            recs = [self.sb(es, "sprc%d" % k, [128, 4], F32) for k in range(NQ)]
            recu = [self.sb(es, "spru%d" % k, [128, 4], U32) for k in range(NQ)]
            hbs = [self.sb(es, "sphb%d" % k, [128, D], BF) for k in range(NQ)]
            hTs = [self.sb(es, "sphT%d" % k, [128, 8, 128], BF) for k in range(NQ)]
            sGs = [self.sb(es, "spsg%d" % k, [128, 512], F32) for k in range(2)]
            Hhs = [self.sb(es, "spHh%d" % k, [128, 512], BF) for k in range(2)]
            HhTs = [self.sb(es, "spHhT%d" % k, [128, 4, 128], BF) for k in range(2)]
            ys = [self.sb(es, "spy%d" % k, [128, D], F32) for k in range(2)]
            def gather(dst_ap, src, idx_ap, r, w, skip=False):
                if skip:
                    S.dma_fn("pool", (lambda e: e.indirect_dma_start(out=dst_ap, out_offset=None, in_=src, in_offset=bass.IndirectOffsetOnAxis(ap=idx_ap, axis=0),
                                                                     bounds_check=self._wbound_reg(e), oob_is_err=False)), r, w)
                else:
                    S.dma_fn("pool", (lambda e: e.indirect_dma_start(out=dst_ap, out_offset=None, in_=src, in_offset=bass.IndirectOffsetOnAxis(ap=idx_ap, axis=0))), r, w)

            def proA(j):
                rc, ru, hb = recs[j % NQ], recu[j % NQ], hbs[j % NQ]
                S.dma("sp", rc[:], self.ROWS[j * 128:(j + 1) * 128, :], [rows_t], [rc])
                self.V("dve", "tensor_copy", [rc], [ru], out=ru[:], in_=rc[:])
                gather(hb[:], self.H2, ru[:, 0:1], [ru, h2_t], [hb])

            def proB(j):
                hb, hT = hbs[j % NQ], hTs[j % NQ]
                pt = self.psb()
                ptv = pt[:].bitcast(BF).rearrange("p (k n) -> p k n", k=8)
                hbv = hb[:].rearrange("t (p j) -> t p j", j=8)
                for jx in range(8):
                    self.tr(ptv[:, jx, :], hbv[:, :, jx], self.identb[:], [hb, self.identb], [pt])
                self.act(hT[:], ptv, AF.Copy, [pt], [hT])

            def wload(j):
                wg, wu, wd = wts[j % 2]
                for h_ in range(2):
                    gather(wg[:, h_ * 4:(h_ + 1) * 4, :].rearrange("p j f -> p (j f)"), wgv, widx[:, h_, j:j + 1], [widx], [wg], skip=True)
                    gather(wu[:, h_ * 4:(h_ + 1) * 4, :].rearrange("p j f -> p (j f)"), wuv, widx[:, h_, j:j + 1], [widx], [wu], skip=True)
                    gather(wd[:, h_ * 2:(h_ + 1) * 2, :].rearrange("p j f -> p (j f)"), wdv, widx[:, h_, j:j + 1], [widx], [wd], skip=True)

            proA(0)
            proA(1)
            wload(0)
            proB(0)
            for j in range(NB):
                z = j % 2
                rc, ru, hT = recs[j % NQ], recu[j % NQ], hTs[j % NQ]
                sg, Hh, HhT, y = sGs[z], Hhs[z], HhTs[z], ys[z]
                wg, wu, wd = wts[z]
                if j + 2 < NB:
                    proA(j + 2)
                if j + 1 < NB:
                    wload(j + 1)
                pG, pU = self.psb(), self.psb()
                for jx in range(8):
                    self.mm(pG[:, :], hT[:, jx, :], wg[:, jx, :], jx == 0, jx == 7, [hT, wg], [pG])
                for jx in range(8):
                    self.mm(pU[:, :], hT[:, jx, :], wu[:, jx, :], jx == 0, jx == 7, [hT, wu], [pU])
                self.act(sg[:], pG[:, :], AF.Silu, [pG], [sg])
                self.V("dve", "scalar_tensor_tensor", [pU, rc, sg], [Hh], out=Hh[:], in0=pU[:, :], scalar=rc[:, 1:2], in1=sg[:], op0=ALU.mult, op1=ALU.mult)
                if j + 1 < NB:
                    proB(j + 1)
                pt2 = self.psb()
                pt2v = pt2[:].bitcast(BF)[:, 0:512].rearrange("p (k n) -> p k n", k=4)
                Hhv = Hh[:].rearrange("t (p j) -> t p j", j=4)
                for jx in range(4):
                    self.tr(pt2v[:, jx, :], Hhv[:, :, jx], self.identb[:], [Hh, self.identb], [pt2])
                self.act(HhT[:], pt2v, AF.Copy, [pt2], [HhT])
                for half in range(2):
                    pD = self.psb()
                    for jx in range(4):
                        self.mm(pD[:, :], HhT[:, jx, :], wd[:, jx, half * 512:(half + 1) * 512], jx == 0, jx == 3, [HhT, wd], [pD])
                    if half == 0:
                        self.act(y[:, 0:512], pD[:, :], AF.Copy, [pD], [y])
                    else:
                        self.V("dve", "tensor_copy", [pD], [y], out=y[:, 512:1024], in_=pD[:, :])
                for h_ in range(2):
                    S.dma_fn("pool", (lambda e, y=y, ru=ru, h_=h_: e.indirect_dma_start(out=self.ACC2.rearrange("r (h x) -> (r h) x", h=2),
                                                                                        out_offset=bass.IndirectOffsetOnAxis(ap=ru[:, 2 + h_:3 + h_], axis=0),
                                                                                        in_=y[:, h_ * 512:(h_ + 1) * 512], in_offset=None,
                                                                                        bounds_check=self._bound_reg(e), oob_is_err=False)), [y, ru], [acc_t])
            self.residual_setup(es, 5)
            a0 = [self.sb(es, "spa0%d" % k, [128, D], F32) for k in range(2)]
            a1 = [self.sb(es, "spa1%d" % k, [128, D], F32) for k in range(2)]
            for c in range(NCH):
                u0, u1 = a0[c % 2], a1[c % 2]
                S.dma("sp", u0[:], self.ACC2[c * 128:(c + 1) * 128, :], [acc_t], [u0])
                S.dma("sp", u1[:], self.ACC2[T + c * 128:T + (c + 1) * 128, :], [acc_t], [u1])
                self.V("pool", "tensor_tensor", [u0, u1], [u0], out=u0[:], in0=u0[:], in1=u1[:], op=ALU.add)
                self.residual(c, lambda half, u0=u0: u0[:, half * 512:(half + 1) * 512], [u0, u0])
            S.barrier()

    def _wbound_reg(self, e):
        if getattr(self, "_wbreg", None) is None:
            self._wbreg = e.to_reg(self.n_layers * 32 * 128 * 2 - 1)
        return self._wbreg

    def _bound_reg(self, e):
        if getattr(self, "_breg", None) is None:
            self._breg = e.to_reg(4 * T - 1)
        return self._breg

    def final_norm(self):
        S = self.S
        with contextlib.ExitStack() as es:
            g = self.sb(es, "fng", [128, D], F32)
            S.dma("sp", g[:], self.i["final_g"].partition_broadcast(128), [], [g])
            xs = [self.sb(es, "fnx%d" % k, [128, D], F32) for k in range(3)]
            sq = self.sb(es, "fnsq", [128, D], F32)
            st = [self.sb(es, "fnst%d" % k, [128, 4], F32) for k in range(2)]
            ot = [self.sb(es, "fno%d" % k, [128, D], F32) for k in range(2)]
            for c in range(2, NCH):
                x = xs[c % 3]
                S.dma("sp", x[:], self.X[c * 128:(c + 1) * 128, :], [Tl(None, self.Xt[c])], [x])
                s_ = st[c % 2]
                self.act(sq[:], x[:], AF.Square, [x], [sq, s_], accum_out=s_[:, 0:1])
                self.V("dve", "tensor_scalar", [s_], [s_], out=s_[:, 1:2], in0=s_[:, 0:1], scalar1=1.0 / D, scalar2=EPS, op0=ALU.mult, op1=ALU.add)
                self.V("dve", "reciprocal", [s_], [s_], out=s_[:, 2:3], in_=s_[:, 1:2])
                self.act(s_[:, 3:4], s_[:, 2:3], AF.Sqrt, [s_], [s_])
                o = ot[c % 2]
                self.V("dve", "scalar_tensor_tensor", [x, s_, g], [o], out=o[:], in0=x[:], scalar=s_[:, 3:4], in1=g[:], op0=ALU.mult, op1=ALU.mult)
                S.dma("sp", self.out[(c - 2) * 128:(c - 1) * 128, :], o[:], [o], [Tl(None, self.outt)])


def make_consts():
    cst = np.zeros((128, 1056), np.float32)
    cst[:, 0:128] = np.eye(128, dtype=np.float32)
    cst[:, 128:256] = 1.0
    R = np.zeros((128, 128), np.float32)
    for blk in range(2):
        o = blk * 64
        for q in range(16):
            R[o + 16 + q, o + q] = -1.0
            R[o + q, o + 16 + q] = 1.0
            R[o + 48 + q, o + 32 + q] = -1.0
            R[o + 32 + q, o + 48 + q] = 1.0
    cst[:, 256:384] = R
    s = np.arange(128)[:, None]
    t = np.arange(128)[None, :]
    same = (s // 64) == (t // 64)
    same32 = (s // 32) == (t // 32)
    cst[:, 384:512] = (same32 & (t >= s)).astype(np.float32)
    cst[:, 512:640] = (same32 & (t <= s)).astype(np.float32)
    cst[:, 640:768] = (same & (s % 64 < 32) & (t % 64 >= 32)).astype(np.float32)
    cst[:, 768:896] = (same & (s % 64 >= 32) & (t % 64 < 32)).astype(np.float32)
    cst[:, 896:1024] = 128.0 * np.arange(128, dtype=np.float32)[None, :]
    cst[:, 1024] = np.arange(128, dtype=np.float32)
    cst[:, 1025:1057 - 1 + 0] = 0.0
    inv_freq = (10000.0 ** (-np.arange(0, 32, 2, dtype=np.float32) / 32)).astype(np.float32)
    pos = np.arange(NLAT)
    row = (pos // 64).astype(np.float32)
    col = (pos % 64).astype(np.float32)
    ang_r = row[:, None] * inv_freq
    ang_c = col[:, None] * inv_freq
    ang = np.concatenate([ang_r, ang_r, ang_c, ang_c], axis=-1).astype(np.float32)
    rope = np.zeros((2, 128, T), np.float32)
    rope[0, :, :NCTX] = 1.0
    rope[0, 0:64, NCTX:] = np.cos(ang).T
    rope[0, 64:128, NCTX:] = np.cos(ang).T
    rope[1, 0:64, NCTX:] = np.sin(ang).T
    rope[1, 64:128, NCTX:] = np.sin(ang).T
    return cst, rope


def make_in_maps(inputs, cores, L=DEPTH, nexp=32):
    f = lambda a: np.ascontiguousarray(np.asarray(a, dtype=np.float32))
    cst, rope = make_consts()
    shared = {
        "c_ctx": f(inputs["c_ctx"]).reshape(1, D),
        "ada_w": f(inputs["ada_w"][:L]), "ada_b": f(inputs["ada_b"]),
        "norm1_g": f(inputs["norm1_g"]), "norm2_g": f(inputs["norm2_g"]),
        "w_in": f(inputs["w_in"][:L]), "w_branch": f(inputs["w_branch"][:L]), "w_out": f(inputs["w_out"][:L]),
        "hg_lb": f(inputs["hg_lb_logits"]), "hg_norm_g": f(inputs["hg_norm_g"]),
        "da_lambda": f(inputs["da_lambda"]).reshape(DEPTH, 256), "da_norm_g": f(inputs["da_norm_g"]),
        "cv_dw_w": f(inputs["cv_dw_w"]), "cv_dw_b": f(inputs["cv_dw_b"]),
        "cv_ln_g": f(inputs["cv_ln_g"]), "cv_ln_b": f(inputs["cv_ln_b"]),
        "sc_w": f(inputs["sc_w"]),
        "moe_w_r": np.ascontiguousarray(np.concatenate([f(inputs["moe_w_grp"]), f(inputs["moe_w_exp"])], axis=-1)),
        "moe_b_r": np.ascontiguousarray(np.concatenate([f(inputs["moe_b_grp"]), f(inputs["moe_b_exp"])], axis=-1)),
        "moe_w_gate": f(inputs["moe_w_gate"][:L, :nexp]), "moe_w_up": f(inputs["moe_w_up"][:L, :nexp]), "moe_w_down": f(inputs["moe_w_down"][:L, :nexp]),
        "final_g": f(inputs["final_g"]).reshape(1, D),
        "cst": cst, "rope": rope, "ltri": np.triu(np.ones((128, 128), np.float32), 1),
    }
    maps = []
    for cid in cores:
        b = cid % 4
        m = dict(shared)
        m["x"] = f(inputs["x"][b])
        m["c"] = f(inputs["c"][b]).reshape(1, D)
        m["ctx"] = f(inputs["ctx"][b])
        maps.append(m)
    return maps


def kernel(**inputs):
    nc = bass.Bass("TRN2", target_bir_lowering=False)
    Prog(nc).build()
    maps = make_in_maps(inputs, list(range(4)))
    res = run_bass_kernel_spmd(nc, maps, core_ids=list(range(4)))
    return np.stack([np.asarray(res.results[b]["out"], dtype=np.float32) for b in range(4)], axis=0)
```

```python
import contextlib
import math
import numpy as np
import concourse.bass as bass
import concourse.mybir as mybir
from concourse.bass_utils import run_bass_kernel_spmd

F32 = mybir.dt.float32
BF = mybir.dt.bfloat16
U32 = mybir.dt.uint32
AF = mybir.ActivationFunctionType
ALU = mybir.AluOpType
AX = mybir.AxisListType

D = 1024
NCTX = 256
NLAT = 4096
T = NCTX + NLAT
NCH = T // 128
NB = 2 * T // 128 + 32
SPARSE = True
DEPTH = 4
W = 512
INW = 10752
EPS = 1e-6
TILES = [(0, 256)] + [(256 + 512 * i, 512) for i in range(8)]
C_HQ, C_HI, C_HFF, C_HFB, C_HG = 0, 512, 1024, 1536, 2048
C_DQ, C_DK, C_DV = 2560, 3072, 3584
C_CVA, C_CVG = 4096, 4608
C_SB, C_SC, C_SX = 5120, 5632, 6144
C_GATE = 6656


class Trk:
    __slots__ = ("w", "r")

    def __init__(self):
        self.w = None
        self.r = {}


class Tl:
    def __init__(self, h, trk=None):
        self.h = h
        self.t = trk or Trk()

    def __getitem__(self, k):
        return self.h[k]


class Stream:
    def __init__(self, name):
        self.name = name
        self.ops = []
        self.seen = {}
        self.sem = None
        self.cnt = 0
        self.dslots = []
        self.dnext = 0


class Sch:
    SEM_MAX = 30000

    def __init__(self, nc, es):
        self.nc = nc
        self.es = es
        self.st = {k: Stream(k) for k in ("pe", "act", "dve", "pool", "sp")}
        self.nsem = 0
        for k, s in self.st.items():
            self._newsem(s)
        for k, n in (("sp", 24), ("pool", 12), ("act", 6)):
            s = self.st[k]
            for i in range(n):
                s.dslots.append([self._sem(), 0])

    def _sem(self):
        self.nsem += 1
        return self.es.enter_context(self.nc.semaphore("s%d" % self.nsem))

    def _newsem(self, s):
        s.sem = self._sem()
        s.cnt = 0

    def _need(self, s, tok, waits):
        if tok is None:
            return
        sem, val, src = tok
        if src == "pe" and s.name == "pe":
            return
        if s.seen.get(id(sem), 0) >= val:
            return
        k = id(sem)
        if k not in waits or waits[k][1] < val:
            waits[k] = (sem, val)

    def _deps(self, s, reads, writes):
        waits = {}
        for b in reads:
            self._need(s, b.t.w, waits)
        for b in writes:
            self._need(s, b.t.w, waits)
            for tok in b.t.r.values():
                self._need(s, tok, waits)
        for k, (sem, val) in waits.items():
            s.seen[k] = val
        return list(waits.values())

    def _mark(self, tok, reads, writes):
        for b in reads:
            b.t.r[id(tok[0])] = tok
        for b in writes:
            b.t.w = tok
            b.t.r = {}

    def op(self, eng, fn, reads=(), writes=()):
        s = self.st[eng]
        if s.cnt >= self.SEM_MAX:
            self._newsem(s)
        waits = self._deps(s, reads, writes)
        s.cnt += 1
        tok = (s.sem, s.cnt, eng)
        s.ops.append((waits, fn, (s.sem, 1)))
        self._mark(tok, reads, writes)
        return tok

    def dma(self, q, out, in_, reads=(), writes=(), **kw):
        return self.dma_fn(q, (lambda e: e.dma_start(out=out, in_=in_, **kw)), reads, writes)

    def dma_fn(self, q, fn, reads=(), writes=()):
        s = self.st[q]
        slot = s.dslots[s.dnext % len(s.dslots)]
        s.dnext += 1
        waits = self._deps(s, reads, writes)
        if slot[1] > 0 and s.seen.get(id(slot[0]), 0) < slot[1]:
            waits.append((slot[0], slot[1]))
            s.seen[id(slot[0])] = slot[1]
        slot[1] += 16
        tok = (slot[0], slot[1], "dma")
        s.ops.append((waits, fn, (slot[0], 16)))
        self._mark(tok, reads, writes)
        return tok

    def barrier(self):
        toks = []
        for s in self.st.values():
            if s.cnt > 0:
                toks.append((s.sem, s.cnt))
            for sl in s.dslots:
                if sl[1] > 0:
                    toks.append((sl[0], sl[1]))
        for s in self.st.values():
            waits = []
            for sem, val in toks:
                if sem is s.sem:
                    continue
                if s.seen.get(id(sem), 0) < val:
                    waits.append((sem, val))
                    s.seen[id(sem)] = val
            if waits:
                s.ops.append((waits, None, None))

    def emit(self):
        nc = self.nc
        self.barrier()
        with nc.Block() as block:
            def run(s):
                def f(e):
                    for waits, fn, inc in s.ops:
                        for sem, val in waits:
                            e.wait_ge(sem, val)
                        if fn is not None:
                            fn(e).then_inc(inc[0], inc[1])
                return f
            block.tensor(run(self.st["pe"]))
            block.scalar(run(self.st["act"]))
            block.vector(run(self.st["dve"]))
            block.gpsimd(run(self.st["pool"]))
            block.sync(run(self.st["sp"]))


class Prog:
    def __init__(self, nc, n_layers=DEPTH, dbg=None, nexp=32):
        self.nc = nc
        self.nexp = nexp
        self.n_layers = n_layers
        self.dbg = dbg or {}

    def sb(self, es, name, shape, dt):
        self.uid = getattr(self, "uid", 0) + 1
        return Tl(es.enter_context(self.nc.sbuf_tensor("%s_u%d" % (name, self.uid), list(shape), dt)))

    def dram(self, name, shape, dt, kind="Internal"):
        return self.nc.dram_tensor(name, list(shape), dt, kind=kind).ap()

    def mm(self, out, lhsT, rhs, start, stop, r, w):
        self.S.op("pe", lambda e: e.matmul(out, lhsT=lhsT, rhs=rhs, start=start, stop=stop), r, w)

    def tr(self, out, in_, ident, r, w):
        self.S.op("pe", lambda e: e.transpose(out=out, in_=in_, identity=ident), r, w)

    def act(self, out, in_, func, r, w, **kw):
        self.S.op("act", lambda e: e.activation(out=out, in_=in_, func=func, **kw), r, w)

    def V(self, eng, name, r, w, *a, **kw):
        self.S.op(eng, lambda e: getattr(e, name)(*a, **kw), r, w)

    def psb(self):
        b = self.banks[self.bi % 8]
        self.bi += 1
        return b

    def build(self):
        nc = self.nc
        L = self.n_layers
        i = {}
        def inp(name, shape, dt=F32):
            i[name] = self.dram(name, shape, dt, kind="ExternalInput")
        inp("x", [NLAT, D]); inp("c", [1, D]); inp("ctx", [NCTX, D]); inp("c_ctx", [1, D])
        inp("ada_w", [L, D, 6 * D]); inp("ada_b", [DEPTH, 6 * D])
        inp("norm1_g", [DEPTH, D]); inp("norm2_g", [DEPTH, D])
        inp("w_in", [L, D, INW]); inp("w_branch", [L, 4, W, D]); inp("w_out", [L, D, D])
        inp("hg_lb", [DEPTH, 2, W]); inp("hg_norm_g", [DEPTH, W])
        inp("da_lambda", [DEPTH, 256]); inp("da_norm_g", [DEPTH, 128])
        inp("cv_dw_w", [DEPTH, 31, W]); inp("cv_dw_b", [DEPTH, W]); inp("cv_ln_g", [DEPTH, W]); inp("cv_ln_b", [DEPTH, W])
        inp("sc_w", [DEPTH, 3, W])
        inp("moe_w_r", [DEPTH, D, 36]); inp("moe_b_r", [DEPTH, 36])
        inp("moe_w_gate", [L, self.nexp, D, W]); inp("moe_w_up", [L, self.nexp, D, W]); inp("moe_w_down", [L, self.nexp, W, D])
        inp("final_g", [1, D]); inp("ltri", [128, 128])
        inp("cst", [128, 1056]); inp("rope", [2, 128, T])
        self.i = i
        self.out = self.dram("out", [NLAT, D], F32, kind="ExternalOutput")
        self.X = self.dram("Xs", [T, D], F32)
        self.HT = self.dram("HTs", [8, 128, T], BF)
        self.Y = self.dram("Ys", [4, 4, 128, T], BF)
        self.MT = self.dram("MTs", [8, 128, T], BF)
        self.MODR = self.dram("MODs", [2, 128, 6 * D], F32)
        self.H2 = self.dram("H2s", [T + 1, D], BF)
        self.ROWS = self.dram("ROWSs", [NB * 128, 4], F32)
        self.ACC2 = self.dram("ACC2s", [2 * T, D], F32)
        self.H2t = Trk(); self.ROWSt = Trk(); self.ACC2t = Trk()
        self.Xt = [Trk() for _ in range(NCH)]
        self.HTt = [Trk() for _ in range(9)]
        self.Yt = [[Trk() for _ in range(9)] for _ in range(4)]
        self.MTt = [Trk() for _ in range(9)]
        self.MODt = Trk()
        self.outt = Trk()
        dbg_out = {}
        for k, shp in self.dbg.items():
            if not isinstance(shp, tuple):
                continue
            dbg_out[k] = self.dram("dbg_" + k, shp[0], shp[1], kind="ExternalOutput")
        self.dbg_out = dbg_out

        with contextlib.ExitStack() as es:
            self.S = S = Sch(nc, es)
            self.banks = [Tl(es.enter_context(nc.psum_tensor("pb%d" % k, [128, 512], F32))) for k in range(8)]
            self.bi = 0
            self.cst = self.sb(es, "cst_sb", [128, 1056], F32)
            S.dma("sp", self.cst[:], i["cst"], [], [self.cst])
            self.identb = self.sb(es, "identb", [128, 128], BF)
            self.onesb = self.sb(es, "onesb", [128, 128], BF)
            self.Rb = self.sb(es, "Rb", [128, 128], BF)
            self.identf = Tl(self.cst.h, self.cst.t)
            self.V("dve", "tensor_copy", [self.cst], [self.identb], out=self.identb[:], in_=self.cst[:, 0:128])
            self.V("dve", "tensor_copy", [self.cst], [self.onesb], out=self.onesb[:], in_=self.cst[:, 128:256])
            self.V("dve", "tensor_copy", [self.cst], [self.Rb], out=self.Rb[:], in_=self.cst[:, 256:384])
            self.sT = []
            for which, src in enumerate((i["c"], i["c_ctx"])):
                cT = self.sb(es, "cT%d" % which, [128, 8], F32)
                S.dma("sp", cT[:], src.rearrange("o (kc p) -> p (o kc)", p=128), [], [cT], allow_slow_non_contiguous=True)
                sg = self.sb(es, "cS%d" % which, [128, 8], F32)
                self.act(sg[:], cT[:], AF.Silu, [cT], [sg])
                rep = self.sb(es, "sT%d" % which, [128, 8, 128], F32)
                self.V("dve", "tensor_copy", [sg], [rep], out=rep[:], in_=sg[:].unsqueeze(2).to_broadcast([128, 8, 128]))
                self.sT.append(rep)
            self.P = self.sb(es, "Pparams", [128, 600], F32)
            self.RW = self.sb(es, "RW", [128, NCH, 32], F32)
            self.RK = self.sb(es, "RK", [128, NCH, 32], F32)
            self.Asum = self.sb(es, "Asum", [128, 32], F32)
            self.ltri = self.sb(es, "ltri_sb", [128, 128], F32)
            S.dma("sp", self.ltri[:], i["ltri"], [], [self.ltri])
            zr = self.sb(es, "zrow", [1, D], BF)
            self.V("dve", "memset", [], [zr], zr[:], 0.0)
            S.dma("sp", self.H2[T:T + 1, :], zr[:], [zr], [Tl(None, self.H2t)])
            self.LB = self.sb(es, "LB", [128, DEPTH, 8], F32)
            self.OML = self.sb(es, "OML", [128, DEPTH, 8], F32)
            lbe = self.sb(es, "lbe", [128, DEPTH, 8], F32)
            lbt = self.sb(es, "lbt", [128, 16], F32)
            for l_ in range(DEPTH):
                for d_ in range(2):
                    for j_ in range(4):
                        S.dma("sp", lbe[:, l_, d_ * 4 + j_:d_ * 4 + j_ + 1], i["hg_lb"][l_, d_:d_ + 1, j_ * 128:(j_ + 1) * 128].rearrange("o p -> p o"),
                              [], [lbe], allow_slow_non_contiguous=True)
            self.act(lbe[:], lbe[:], AF.Exp, [lbe], [lbe])
            self.V("dve", "tensor_tensor", [lbe], [lbt], out=lbt[:, 0:8], in0=lbe[:, 0, :], in1=lbe[:, 1, :], op=ALU.add)
            self.V("dve", "tensor_tensor", [lbe, lbt], [lbt], out=lbt[:, 0:8], in0=lbt[:, 0:8], in1=lbe[:, 2, :], op=ALU.add)
            self.V("dve", "tensor_tensor", [lbe, lbt], [lbt], out=lbt[:, 0:8], in0=lbt[:, 0:8], in1=lbe[:, 3, :], op=ALU.add)
            self.V("dve", "reciprocal", [lbt], [lbt], out=lbt[:, 8:16], in_=lbt[:, 0:8])
            self.V("dve", "memset", [], [self.LB], self.LB[:], 0.0)
            for l_ in range(1, DEPTH):
                self.V("dve", "tensor_tensor", [lbe, lbt], [lbe], out=lbe[:, l_, :], in0=lbe[:, l_, :], in1=lbt[:, 8:16], op=ALU.mult)
                self.V("dve", "tensor_tensor", [lbe, self.LB], [self.LB], out=self.LB[:, l_, :], in0=self.LB[:, l_ - 1, :], in1=lbe[:, l_, :], op=ALU.add)
            self.V("dve", "tensor_scalar", [self.LB], [self.OML], out=self.OML[:], in0=self.LB[:], scalar1=-1.0, scalar2=1.0, op0=ALU.mult, op1=ALU.add)
            self.seg = self.sb(es, "seg", [128, 512], F32)
            self.V("dve", "memset", [], [self.seg], self.seg[:], 1.0)
            self.V("dve", "memset", [self.seg], [self.seg], self.seg[:].rearrange("p (c s) -> p c s", s=64)[:, :, 0:1], 0.0)
            S.barrier()
            S.dma("sp", self.X[0:NCTX, :], i["ctx"], [], self._xt(0, 2))
            for q in range(4):
                S.dma("sp", self.X[NCTX + q * 1024: NCTX + (q + 1) * 1024, :], i["x"][q * 1024:(q + 1) * 1024, :], [],
                      self._xt(2 + q * 8, 8))
            for l in range(L):
                self.layer(l)
            self.final_norm()
            S.emit()

    def _xt(self, c0, n):
        return [Tl(None, t) for t in self.Xt[c0:c0 + n]]

    def layer(self, l):
        stop = self.dbg.get("stop")
        self.phase_mod(l)
        self.load_params(l)
        self.phase_norm(l, 1)
        if stop == "norm1":
            return
        self.phase_sconv(l)
        self.phase_conv(l)
        if stop == "convs":
            return self.dump_dbg()
        self.phase_attn(l)
        if stop == "attn":
            return self.dump_dbg()
        self.phase_hgrn(l)
        if stop == "hgrn":
            return self.dump_dbg()
        self.phase_merge(l)
        if stop == "merge":
            return self.dump_dbg()
        self.phase_norm(l, 2)
        if SPARSE:
            self.phase_moe_sparse(l)
        else:
            self.phase_moe(l)
        if stop == "moe":
            return self.dump_dbg()

    def dump_dbg(self):
        S = self.S
        if "y" in self.dbg_out:
            S.dma("sp", self.dbg_out["y"], self.Y, [Tl(None, t) for k in range(4) for t in self.Yt[k]], [Tl(None, Trk())])
        if "x" in self.dbg_out:
            S.dma("sp", self.dbg_out["x"], self.X, [Tl(None, t) for t in self.Xt], [Tl(None, Trk())])

    def load_w(self, es, name, l, c0, ncols):
        w = self.sb(es, name, [128, 8, ncols], BF)
        self.S.dma("pool", w[:], self.i["w_in"][l, :, c0:c0 + ncols].rearrange("(kc p) n -> p kc n", p=128), [], [w])
        return w

    def load_ht(self, buf, ti):
        t0, N = TILES[ti]
        self.S.dma("sp", buf[:, :, 0:N], self.HT[:, :, t0:t0 + N].rearrange("k p n -> p k n"), [Tl(None, self.HTt[ti])], [buf])

    def proj(self, ps, w, j0, ht, N, width=128):
        for kc in range(8):
            self.mm(ps[0:width, 0:N], w[:, kc, j0:j0 + width], ht[:, kc, 0:N], kc == 0, kc == 7, [w, ht], [ps])

    def rstd_from(self, src_ap, scale, out_t, tmp_t, N, r):
        self.V("dve", "tensor_scalar", r, [tmp_t], out=tmp_t[:, 0:N], in0=src_ap, scalar1=scale, scalar2=EPS, op0=ALU.mult, op1=ALU.add)
        self.V("dve", "reciprocal", [tmp_t], [tmp_t], out=tmp_t[:, 0:N], in_=tmp_t[:, 0:N])
        self.act(out_t[:, 0:N], tmp_t[:, 0:N], AF.Sqrt, [tmp_t], [out_t])

    def load_params(self, l):
        S = self.S
        i = self.i
        P = self.P
        nc = True
        def ld(dst, src):
            S.dma("sp", dst, src, [], [P], allow_slow_non_contiguous=True)
        for j in range(4):
            sl = slice(j * 128, (j + 1) * 128)
            ld(P[:, j * 3:j * 3 + 3], i["sc_w"][l][:, sl].rearrange("k p -> p k"))
            ld(P[:, 12 + j * 31:12 + (j + 1) * 31], i["cv_dw_w"][l][:, sl].rearrange("k p -> p k"))
            ld(P[:, 136 + j:137 + j], i["cv_dw_b"][l:l + 1, sl].rearrange("o p -> p o"))
            ld(P[:, 140 + j:141 + j], i["cv_ln_g"][l:l + 1, sl].rearrange("o p -> p o"))
            ld(P[:, 144 + j:145 + j], i["cv_ln_b"][l:l + 1, sl].rearrange("o p -> p o"))
            ld(P[:, 148 + j:149 + j], i["hg_norm_g"][l:l + 1, sl].rearrange("o p -> p o"))
        ld(P[:, 152:153], i["da_norm_g"][l:l + 1, :].rearrange("o p -> p o"))
        S.dma("sp", P[:, 160:416], i["da_lambda"][l:l + 1, :].partition_broadcast(128), [], [P])
        S.dma("sp", P[:, 416:452], i["moe_b_r"][l:l + 1, :].partition_broadcast(128), [], [P])
        lam_init = 0.8 - 0.6 * math.exp(-0.3 * l)
        self.V("dve", "tensor_tensor", [P], [P], out=P[:, 460:524], in0=P[:, 160:224], in1=P[:, 224:288], op=ALU.mult)
        self.V("dve", "tensor_tensor", [P], [P], out=P[:, 524:588], in0=P[:, 288:352], in1=P[:, 352:416], op=ALU.mult)
        self.V("dve", "tensor_reduce", [P], [P], out=P[:, 155:157], in_=P[:, 460:588].rearrange("p (a b) -> p a b", a=2), axis=AX.X, op=ALU.add)
        self.act(P[:, 157:159], P[:, 155:157], AF.Exp, [P], [P])
        self.V("dve", "tensor_tensor", [P], [P], out=P[:, 159:160], in0=P[:, 158:159], in1=P[:, 157:158], op=ALU.subtract)
        self.V("dve", "tensor_scalar", [P], [P], out=P[:, 153:154], in0=P[:, 159:160], scalar1=-lam_init, scalar2=1.0, op0=ALU.add, op1=ALU.mult)
        self.V("dve", "tensor_scalar", [P], [P], out=P[:, 154:155], in0=P[:, 152:153], scalar1=1.0 - lam_init, scalar2=0.0, op0=ALU.mult, op1=ALU.add)

    def phase_mod(self, l):
        S = self.S
        i = self.i
        modt = Tl(None, self.MODt)
        with contextlib.ExitStack() as es:
            wt = [self.sb(es, "adaw%d" % k, [128, 8, 512], F32) for k in range(2)]
            bt = [self.sb(es, "adab%d" % k, [1, 512], F32) for k in range(2)]
            ot = [self.sb(es, "adao%d" % k, [128, 512], F32) for k in range(2)]
            n = 0
            for blk in range(12):
                w = wt[blk % 2]
                b = bt[blk % 2]
                S.dma("sp", w[:], i["ada_w"][l, :, blk * 512:(blk + 1) * 512].rearrange("(kc p) n -> p kc n", p=128), [], [w])
                S.dma("sp", b[:], i["ada_b"][l:l + 1, blk * 512:(blk + 1) * 512], [], [b])
                for which in range(2):
                    ps = self.psb()
                    for kc in range(8):
                        self.mm(ps[:], self.sT[which][:, kc, :], w[:, kc, :], kc == 0, False, [self.sT[which], w], [ps])
                    self.mm(ps[:], self.cst[0:1, 128:256], b[0:1, :], False, True, [self.cst, b], [ps])
                    o = ot[n % 2]
                    n += 1
                    self.V("dve", "tensor_copy", [ps], [o], out=o[:], in_=ps[:])
                    S.dma("sp", self.MODR[which, :, blk * 512:(blk + 1) * 512], o[:], [o], [modt])
            S.barrier()

    def phase_norm(self, l, which):
        S = self.S
        i = self.i
        gsrc = i["norm1_g"] if which == 1 else i["norm2_g"]
        sh, sc = (0, 1) if which == 1 else (3, 4)
        modt = Tl(None, self.MODt)
        with contextlib.ExitStack() as es:
            A = [self.sb(es, "nA%d" % k, [128, D], F32) for k in range(2)]
            B = [self.sb(es, "nB%d" % k, [128, D], F32) for k in range(2)]
            g = self.sb(es, "ng", [128, D], F32)
            S.dma("sp", g[:], gsrc[l:l + 1, :].partition_broadcast(128), [], [g])
            for k in range(2):
                S.dma("sp", A[k][:], self.MODR[k, :, sc * D:(sc + 1) * D], [modt], [A[k]])
                S.dma("sp", B[k][:], self.MODR[k, :, sh * D:(sh + 1) * D], [modt], [B[k]])
                self.V("dve", "scalar_tensor_tensor", [A[k], g], [A[k]], out=A[k][:], in0=A[k][:], scalar=1.0, in1=g[:],
                       op0=ALU.add, op1=ALU.mult)
            xs = [self.sb(es, "nx%d" % k, [128, D], F32) for k in range(3)]
            sq = self.sb(es, "nsq", [128, D], F32)
            t1 = [self.sb(es, "nt%d" % k, [128, D], F32) for k in range(2)]
            hb = [self.sb(es, "nhb%d" % k, [128, D], BF) for k in range(2)]
            st = [self.sb(es, "nst%d" % k, [128, 4], F32) for k in range(2)]
            hT = [self.sb(es, "nhT%d" % k, [128, 8, 512], BF) for k in range(2)]
            if which == 2:
                wr = self.sb(es, "nwr", [128, 8, 36], F32)
                S.dma("sp", wr[:], i["moe_w_r"][l].rearrange("(kc p) n -> p kc n", p=128), [], [wr])
                h32 = [self.sb(es, "nh32%d" % k, [128, D], F32) for k in range(2)]
                h32T = [self.sb(es, "nh32T%d" % k, [128, 8, 128], F32) for k in range(2)]
                Rt = [self.sb(es, "nR%d" % k, [128, 160], F32) for k in range(2)]
            for ti, (t0, N) in enumerate(TILES):
                ht = hT[ti % 2]
                for cc in range(N // 128):
                    c = t0 // 128 + cc
                    lat = 0 if c >= 2 else 1
                    x = xs[c % 3]
                    xt = Tl(None, self.Xt[c])
                    S.dma("sp", x[:], self.X[c * 128:(c + 1) * 128, :], [xt], [x])
                    s_ = st[c % 2]
                    self.act(sq[:], x[:], AF.Square, [x], [sq, s_], accum_out=s_[:, 0:1])
                    self.V("dve", "tensor_scalar", [s_], [s_], out=s_[:, 1:2], in0=s_[:, 0:1], scalar1=1.0 / D, scalar2=EPS,
                           op0=ALU.mult, op1=ALU.add)
                    self.V("dve", "reciprocal", [s_], [s_], out=s_[:, 2:3], in_=s_[:, 1:2])
                    self.act(s_[:, 3:4], s_[:, 2:3], AF.Sqrt, [s_], [s_])
                    t = t1[c % 2]
                    self.V("dve", "scalar_tensor_tensor", [x, s_, A[lat]], [t], out=t[:], in0=x[:], scalar=s_[:, 3:4],
                           in1=A[lat][:], op0=ALU.mult, op1=ALU.mult)
                    h = hb[c % 2]
                    self.V("pool", "tensor_tensor", [t, B[lat]], [h], out=h[:], in0=t[:], in1=B[lat][:], op=ALU.add)
                    ps = self.psb()
                    pv = ps[:].bitcast(BF).rearrange("p (k n) -> p k n", k=8)
                    for k in range(8):
                        self.tr(pv[:, k, :], h[:, k * 128:(k + 1) * 128], self.identb[:], [h, self.identb], [ps])
                    self.act(ht[:, :, cc * 128:(cc + 1) * 128], pv, AF.Copy, [ps], [ht])
                    if which == 2:
                        self.route(c, t, B[lat], h32[c % 2], h32T[c % 2], wr, Rt[c % 2])
                        if SPARSE:
                            S.dma("sp", self.H2[c * 128:(c + 1) * 128, :], h[:], [h], [Tl(None, self.H2t)])
                            self.rank(c, Rt[c % 2])
                S.dma("sp", self.HT[:, :, t0:t0 + N].rearrange("k p n -> p k n"), ht[:, :, 0:N], [ht], [Tl(None, self.HTt[ti])])
            S.barrier()
        if "h1" in self.dbg_out and l == self.dbg.get("layer", 0) and which == 1:
            S.dma("sp", self.dbg_out["h1"], self.HT, [Tl(None, t) for t in self.HTt], [Tl(None, Trk())])


    def phase_sconv(self, l):
        S = self.S
        P = self.P
        with contextlib.ExitStack() as es:
            wb = self.load_w(es, "scwb", l, C_SB, 512)
            wc = self.load_w(es, "scwc", l, C_SC, 512)
            wx = self.load_w(es, "scwx", l, C_SX, 512)
            ub = self.sb(es, "scu", [128, T + 3], F32)
            bb = self.sb(es, "scb", [128, T], F32)
            hts = [self.sb(es, "scht%d" % k, [128, 8, 512], BF) for k in range(2)]
            cs = [self.sb(es, "sccs%d" % k, [128, 512], F32) for k in range(2)]
            acc = [self.sb(es, "scacc%d" % k, [128, 512], F32) for k in range(2)]
            ys = [self.sb(es, "scy%d" % k, [128, 512], BF) for k in range(2)]
            self.V("pool", "memset", [], [ub], ub[:], 0.0)
            n = 0
            for j in range(4):
                for ti, (t0, N) in enumerate(TILES):
                    ht = hts[n % 2]
                    c_ = cs[n % 2]
                    n += 1
                    self.load_ht(ht, ti)
                    base = t0 + 1 if ti == 0 else t0 + 2
                    pb, pc, px = self.psb(), self.psb(), self.psb()
                    self.proj(pb, wb, j * 128, ht, N)
                    self.proj(pc, wc, j * 128, ht, N)
                    self.proj(px, wx, j * 128, ht, N)
                    self.act(c_[:, 0:N], pc[:, 0:N], AF.Copy, [pc], [c_])
                    self.V("dve", "tensor_tensor", [c_, px], [ub], out=ub[:, base:base + N], in0=c_[:, 0:N], in1=px[:, 0:N], op=ALU.mult)
                    self.act(bb[:, t0:t0 + N], pb[:, 0:N], AF.Copy, [pb], [bb])
                for ti, (t0, N) in enumerate(TILES):
                    base = t0 + 1 if ti == 0 else t0 + 2
                    a = acc[ti % 2]
                    y = ys[ti % 2]
                    self.V("dve", "tensor_scalar", [ub, P], [a], out=a[:, 0:N], in0=ub[:, base - 1:base - 1 + N], scalar1=P[:, j * 3:j * 3 + 1],
                           scalar2=0.0, op0=ALU.mult, op1=ALU.add)
                    for k in (1, 2):
                        self.V("dve", "scalar_tensor_tensor", [ub, P, a], [a], out=a[:, 0:N], in0=ub[:, base - 1 + k:base - 1 + k + N],
                               scalar=P[:, j * 3 + k:j * 3 + k + 1], in1=a[:, 0:N], op0=ALU.mult, op1=ALU.add)
                    self.V("pool", "tensor_tensor", [a, bb], [y], out=y[:, 0:N], in0=a[:, 0:N], in1=bb[:, t0:t0 + N], op=ALU.mult)
                    S.dma("sp", self.Y[3, j, :, t0:t0 + N], y[:, 0:N], [y], [Tl(None, self.Yt[3][ti])])
            S.barrier()

    def phase_conv(self, l):
        S = self.S
        P = self.P
        onesf = self.cst[:, 128:256]
        with contextlib.ExitStack() as es:
            wa = self.load_w(es, "cvwa", l, C_CVA, 512)
            wg = self.load_w(es, "cvwg", l, C_CVG, 512)
            vb = self.sb(es, "cvv", [128, 4, T + 45], BF)
            hts = [self.sb(es, "cvht%d" % k, [128, 8, 512], BF) for k in range(2)]
            sg = [self.sb(es, "cvsg%d" % k, [128, 512], F32) for k in range(2)]
            self.V("pool", "memset", [], [vb], vb[:], 0.0)
            n = 0
            for ti, (t0, N) in enumerate(TILES):
                ht = hts[ti % 2]
                self.load_ht(ht, ti)
                base = t0 + 15 if ti == 0 else t0 + 30
                for j in range(4):
                    pa, pg = self.psb(), self.psb()
                    self.proj(pa, wa, j * 128, ht, N)
                    self.proj(pg, wg, j * 128, ht, N)
                    s_ = sg[n % 2]
                    n += 1
                    self.act(s_[:, 0:N], pg[:, 0:N], AF.Sigmoid, [pg], [s_])
                    self.V("dve", "tensor_tensor", [s_, pa], [vb], out=vb[:, j, base:base + N], in0=pa[:, 0:N], in1=s_[:, 0:N], op=ALU.mult)
            DG = self.sb(es, "cvDG", [128, 124, 128], BF)
            for jk in range(124):
                self.V("pool", "tensor_scalar", [self.identb, P], [DG], out=DG[:, jk, :], in0=self.identb[:], scalar1=P[:, 12 + jk:13 + jk], scalar2=0.0,
                       op0=ALU.mult, op1=ALU.add)
            ca = [self.sb(es, "cvca%d" % k, [128, 4, 512], F32) for k in range(2)]
            cp = self.sb(es, "cvcp", [128, 512], F32)
            sq = self.sb(es, "cvsq", [128, 4, 512], F32)
            mean = self.sb(es, "cvmean", [128, 512], F32)
            tmp = self.sb(es, "cvtmp", [128, 512], F32)
            rstd = self.sb(es, "cvrstd", [128, 512], F32)
            dd = [self.sb(es, "cvd%d" % k, [128, 512], F32) for k in range(2)]
            ys = [self.sb(es, "cvy%d" % k, [128, 4, 512], BF) for k in range(2)]
            for ti, (t0, N) in enumerate(TILES):
                base = t0 + 15 if ti == 0 else t0 + 30
                a = ca[ti % 2]
                for j in range(4):
                    pc = self.psb()
                    for k in range(31):
                        self.mm(pc[:, 0:N], DG[:, j * 31 + k, :], vb[:, j, base - 15 + k:base - 15 + k + N], k == 0, k == 30, [DG, vb], [pc])
                    self.act(a[:, j, 0:N], pc[:, 0:N], AF.Identity, [pc, P], [a], bias=P[:, 136 + j:137 + j])
                    self.act(sq[:, j, 0:N], a[:, j, 0:N], AF.Square, [a], [sq])
                p1, p2 = self.psb(), self.psb()
                for j in range(4):
                    self.mm(p1[:, 0:N], onesf, a[:, j, 0:N], j == 0, j == 3, [self.cst, a], [p1])
                for j in range(4):
                    self.mm(p2[:, 0:N], onesf, sq[:, j, 0:N], j == 0, j == 3, [self.cst, sq], [p2])
                self.act(mean[:, 0:N], p1[:, 0:N], AF.Copy, [p1], [mean], scale=1.0 / W)
                self.V("pool", "tensor_tensor", [mean], [tmp], out=tmp[:, 0:N], in0=mean[:, 0:N], in1=mean[:, 0:N], op=ALU.mult)
                self.V("dve", "scalar_tensor_tensor", [p2, tmp], [tmp], out=tmp[:, 0:N], in0=p2[:, 0:N], scalar=1.0 / W, in1=tmp[:, 0:N],
                       op0=ALU.mult, op1=ALU.subtract)
                self.rstd_from(tmp[:, 0:N], 1.0, rstd, tmp, N, [tmp])
                y = ys[ti % 2]
                for j in range(4):
                    d = dd[j % 2]
                    self.V("dve", "tensor_tensor", [a, mean], [d], out=d[:, 0:N], in0=a[:, j, 0:N], in1=mean[:, 0:N], op=ALU.subtract)
                    self.V("pool", "tensor_tensor", [d, rstd], [d], out=d[:, 0:N], in0=d[:, 0:N], in1=rstd[:, 0:N], op=ALU.mult)
                    self.V("dve", "tensor_scalar", [d, P], [d], out=d[:, 0:N], in0=d[:, 0:N], scalar1=P[:, 140 + j:141 + j], scalar2=P[:, 144 + j:145 + j],
                           op0=ALU.mult, op1=ALU.add)
                    self.act(y[:, j, 0:N], d[:, 0:N], AF.Silu, [d], [y])
                S.dma("sp", self.Y[2, :, :, t0:t0 + N].rearrange("j p n -> p j n"), y[:, :, 0:N], [y], [Tl(None, self.Yt[2][ti])])
            S.barrier()

    def rope(self, ps, raw, cos, sin, t1, t2, dst_ap, dst_t, N, rot_ps):
        self.act(raw[:, 0:N], ps[:, 0:N], AF.Copy, [ps], [raw])
        self.mm(rot_ps[:, 0:N], self.Rb[:], raw[:, 0:N], True, True, [self.Rb, raw], [rot_ps])
        self.V("pool", "tensor_tensor", [raw, cos], [t1], out=t1[:, 0:N], in0=raw[:, 0:N], in1=cos[:, 0:N], op=ALU.mult)
        self.V("dve", "tensor_tensor", [rot_ps, sin], [t2], out=t2[:, 0:N], in0=rot_ps[:, 0:N], in1=sin[:, 0:N], op=ALU.mult)
        self.V("pool", "tensor_tensor", [t1, t2], [dst_t], out=dst_ap, in0=t1[:, 0:N], in1=t2[:, 0:N], op=ALU.add)

    def phase_attn(self, l):
        S = self.S
        P = self.P
        i = self.i
        onesf = self.cst[:, 128:256]
        B = self.banks
        with contextlib.ExitStack() as es:
            wq = self.load_w(es, "dawq", l, C_DQ, 512)
            wk = self.load_w(es, "dawk", l, C_DK, 512)
            wv = self.load_w(es, "dawv", l, C_DV, 512)
            KT = self.sb(es, "daKT", [128, 4, T], BF)
            Vt = self.sb(es, "daV", [128, NCH, 512], BF)
            hts = [self.sb(es, "daht%d" % k, [128, 8, 512], BF) for k in range(2)]
            cos = [self.sb(es, "dacos%d" % k, [128, 512], F32) for k in range(2)]
            sin = [self.sb(es, "dasin%d" % k, [128, 512], F32) for k in range(2)]
            raw = [self.sb(es, "daraw%d" % k, [128, 512], BF) for k in range(2)]
            t1 = [self.sb(es, "dat1%d" % k, [128, 512], F32) for k in range(2)]
            t2 = [self.sb(es, "dat2%d" % k, [128, 512], F32) for k in range(2)]
            n = 0
            for ti, (t0, N) in enumerate(TILES):
                ht = hts[ti % 2]
                self.load_ht(ht, ti)
                S.dma("sp", cos[ti % 2][:, 0:N], i["rope"][0, :, t0:t0 + N], [], [cos[ti % 2]])
                S.dma("sp", sin[ti % 2][:, 0:N], i["rope"][1, :, t0:t0 + N], [], [sin[ti % 2]])
                for h in range(4):
                    ps, rp = self.psb(), self.psb()
                    self.proj(ps, wk, h * 128, ht, N)
                    self.rope(ps, raw[n % 2], cos[ti % 2], sin[ti % 2], t1[n % 2], t2[n % 2], KT[:, h, t0:t0 + N], KT, N, rp)
                    n += 1
                for cc in range(N // 128):
                    ps = self.psb()
                    for kc in range(8):
                        self.mm(ps[:, :], ht[:, kc, cc * 128:(cc + 1) * 128], wv[:, kc, :], kc == 0, kc == 7, [ht, wv], [ps])
                    self.act(Vt[:, t0 // 128 + cc, :], ps[:, :], AF.Copy, [ps], [Vt])
            QT = [self.sb(es, "daQT%d" % k, [128, 512], BF) for k in range(2)]
            QZ = [[self.sb(es, "daQZ%d_%d" % (k, c), [128, 512], BF) for c in range(2)] for k in range(2)]
            for k in range(2):
                for c in range(2):
                    self.V("pool", "memset", [], [QZ[k][c]], QZ[k][c][:], 0.0)
            pT = [self.sb(es, "dapT%d" % k, [128, 512], BF) for k in range(4)]
            rd = [self.sb(es, "dard%d" % k, [128, 512], F32) for k in range(2)]
            rp_ = [self.sb(es, "darp%d" % k, [128, 512], F32) for k in range(2)]
            rinv = self.sb(es, "darinv", [128, 512], F32)
            Oc = [self.sb(es, "daOc%d" % k, [128, 512], F32) for k in range(2)]
            o = self.sb(es, "dao", [128, 512], F32)
            sq = self.sb(es, "dasq", [128, 512], F32)
            tmp = self.sb(es, "datmp", [128, 512], F32)
            rstd = self.sb(es, "darstd", [128, 512], F32)
            ys = [self.sb(es, "day%d" % k, [128, 4, 512], BF) for k in range(2)]
            it = 0
            for ti, (t0, N) in enumerate(TILES):
                ht = hts[ti % 2]
                self.load_ht(ht, ti)
                S.dma("sp", cos[ti % 2][:, 0:N], i["rope"][0, :, t0:t0 + N], [], [cos[ti % 2]])
                S.dma("sp", sin[ti % 2][:, 0:N], i["rope"][1, :, t0:t0 + N], [], [sin[ti % 2]])
                nk = 2 if ti == 0 else NCH
                y = ys[ti % 2]
                for h in range(4):
                    qt = QT[h % 2]
                    self.proj(B[7], wq, h * 128, ht, N)
                    self.rope(B[7], raw[n % 2], cos[ti % 2], sin[ti % 2], t1[n % 2], t2[n % 2], qt[:, 0:N], qt, N, B[2])
                    n += 1
                    qz = QZ[h % 2]
                    self.V("pool", "tensor_copy", [qt], [qz[0]], out=qz[0][0:64, 0:N], in_=qt[0:64, 0:N])
                    self.V("dve", "tensor_copy", [qt], [qz[1]], out=qz[1][64:128, 0:N], in_=qt[64:128, 0:N])
                    its = [(c, kc) for c in range(2) for kc in range(nk)]
                    slots = {}
                    def qk(j):
                        c, kc = its[j]
                        sps = B[2 + it_base[0] % 3]
                        p = pT[it_base[0] % 4]
                        it_base[0] += 1
                        slots[j] = (sps, p)
                        p0 = c * 64
                        self.mm(sps[:, 0:N], KT[:, h, kc * 128:(kc + 1) * 128], qz[c][:, 0:N], True, True, [KT, qz[c]], [sps])
                    it_base = [it]
                    qk(0)
                    if len(its) > 1:
                        qk(1)
                    for j, (c, kc) in enumerate(its):
                        sps, p = slots.pop(j)
                        oacc = B[c]
                        self.act(p[:, 0:N], sps[:, 0:N], AF.Exp, [sps], [p], scale=0.125)
                        if j + 2 < len(its):
                            qk(j + 2)
                        self.mm(oacc[:, 0:N], Vt[:, kc, h * 128:(h + 1) * 128], p[:, 0:N], kc == 0, kc == nk - 1, [Vt, p], [oacc])
                        rsb = B[5 + c]
                        self.mm(rsb[:, 0:N], self.onesb[:], p[:, 0:N], kc == 0, kc == nk - 1, [self.onesb, p], [rsb])
                        if kc == nk - 1:
                            self.V("dve", "reciprocal", [rsb], [rinv], out=rinv[:, 0:N], in_=rsb[:, 0:N])
                            self.V("dve", "tensor_tensor", [oacc, rinv], [Oc[c]], out=Oc[c][:, 0:N], in0=oacc[:, 0:N], in1=rinv[:, 0:N], op=ALU.mult)
                    it = it_base[0]
                    self.V("dve", "scalar_tensor_tensor", [Oc[0], Oc[1], P], [o], out=o[:, 0:N], in0=Oc[1][:, 0:N], scalar=P[:, 153:154], in1=Oc[0][:, 0:N],
                           op0=ALU.mult, op1=ALU.add)
                    self.act(sq[:, 0:N], o[:, 0:N], AF.Square, [o], [sq])
                    self.mm(B[7][:, 0:N], onesf, sq[:, 0:N], True, True, [self.cst, sq], [B[7]])
                    self.rstd_from(B[7][:, 0:N], 1.0 / 128, rstd, tmp, N, [B[7]])
                    self.V("dve", "scalar_tensor_tensor", [o, rstd, P], [y], out=y[:, h, 0:N], in0=o[:, 0:N], scalar=P[:, 154:155], in1=rstd[:, 0:N],
                           op0=ALU.mult, op1=ALU.mult)
                S.dma("sp", self.Y[1, :, :, t0:t0 + N].rearrange("j p n -> p j n"), y[:, :, 0:N], [y], [Tl(None, self.Yt[1][ti])])
            S.barrier()

    def phase_hgrn(self, l):
        S = self.S
        B = self.banks
        with contextlib.ExitStack() as es:
            rot = [2]
            def rb():
                b = B[rot[0]]
                rot[0] = 2 + (rot[0] - 1) % 6
                return b
            sh = {k: self.sb(es, "hgs_" + k, [128, 512], F32) for k in ("osum", "sq", "tmp", "rstd", "sgg", "y1")}
            sh["ys"] = [self.sb(es, "hgys%d" % k, [128, 512], BF) for k in range(2)]
            sh["n"] = 0
            chains = []
            for k in range(2):
                c = {"k": k}
                c["OF"] = self.sb(es, "hgOF%d" % k, [128, T], F32)
                c["S32"] = self.sb(es, "hgS%d" % k, [128, 128], F32)
                c["Sbf"] = [self.sb(es, "hgSb%d_%d" % (k, j), [128, 128], BF) for j in range(2)]
                c["ht"] = self.sb(es, "hght%d" % k, [128, 8, 512], BF)
                c["w"] = [self.sb(es, "hgw%d_%d" % (k, j), [128, 8, 128], BF) for j in range(4)]
                for nm in ("q32", "ee", "ff", "lf", "kk", "pre", "bb", "d3", "d2", "d3a"):
                    c[nm] = self.sb(es, "hg%s%d" % (nm, k), [128, 512], F32)
                for nm in ("qE1", "qE3", "kE4", "kE2", "qE3b", "kE4b"):
                    c[nm] = self.sb(es, "hg%s%d" % (nm, k), [128, 512], BF)
                c["iT"] = self.sb(es, "hgiT%d" % k, [128, 4, 128], BF)
                c["kT"] = self.sb(es, "hgkT%d" % k, [128, 4, 128], BF)
                c["AM"] = [self.sb(es, "hgAM%d_%d" % (k, j), [128, 128], BF) for j in range(2)]
                c["a1"] = [self.sb(es, "hga1%d_%d" % (k, j), [128, 128], F32) for j in range(2)]
                c["a2"] = [self.sb(es, "hga2%d_%d" % (k, j), [128, 128], F32) for j in range(2)]
                c["ops"] = B[k]
                chains.append(c)
            for pair in ((0, 1), (2, 3)):
                for d in range(2):
                    gens = [self.hgrn_chain(l, h, d, chains[k], sh, rb) for k, h in enumerate(pair)]
                    live = list(gens)
                    while live:
                        for g in list(live):
                            try:
                                next(g)
                            except StopIteration:
                                live.remove(g)
            S.barrier()

    def hgrn_chain(self, l, h, d, c, sh, rb):
        S = self.S
        P = self.P
        onesf = self.cst[:, 128:256]
        w = c["w"]
        OF, S32, Sbf, ht, ops = c["OF"], c["S32"], c["Sbf"], c["ht"], c["ops"]
        q32, ee, ff, lf, kk, pre, bb, d3, d2, d3a = (c[n] for n in ("q32", "ee", "ff", "lf", "kk", "pre", "bb", "d3", "d2", "d3a"))
        qE1, qE3, kE4, kE2, qE3b, kE4b = (c[n] for n in ("qE1", "qE3", "kE4", "kE2", "qE3b", "kE4b"))
        iT, kT = c["iT"], c["kT"]
        E1, E3, E3b, E4b, E2, E4 = ee, ff, lf, d3, d2, d3a
        cols = (C_HQ, C_HFF if d == 0 else C_HFB, C_HI, C_HG)
        for k in range(4):
            S.dma("pool", w[k][:], self.i["w_in"][l, :, cols[k] + h * 128:cols[k] + (h + 1) * 128].rearrange("(kc p) n -> p kc n", p=128), [], [w[k]])
        self.V("pool", "memset", [], [S32], S32[:], 0.0)
        self.V("pool", "memset", [], [Sbf[0]], Sbf[0][:], 0.0)
        cur = 0
        am_i = 0
        lbc = self.LB[:, l, d * 4 + h:d * 4 + h + 1]
        omc = self.OML[:, l, d * 4 + h:d * 4 + h + 1]
        order = list(range(9)) if d == 0 else [0] + list(range(8, 0, -1))
        mask = self.cst[:, 384:512] if d == 0 else self.cst[:, 512:640]
        masko = self.cst[:, 640:768] if d == 0 else self.cst[:, 768:896]
        for ti in order:
            t0, N = TILES[ti]
            nb = N // 128
            nch = N // 64
            self.load_ht(ht, ti)
            pq, pf = rb(), rb()
            self.proj(pq, w[0], 0, ht, N)
            self.proj(pf, w[1], 0, ht, N)
            self.act(q32[:, 0:N], pq[:, 0:N], AF.Copy, [pq], [q32])
            self.act(ee[:, 0:N], pf[:, 0:N], AF.Exp, [pf], [ee], scale=-1.0)
            self.V("dve", "tensor_scalar", [ee], [ee], out=ee[:, 0:N], in0=ee[:, 0:N], scalar1=1.0, scalar2=1.0, op0=ALU.add, op1=ALU.mult)
            self.V("dve", "reciprocal", [ee], [ee], out=ee[:, 0:N], in_=ee[:, 0:N])
            self.V("dve", "tensor_scalar", [ee, self.LB, self.OML], [ff], out=ff[:, 0:N], in0=ee[:, 0:N], scalar1=omc, scalar2=lbc, op0=ALU.mult, op1=ALU.add)
            self.act(lf[:, 0:N], ff[:, 0:N], AF.Ln, [ff], [lf])
            self.V("pool", "tensor_scalar", [ff], [kk], out=kk[:, 0:N], in0=ff[:, 0:N], scalar1=-1.0, scalar2=1.0, op0=ALU.mult, op1=ALU.add)
            self.V("dve", "tensor_tensor_scan", [self.seg, lf], [pre], out=pre[:, 0:N], data0=self.seg[:, 0:N], data1=lf[:, 0:N], initial=0.0, op0=ALU.mult, op1=ALU.add)
            v3 = lambda t_: t_[:, 0:N].rearrange("p (c s) -> p c s", s=64)
            v32 = lambda t_: t_[:, 0:N].rearrange("p (c s) -> p c s", s=32)
            bc = lambda t_, col: v3(t_)[:, :, col:col + 1].to_broadcast([128, nch, 64])
            if d == 0:
                b_ = pre
                cend = 63
            else:
                b_ = bb
                cend = 0
                self.V("dve", "tensor_tensor", [lf, pre], [bb], out=bb[:, 0:N], in0=lf[:, 0:N], in1=pre[:, 0:N], op=ALU.subtract)
                self.V("dve", "tensor_tensor", [bb, pre], [bb], out=v3(bb), in0=v3(bb), in1=bc(pre, 63), op=ALU.add)
            yield
            self.V("dve", "tensor_tensor", [b_], [d3], out=v3(d3), in0=v3(b_), in1=bc(b_, 32), op=ALU.subtract)
            self.V("pool", "tensor_tensor", [b_], [d2], out=v3(d2), in0=v3(b_), in1=bc(b_, cend), op=ALU.subtract)
            self.V("dve", "tensor_tensor", [b_], [d3a], out=v32(d3a), in0=v32(b_), in1=v32(b_)[:, :, 16:17].to_broadcast([128, 2 * nch, 32]), op=ALU.subtract)
            self.act(E1[:, 0:N], b_[:, 0:N], AF.Exp, [b_], [E1])
            self.act(E2[:, 0:N], d2[:, 0:N], AF.Exp, [d2], [E2], scale=-1.0)
            self.act(E3[:, 0:N], d3a[:, 0:N], AF.Exp, [d3a], [E3])
            self.act(E4[:, 0:N], d3a[:, 0:N], AF.Exp, [d3a], [E4], scale=-1.0)
            self.act(E3b[:, 0:N], d3[:, 0:N], AF.Exp, [d3], [E3b])
            self.act(E4b[:, 0:N], d3[:, 0:N], AF.Exp, [d3], [E4b], scale=-1.0)
            self.V("dve", "tensor_tensor", [q32, E1], [qE1], out=qE1[:, 0:N], in0=q32[:, 0:N], in1=E1[:, 0:N], op=ALU.mult)
            self.V("pool", "tensor_tensor", [q32, E3], [qE3], out=qE3[:, 0:N], in0=q32[:, 0:N], in1=E3[:, 0:N], op=ALU.mult)
            self.V("dve", "tensor_tensor", [kk, E4], [kE4], out=kE4[:, 0:N], in0=kk[:, 0:N], in1=E4[:, 0:N], op=ALU.mult)
            self.V("pool", "tensor_tensor", [kk, E2], [kE2], out=kE2[:, 0:N], in0=kk[:, 0:N], in1=E2[:, 0:N], op=ALU.mult)
            self.V("dve", "tensor_tensor", [q32, E3b], [qE3b], out=qE3b[:, 0:N], in0=q32[:, 0:N], in1=E3b[:, 0:N], op=ALU.mult)
            self.V("pool", "tensor_tensor", [kk, E4b], [kE4b], out=kE4b[:, 0:N], in0=kk[:, 0:N], in1=E4b[:, 0:N], op=ALU.mult)
            qz, kz = (slice(0, 32), slice(32, 64)) if d == 0 else (slice(32, 64), slice(0, 32))
            self.V("dve", "memset", [qE3b], [qE3b], v3(qE3b)[:, :, qz], 0.0)
            self.V("pool", "memset", [kE4b], [kE4b], v3(kE4b)[:, :, kz], 0.0)
            yield
            for cc in range(nb):
                pi = rb()
                for kc in range(8):
                    self.mm(pi[:, 0:128], ht[:, kc, cc * 128:(cc + 1) * 128], w[2][:, kc, :], kc == 0, kc == 7, [ht, w[2]], [pi])
                self.act(iT[:, cc, :], pi[:, 0:128], AF.Copy, [pi], [iT])
                pt = rb()
                ptv = pt[:].bitcast(BF)
                self.tr(ptv[:, 0:128], kE2[:, cc * 128:(cc + 1) * 128], self.identb[:], [kE2, self.identb], [pt])
                self.act(kT[:, cc, :], ptv[:, 0:128], AF.Copy, [pt], [kT])
            yield
            blks = list(range(nb)) if d == 0 else list(range(nb - 1, -1, -1))
            for blk in blks:
                pa = rb()
                bs = slice(blk * 128, (blk + 1) * 128)
                self.mm(pa[:, 0:128], kE4[:, bs], qE3[:, bs], True, True, [kE4, qE3], [pa])
                pb_ = rb()
                self.mm(pb_[:, 0:128], kE4b[:, bs], qE3b[:, bs], True, True, [kE4b, qE3b], [pb_])
                am = c["AM"][am_i % 2]
                a1 = c["a1"][am_i % 2]
                a2 = c["a2"][am_i % 2]
                am_i += 1
                self.V("dve", "tensor_tensor", [pa, self.cst], [a1], out=a1[:], in0=pa[:, 0:128], in1=mask, op=ALU.mult)
                self.V("dve", "tensor_tensor", [pb_, self.cst], [a2], out=a2[:], in0=pb_[:, 0:128], in1=masko, op=ALU.mult)
                self.V("pool", "tensor_tensor", [a1, a2], [am], out=am[:], in0=a1[:], in1=a2[:], op=ALU.add)
                for ch in ((0, 1) if d == 0 else (1, 0)):
                    p0 = ch * 64
                    c0 = blk * 128 + p0
                    self.mm(ops[:, c0:c0 + 64], Sbf[cur][:], qE1[:, c0:c0 + 64], True, False, [Sbf[cur], qE1], [ops])
                    self.mm(ops[:, c0:c0 + 64], iT[p0:p0 + 64, blk, :], am[p0:p0 + 64, p0:p0 + 64], False, True, [iT, am], [ops])
                    pS = rb()
                    self.mm(pS[:, 0:128], kT[p0:p0 + 64, blk, :], iT[p0:p0 + 64, blk, :], True, True, [kT, iT], [pS])
                    ce = c0 + cend
                    self.V("dve", "scalar_tensor_tensor", [S32, E1, pS], [S32], out=S32[:], in0=S32[:], scalar=E1[:, ce:ce + 1], in1=pS[:, 0:128],
                           op0=ALU.mult, op1=ALU.add)
                    cur = 1 - cur
                    self.act(Sbf[cur][:], S32[:], AF.Copy, [S32], [Sbf[cur]])
                    yield
            if d == 0:
                self.act(OF[:, t0:t0 + N], ops[:, 0:N], AF.Copy, [ops], [OF])
            else:
                osum, sq, tmp, rstd, sgg, y1 = (sh[n_] for n_ in ("osum", "sq", "tmp", "rstd", "sgg", "y1"))
                self.V("dve", "tensor_tensor", [OF, ops], [osum], out=osum[:, 0:N], in0=OF[:, t0:t0 + N], in1=ops[:, 0:N], op=ALU.add)
                self.act(sq[:, 0:N], osum[:, 0:N], AF.Square, [osum], [sq])
                pss = rb()
                self.mm(pss[:, 0:N], onesf, sq[:, 0:N], True, True, [self.cst, sq], [pss])
                self.rstd_from(pss[:, 0:N], 1.0 / 128, rstd, tmp, N, [pss])
                pg = rb()
                self.proj(pg, w[3], 0, ht, N)
                self.act(sgg[:, 0:N], pg[:, 0:N], AF.Sigmoid, [pg], [sgg])
                self.V("dve", "scalar_tensor_tensor", [osum, rstd, P], [y1], out=y1[:, 0:N], in0=osum[:, 0:N], scalar=P[:, 148 + h:149 + h], in1=rstd[:, 0:N],
                       op0=ALU.mult, op1=ALU.mult)
                y = sh["ys"][sh["n"] % 2]
                sh["n"] += 1
                self.V("pool", "tensor_tensor", [y1, sgg], [y], out=y[:, 0:N], in0=y1[:, 0:N], in1=sgg[:, 0:N], op=ALU.mult)
                S.dma("sp", self.Y[0, h, :, t0:t0 + N], y[:, 0:N], [y], [Tl(None, self.Yt[0][ti])])
            yield

    def phase_merge(self, l):
        S = self.S
        i = self.i
        with contextlib.ExitStack() as es:
            wg = self.sb(es, "mgwg", [128, 8, 4096], BF)
            for k in range(4):
                S.dma("pool", wg[:, :, k * 1024:(k + 1) * 1024], i["w_in"][l, :, C_GATE + k * 1024:C_GATE + (k + 1) * 1024].rearrange("(kc p) n -> p kc n", p=128), [], [wg])
            wb = self.sb(es, "mgwb", [128, 4, 4, 1024], BF)
            for k in range(4):
                S.dma("pool", wb[:, k, :, :], i["w_branch"][l, k].rearrange("(cc p) n -> p cc n", p=128), [], [wb])
            ht = self.sb(es, "mght", [128, 8, 512], BF)
            Yk = [self.sb(es, "mgY%d" % k, [128, 4, 512], BF) for k in range(4)]
            sg = [self.sb(es, "mgsg%d" % k, [128, 512], F32) for k in range(2)]
            tmp = [self.sb(es, "mgtmp%d" % k, [128, 512], F32) for k in range(2)]
            macc = [self.sb(es, "mgacc%d" % k, [128, 512], F32) for k in range(2)]
            mT = [self.sb(es, "mgmT%d" % k, [128, 8, 512], BF) for k in range(2)]
            n = 0
            for ti, (t0, N) in enumerate(TILES):
                self.load_ht(ht, ti)
                for k in range(4):
                    S.dma("sp", Yk[k][:, :, 0:N], self.Y[k, :, :, t0:t0 + N].rearrange("j p n -> p j n"), [Tl(None, self.Yt[k][ti])], [Yk[k]])
                m = mT[ti % 2]
                for nch in range(8):
                    ma = macc[nch % 2]
                    for k in range(4):
                        pg, pp = self.psb(), self.psb()
                        self.proj(pg, wg, k * 1024 + nch * 128, ht, N)
                        for cc in range(4):
                            self.mm(pp[:, 0:N], wb[:, k, cc, nch * 128:(nch + 1) * 128], Yk[k][:, cc, 0:N], cc == 0, cc == 3, [wb, Yk[k]], [pp])
                        s_ = sg[n % 2]
                        t_ = tmp[n % 2]
                        n += 1
                        self.act(s_[:, 0:N], pg[:, 0:N], AF.Sigmoid, [pg], [s_])
                        if k == 0:
                            self.V("dve", "tensor_tensor", [pp, s_], [ma], out=ma[:, 0:N], in0=pp[:, 0:N], in1=s_[:, 0:N], op=ALU.mult)
                        else:
                            self.V("dve", "tensor_tensor", [pp, s_], [t_], out=t_[:, 0:N], in0=pp[:, 0:N], in1=s_[:, 0:N], op=ALU.mult)
                            self.V("pool", "tensor_tensor", [ma, t_], [ma], out=ma[:, 0:N], in0=ma[:, 0:N], in1=t_[:, 0:N], op=ALU.add)
                    self.V("pool", "tensor_copy", [ma], [m], out=m[:, nch, 0:N], in_=ma[:, 0:N])
                S.dma("sp", self.MT[:, :, t0:t0 + N].rearrange("k p n -> p k n"), m[:, :, 0:N], [m], [Tl(None, self.MTt[ti])])
            S.barrier()
        with contextlib.ExitStack() as es:
            wo = self.sb(es, "mgwo", [128, 8, 1024], BF)
            S.dma("pool", wo[:], i["w_out"][l].rearrange("(kc p) n -> p kc n", p=128), [], [wo])
            mts = [self.sb(es, "mgmt%d" % k, [128, 8, 512], BF) for k in range(2)]
            self.residual_setup(es, 2)
            for ti, (t0, N) in enumerate(TILES):
                mt = mts[ti % 2]
                S.dma("sp", mt[:, :, 0:N], self.MT[:, :, t0:t0 + N].rearrange("k p n -> p k n"), [Tl(None, self.MTt[ti])], [mt])
                for cc in range(N // 128):
                    c = t0 // 128 + cc
                    halves = []
                    for half in range(2):
                        po = self.psb()
                        for kc in range(8):
                            self.mm(po[:, :], mt[:, kc, cc * 128:(cc + 1) * 128], wo[:, kc, half * 512:(half + 1) * 512], kc == 0, kc == 7, [mt, wo], [po])
                        halves.append(po)
                    self.residual(c, lambda half: halves[half][:, :], halves)
            S.barrier()

    def residual_setup(self, es, modidx):
        S = self.S
        self.rmod = [self.sb(es, "rsmod%d" % k, [128, D], F32) for k in range(2)]
        for k in range(2):
            S.dma("sp", self.rmod[k][:], self.MODR[k, :, modidx * D:(modidx + 1) * D], [Tl(None, self.MODt)], [self.rmod[k]])
        self.rx = [self.sb(es, "rsx%d" % k, [128, D], F32) for k in range(2)]
        self.rtmp = [self.sb(es, "rstmp%d" % k, [128, D], F32) for k in range(2)]

    def residual(self, c, delta_ap, delta_tiles):
        S = self.S
        lat = 0 if c >= 2 else 1
        x = self.rx[c % 2]
        t = self.rtmp[c % 2]
        xt = Tl(None, self.Xt[c])
        S.dma("sp", x[:], self.X[c * 128:(c + 1) * 128, :], [xt], [x])
        for half in range(2):
            hs = slice(half * 512, (half + 1) * 512)
            self.V("dve", "tensor_tensor", [delta_tiles[half], self.rmod[lat]], [t], out=t[:, hs], in0=delta_ap(half), in1=self.rmod[lat][:, hs], op=ALU.mult)
        self.V("dve", "tensor_tensor", [x, t], [x], out=x[:], in0=x[:], in1=t[:], op=ALU.add)
        S.dma("sp", self.X[c * 128:(c + 1) * 128, :], x[:], [x], [xt])

    def route(self, c, t, Bm, h32, h32T, wr, R):
        P = self.P
        self.V("pool", "tensor_tensor", [t, Bm], [h32], out=h32[:], in0=t[:], in1=Bm[:], op=ALU.add)
        for g in range(2):
            ps = self.psb()
            for k in range(4):
                kk = g * 4 + k
                self.tr(ps[:, k * 128:(k + 1) * 128], h32[:, kk * 128:(kk + 1) * 128], self.cst[:, 0:128], [h32, self.cst], [ps])
            self.act(h32T[:, g * 4:(g + 1) * 4, :], ps[:].rearrange("p (k n) -> p k n", k=4), AF.Copy, [ps], [h32T])
        pl = self.psb()
        for kc in range(8):
            self.mm(pl[:, 0:36], h32T[:, kc, :], wr[:, kc, :], kc == 0, kc == 7, [h32T, wr], [pl])
        dv = lambda name, w_, **kw: self.V("dve", name, [R, P] + w_[1:], [w_[0]], **kw)
        self.V("dve", "tensor_tensor", [pl, P], [R], out=R[:, 0:36], in0=pl[:, 0:36], in1=P[:, 416:452], op=ALU.add)
        RR = [R]
        dv("tensor_reduce", RR, out=R[:, 36:37], in_=R[:, 0:4], axis=AX.X, op=ALU.max)
        dv("tensor_scalar", RR, out=R[:, 37:38], in0=R[:, 36:37], scalar1=-1.0, scalar2=0.0, op0=ALU.mult, op1=ALU.add)
        self.act(R[:, 44:48], R[:, 0:4], AF.Exp, [R], [R], bias=R[:, 37:38], accum_out=R[:, 38:39])
        dv("reciprocal", RR, out=R[:, 39:40], in_=R[:, 38:39])
        dv("tensor_scalar", RR, out=R[:, 40:44], in0=R[:, 0:4], scalar1=R[:, 36:37], scalar2=1.0, op0=ALU.is_equal, op1=ALU.mult)
        dv("tensor_tensor", RR, out=R[:, 48:80].rearrange("p (g e) -> p g e", g=4), in0=R[:, 4:36].rearrange("p (g e) -> p g e", g=4),
           in1=R[:, 40:44].unsqueeze(2).to_broadcast([128, 4, 8]), op=ALU.mult)
        dv("tensor_reduce", RR, out=R[:, 80:88], in_=R[:, 48:80].rearrange("p (g e) -> p e g", g=4), axis=AX.X, op=ALU.add)
        dv("tensor_reduce", RR, out=R[:, 88:89], in_=R[:, 80:88], axis=AX.X, op=ALU.max)
        dv("tensor_scalar", RR, out=R[:, 89:97], in0=R[:, 80:88], scalar1=R[:, 88:89], scalar2=1.0, op0=ALU.is_equal, op1=ALU.mult)
        dv("scalar_tensor_tensor", RR, out=R[:, 97:105], in0=R[:, 89:97], scalar=-1e30, in1=R[:, 80:88], op0=ALU.mult, op1=ALU.add)
        dv("tensor_reduce", RR, out=R[:, 105:106], in_=R[:, 97:105], axis=AX.X, op=ALU.max)
        dv("tensor_scalar", RR, out=R[:, 106:114], in0=R[:, 97:105], scalar1=R[:, 105:106], scalar2=1.0, op0=ALU.is_equal, op1=ALU.mult)
        dv("tensor_tensor", RR, out=R[:, 114:115], in0=R[:, 105:106], in1=R[:, 88:89], op=ALU.subtract)
        self.act(R[:, 115:116], R[:, 114:115], AF.Exp, [R], [R])
        dv("tensor_scalar", RR, out=R[:, 116:117], in0=R[:, 115:116], scalar1=1.0, scalar2=1.0, op0=ALU.add, op1=ALU.mult)
        dv("reciprocal", RR, out=R[:, 116:117], in_=R[:, 116:117])
        dv("tensor_tensor", RR, out=R[:, 117:118], in0=R[:, 116:117], in1=R[:, 39:40], op=ALU.mult)
        dv("tensor_tensor", RR, out=R[:, 118:119], in0=R[:, 39:40], in1=R[:, 117:118], op=ALU.subtract)
        dv("tensor_scalar", RR, out=R[:, 119:127], in0=R[:, 89:97], scalar1=R[:, 117:118], scalar2=0.0, op0=ALU.mult, op1=ALU.add)
        dv("scalar_tensor_tensor", RR, out=R[:, 119:127], in0=R[:, 106:114], scalar=R[:, 118:119], in1=R[:, 119:127], op0=ALU.mult, op1=ALU.add)
        self.V("dve", "tensor_tensor", [R], [self.RW], out=self.RW[:, c, :].rearrange("p (g e) -> p g e", g=4),
               in0=R[:, 40:44].unsqueeze(2).to_broadcast([128, 4, 8]), in1=R[:, 119:127].unsqueeze(1).to_broadcast([128, 4, 8]), op=ALU.mult)

    def phase_moe(self, l):
        S = self.S
        i = self.i
        with contextlib.ExitStack() as es:
            acc = self.sb(es, "moacc", [128, 10, D], F32)
            hTb = self.sb(es, "mohT", [128, 8, 1280], BF)
            wts = [(self.sb(es, "mowg%d" % k, [128, 8, 512], BF), self.sb(es, "mowu%d" % k, [128, 8, 512], BF),
                    self.sb(es, "mowd%d" % k, [128, 4, D], BF)) for k in range(2)]
            sG = [self.sb(es, "mosg%d" % k, [128, 512], F32) for k in range(2)]
            Hh = [self.sb(es, "moHh%d" % k, [128, 4, 512], BF) for k in range(2)]
            self.residual_setup(es, 5)
            n = 0
            m = 0
            for blk in ((0, 1, 2), (3, 4), (5, 6), (7, 8)):
                col = 0
                tcs = []
                for ti in blk:
                    t0, N = TILES[ti]
                    S.dma("sp", hTb[:, :, col:col + N], self.HT[:, :, t0:t0 + N].rearrange("k p n -> p k n"), [Tl(None, self.HTt[ti])], [hTb])
                    tcs.append((col, N, t0))
                    col += N
                for e in range(self.nexp):
                    wg, wu, wd = wts[e % 2]
                    S.dma("pool", wg[:], i["moe_w_gate"][l, e].rearrange("(kc p) f -> p kc f", p=128), [], [wg])
                    S.dma("pool", wu[:], i["moe_w_up"][l, e].rearrange("(kc p) f -> p kc f", p=128), [], [wu])
                    S.dma("pool", wd[:], i["moe_w_down"][l, e].rearrange("(fc p) n -> p fc n", p=128), [], [wd])
                    for (col, N, t0) in tcs:
                        hh = Hh[m % 2]
                        m += 1
                        for fc in range(4):
                            pG, pU = self.psb(), self.psb()
                            for kc in range(8):
                                self.mm(pG[:, 0:N], wg[:, kc, fc * 128:(fc + 1) * 128], hTb[:, kc, col:col + N], kc == 0, kc == 7, [wg, hTb], [pG])
                            for kc in range(8):
                                self.mm(pU[:, 0:N], wu[:, kc, fc * 128:(fc + 1) * 128], hTb[:, kc, col:col + N], kc == 0, kc == 7, [wu, hTb], [pU])
                            sg = sG[n % 2]
                            n += 1
                            self.act(sg[:, 0:N], pG[:, 0:N], AF.Silu, [pG], [sg])
                            self.V("dve", "tensor_tensor", [sg, pU], [hh], out=hh[:, fc, 0:N], in0=sg[:, 0:N], in1=pU[:, 0:N], op=ALU.mult)
                        for cc in range(N // 128):
                            ci = col // 128 + cc
                            c = t0 // 128 + cc
                            for half in range(2):
                                hs = slice(half * 512, (half + 1) * 512)
                                pD = self.psb()
                                for fc in range(4):
                                    self.mm(pD[:, :], hh[:, fc, cc * 128:(cc + 1) * 128], wd[:, fc, hs], fc == 0, fc == 3, [hh, wd], [pD])
                                if e == 0:
                                    self.V("dve", "tensor_scalar", [pD, self.RW], [acc], out=acc[:, ci, hs], in0=pD[:, :], scalar1=self.RW[:, c, e:e + 1], scalar2=0.0,
                                           op0=ALU.mult, op1=ALU.add)
                                else:
                                    self.V("dve", "scalar_tensor_tensor", [pD, self.RW, acc], [acc], out=acc[:, ci, hs], in0=pD[:, :], scalar=self.RW[:, c, e:e + 1],
                                           in1=acc[:, ci, hs], op0=ALU.mult, op1=ALU.add)
                for (col, N, t0) in tcs:
                    for cc in range(N // 128):
                        ci = col // 128 + cc
                        c = t0 // 128 + cc
                        self.residual(c, lambda half, ci=ci: acc[:, ci, half * 512:(half + 1) * 512], [acc, acc])
            S.barrier()

    def rank(self, c, R):
        A = R[:, 128:160]
        self.V("dve", "tensor_scalar", [self.RW], [R], out=A, in0=self.RW[:, c, :], scalar1=0.0, scalar2=1.0, op0=ALU.is_gt, op1=ALU.mult)
        if c == 0:
            self.V("dve", "memset", [], [self.Asum], self.Asum[:], 0.0)
        ps = self.psb()
        self.mm(ps[:, 0:32], self.ltri[:], A, True, False, [self.ltri, R], [ps])
        self.mm(ps[:, 0:32], self.cst[:, 128:256], self.Asum[:], False, True, [self.cst, self.Asum], [ps])
        self.act(self.RK[:, c, :], ps[:, 0:32], AF.Copy, [ps], [self.RK])
        self.V("dve", "tensor_tensor", [self.Asum, R], [self.Asum], out=self.Asum[:], in0=self.Asum[:], in1=A, op=ALU.add)

    def phase_moe_sparse(self, l):
        S = self.S
        i = self.i
        onesf = self.cst[:, 128:256]
        rows_t = Tl(None, self.ROWSt)
        acc_t = Tl(None, self.ACC2t)
        h2_t = Tl(None, self.H2t)
        with contextlib.ExitStack() as es:
            G = self.sb(es, "spG", [128, 1024], F32)
            widx = self.sb(es, "spwidx", [128, 2, NB], U32)
            init = self.sb(es, "spinit", [128, 128, 4], F32)
            self.V("pool", "memset", [], [init], init[:], 0.0)
            self.V("pool", "memset", [init], [init], init[:, :, 0:1], float(T))
            self.V("pool", "memset", [init], [init], init[:, :, 2:4], 1.0e6)
            S.dma("sp", self.ROWS.rearrange("(j p) c -> j (p c)", p=128), init[0:NB, :, :].rearrange("j p c -> j (p c)"), [init], [rows_t])
            ps = self.psb()
            self.mm(ps[:, 0:32], onesf, self.Asum[:], True, True, [self.cst, self.Asum], [ps])
            cnt, pad, pend, pst = G[:, 0:32], G[:, 32:64], G[:, 64:96], G[:, 96:128]
            cmp = self.sb(es, "spcmp", [128, NB, 32], F32)
            self.V("dve", "tensor_copy", [ps], [G], out=cnt, in_=ps[:, 0:32])
            cmp2 = cmp[:].rearrange("p a b -> p (a b)")[:, 0:32 * 68].rearrange("p (e m) -> p e m", m=68)
            self.V("dve", "tensor_tensor", [G, self.cst], [cmp], out=cmp2, in0=cnt.unsqueeze(2).to_broadcast([128, 32, 68]),
                   in1=self.cst[:, 896:896 + 68].unsqueeze(1).to_broadcast([128, 32, 68]), op=ALU.is_gt)
            self.V("dve", "tensor_reduce", [cmp], [G], out=pad, in_=cmp2, axis=AX.X, op=ALU.add)
            self.V("dve", "tensor_scalar", [G], [G], out=pad, in0=pad, scalar1=128.0, scalar2=0.0, op0=ALU.mult, op1=ALU.add)
            self.V("dve", "tensor_tensor_scan", [G, self.cst], [G], out=pend, data0=onesf[:, 0:32], data1=pad, initial=0.0, op0=ALU.mult, op1=ALU.add)
            self.V("dve", "tensor_tensor", [G], [G], out=pst, in0=pend, in1=pad, op=ALU.subtract)
            self.V("dve", "tensor_tensor", [G, self.cst], [cmp], out=cmp[:], in0=pend.unsqueeze(1).to_broadcast([128, NB, 32]),
                   in1=self.cst[:, 896:896 + NB].unsqueeze(2).to_broadcast([128, NB, 32]), op=ALU.is_le)
            be = G[:, 128:128 + NB]
            self.V("dve", "tensor_reduce", [cmp], [G], out=be, in_=cmp[:], axis=AX.X, op=ALU.add)
            self.V("dve", "tensor_scalar", [G], [G], out=be, in0=be, scalar1=31.0, scalar2=128.0, op0=ALU.min, op1=ALU.mult)
            same = G[:, 384:384 + NB]
            self.V("dve", "memset", [G], [G], same, 0.0)
            self.V("dve", "tensor_tensor", [G], [G], out=G[:, 386:384 + NB], in0=G[:, 130:128 + NB], in1=G[:, 128:126 + NB], op=ALU.is_equal)
            wf = G[:, 256:256 + NB]
            self.V("dve", "tensor_scalar", [G, self.cst], [G], out=wf, in0=be, scalar1=self.cst[:, 1024:1025], scalar2=2.0, op0=ALU.add, op1=ALU.mult)
            self.V("dve", "tensor_scalar", [G], [G], out=wf, in0=wf, scalar1=float(l * 8192), scalar2=1.0, op0=ALU.add, op1=ALU.mult)
            self.V("dve", "scalar_tensor_tensor", [G], [G], out=wf, in0=same, scalar=1.0e8, in1=wf, op0=ALU.mult, op1=ALU.add)
            self.V("dve", "tensor_copy", [G], [widx], out=widx[:, 0, :], in_=wf)
            self.V("dve", "tensor_scalar", [G], [G], out=wf, in0=wf, scalar1=1.0, scalar2=1.0, op0=ALU.add, op1=ALU.mult)
            self.V("dve", "tensor_copy", [G], [widx], out=widx[:, 1, :], in_=wf)
            Q = [self.sb(es, "spQ%d" % k, [128, 160], F32) for k in range(2)]
            rec = [self.sb(es, "sprec%d" % k, [128, 2, 4], F32) for k in range(2)]
            didx = [self.sb(es, "spdidx%d" % k, [128, 2], U32) for k in range(2)]
            for c in range(NCH):
                q = Q[c % 2]
                r_ = rec[c % 2]
                di = didx[c % 2]
                A, dst, d1, m1 = q[:, 0:32], q[:, 32:64], q[:, 64:96], q[:, 96:128]
                rd = [self.RW, self.RK, G, q]
                self.V("dve", "tensor_scalar", rd, [q], out=A, in0=self.RW[:, c, :], scalar1=0.0, scalar2=1.0, op0=ALU.is_gt, op1=ALU.mult)
                self.V("dve", "tensor_tensor", rd, [q], out=dst, in0=self.RK[:, c, :], in1=pst, op=ALU.add)
                self.V("dve", "scalar_tensor_tensor", rd, [q], out=d1, in0=dst, scalar=1.0, in1=A, op0=ALU.add, op1=ALU.mult)
                self.V("dve", "tensor_reduce", rd, [q], out=q[:, 128:129], in_=d1, axis=AX.X, op=ALU.max)
                self.V("dve", "tensor_scalar", rd, [q], out=m1, in0=d1, scalar1=q[:, 128:129], scalar2=1.0, op0=ALU.is_equal, op1=ALU.mult)
                self.V("dve", "tensor_tensor", rd, [q], out=m1, in0=m1, in1=self.RW[:, c, :], op=ALU.mult)
                self.V("dve", "tensor_reduce", rd, [q], out=q[:, 129:130], in_=m1, axis=AX.X, op=ALU.add)
                self.V("dve", "tensor_reduce", rd, [q], out=q[:, 130:131], in_=self.RW[:, c, :], axis=AX.X, op=ALU.add)
                self.V("dve", "tensor_scalar", rd, [q], out=m1, in0=A, scalar1=-1.0e9, scalar2=1.0e9, op0=ALU.mult, op1=ALU.add)
                self.V("dve", "tensor_tensor", rd, [q], out=m1, in0=m1, in1=dst, op=ALU.add)
                self.V("dve", "tensor_reduce", rd, [q], out=q[:, 131:132], in_=m1, axis=AX.X, op=ALU.min)
                self.V("dve", "tensor_scalar", rd, [q], out=q[:, 132:133], in0=q[:, 128:129], scalar1=-1.0, scalar2=1.0, op0=ALU.add, op1=ALU.mult)
                self.V("dve", "memset", [], [r_], r_[:], 0.0)
                for k in range(2):
                    self.V("dve", "tensor_scalar", [self.cst, r_], [r_], out=r_[:, k, 0:1], in0=self.cst[:, 1024:1025], scalar1=float(c * 128), scalar2=1.0,
                           op0=ALU.add, op1=ALU.mult)
                    self.V("dve", "tensor_scalar", [self.cst, r_], [r_], out=r_[:, k, 2:3], in0=self.cst[:, 1024:1025], scalar1=float(c * 128 + k * T), scalar2=2.0,
                           op0=ALU.add, op1=ALU.mult)
                    self.V("dve", "tensor_scalar", [r_], [r_], out=r_[:, k, 3:4], in0=r_[:, k, 2:3], scalar1=1.0, scalar2=1.0, op0=ALU.add, op1=ALU.mult)
                self.V("dve", "tensor_tensor", [q, r_], [r_], out=r_[:, 0, 1:2], in0=q[:, 130:131], in1=q[:, 129:130], op=ALU.subtract)
                self.V("dve", "tensor_copy", [q, r_], [r_], out=r_[:, 1, 1:2], in_=q[:, 129:130])
                self.V("dve", "tensor_copy", [q], [di], out=di[:, 0:1], in_=q[:, 131:132])
                self.V("dve", "tensor_copy", [q], [di], out=di[:, 1:2], in_=q[:, 132:133])
                for k in range(2):
                    S.dma_fn("pool", (lambda e, r_=r_, di=di, k=k: e.indirect_dma_start(out=self.ROWS, out_offset=bass.IndirectOffsetOnAxis(ap=di[:, k:k + 1], axis=0),
                                                                                      in_=r_[:, k, :], in_offset=None)), [r_, di], [rows_t])
            wgv = i["moe_w_gate"].rearrange("l e (p j) f -> (l e p) (j f)", j=8).rearrange("r (h x) -> (r h) x", h=2)
            wuv = i["moe_w_up"].rearrange("l e (p j) f -> (l e p) (j f)", j=8).rearrange("r (h x) -> (r h) x", h=2)
            wdv = i["moe_w_down"].rearrange("l e (p j) n -> (l e p) (j n)", j=4).rearrange("r (h x) -> (r h) x", h=2)
            wts = [(self.sb(es, "spwg%d" % k, [128, 8, 512], BF), self.sb(es, "spwu%d" % k, [128, 8, 512], BF),
                    self.sb(es, "spwd%d" % k, [128, 4, D], BF)) for k in range(2)]
            NQ = 4
            recs = [self.sb(es, "sprc%d" % k, [128, 4], F32) for k in range(NQ)]
            recu = [self.sb(es, "spru%d" % k, [128, 4], U32) for k in range(NQ)]
            hbs = [self.sb(es, "sphb%d" % k, [128, D], BF) for k in range(NQ)]
            hTs = [self.sb(es, "sphT%d" % k, [128, 8, 128], BF) for k in range(NQ)]
            sGs = [self.sb(es, "spsg%d" % k, [128, 512], F32) for k in range(2)]
            Hhs = [self.sb(es, "spHh%d" % k, [128, 512], BF) for k in range(2)]
            HhTs = [self.sb(es, "spHhT%d" % k, [128, 4, 128], BF) for k in range(2)]
            ys = [self.sb(es, "spy%d" % k, [128, D], F32) for k in range(2)]
            def gather(dst_ap, src, idx_ap, r, w, skip=False):
                if skip:
                    S.dma_fn("pool", (lambda e: e.indirect_dma_start(out=dst_ap, out_offset=None, in_=src, in_offset=bass.IndirectOffsetOnAxis(ap=idx_ap, axis=0),
                                                                     bounds_check=self._wbound_reg(e), oob_is_err=False)), r, w)
                else:
                    S.dma_fn("pool", (lambda e: e.indirect_dma_start(out=dst_ap, out_offset=None, in_=src, in_offset=bass.IndirectOffsetOnAxis(ap=idx_ap, axis=0))), r, w)

            def proA(j):
                rc, ru, hb = recs[j % NQ], recu[j % NQ], hbs[j % NQ]
                S.dma("sp", rc[:], self.ROWS[j * 128:(j + 1) * 128, :], [rows_t], [rc])
                self.V("dve", "tensor_copy", [rc], [ru], out=ru[:], in_=rc[:])
                gather(hb[:], self.H2, ru[:, 0:1], [ru, h2_t], [hb])

            def proB(j):
                hb, hT = hbs[j % NQ], hTs[j % NQ]
                pt = self.psb()
                ptv = pt[:].bitcast(BF).rearrange("p (k n) -> p k n", k=8)
                hbv = hb[:].rearrange("t (p j) -> t p j", j=8)
                for jx in range(8):
                    self.tr(ptv[:, jx, :], hbv[:, :, jx], self.identb[:], [hb, self.identb], [pt])
                self.act(hT[:], ptv, AF.Copy, [pt], [hT])

            def wload(j):
                wg, wu, wd = wts[j % 2]
                for h_ in range(2):
                    gather(wg[:, h_ * 4:(h_ + 1) * 4, :].rearrange("p j f -> p (j f)"), wgv, widx[:, h_, j:j + 1], [widx], [wg], skip=True)
                    gather(wu[:, h_ * 4:(h_ + 1) * 4, :].rearrange("p j f -> p (j f)"), wuv, widx[:, h_, j:j + 1], [widx], [wu], skip=True)
                    gather(wd[:, h_ * 2:(h_ + 1) * 2, :].rearrange("p j f -> p (j f)"), wdv, widx[:, h_, j:j + 1], [widx], [wd], skip=True)

            proA(0)
            proA(1)
            wload(0)
            proB(0)
            for j in range(NB):
                z = j % 2
                rc, ru, hT = recs[j % NQ], recu[j % NQ], hTs[j % NQ]
                sg, Hh, HhT, y = sGs[z], Hhs[z], HhTs[z], ys[z]
                wg, wu, wd = wts[z]
                if j + 2 < NB:
                    proA(j + 2)
                if j + 1 < NB:
                    wload(j + 1)
                pG, pU = self.psb(), self.psb()
                for jx in range(8):
                    self.mm(pG[:, :], hT[:, jx, :], wg[:, jx, :], jx == 0, jx == 7, [hT, wg], [pG])
                for jx in range(8):
                    self.mm(pU[:, :], hT[:, jx, :], wu[:, jx, :], jx == 0, jx == 7, [hT, wu], [pU])
                self.act(sg[:], pG[:, :], AF.Silu, [pG], [sg])
                self.V("dve", "scalar_tensor_tensor", [pU, rc, sg], [Hh], out=Hh[:], in0=pU[:, :], scalar=rc[:, 1:2], in1=sg[:], op0=ALU.mult, op1=ALU.mult)
                if j + 1 < NB:
                    proB(j + 1)
                pt2 = self.psb()
                pt2v = pt2[:].bitcast(BF)[:, 0:512].rearrange("p (k n) -> p k n", k=4)
                Hhv = Hh[:].rearrange("t (p j) -> t p j", j=4)
                for jx in range(4):
                    self.tr(pt2v[:, jx, :], Hhv[:, :, jx], self.identb[:], [Hh, self.identb], [pt2])
                self.act(HhT[:], pt2v, AF.Copy, [pt2], [HhT])
                for half in range(2):
                    pD = self.psb()
                    for jx in range(4):
                        self.mm(pD[:, :], HhT[:, jx, :], wd[:, jx, half * 512:(half + 1) * 512], jx == 0, jx == 3, [HhT, wd], [pD])
                    if half == 0:
                        self.act(y[:, 0:512], pD[:, :], AF.Copy, [pD], [y])
                    else:
                        self.V("dve", "tensor_copy", [pD], [y], out=y[:, 512:1024], in_=pD[:, :])
                for h_ in range(2):
                    S.dma_fn("pool", (lambda e, y=y, ru=ru, h_=h_: e.indirect_dma_start(out=self.ACC2.rearrange("r (h x) -> (r h) x", h=2),
                                                                                        out_offset=bass.IndirectOffsetOnAxis(ap=ru[:, 2 + h_:3 + h_], axis=0),
                                                                                        in_=y[:, h_ * 512:(h_ + 1) * 512], in_offset=None,
                                                                                        bounds_check=self._bound_reg(e), oob_is_err=False)), [y, ru], [acc_t])
            self.residual_setup(es, 5)
            a0 = [self.sb(es, "spa0%d" % k, [128, D], F32) for k in range(2)]
            a1 = [self.sb(es, "spa1%d" % k, [128, D], F32) for k in range(2)]
            for c in range(NCH):
                u0, u1 = a0[c % 2], a1[c % 2]
                S.dma("sp", u0[:], self.ACC2[c * 128:(c + 1) * 128, :], [acc_t], [u0])
                S.dma("sp", u1[:], self.ACC2[T + c * 128:T + (c + 1) * 128, :], [acc_t], [u1])
                self.V("pool", "tensor_tensor", [u0, u1], [u0], out=u0[:], in0=u0[:], in1=u1[:], op=ALU.add)
                self.residual(c, lambda half, u0=u0: u0[:, half * 512:(half + 1) * 512], [u0, u0])
            S.barrier()

    def _wbound_reg(self, e):
        if getattr(self, "_wbreg", None) is None:
            self._wbreg = e.to_reg(self.n_layers * 32 * 128 * 2 - 1)
        return self._wbreg

    def _bound_reg(self, e):
        if getattr(self, "_breg", None) is None:
            self._breg = e.to_reg(4 * T - 1)
        return self._breg

    def final_norm(self):
        S = self.S
        with contextlib.ExitStack() as es:
            g = self.sb(es, "fng", [128, D], F32)
            S.dma("sp", g[:], self.i["final_g"].partition_broadcast(128), [], [g])
            xs = [self.sb(es, "fnx%d" % k, [128, D], F32) for k in range(3)]
            sq = self.sb(es, "fnsq", [128, D], F32)
            st = [self.sb(es, "fnst%d" % k, [128, 4], F32) for k in range(2)]
            ot = [self.sb(es, "fno%d" % k, [128, D], F32) for k in range(2)]
            for c in range(2, NCH):
                x = xs[c % 3]
                S.dma("sp", x[:], self.X[c * 128:(c + 1) * 128, :], [Tl(None, self.Xt[c])], [x])
                s_ = st[c % 2]
                self.act(sq[:], x[:], AF.Square, [x], [sq, s_], accum_out=s_[:, 0:1])
                self.V("dve", "tensor_scalar", [s_], [s_], out=s_[:, 1:2], in0=s_[:, 0:1], scalar1=1.0 / D, scalar2=EPS, op0=ALU.mult, op1=ALU.add)
                self.V("dve", "reciprocal", [s_], [s_], out=s_[:, 2:3], in_=s_[:, 1:2])
                self.act(s_[:, 3:4], s_[:, 2:3], AF.Sqrt, [s_], [s_])
                o = ot[c % 2]
                self.V("dve", "scalar_tensor_tensor", [x, s_, g], [o], out=o[:], in0=x[:], scalar=s_[:, 3:4], in1=g[:], op0=ALU.mult, op1=ALU.mult)
                S.dma("sp", self.out[(c - 2) * 128:(c - 1) * 128, :], o[:], [o], [Tl(None, self.outt)])


def make_consts():
    cst = np.zeros((128, 1056), np.float32)
    cst[:, 0:128] = np.eye(128, dtype=np.float32)
    cst[:, 128:256] = 1.0
    R = np.zeros((128, 128), np.float32)
    for blk in range(2):
        o = blk * 64
        for q in range(16):
            R[o + 16 + q, o + q] = -1.0
            R[o + q, o + 16 + q] = 1.0
            R[o + 48 + q, o + 32 + q] = -1.0
            R[o + 32 + q, o + 48 + q] = 1.0
    cst[:, 256:384] = R
    s = np.arange(128)[:, None]
    t = np.arange(128)[None, :]
    same = (s // 64) == (t // 64)
    same32 = (s // 32) == (t // 32)
    cst[:, 384:512] = (same32 & (t >= s)).astype(np.float32)
    cst[:, 512:640] = (same32 & (t <= s)).astype(np.float32)
    cst[:, 640:768] = (same & (s % 64 < 32) & (t % 64 >= 32)).astype(np.float32)
    cst[:, 768:896] = (same & (s % 64 >= 32) & (t % 64 < 32)).astype(np.float32)
    cst[:, 896:1024] = 128.0 * np.arange(128, dtype=np.float32)[None, :]
    cst[:, 1024] = np.arange(128, dtype=np.float32)
    cst[:, 1025:1057 - 1 + 0] = 0.0
    inv_freq = (10000.0 ** (-np.arange(0, 32, 2, dtype=np.float32) / 32)).astype(np.float32)
    pos = np.arange(NLAT)
    row = (pos // 64).astype(np.float32)
    col = (pos % 64).astype(np.float32)
    ang_r = row[:, None] * inv_freq
    ang_c = col[:, None] * inv_freq
    ang = np.concatenate([ang_r, ang_r, ang_c, ang_c], axis=-1).astype(np.float32)
    rope = np.zeros((2, 128, T), np.float32)
    rope[0, :, :NCTX] = 1.0
    rope[0, 0:64, NCTX:] = np.cos(ang).T
    rope[0, 64:128, NCTX:] = np.cos(ang).T
    rope[1, 0:64, NCTX:] = np.sin(ang).T
    rope[1, 64:128, NCTX:] = np.sin(ang).T
    return cst, rope


def make_in_maps(inputs, cores, L=DEPTH, nexp=32):
    f = lambda a: np.ascontiguousarray(np.asarray(a, dtype=np.float32))
    cst, rope = make_consts()
    shared = {
        "c_ctx": f(inputs["c_ctx"]).reshape(1, D),
        "ada_w": f(inputs["ada_w"][:L]), "ada_b": f(inputs["ada_b"]),
        "norm1_g": f(inputs["norm1_g"]), "norm2_g": f(inputs["norm2_g"]),
        "w_in": f(inputs["w_in"][:L]), "w_branch": f(inputs["w_branch"][:L]), "w_out": f(inputs["w_out"][:L]),
        "hg_lb": f(inputs["hg_lb_logits"]), "hg_norm_g": f(inputs["hg_norm_g"]),
        "da_lambda": f(inputs["da_lambda"]).reshape(DEPTH, 256), "da_norm_g": f(inputs["da_norm_g"]),
        "cv_dw_w": f(inputs["cv_dw_w"]), "cv_dw_b": f(inputs["cv_dw_b"]),
        "cv_ln_g": f(inputs["cv_ln_g"]), "cv_ln_b": f(inputs["cv_ln_b"]),
        "sc_w": f(inputs["sc_w"]),
        "moe_w_r": np.ascontiguousarray(np.concatenate([f(inputs["moe_w_grp"]), f(inputs["moe_w_exp"])], axis=-1)),
        "moe_b_r": np.ascontiguousarray(np.concatenate([f(inputs["moe_b_grp"]), f(inputs["moe_b_exp"])], axis=-1)),
        "moe_w_gate": f(inputs["moe_w_gate"][:L, :nexp]), "moe_w_up": f(inputs["moe_w_up"][:L, :nexp]), "moe_w_down": f(inputs["moe_w_down"][:L, :nexp]),
        "final_g": f(inputs["final_g"]).reshape(1, D),
        "cst": cst, "rope": rope, "ltri": np.triu(np.ones((128, 128), np.float32), 1),
    }
    maps = []
    for cid in cores:
        b = cid % 4
        m = dict(shared)
        m["x"] = f(inputs["x"][b])
        m["c"] = f(inputs["c"][b]).reshape(1, D)
        m["ctx"] = f(inputs["ctx"][b])
        maps.append(m)
    return maps


def kernel(**inputs):
    nc = bass.Bass("TRN2", target_bir_lowering=False)
    Prog(nc).build()
    maps = make_in_maps(inputs, list(range(4)))
    res = run_bass_kernel_spmd(nc, maps, core_ids=list(range(4)))
    return np.stack([np.asarray(res.results[b]["out"], dtype=np.float32) for b in range(4)], axis=0)
```

```python
import contextlib
import math
import numpy as np
import concourse.bass as bass
import concourse.mybir as mybir
from concourse.bass_utils import run_bass_kernel_spmd

F32 = mybir.dt.float32
BF = mybir.dt.bfloat16
U32 = mybir.dt.uint32
AF = mybir.ActivationFunctionType
ALU = mybir.AluOpType
AX = mybir.AxisListType

D = 1024
NCTX = 256
NLAT = 4096
T = NCTX + NLAT
NCH = T // 128
NB = 2 * T // 128 + 32
SPARSE = True
DEPTH = 4
W = 512
INW = 10752
EPS = 1e-6
TILES = [(0, 256)] + [(256 + 512 * i, 512) for i in range(8)]
C_HQ, C_HI, C_HFF, C_HFB, C_HG = 0, 512, 1024, 1536, 2048
C_DQ, C_DK, C_DV = 2560, 3072, 3584
C_CVA, C_CVG = 4096, 4608
C_SB, C_SC, C_SX = 5120, 5632, 6144
C_GATE = 6656


class Trk:
    __slots__ = ("w", "r")

    def __init__(self):
        self.w = None
        self.r = {}


class Tl:
    def __init__(self, h, trk=None):
        self.h = h
        self.t = trk or Trk()

    def __getitem__(self, k):
        return self.h[k]


class Stream:
    def __init__(self, name):
        self.name = name
        self.ops = []
        self.seen = {}
        self.sem = None
        self.cnt = 0
        self.dslots = []
        self.dnext = 0


class Sch:
    SEM_MAX = 30000

    def __init__(self, nc, es):
        self.nc = nc
        self.es = es
        self.st = {k: Stream(k) for k in ("pe", "act", "dve", "pool", "sp")}
        self.nsem = 0
        for k, s in self.st.items():
            self._newsem(s)
        for k, n in (("sp", 24), ("pool", 12), ("act", 6)):
            s = self.st[k]
            for i in range(n):
                s.dslots.append([self._sem(), 0])

    def _sem(self):
        self.nsem += 1
        return self.es.enter_context(self.nc.semaphore("s%d" % self.nsem))

    def _newsem(self, s):
        s.sem = self._sem()
        s.cnt = 0

    def _need(self, s, tok, waits):
        if tok is None:
            return
        sem, val, src = tok
        if src == "pe" and s.name == "pe":
            return
        if s.seen.get(id(sem), 0) >= val:
            return
        k = id(sem)
        if k not in waits or waits[k][1] < val:
            waits[k] = (sem, val)

    def _deps(self, s, reads, writes):
        waits = {}
        for b in reads:
            self._need(s, b.t.w, waits)
        for b in writes:
            self._need(s, b.t.w, waits)
            for tok in b.t.r.values():
                self._need(s, tok, waits)
        for k, (sem, val) in waits.items():
            s.seen[k] = val
        return list(waits.values())

    def _mark(self, tok, reads, writes):
        for b in reads:
            b.t.r[id(tok[0])] = tok
        for b in writes:
            b.t.w = tok
            b.t.r = {}

    def op(self, eng, fn, reads=(), writes=()):
        s = self.st[eng]
        if s.cnt >= self.SEM_MAX:
            self._newsem(s)
        waits = self._deps(s, reads, writes)
        s.cnt += 1
        tok = (s.sem, s.cnt, eng)
        s.ops.append((waits, fn, (s.sem, 1)))
        self._mark(tok, reads, writes)
        return tok

    def dma(self, q, out, in_, reads=(), writes=(), **kw):
        return self.dma_fn(q, (lambda e: e.dma_start(out=out, in_=in_, **kw)), reads, writes)

    def dma_fn(self, q, fn, reads=(), writes=()):
        s = self.st[q]
        slot = s.dslots[s.dnext % len(s.dslots)]
        s.dnext += 1
        waits = self._deps(s, reads, writes)
        if slot[1] > 0 and s.seen.get(id(slot[0]), 0) < slot[1]:
            waits.append((slot[0], slot[1]))
            s.seen[id(slot[0])] = slot[1]
        slot[1] += 16
        tok = (slot[0], slot[1], "dma")
        s.ops.append((waits, fn, (slot[0], 16)))
        self._mark(tok, reads, writes)
        return tok

    def barrier(self):
        toks = []
        for s in self.st.values():
            if s.cnt > 0:
                toks.append((s.sem, s.cnt))
            for sl in s.dslots:
                if sl[1] > 0:
                    toks.append((sl[0], sl[1]))
        for s in self.st.values():
            waits = []
            for sem, val in toks:
                if sem is s.sem:
                    continue
                if s.seen.get(id(sem), 0) < val:
                    waits.append((sem, val))
                    s.seen[id(sem)] = val
            if waits:
                s.ops.append((waits, None, None))

    def emit(self):
        nc = self.nc
        self.barrier()
        with nc.Block() as block:
            def run(s):
                def f(e):
                    for waits, fn, inc in s.ops:
                        for sem, val in waits:
                            e.wait_ge(sem, val)
                        if fn is not None:
                            fn(e).then_inc(inc[0], inc[1])
                return f
            block.tensor(run(self.st["pe"]))
            block.scalar(run(self.st["act"]))
            block.vector(run(self.st["dve"]))
            block.gpsimd(run(self.st["pool"]))
            block.sync(run(self.st["sp"]))


class Prog:
    def __init__(self, nc, n_layers=DEPTH, dbg=None, nexp=32):
        self.nc = nc
        self.nexp = nexp
        self.n_layers = n_layers
        self.dbg = dbg or {}

    def sb(self, es, name, shape, dt):
        self.uid = getattr(self, "uid", 0) + 1
        return Tl(es.enter_context(self.nc.sbuf_tensor("%s_u%d" % (name, self.uid), list(shape), dt)))

    def dram(self, name, shape, dt, kind="Internal"):
        return self.nc.dram_tensor(name, list(shape), dt, kind=kind).ap()

    def mm(self, out, lhsT, rhs, start, stop, r, w):
        self.S.op("pe", lambda e: e.matmul(out, lhsT=lhsT, rhs=rhs, start=start, stop=stop), r, w)

    def tr(self, out, in_, ident, r, w):
        self.S.op("pe", lambda e: e.transpose(out=out, in_=in_, identity=ident), r, w)

    def act(self, out, in_, func, r, w, **kw):
        self.S.op("act", lambda e: e.activation(out=out, in_=in_, func=func, **kw), r, w)

    def V(self, eng, name, r, w, *a, **kw):
        self.S.op(eng, lambda e: getattr(e, name)(*a, **kw), r, w)

    def psb(self):
        b = self.banks[self.bi % 8]
        self.bi += 1
        return b

    def build(self):
        nc = self.nc
        L = self.n_layers
        i = {}
        def inp(name, shape, dt=F32):
            i[name] = self.dram(name, shape, dt, kind="ExternalInput")
        inp("x", [NLAT, D]); inp("c", [1, D]); inp("ctx", [NCTX, D]); inp("c_ctx", [1, D])
        inp("ada_w", [L, D, 6 * D]); inp("ada_b", [DEPTH, 6 * D])
        inp("norm1_g", [DEPTH, D]); inp("norm2_g", [DEPTH, D])
        inp("w_in", [L, D, INW]); inp("w_branch", [L, 4, W, D]); inp("w_out", [L, D, D])
        inp("hg_lb", [DEPTH, 2, W]); inp("hg_norm_g", [DEPTH, W])
        inp("da_lambda", [DEPTH, 256]); inp("da_norm_g", [DEPTH, 128])
        inp("cv_dw_w", [DEPTH, 31, W]); inp("cv_dw_b", [DEPTH, W]); inp("cv_ln_g", [DEPTH, W]); inp("cv_ln_b", [DEPTH, W])
        inp("sc_w", [DEPTH, 3, W])
        inp("moe_w_r", [DEPTH, D, 36]); inp("moe_b_r", [DEPTH, 36])
        inp("moe_w_gate", [L, self.nexp, D, W]); inp("moe_w_up", [L, self.nexp, D, W]); inp("moe_w_down", [L, self.nexp, W, D])
        inp("final_g", [1, D]); inp("ltri", [128, 128])
        inp("cst", [128, 1056]); inp("rope", [2, 128, T])
        self.i = i
        self.out = self.dram("out", [NLAT, D], F32, kind="ExternalOutput")
        self.X = self.dram("Xs", [T, D], F32)
        self.HT = self.dram("HTs", [8, 128, T], BF)
        self.Y = self.dram("Ys", [4, 4, 128, T], BF)
        self.MT = self.dram("MTs", [8, 128, T], BF)
        self.MODR = self.dram("MODs", [2, 128, 6 * D], F32)
        self.H2 = self.dram("H2s", [T + 1, D], BF)
        self.ROWS = self.dram("ROWSs", [NB * 128, 4], F32)
        self.ACC2 = self.dram("ACC2s", [2 * T, D], F32)
        self.H2t = Trk(); self.ROWSt = Trk(); self.ACC2t = Trk()
        self.Xt = [Trk() for _ in range(NCH)]
        self.HTt = [Trk() for _ in range(9)]
        self.Yt = [[Trk() for _ in range(9)] for _ in range(4)]
        self.MTt = [Trk() for _ in range(9)]
        self.MODt = Trk()
        self.outt = Trk()
        dbg_out = {}
        for k, shp in self.dbg.items():
            if not isinstance(shp, tuple):
                continue
            dbg_out[k] = self.dram("dbg_" + k, shp[0], shp[1], kind="ExternalOutput")
        self.dbg_out = dbg_out

        with contextlib.ExitStack() as es:
            self.S = S = Sch(nc, es)
            self.banks = [Tl(es.enter_context(nc.psum_tensor("pb%d" % k, [128, 512], F32))) for k in range(8)]
            self.bi = 0
            self.cst = self.sb(es, "cst_sb", [128, 1056], F32)
            S.dma("sp", self.cst[:], i["cst"], [], [self.cst])
            self.identb = self.sb(es, "identb", [128, 128], BF)
            self.onesb = self.sb(es, "onesb", [128, 128], BF)
            self.Rb = self.sb(es, "Rb", [128, 128], BF)
            self.identf = Tl(self.cst.h, self.cst.t)
            self.V("dve", "tensor_copy", [self.cst], [self.identb], out=self.identb[:], in_=self.cst[:, 0:128])
            self.V("dve", "tensor_copy", [self.cst], [self.onesb], out=self.onesb[:], in_=self.cst[:, 128:256])
            self.V("dve", "tensor_copy", [self.cst], [self.Rb], out=self.Rb[:], in_=self.cst[:, 256:384])
            self.sT = []
            for which, src in enumerate((i["c"], i["c_ctx"])):
                cT = self.sb(es, "cT%d" % which, [128, 8], F32)
                S.dma("sp", cT[:], src.rearrange("o (kc p) -> p (o kc)", p=128), [], [cT], allow_slow_non_contiguous=True)
                sg = self.sb(es, "cS%d" % which, [128, 8], F32)
                self.act(sg[:], cT[:], AF.Silu, [cT], [sg])
                rep = self.sb(es, "sT%d" % which, [128, 8, 128], F32)
                self.V("dve", "tensor_copy", [sg], [rep], out=rep[:], in_=sg[:].unsqueeze(2).to_broadcast([128, 8, 128]))
                self.sT.append(rep)
            self.P = self.sb(es, "Pparams", [128, 600], F32)
            self.RW = self.sb(es, "RW", [128, NCH, 32], F32)
            self.RK = self.sb(es, "RK", [128, NCH, 32], F32)
            self.Asum = self.sb(es, "Asum", [128, 32], F32)
            self.ltri = self.sb(es, "ltri_sb", [128, 128], F32)
            S.dma("sp", self.ltri[:], i["ltri"], [], [self.ltri])
            zr = self.sb(es, "zrow", [1, D], BF)
            self.V("dve", "memset", [], [zr], zr[:], 0.0)
            S.dma("sp", self.H2[T:T + 1, :], zr[:], [zr], [Tl(None, self.H2t)])
            self.LB = self.sb(es, "LB", [128, DEPTH, 8], F32)
            self.OML = self.sb(es, "OML", [128, DEPTH, 8], F32)
            lbe = self.sb(es, "lbe", [128, DEPTH, 8], F32)
            lbt = self.sb(es, "lbt", [128, 16], F32)
            for l_ in range(DEPTH):
                for d_ in range(2):
                    for j_ in range(4):
                        S.dma("sp", lbe[:, l_, d_ * 4 + j_:d_ * 4 + j_ + 1], i["hg_lb"][l_, d_:d_ + 1, j_ * 128:(j_ + 1) * 128].rearrange("o p -> p o"),
                              [], [lbe], allow_slow_non_contiguous=True)
            self.act(lbe[:], lbe[:], AF.Exp, [lbe], [lbe])
            self.V("dve", "tensor_tensor", [lbe], [lbt], out=lbt[:, 0:8], in0=lbe[:, 0, :], in1=lbe[:, 1, :], op=ALU.add)
            self.V("dve", "tensor_tensor", [lbe, lbt], [lbt], out=lbt[:, 0:8], in0=lbt[:, 0:8], in1=lbe[:, 2, :], op=ALU.add)
            self.V("dve", "tensor_tensor", [lbe, lbt], [lbt], out=lbt[:, 0:8], in0=lbt[:, 0:8], in1=lbe[:, 3, :], op=ALU.add)
            self.V("dve", "reciprocal", [lbt], [lbt], out=lbt[:, 8:16], in_=lbt[:, 0:8])
            self.V("dve", "memset", [], [self.LB], self.LB[:], 0.0)
            for l_ in range(1, DEPTH):
                self.V("dve", "tensor_tensor", [lbe, lbt], [lbe], out=lbe[:, l_, :], in0=lbe[:, l_, :], in1=lbt[:, 8:16], op=ALU.mult)
                self.V("dve", "tensor_tensor", [lbe, self.LB], [self.LB], out=self.LB[:, l_, :], in0=self.LB[:, l_ - 1, :], in1=lbe[:, l_, :], op=ALU.add)
            self.V("dve", "tensor_scalar", [self.LB], [self.OML], out=self.OML[:], in0=self.LB[:], scalar1=-1.0, scalar2=1.0, op0=ALU.mult, op1=ALU.add)
            self.seg = self.sb(es, "seg", [128, 512], F32)
            self.V("dve", "memset", [], [self.seg], self.seg[:], 1.0)
            self.V("dve", "memset", [self.seg], [self.seg], self.seg[:].rearrange("p (c s) -> p c s", s=64)[:, :, 0:1], 0.0)
            S.barrier()
            S.dma("sp", self.X[0:NCTX, :], i["ctx"], [], self._xt(0, 2))
            for q in range(4):
                S.dma("sp", self.X[NCTX + q * 1024: NCTX + (q + 1) * 1024, :], i["x"][q * 1024:(q + 1) * 1024, :], [],
                      self._xt(2 + q * 8, 8))
            for l in range(L):
                self.layer(l)
            self.final_norm()
            S.emit()

    def _xt(self, c0, n):
        return [Tl(None, t) for t in self.Xt[c0:c0 + n]]

    def layer(self, l):
        stop = self.dbg.get("stop")
        self.phase_mod(l)
        self.load_params(l)
        self.phase_norm(l, 1)
        if stop == "norm1":
            return
        self.phase_sconv(l)
        self.phase_conv(l)
        if stop == "convs":
            return self.dump_dbg()
        self.phase_attn(l)
        if stop == "attn":
            return self.dump_dbg()
        self.phase_hgrn(l)
        if stop == "hgrn":
            return self.dump_dbg()
        self.phase_merge(l)
        if stop == "merge":
            return self.dump_dbg()
        self.phase_norm(l, 2)
        if SPARSE:
            self.phase_moe_sparse(l)
        else:
            self.phase_moe(l)
        if stop == "moe":
            return self.dump_dbg()

    def dump_dbg(self):
        S = self.S
        if "y" in self.dbg_out:
            S.dma("sp", self.dbg_out["y"], self.Y, [Tl(None, t) for k in range(4) for t in self.Yt[k]], [Tl(None, Trk())])
        if "x" in self.dbg_out:
            S.dma("sp", self.dbg_out["x"], self.X, [Tl(None, t) for t in self.Xt], [Tl(None, Trk())])

    def load_w(self, es, name, l, c0, ncols):
        w = self.sb(es, name, [128, 8, ncols], BF)
        self.S.dma("pool", w[:], self.i["w_in"][l, :, c0:c0 + ncols].rearrange("(kc p) n -> p kc n", p=128), [], [w])
        return w

    def load_ht(self, buf, ti):
        t0, N = TILES[ti]
        self.S.dma("sp", buf[:, :, 0:N], self.HT[:, :, t0:t0 + N].rearrange("k p n -> p k n"), [Tl(None, self.HTt[ti])], [buf])

    def proj(self, ps, w, j0, ht, N, width=128):
        for kc in range(8):
            self.mm(ps[0:width, 0:N], w[:, kc, j0:j0 + width], ht[:, kc, 0:N], kc == 0, kc == 7, [w, ht], [ps])

    def rstd_from(self, src_ap, scale, out_t, tmp_t, N, r):
        self.V("dve", "tensor_scalar", r, [tmp_t], out=tmp_t[:, 0:N], in0=src_ap, scalar1=scale, scalar2=EPS, op0=ALU.mult, op1=ALU.add)
        self.V("dve", "reciprocal", [tmp_t], [tmp_t], out=tmp_t[:, 0:N], in_=tmp_t[:, 0:N])
        self.act(out_t[:, 0:N], tmp_t[:, 0:N], AF.Sqrt, [tmp_t], [out_t])

    def load_params(self, l):
        S = self.S
        i = self.i
        P = self.P
        nc = True
        def ld(dst, src):
            S.dma("sp", dst, src, [], [P], allow_slow_non_contiguous=True)
        for j in range(4):
            sl = slice(j * 128, (j + 1) * 128)
            ld(P[:, j * 3:j * 3 + 3], i["sc_w"][l][:, sl].rearrange("k p -> p k"))
            ld(P[:, 12 + j * 31:12 + (j + 1) * 31], i["cv_dw_w"][l][:, sl].rearrange("k p -> p k"))
            ld(P[:, 136 + j:137 + j], i["cv_dw_b"][l:l + 1, sl].rearrange("o p -> p o"))
            ld(P[:, 140 + j:141 + j], i["cv_ln_g"][l:l + 1, sl].rearrange("o p -> p o"))
            ld(P[:, 144 + j:145 + j], i["cv_ln_b"][l:l + 1, sl].rearrange("o p -> p o"))
            ld(P[:, 148 + j:149 + j], i["hg_norm_g"][l:l + 1, sl].rearrange("o p -> p o"))
        ld(P[:, 152:153], i["da_norm_g"][l:l + 1, :].rearrange("o p -> p o"))
        S.dma("sp", P[:, 160:416], i["da_lambda"][l:l + 1, :].partition_broadcast(128), [], [P])
        S.dma("sp", P[:, 416:452], i["moe_b_r"][l:l + 1, :].partition_broadcast(128), [], [P])
        lam_init = 0.8 - 0.6 * math.exp(-0.3 * l)
        self.V("dve", "tensor_tensor", [P], [P], out=P[:, 460:524], in0=P[:, 160:224], in1=P[:, 224:288], op=ALU.mult)
        self.V("dve", "tensor_tensor", [P], [P], out=P[:, 524:588], in0=P[:, 288:352], in1=P[:, 352:416], op=ALU.mult)
        self.V("dve", "tensor_reduce", [P], [P], out=P[:, 155:157], in_=P[:, 460:588].rearrange("p (a b) -> p a b", a=2), axis=AX.X, op=ALU.add)
        self.act(P[:, 157:159], P[:, 155:157], AF.Exp, [P], [P])
        self.V("dve", "tensor_tensor", [P], [P], out=P[:, 159:160], in0=P[:, 158:159], in1=P[:, 157:158], op=ALU.subtract)
        self.V("dve", "tensor_scalar", [P], [P], out=P[:, 153:154], in0=P[:, 159:160], scalar1=-lam_init, scalar2=1.0, op0=ALU.add, op1=ALU.mult)
        self.V("dve", "tensor_scalar", [P], [P], out=P[:, 154:155], in0=P[:, 152:153], scalar1=1.0 - lam_init, scalar2=0.0, op0=ALU.mult, op1=ALU.add)

    def phase_mod(self, l):
        S = self.S
        i = self.i
        modt = Tl(None, self.MODt)
        with contextlib.ExitStack() as es:
            wt = [self.sb(es, "adaw%d" % k, [128, 8, 512], F32) for k in range(2)]
            bt = [self.sb(es, "adab%d" % k, [1, 512], F32) for k in range(2)]
            ot = [self.sb(es, "adao%d" % k, [128, 512], F32) for k in range(2)]
            n = 0
            for blk in range(12):
                w = wt[blk % 2]
                b = bt[blk % 2]
                S.dma("sp", w[:], i["ada_w"][l, :, blk * 512:(blk + 1) * 512].rearrange("(kc p) n -> p kc n", p=128), [], [w])
                S.dma("sp", b[:], i["ada_b"][l:l + 1, blk * 512:(blk + 1) * 512], [], [b])
                for which in range(2):
                    ps = self.psb()
                    for kc in range(8):
                        self.mm(ps[:], self.sT[which][:, kc, :], w[:, kc, :], kc == 0, False, [self.sT[which], w], [ps])
                    self.mm(ps[:], self.cst[0:1, 128:256], b[0:1, :], False, True, [self.cst, b], [ps])
                    o = ot[n % 2]
                    n += 1
                    self.V("dve", "tensor_copy", [ps], [o], out=o[:], in_=ps[:])
                    S.dma("sp", self.MODR[which, :, blk * 512:(blk + 1) * 512], o[:], [o], [modt])
            S.barrier()

    def phase_norm(self, l, which):
        S = self.S
        i = self.i
        gsrc = i["norm1_g"] if which == 1 else i["norm2_g"]
        sh, sc = (0, 1) if which == 1 else (3, 4)
        modt = Tl(None, self.MODt)
        with contextlib.ExitStack() as es:
            A = [self.sb(es, "nA%d" % k, [128, D], F32) for k in range(2)]
            B = [self.sb(es, "nB%d" % k, [128, D], F32) for k in range(2)]
            g = self.sb(es, "ng", [128, D], F32)
            S.dma("sp", g[:], gsrc[l:l + 1, :].partition_broadcast(128), [], [g])
            for k in range(2):
                S.dma("sp", A[k][:], self.MODR[k, :, sc * D:(sc + 1) * D], [modt], [A[k]])
                S.dma("sp", B[k][:], self.MODR[k, :, sh * D:(sh + 1) * D], [modt], [B[k]])
                self.V("dve", "scalar_tensor_tensor", [A[k], g], [A[k]], out=A[k][:], in0=A[k][:], scalar=1.0, in1=g[:],
                       op0=ALU.add, op1=ALU.mult)
            xs = [self.sb(es, "nx%d" % k, [128, D], F32) for k in range(3)]
            sq = self.sb(es, "nsq", [128, D], F32)
            t1 = [self.sb(es, "nt%d" % k, [128, D], F32) for k in range(2)]
            hb = [self.sb(es, "nhb%d" % k, [128, D], BF) for k in range(2)]
            st = [self.sb(es, "nst%d" % k, [128, 4], F32) for k in range(2)]
            hT = [self.sb(es, "nhT%d" % k, [128, 8, 512], BF) for k in range(2)]
            if which == 2:
                wr = self.sb(es, "nwr", [128, 8, 36], F32)
                S.dma("sp", wr[:], i["moe_w_r"][l].rearrange("(kc p) n -> p kc n", p=128), [], [wr])
                h32 = [self.sb(es, "nh32%d" % k, [128, D], F32) for k in range(2)]
                h32T = [self.sb(es, "nh32T%d" % k, [128, 8, 128], F32) for k in range(2)]
                Rt = [self.sb(es, "nR%d" % k, [128, 160], F32) for k in range(2)]
            st3 = st + [self.sb(es, "nst2", [128, 4], F32)]
            sq2 = [sq, self.sb(es, "nsq2", [128, D], F32)]
            chunks = [(ti, t0, N, cc) for ti, (t0, N) in enumerate(TILES) for cc in range(N // 128)]

            def stageA(c):
                x = xs[c % 3]
                s_ = st3[c % 3]
                S.dma("sp", x[:], self.X[c * 128:(c + 1) * 128, :], [Tl(None, self.Xt[c])], [x])
                self.act(sq2[c % 2][:], x[:], AF.Square, [x], [sq2[c % 2], s_], accum_out=s_[:, 0:1])
                self.V("dve", "tensor_scalar", [s_], [s_], out=s_[:, 1:2], in0=s_[:, 0:1], scalar1=1.0 / D, scalar2=EPS,
                       op0=ALU.mult, op1=ALU.add)
                self.V("dve", "reciprocal", [s_], [s_], out=s_[:, 2:3], in_=s_[:, 1:2])
                self.act(s_[:, 3:4], s_[:, 2:3], AF.Sqrt, [s_], [s_])

            def stageB(ti, t0, N, cc):
                c = t0 // 128 + cc
                lat = 0 if c >= 2 else 1
                ht = hT[ti % 2]
                x = xs[c % 3]
                s_ = st3[c % 3]
                t = t1[c % 2]
                self.V("dve", "scalar_tensor_tensor", [x, s_, A[lat]], [t], out=t[:], in0=x[:], scalar=s_[:, 3:4],
                       in1=A[lat][:], op0=ALU.mult, op1=ALU.mult)
                h = hb[c % 2]
                self.V("pool", "tensor_tensor", [t, B[lat]], [h], out=h[:], in0=t[:], in1=B[lat][:], op=ALU.add)
                ps = self.psb()
                pv = ps[:].bitcast(BF).rearrange("p (k n) -> p k n", k=8)
                for k in range(8):
                    self.tr(pv[:, k, :], h[:, k * 128:(k + 1) * 128], self.identb[:], [h, self.identb], [ps])
                self.V("dve", "tensor_copy", [ps], [ht], out=ht[:, :, cc * 128:(cc + 1) * 128], in_=pv)
                if which == 2:
                    self.route(c, t, B[lat], h32[c % 2], h32T[c % 2], wr, Rt[c % 2])
                    if SPARSE:
                        S.dma("sp", self.H2[c * 128:(c + 1) * 128, :], h[:], [h], [Tl(None, self.H2t)])
                        self.rank(c, Rt[c % 2])
                if cc == N // 128 - 1:
                    S.dma("sp", self.HT[:, :, t0:t0 + N].rearrange("k p n -> p k n"), ht[:, :, 0:N], [ht], [Tl(None, self.HTt[ti])])

            stageA(0)
            for idx, (ti, t0, N, cc) in enumerate(chunks):
                if idx + 1 < len(chunks):
                    stageA(idx + 1)
                stageB(ti, t0, N, cc)
            S.barrier()
        if "h1" in self.dbg_out and l == self.dbg.get("layer", 0) and which == 1:
            S.dma("sp", self.dbg_out["h1"], self.HT, [Tl(None, t) for t in self.HTt], [Tl(None, Trk())])


    def phase_sconv(self, l):
        S = self.S
        P = self.P
        with contextlib.ExitStack() as es:
            wb = self.load_w(es, "scwb", l, C_SB, 512)
            wc = self.load_w(es, "scwc", l, C_SC, 512)
            wx = self.load_w(es, "scwx", l, C_SX, 512)
            ub = self.sb(es, "scu", [128, T + 3], F32)
            bb = self.sb(es, "scb", [128, T], F32)
            hts = [self.sb(es, "scht%d" % k, [128, 8, 512], BF) for k in range(2)]
            cs = [self.sb(es, "sccs%d" % k, [128, 512], F32) for k in range(2)]
            acc = [self.sb(es, "scacc%d" % k, [128, 512], F32) for k in range(2)]
            ys = [self.sb(es, "scy%d" % k, [128, 512], BF) for k in range(2)]
            self.V("pool", "memset", [], [ub], ub[:], 0.0)
            n = 0
            for j in range(4):
                for ti, (t0, N) in enumerate(TILES):
                    ht = hts[n % 2]
                    c_ = cs[n % 2]
                    n += 1
                    self.load_ht(ht, ti)
                    base = t0 + 1 if ti == 0 else t0 + 2
                    pb, pc, px = self.psb(), self.psb(), self.psb()
                    self.proj(pb, wb, j * 128, ht, N)
                    self.proj(pc, wc, j * 128, ht, N)
                    self.proj(px, wx, j * 128, ht, N)
                    self.act(c_[:, 0:N], pc[:, 0:N], AF.Copy, [pc], [c_])
                    self.V("dve", "tensor_tensor", [c_, px], [ub], out=ub[:, base:base + N], in0=c_[:, 0:N], in1=px[:, 0:N], op=ALU.mult)
                    self.act(bb[:, t0:t0 + N], pb[:, 0:N], AF.Copy, [pb], [bb])
                for ti, (t0, N) in enumerate(TILES):
                    base = t0 + 1 if ti == 0 else t0 + 2
                    a = acc[ti % 2]
                    y = ys[ti % 2]
                    self.V("dve", "tensor_scalar", [ub, P], [a], out=a[:, 0:N], in0=ub[:, base - 1:base - 1 + N], scalar1=P[:, j * 3:j * 3 + 1],
                           scalar2=0.0, op0=ALU.mult, op1=ALU.add)
                    for k in (1, 2):
                        self.V("dve", "scalar_tensor_tensor", [ub, P, a], [a], out=a[:, 0:N], in0=ub[:, base - 1 + k:base - 1 + k + N],
                               scalar=P[:, j * 3 + k:j * 3 + k + 1], in1=a[:, 0:N], op0=ALU.mult, op1=ALU.add)
                    self.V("pool", "tensor_tensor", [a, bb], [y], out=y[:, 0:N], in0=a[:, 0:N], in1=bb[:, t0:t0 + N], op=ALU.mult)
                    S.dma("sp", self.Y[3, j, :, t0:t0 + N], y[:, 0:N], [y], [Tl(None, self.Yt[3][ti])])
            S.barrier()

    def phase_conv(self, l):
        S = self.S
        P = self.P
        onesf = self.cst[:, 128:256]
        with contextlib.ExitStack() as es:
            wa = self.load_w(es, "cvwa", l, C_CVA, 512)
            wg = self.load_w(es, "cvwg", l, C_CVG, 512)
            vb = self.sb(es, "cvv", [128, 4, T + 45], BF)
            hts = [self.sb(es, "cvht%d" % k, [128, 8, 512], BF) for k in range(2)]
            sg = [self.sb(es, "cvsg%d" % k, [128, 512], F32) for k in range(2)]
            self.V("pool", "memset", [], [vb], vb[:], 0.0)
            n = 0
            for ti, (t0, N) in enumerate(TILES):
                ht = hts[ti % 2]
                self.load_ht(ht, ti)
                base = t0 + 15 if ti == 0 else t0 + 30
                for j in range(4):
                    pa, pg = self.psb(), self.psb()
                    self.proj(pa, wa, j * 128, ht, N)
                    self.proj(pg, wg, j * 128, ht, N)
                    s_ = sg[n % 2]
                    n += 1
                    self.act(s_[:, 0:N], pg[:, 0:N], AF.Sigmoid, [pg], [s_])
                    self.V("dve", "tensor_tensor", [s_, pa], [vb], out=vb[:, j, base:base + N], in0=pa[:, 0:N], in1=s_[:, 0:N], op=ALU.mult)
            DG = self.sb(es, "cvDG", [128, 124, 128], BF)
            for jk in range(124):
                self.V("pool", "tensor_scalar", [self.identb, P], [DG], out=DG[:, jk, :], in0=self.identb[:], scalar1=P[:, 12 + jk:13 + jk], scalar2=0.0,
                       op0=ALU.mult, op1=ALU.add)
            ca = [self.sb(es, "cvca%d" % k, [128, 4, 512], F32) for k in range(2)]
            cp = self.sb(es, "cvcp", [128, 512], F32)
            sq = self.sb(es, "cvsq", [128, 4, 512], F32)
            mean = self.sb(es, "cvmean", [128, 512], F32)
            tmp = self.sb(es, "cvtmp", [128, 512], F32)
            rstd = self.sb(es, "cvrstd", [128, 512], F32)
            dd = [self.sb(es, "cvd%d" % k, [128, 512], F32) for k in range(2)]
            ys = [self.sb(es, "cvy%d" % k, [128, 4, 512], BF) for k in range(2)]
            for ti, (t0, N) in enumerate(TILES):
                base = t0 + 15 if ti == 0 else t0 + 30
                a = ca[ti % 2]
                for j in range(4):
                    pc = self.psb()
                    for k in range(31):
                        self.mm(pc[:, 0:N], DG[:, j * 31 + k, :], vb[:, j, base - 15 + k:base - 15 + k + N], k == 0, k == 30, [DG, vb], [pc])
                    self.act(a[:, j, 0:N], pc[:, 0:N], AF.Identity, [pc, P], [a], bias=P[:, 136 + j:137 + j])
                    self.act(sq[:, j, 0:N], a[:, j, 0:N], AF.Square, [a], [sq])
                p1, p2 = self.psb(), self.psb()
                for j in range(4):
                    self.mm(p1[:, 0:N], onesf, a[:, j, 0:N], j == 0, j == 3, [self.cst, a], [p1])
                for j in range(4):
                    self.mm(p2[:, 0:N], onesf, sq[:, j, 0:N], j == 0, j == 3, [self.cst, sq], [p2])
                self.act(mean[:, 0:N], p1[:, 0:N], AF.Copy, [p1], [mean], scale=1.0 / W)
                self.V("pool", "tensor_tensor", [mean], [tmp], out=tmp[:, 0:N], in0=mean[:, 0:N], in1=mean[:, 0:N], op=ALU.mult)
                self.V("dve", "scalar_tensor_tensor", [p2, tmp], [tmp], out=tmp[:, 0:N], in0=p2[:, 0:N], scalar=1.0 / W, in1=tmp[:, 0:N],
                       op0=ALU.mult, op1=ALU.subtract)
                self.rstd_from(tmp[:, 0:N], 1.0, rstd, tmp, N, [tmp])
                y = ys[ti % 2]
                for j in range(4):
                    d = dd[j % 2]
                    self.V("dve", "tensor_tensor", [a, mean], [d], out=d[:, 0:N], in0=a[:, j, 0:N], in1=mean[:, 0:N], op=ALU.subtract)
                    self.V("pool", "tensor_tensor", [d, rstd], [d], out=d[:, 0:N], in0=d[:, 0:N], in1=rstd[:, 0:N], op=ALU.mult)
                    self.V("dve", "tensor_scalar", [d, P], [d], out=d[:, 0:N], in0=d[:, 0:N], scalar1=P[:, 140 + j:141 + j], scalar2=P[:, 144 + j:145 + j],
                           op0=ALU.mult, op1=ALU.add)
                    self.act(y[:, j, 0:N], d[:, 0:N], AF.Silu, [d], [y])
                S.dma("sp", self.Y[2, :, :, t0:t0 + N].rearrange("j p n -> p j n"), y[:, :, 0:N], [y], [Tl(None, self.Yt[2][ti])])
            S.barrier()

    def rope(self, ps, raw, cos, sin, t1, t2, dst_ap, dst_t, N, rot_ps):
        self.act(raw[:, 0:N], ps[:, 0:N], AF.Copy, [ps], [raw])
        self.mm(rot_ps[:, 0:N], self.Rb[:], raw[:, 0:N], True, True, [self.Rb, raw], [rot_ps])
        self.V("pool", "tensor_tensor", [raw, cos], [t1], out=t1[:, 0:N], in0=raw[:, 0:N], in1=cos[:, 0:N], op=ALU.mult)
        self.V("dve", "tensor_tensor", [rot_ps, sin], [t2], out=t2[:, 0:N], in0=rot_ps[:, 0:N], in1=sin[:, 0:N], op=ALU.mult)
        self.V("pool", "tensor_tensor", [t1, t2], [dst_t], out=dst_ap, in0=t1[:, 0:N], in1=t2[:, 0:N], op=ALU.add)

    def phase_attn(self, l):
        S = self.S
        P = self.P
        i = self.i
        onesf = self.cst[:, 128:256]
        B = self.banks
        with contextlib.ExitStack() as es:
            wq = self.load_w(es, "dawq", l, C_DQ, 512)
            wk = self.load_w(es, "dawk", l, C_DK, 512)
            wv = self.load_w(es, "dawv", l, C_DV, 512)
            KT = self.sb(es, "daKT", [128, 4, T], BF)
            Vt = self.sb(es, "daV", [128, NCH, 512], BF)
            hts = [self.sb(es, "daht%d" % k, [128, 8, 512], BF) for k in range(2)]
            cos = [self.sb(es, "dacos%d" % k, [128, 512], F32) for k in range(2)]
            sin = [self.sb(es, "dasin%d" % k, [128, 512], F32) for k in range(2)]
            raw = [self.sb(es, "daraw%d" % k, [128, 512], BF) for k in range(2)]
            t1 = [self.sb(es, "dat1%d" % k, [128, 512], F32) for k in range(2)]
            t2 = [self.sb(es, "dat2%d" % k, [128, 512], F32) for k in range(2)]
            n = 0
            for ti, (t0, N) in enumerate(TILES):
                ht = hts[ti % 2]
                self.load_ht(ht, ti)
                S.dma("sp", cos[ti % 2][:, 0:N], i["rope"][0, :, t0:t0 + N], [], [cos[ti % 2]])
                S.dma("sp", sin[ti % 2][:, 0:N], i["rope"][1, :, t0:t0 + N], [], [sin[ti % 2]])
                for h in range(4):
                    ps, rp = self.psb(), self.psb()
                    self.proj(ps, wk, h * 128, ht, N)
                    self.rope(ps, raw[n % 2], cos[ti % 2], sin[ti % 2], t1[n % 2], t2[n % 2], KT[:, h, t0:t0 + N], KT, N, rp)
                    n += 1
                for cc in range(N // 128):
                    ps = self.psb()
                    for kc in range(8):
                        self.mm(ps[:, :], ht[:, kc, cc * 128:(cc + 1) * 128], wv[:, kc, :], kc == 0, kc == 7, [ht, wv], [ps])
                    self.act(Vt[:, t0 // 128 + cc, :], ps[:, :], AF.Copy, [ps], [Vt])
            QT = [self.sb(es, "daQT%d" % k, [128, 512], BF) for k in range(2)]
            QZ = [[self.sb(es, "daQZ%d_%d" % (k, c), [128, 512], BF) for c in range(2)] for k in range(2)]
            for k in range(2):
                for c in range(2):
                    self.V("pool", "memset", [], [QZ[k][c]], QZ[k][c][:], 0.0)
            pT = [self.sb(es, "dapT%d" % k, [128, 512], BF) for k in range(4)]
            rd = [self.sb(es, "dard%d" % k, [128, 512], F32) for k in range(2)]
            rp_ = [self.sb(es, "darp%d" % k, [128, 512], F32) for k in range(2)]
            rinv = self.sb(es, "darinv", [128, 512], F32)
            Oc = [self.sb(es, "daOc%d" % k, [128, 512], F32) for k in range(2)]
            o = self.sb(es, "dao", [128, 512], F32)
            sq = self.sb(es, "dasq", [128, 512], F32)
            tmp = self.sb(es, "datmp", [128, 512], F32)
            rstd = self.sb(es, "darstd", [128, 512], F32)
            ys = [self.sb(es, "day%d" % k, [128, 4, 512], BF) for k in range(2)]
            it = 0
            for ti, (t0, N) in enumerate(TILES):
                ht = hts[ti % 2]
                self.load_ht(ht, ti)
                S.dma("sp", cos[ti % 2][:, 0:N], i["rope"][0, :, t0:t0 + N], [], [cos[ti % 2]])
                S.dma("sp", sin[ti % 2][:, 0:N], i["rope"][1, :, t0:t0 + N], [], [sin[ti % 2]])
                nk = 2 if ti == 0 else NCH
                y = ys[ti % 2]
                for h in range(4):
                    qt = QT[h % 2]
                    self.proj(B[7], wq, h * 128, ht, N)
                    self.rope(B[7], raw[n % 2], cos[ti % 2], sin[ti % 2], t1[n % 2], t2[n % 2], qt[:, 0:N], qt, N, B[2])
                    n += 1
                    qz = QZ[h % 2]
                    self.V("pool", "tensor_copy", [qt], [qz[0]], out=qz[0][0:64, 0:N], in_=qt[0:64, 0:N])
                    self.V("dve", "tensor_copy", [qt], [qz[1]], out=qz[1][64:128, 0:N], in_=qt[64:128, 0:N])
                    its = [(c, kc) for c in range(2) for kc in range(nk)]
                    slots = {}
                    def qk(j):
                        c, kc = its[j]
                        sps = B[2 + it_base[0] % 3]
                        p = pT[it_base[0] % 4]
                        it_base[0] += 1
                        slots[j] = (sps, p)
                        p0 = c * 64
                        self.mm(sps[:, 0:N], KT[:, h, kc * 128:(kc + 1) * 128], qz[c][:, 0:N], True, True, [KT, qz[c]], [sps])
                    it_base = [it]
                    qk(0)
                    if len(its) > 1:
                        qk(1)
                    for j, (c, kc) in enumerate(its):
                        sps, p = slots.pop(j)
                        oacc = B[c]
                        self.act(p[:, 0:N], sps[:, 0:N], AF.Exp, [sps], [p], scale=0.125)
                        if j + 2 < len(its):
                            qk(j + 2)
                        self.mm(oacc[:, 0:N], Vt[:, kc, h * 128:(h + 1) * 128], p[:, 0:N], kc == 0, kc == nk - 1, [Vt, p], [oacc])
                        rsb = B[5 + c]
                        self.mm(rsb[:, 0:N], self.onesb[:], p[:, 0:N], kc == 0, kc == nk - 1, [self.onesb, p], [rsb])
                        if kc == nk - 1:
                            self.V("dve", "reciprocal", [rsb], [rinv], out=rinv[:, 0:N], in_=rsb[:, 0:N])
                            self.V("dve", "tensor_tensor", [oacc, rinv], [Oc[c]], out=Oc[c][:, 0:N], in0=oacc[:, 0:N], in1=rinv[:, 0:N], op=ALU.mult)
                    it = it_base[0]
                    self.V("dve", "scalar_tensor_tensor", [Oc[0], Oc[1], P], [o], out=o[:, 0:N], in0=Oc[1][:, 0:N], scalar=P[:, 153:154], in1=Oc[0][:, 0:N],
                           op0=ALU.mult, op1=ALU.add)
                    self.act(sq[:, 0:N], o[:, 0:N], AF.Square, [o], [sq])
                    self.mm(B[7][:, 0:N], onesf, sq[:, 0:N], True, True, [self.cst, sq], [B[7]])
                    self.rstd_from(B[7][:, 0:N], 1.0 / 128, rstd, tmp, N, [B[7]])
                    self.V("dve", "scalar_tensor_tensor", [o, rstd, P], [y], out=y[:, h, 0:N], in0=o[:, 0:N], scalar=P[:, 154:155], in1=rstd[:, 0:N],
                           op0=ALU.mult, op1=ALU.mult)
                S.dma("sp", self.Y[1, :, :, t0:t0 + N].rearrange("j p n -> p j n"), y[:, :, 0:N], [y], [Tl(None, self.Yt[1][ti])])
            S.barrier()

    def phase_hgrn(self, l):
        S = self.S
        B = self.banks
        with contextlib.ExitStack() as es:
            rot = [2]
            def rb():
                b = B[rot[0]]
                rot[0] = 2 + (rot[0] - 1) % 6
                return b
            sh = {k: self.sb(es, "hgs_" + k, [128, 512], F32) for k in ("osum", "sq", "tmp", "rstd", "sgg", "y1")}
            sh["ys"] = [self.sb(es, "hgys%d" % k, [128, 512], BF) for k in range(2)]
            sh["n"] = 0
            chains = []
            for k in range(2):
                c = {"k": k}
                c["OF"] = self.sb(es, "hgOF%d" % k, [128, T], F32)
                c["S32"] = self.sb(es, "hgS%d" % k, [128, 128], F32)
                c["Sbf"] = [self.sb(es, "hgSb%d_%d" % (k, j), [128, 128], BF) for j in range(2)]
                c["ht"] = self.sb(es, "hght%d" % k, [128, 8, 512], BF)
                c["w"] = [self.sb(es, "hgw%d_%d" % (k, j), [128, 8, 128], BF) for j in range(4)]
                for nm in ("q32", "ee", "ff", "lf", "kk", "pre", "bb", "d3", "d2", "d3a"):
                    c[nm] = self.sb(es, "hg%s%d" % (nm, k), [128, 512], F32)
                for nm in ("qE1", "qE3", "kE4", "kE2", "qE3b", "kE4b"):
                    c[nm] = self.sb(es, "hg%s%d" % (nm, k), [128, 512], BF)
                c["iT"] = self.sb(es, "hgiT%d" % k, [128, 4, 128], BF)
                c["kT"] = self.sb(es, "hgkT%d" % k, [128, 4, 128], BF)
                c["AM"] = [self.sb(es, "hgAM%d_%d" % (k, j), [128, 128], BF) for j in range(2)]
                c["a1"] = [self.sb(es, "hga1%d_%d" % (k, j), [128, 128], F32) for j in range(2)]
                c["a2"] = [self.sb(es, "hga2%d_%d" % (k, j), [128, 128], F32) for j in range(2)]
                c["ops"] = B[k]
                chains.append(c)
            for pair in ((0, 1), (2, 3)):
                for d in range(2):
                    gens = [self.hgrn_chain(l, h, d, chains[k], sh, rb) for k, h in enumerate(pair)]
                    live = list(gens)
                    while live:
                        for g in list(live):
                            try:
                                next(g)
                            except StopIteration:
                                live.remove(g)
            S.barrier()

    def hgrn_chain(self, l, h, d, c, sh, rb):
        S = self.S
        P = self.P
        onesf = self.cst[:, 128:256]
        w = c["w"]
        OF, S32, Sbf, ht, ops = c["OF"], c["S32"], c["Sbf"], c["ht"], c["ops"]
        q32, ee, ff, lf, kk, pre, bb, d3, d2, d3a = (c[n] for n in ("q32", "ee", "ff", "lf", "kk", "pre", "bb", "d3", "d2", "d3a"))
        qE1, qE3, kE4, kE2, qE3b, kE4b = (c[n] for n in ("qE1", "qE3", "kE4", "kE2", "qE3b", "kE4b"))
        iT, kT = c["iT"], c["kT"]
        E1, E3, E3b, E4b, E2, E4 = ee, ff, lf, d3, d2, d3a
        cols = (C_HQ, C_HFF if d == 0 else C_HFB, C_HI, C_HG)
        for k in range(4):
            S.dma("pool", w[k][:], self.i["w_in"][l, :, cols[k] + h * 128:cols[k] + (h + 1) * 128].rearrange("(kc p) n -> p kc n", p=128), [], [w[k]])
        self.V("pool", "memset", [], [S32], S32[:], 0.0)
        self.V("pool", "memset", [], [Sbf[0]], Sbf[0][:], 0.0)
        cur = 0
        am_i = 0
        lbc = self.LB[:, l, d * 4 + h:d * 4 + h + 1]
        omc = self.OML[:, l, d * 4 + h:d * 4 + h + 1]
        order = list(range(9)) if d == 0 else [0] + list(range(8, 0, -1))
        mask = self.cst[:, 384:512] if d == 0 else self.cst[:, 512:640]
        masko = self.cst[:, 640:768] if d == 0 else self.cst[:, 768:896]
        for ti in order:
            t0, N = TILES[ti]
            nb = N // 128
            nch = N // 64
            self.load_ht(ht, ti)
            pq, pf = rb(), rb()
            self.proj(pq, w[0], 0, ht, N)
            self.proj(pf, w[1], 0, ht, N)
            self.act(q32[:, 0:N], pq[:, 0:N], AF.Copy, [pq], [q32])
            self.act(ee[:, 0:N], pf[:, 0:N], AF.Exp, [pf], [ee], scale=-1.0)
            self.V("dve", "tensor_scalar", [ee], [ee], out=ee[:, 0:N], in0=ee[:, 0:N], scalar1=1.0, scalar2=1.0, op0=ALU.add, op1=ALU.mult)
            self.V("dve", "reciprocal", [ee], [ee], out=ee[:, 0:N], in_=ee[:, 0:N])
            self.V("dve", "tensor_scalar", [ee, self.LB, self.OML], [ff], out=ff[:, 0:N], in0=ee[:, 0:N], scalar1=omc, scalar2=lbc, op0=ALU.mult, op1=ALU.add)
            self.act(lf[:, 0:N], ff[:, 0:N], AF.Ln, [ff], [lf])
            self.V("pool", "tensor_scalar", [ff], [kk], out=kk[:, 0:N], in0=ff[:, 0:N], scalar1=-1.0, scalar2=1.0, op0=ALU.mult, op1=ALU.add)
            self.V("dve", "tensor_tensor_scan", [self.seg, lf], [pre], out=pre[:, 0:N], data0=self.seg[:, 0:N], data1=lf[:, 0:N], initial=0.0, op0=ALU.mult, op1=ALU.add)
            v3 = lambda t_: t_[:, 0:N].rearrange("p (c s) -> p c s", s=64)
            v32 = lambda t_: t_[:, 0:N].rearrange("p (c s) -> p c s", s=32)
            bc = lambda t_, col: v3(t_)[:, :, col:col + 1].to_broadcast([128, nch, 64])
            if d == 0:
                b_ = pre
                cend = 63
            else:
                b_ = bb
                cend = 0
                self.V("dve", "tensor_tensor", [lf, pre], [bb], out=bb[:, 0:N], in0=lf[:, 0:N], in1=pre[:, 0:N], op=ALU.subtract)
                self.V("dve", "tensor_tensor", [bb, pre], [bb], out=v3(bb), in0=v3(bb), in1=bc(pre, 63), op=ALU.add)
            yield
            self.V("dve", "tensor_tensor", [b_], [d3], out=v3(d3), in0=v3(b_), in1=bc(b_, 32), op=ALU.subtract)
            self.V("pool", "tensor_tensor", [b_], [d2], out=v3(d2), in0=v3(b_), in1=bc(b_, cend), op=ALU.subtract)
            self.V("dve", "tensor_tensor", [b_], [d3a], out=v32(d3a), in0=v32(b_), in1=v32(b_)[:, :, 16:17].to_broadcast([128, 2 * nch, 32]), op=ALU.subtract)
            self.act(E1[:, 0:N], b_[:, 0:N], AF.Exp, [b_], [E1])
            self.act(E2[:, 0:N], d2[:, 0:N], AF.Exp, [d2], [E2], scale=-1.0)
            self.act(E3[:, 0:N], d3a[:, 0:N], AF.Exp, [d3a], [E3])
            self.act(E4[:, 0:N], d3a[:, 0:N], AF.Exp, [d3a], [E4], scale=-1.0)
            self.act(E3b[:, 0:N], d3[:, 0:N], AF.Exp, [d3], [E3b])
            self.act(E4b[:, 0:N], d3[:, 0:N], AF.Exp, [d3], [E4b], scale=-1.0)
            self.V("dve", "tensor_tensor", [q32, E1], [qE1], out=qE1[:, 0:N], in0=q32[:, 0:N], in1=E1[:, 0:N], op=ALU.mult)
            self.V("pool", "tensor_tensor", [q32, E3], [qE3], out=qE3[:, 0:N], in0=q32[:, 0:N], in1=E3[:, 0:N], op=ALU.mult)
            self.V("dve", "tensor_tensor", [kk, E4], [kE4], out=kE4[:, 0:N], in0=kk[:, 0:N], in1=E4[:, 0:N], op=ALU.mult)
            self.V("pool", "tensor_tensor", [kk, E2], [kE2], out=kE2[:, 0:N], in0=kk[:, 0:N], in1=E2[:, 0:N], op=ALU.mult)
            self.V("dve", "tensor_tensor", [q32, E3b], [qE3b], out=qE3b[:, 0:N], in0=q32[:, 0:N], in1=E3b[:, 0:N], op=ALU.mult)
            self.V("pool", "tensor_tensor", [kk, E4b], [kE4b], out=kE4b[:, 0:N], in0=kk[:, 0:N], in1=E4b[:, 0:N], op=ALU.mult)
            qz, kz = (slice(0, 32), slice(32, 64)) if d == 0 else (slice(32, 64), slice(0, 32))
            self.V("dve", "memset", [qE3b], [qE3b], v3(qE3b)[:, :, qz], 0.0)
            self.V("pool", "memset", [kE4b], [kE4b], v3(kE4b)[:, :, kz], 0.0)
            yield
            for cc in range(nb):
                pi = rb()
                for kc in range(8):
                    self.mm(pi[:, 0:128], ht[:, kc, cc * 128:(cc + 1) * 128], w[2][:, kc, :], kc == 0, kc == 7, [ht, w[2]], [pi])
                self.act(iT[:, cc, :], pi[:, 0:128], AF.Copy, [pi], [iT])
                pt = rb()
                ptv = pt[:].bitcast(BF)
                self.tr(ptv[:, 0:128], kE2[:, cc * 128:(cc + 1) * 128], self.identb[:], [kE2, self.identb], [pt])
                self.act(kT[:, cc, :], ptv[:, 0:128], AF.Copy, [pt], [kT])
            yield
            blks = list(range(nb)) if d == 0 else list(range(nb - 1, -1, -1))
            for blk in blks:
                pa = rb()
                bs = slice(blk * 128, (blk + 1) * 128)
                self.mm(pa[:, 0:128], kE4[:, bs], qE3[:, bs], True, True, [kE4, qE3], [pa])
                pb_ = rb()
                self.mm(pb_[:, 0:128], kE4b[:, bs], qE3b[:, bs], True, True, [kE4b, qE3b], [pb_])
                am = c["AM"][am_i % 2]
                a1 = c["a1"][am_i % 2]
                a2 = c["a2"][am_i % 2]
                am_i += 1
                self.V("dve", "tensor_tensor", [pa, self.cst], [a1], out=a1[:], in0=pa[:, 0:128], in1=mask, op=ALU.mult)
                self.V("dve", "tensor_tensor", [pb_, self.cst], [a2], out=a2[:], in0=pb_[:, 0:128], in1=masko, op=ALU.mult)
                self.V("pool", "tensor_tensor", [a1, a2], [am], out=am[:], in0=a1[:], in1=a2[:], op=ALU.add)
                for ch in ((0, 1) if d == 0 else (1, 0)):
                    p0 = ch * 64
                    c0 = blk * 128 + p0
                    self.mm(ops[:, c0:c0 + 64], Sbf[cur][:], qE1[:, c0:c0 + 64], True, False, [Sbf[cur], qE1], [ops])
                    self.mm(ops[:, c0:c0 + 64], iT[p0:p0 + 64, blk, :], am[p0:p0 + 64, p0:p0 + 64], False, True, [iT, am], [ops])
                    pS = rb()
                    self.mm(pS[:, 0:128], kT[p0:p0 + 64, blk, :], iT[p0:p0 + 64, blk, :], True, True, [kT, iT], [pS])
                    ce = c0 + cend
                    self.V("dve", "scalar_tensor_tensor", [S32, E1, pS], [S32], out=S32[:], in0=S32[:], scalar=E1[:, ce:ce + 1], in1=pS[:, 0:128],
                           op0=ALU.mult, op1=ALU.add)
                    cur = 1 - cur
                    self.act(Sbf[cur][:], S32[:], AF.Copy, [S32], [Sbf[cur]])
                    yield
            if d == 0:
                self.act(OF[:, t0:t0 + N], ops[:, 0:N], AF.Copy, [ops], [OF])
            else:
                osum, sq, tmp, rstd, sgg, y1 = (sh[n_] for n_ in ("osum", "sq", "tmp", "rstd", "sgg", "y1"))
                self.V("dve", "tensor_tensor", [OF, ops], [osum], out=osum[:, 0:N], in0=OF[:, t0:t0 + N], in1=ops[:, 0:N], op=ALU.add)
                self.act(sq[:, 0:N], osum[:, 0:N], AF.Square, [osum], [sq])
                pss = rb()
                self.mm(pss[:, 0:N], onesf, sq[:, 0:N], True, True, [self.cst, sq], [pss])
                self.rstd_from(pss[:, 0:N], 1.0 / 128, rstd, tmp, N, [pss])
                pg = rb()
                self.proj(pg, w[3], 0, ht, N)
                self.act(sgg[:, 0:N], pg[:, 0:N], AF.Sigmoid, [pg], [sgg])
                self.V("dve", "scalar_tensor_tensor", [osum, rstd, P], [y1], out=y1[:, 0:N], in0=osum[:, 0:N], scalar=P[:, 148 + h:149 + h], in1=rstd[:, 0:N],
                       op0=ALU.mult, op1=ALU.mult)
                y = sh["ys"][sh["n"] % 2]
                sh["n"] += 1
                self.V("pool", "tensor_tensor", [y1, sgg], [y], out=y[:, 0:N], in0=y1[:, 0:N], in1=sgg[:, 0:N], op=ALU.mult)
                S.dma("sp", self.Y[0, h, :, t0:t0 + N], y[:, 0:N], [y], [Tl(None, self.Yt[0][ti])])
            yield

    def phase_merge(self, l):
        S = self.S
        i = self.i
        with contextlib.ExitStack() as es:
            wg = self.sb(es, "mgwg", [128, 8, 4096], BF)
            for k in range(4):
                S.dma("pool", wg[:, :, k * 1024:(k + 1) * 1024], i["w_in"][l, :, C_GATE + k * 1024:C_GATE + (k + 1) * 1024].rearrange("(kc p) n -> p kc n", p=128), [], [wg])
            wb = self.sb(es, "mgwb", [128, 4, 4, 1024], BF)
            for k in range(4):
                S.dma("pool", wb[:, k, :, :], i["w_branch"][l, k].rearrange("(cc p) n -> p cc n", p=128), [], [wb])
            ht = self.sb(es, "mght", [128, 8, 512], BF)
            Yk = [self.sb(es, "mgY%d" % k, [128, 4, 512], BF) for k in range(4)]
            sg = [self.sb(es, "mgsg%d" % k, [128, 512], F32) for k in range(2)]
            tmp = [self.sb(es, "mgtmp%d" % k, [128, 512], F32) for k in range(2)]
            macc = [self.sb(es, "mgacc%d" % k, [128, 512], F32) for k in range(2)]
            mT = [self.sb(es, "mgmT%d" % k, [128, 8, 512], BF) for k in range(2)]
            n = 0
            for ti, (t0, N) in enumerate(TILES):
                self.load_ht(ht, ti)
                for k in range(4):
                    S.dma("sp", Yk[k][:, :, 0:N], self.Y[k, :, :, t0:t0 + N].rearrange("j p n -> p j n"), [Tl(None, self.Yt[k][ti])], [Yk[k]])
                m = mT[ti % 2]
                for nch in range(8):
                    ma = macc[nch % 2]
                    for k in range(4):
                        pg, pp = self.psb(), self.psb()
                        self.proj(pg, wg, k * 1024 + nch * 128, ht, N)
                        for cc in range(4):
                            self.mm(pp[:, 0:N], wb[:, k, cc, nch * 128:(nch + 1) * 128], Yk[k][:, cc, 0:N], cc == 0, cc == 3, [wb, Yk[k]], [pp])
                        s_ = sg[n % 2]
                        t_ = tmp[n % 2]
                        n += 1
                        self.act(s_[:, 0:N], pg[:, 0:N], AF.Sigmoid, [pg], [s_])
                        if k == 0:
                            self.V("dve", "tensor_tensor", [pp, s_], [ma], out=ma[:, 0:N], in0=pp[:, 0:N], in1=s_[:, 0:N], op=ALU.mult)
                        else:
                            self.V("dve", "tensor_tensor", [pp, s_], [t_], out=t_[:, 0:N], in0=pp[:, 0:N], in1=s_[:, 0:N], op=ALU.mult)
                            self.V("pool", "tensor_tensor", [ma, t_], [ma], out=ma[:, 0:N], in0=ma[:, 0:N], in1=t_[:, 0:N], op=ALU.add)
                    self.V("pool", "tensor_copy", [ma], [m], out=m[:, nch, 0:N], in_=ma[:, 0:N])
                S.dma("sp", self.MT[:, :, t0:t0 + N].rearrange("k p n -> p k n"), m[:, :, 0:N], [m], [Tl(None, self.MTt[ti])])
            S.barrier()
        with contextlib.ExitStack() as es:
            wo = self.sb(es, "mgwo", [128, 8, 1024], BF)
            S.dma("pool", wo[:], i["w_out"][l].rearrange("(kc p) n -> p kc n", p=128), [], [wo])
            mts = [self.sb(es, "mgmt%d" % k, [128, 8, 512], BF) for k in range(2)]
            self.residual_setup(es, 2)
            for ti, (t0, N) in enumerate(TILES):
                mt = mts[ti % 2]
                S.dma("sp", mt[:, :, 0:N], self.MT[:, :, t0:t0 + N].rearrange("k p n -> p k n"), [Tl(None, self.MTt[ti])], [mt])
                for cc in range(N // 128):
                    c = t0 // 128 + cc
                    halves = []
                    for half in range(2):
                        po = self.psb()
                        for kc in range(8):
                            self.mm(po[:, :], mt[:, kc, cc * 128:(cc + 1) * 128], wo[:, kc, half * 512:(half + 1) * 512], kc == 0, kc == 7, [mt, wo], [po])
                        halves.append(po)
                    self.residual(c, lambda half: halves[half][:, :], halves)
            S.barrier()

    def residual_setup(self, es, modidx):
        S = self.S
        self.rmod = [self.sb(es, "rsmod%d" % k, [128, D], F32) for k in range(2)]
        for k in range(2):
            S.dma("sp", self.rmod[k][:], self.MODR[k, :, modidx * D:(modidx + 1) * D], [Tl(None, self.MODt)], [self.rmod[k]])
        self.rx = [self.sb(es, "rsx%d" % k, [128, D], F32) for k in range(2)]
        self.rtmp = [self.sb(es, "rstmp%d" % k, [128, D], F32) for k in range(2)]

    def residual(self, c, delta_ap, delta_tiles):
        S = self.S
        lat = 0 if c >= 2 else 1
        x = self.rx[c % 2]
        t = self.rtmp[c % 2]
        xt = Tl(None, self.Xt[c])
        S.dma("sp", x[:], self.X[c * 128:(c + 1) * 128, :], [xt], [x])
        for half in range(2):
            hs = slice(half * 512, (half + 1) * 512)
            self.V("dve", "tensor_tensor", [delta_tiles[half], self.rmod[lat]], [t], out=t[:, hs], in0=delta_ap(half), in1=self.rmod[lat][:, hs], op=ALU.mult)
        self.V("dve", "tensor_tensor", [x, t], [x], out=x[:], in0=x[:], in1=t[:], op=ALU.add)
        S.dma("sp", self.X[c * 128:(c + 1) * 128, :], x[:], [x], [xt])

    def route(self, c, t, Bm, h32, h32T, wr, R):
        P = self.P
        self.V("pool", "tensor_tensor", [t, Bm], [h32], out=h32[:], in0=t[:], in1=Bm[:], op=ALU.add)
        for g in range(2):
            ps = self.psb()
            for k in range(4):
                kk = g * 4 + k
                self.tr(ps[:, k * 128:(k + 1) * 128], h32[:, kk * 128:(kk + 1) * 128], self.cst[:, 0:128], [h32, self.cst], [ps])
            self.act(h32T[:, g * 4:(g + 1) * 4, :], ps[:].rearrange("p (k n) -> p k n", k=4), AF.Copy, [ps], [h32T])
        pl = self.psb()
        for kc in range(8):
            self.mm(pl[:, 0:36], h32T[:, kc, :], wr[:, kc, :], kc == 0, kc == 7, [h32T, wr], [pl])
        dv = lambda name, w_, **kw: self.V("dve", name, [R, P] + w_[1:], [w_[0]], **kw)
        self.V("dve", "tensor_tensor", [pl, P], [R], out=R[:, 0:36], in0=pl[:, 0:36], in1=P[:, 416:452], op=ALU.add)
        RR = [R]
        dv("tensor_reduce", RR, out=R[:, 36:37], in_=R[:, 0:4], axis=AX.X, op=ALU.max)
        dv("tensor_scalar", RR, out=R[:, 37:38], in0=R[:, 36:37], scalar1=-1.0, scalar2=0.0, op0=ALU.mult, op1=ALU.add)
        self.act(R[:, 44:48], R[:, 0:4], AF.Exp, [R], [R], bias=R[:, 37:38], accum_out=R[:, 38:39])
        dv("reciprocal", RR, out=R[:, 39:40], in_=R[:, 38:39])
        dv("tensor_scalar", RR, out=R[:, 40:44], in0=R[:, 0:4], scalar1=R[:, 36:37], scalar2=1.0, op0=ALU.is_equal, op1=ALU.mult)
        dv("tensor_tensor", RR, out=R[:, 48:80].rearrange("p (g e) -> p g e", g=4), in0=R[:, 4:36].rearrange("p (g e) -> p g e", g=4),
           in1=R[:, 40:44].unsqueeze(2).to_broadcast([128, 4, 8]), op=ALU.mult)
        dv("tensor_reduce", RR, out=R[:, 80:88], in_=R[:, 48:80].rearrange("p (g e) -> p e g", g=4), axis=AX.X, op=ALU.add)
        dv("tensor_reduce", RR, out=R[:, 88:89], in_=R[:, 80:88], axis=AX.X, op=ALU.max)
        dv("tensor_scalar", RR, out=R[:, 89:97], in0=R[:, 80:88], scalar1=R[:, 88:89], scalar2=1.0, op0=ALU.is_equal, op1=ALU.mult)
        dv("scalar_tensor_tensor", RR, out=R[:, 97:105], in0=R[:, 89:97], scalar=-1e30, in1=R[:, 80:88], op0=ALU.mult, op1=ALU.add)
        dv("tensor_reduce", RR, out=R[:, 105:106], in_=R[:, 97:105], axis=AX.X, op=ALU.max)
        dv("tensor_scalar", RR, out=R[:, 106:114], in0=R[:, 97:105], scalar1=R[:, 105:106], scalar2=1.0, op0=ALU.is_equal, op1=ALU.mult)
        dv("tensor_tensor", RR, out=R[:, 114:115], in0=R[:, 105:106], in1=R[:, 88:89], op=ALU.subtract)
        self.act(R[:, 115:116], R[:, 114:115], AF.Exp, [R], [R])
        dv("tensor_scalar", RR, out=R[:, 116:117], in0=R[:, 115:116], scalar1=1.0, scalar2=1.0, op0=ALU.add, op1=ALU.mult)
        dv("reciprocal", RR, out=R[:, 116:117], in_=R[:, 116:117])
        dv("tensor_tensor", RR, out=R[:, 117:118], in0=R[:, 116:117], in1=R[:, 39:40], op=ALU.mult)
        dv("tensor_tensor", RR, out=R[:, 118:119], in0=R[:, 39:40], in1=R[:, 117:118], op=ALU.subtract)
        dv("tensor_scalar", RR, out=R[:, 119:127], in0=R[:, 89:97], scalar1=R[:, 117:118], scalar2=0.0, op0=ALU.mult, op1=ALU.add)
        dv("scalar_tensor_tensor", RR, out=R[:, 119:127], in0=R[:, 106:114], scalar=R[:, 118:119], in1=R[:, 119:127], op0=ALU.mult, op1=ALU.add)
        self.V("dve", "tensor_tensor", [R], [self.RW], out=self.RW[:, c, :].rearrange("p (g e) -> p g e", g=4),
               in0=R[:, 40:44].unsqueeze(2).to_broadcast([128, 4, 8]), in1=R[:, 119:127].unsqueeze(1).to_broadcast([128, 4, 8]), op=ALU.mult)

    def phase_moe(self, l):
        S = self.S
        i = self.i
        with contextlib.ExitStack() as es:
            acc = self.sb(es, "moacc", [128, 10, D], F32)
            hTb = self.sb(es, "mohT", [128, 8, 1280], BF)
            wts = [(self.sb(es, "mowg%d" % k, [128, 8, 512], BF), self.sb(es, "mowu%d" % k, [128, 8, 512], BF),
                    self.sb(es, "mowd%d" % k, [128, 4, D], BF)) for k in range(2)]
            sG = [self.sb(es, "mosg%d" % k, [128, 512], F32) for k in range(2)]
            Hh = [self.sb(es, "moHh%d" % k, [128, 4, 512], BF) for k in range(2)]
            self.residual_setup(es, 5)
            n = 0
            m = 0
            for blk in ((0, 1, 2), (3, 4), (5, 6), (7, 8)):
                col = 0
                tcs = []
                for ti in blk:
                    t0, N = TILES[ti]
                    S.dma("sp", hTb[:, :, col:col + N], self.HT[:, :, t0:t0 + N].rearrange("k p n -> p k n"), [Tl(None, self.HTt[ti])], [hTb])
                    tcs.append((col, N, t0))
                    col += N
                for e in range(self.nexp):
                    wg, wu, wd = wts[e % 2]
                    S.dma("pool", wg[:], i["moe_w_gate"][l, e].rearrange("(kc p) f -> p kc f", p=128), [], [wg])
                    S.dma("pool", wu[:], i["moe_w_up"][l, e].rearrange("(kc p) f -> p kc f", p=128), [], [wu])
                    S.dma("pool", wd[:], i["moe_w_down"][l, e].rearrange("(fc p) n -> p fc n", p=128), [], [wd])
                    for (col, N, t0) in tcs:
                        hh = Hh[m % 2]
                        m += 1
                        for fc in range(4):
                            pG, pU = self.psb(), self.psb()
                            for kc in range(8):
                                self.mm(pG[:, 0:N], wg[:, kc, fc * 128:(fc + 1) * 128], hTb[:, kc, col:col + N], kc == 0, kc == 7, [wg, hTb], [pG])
                            for kc in range(8):
                                self.mm(pU[:, 0:N], wu[:, kc, fc * 128:(fc + 1) * 128], hTb[:, kc, col:col + N], kc == 0, kc == 7, [wu, hTb], [pU])
                            sg = sG[n % 2]
                            n += 1
                            self.act(sg[:, 0:N], pG[:, 0:N], AF.Silu, [pG], [sg])
                            self.V("dve", "tensor_tensor", [sg, pU], [hh], out=hh[:, fc, 0:N], in0=sg[:, 0:N], in1=pU[:, 0:N], op=ALU.mult)
                        for cc in range(N // 128):
                            ci = col // 128 + cc
                            c = t0 // 128 + cc
                            for half in range(2):
                                hs = slice(half * 512, (half + 1) * 512)
                                pD = self.psb()
                                for fc in range(4):
                                    self.mm(pD[:, :], hh[:, fc, cc * 128:(cc + 1) * 128], wd[:, fc, hs], fc == 0, fc == 3, [hh, wd], [pD])
                                if e == 0:
                                    self.V("dve", "tensor_scalar", [pD, self.RW], [acc], out=acc[:, ci, hs], in0=pD[:, :], scalar1=self.RW[:, c, e:e + 1], scalar2=0.0,
                                           op0=ALU.mult, op1=ALU.add)
                                else:
                                    self.V("dve", "scalar_tensor_tensor", [pD, self.RW, acc], [acc], out=acc[:, ci, hs], in0=pD[:, :], scalar=self.RW[:, c, e:e + 1],
                                           in1=acc[:, ci, hs], op0=ALU.mult, op1=ALU.add)
                for (col, N, t0) in tcs:
                    for cc in range(N // 128):
                        ci = col // 128 + cc
                        c = t0 // 128 + cc
                        self.residual(c, lambda half, ci=ci: acc[:, ci, half * 512:(half + 1) * 512], [acc, acc])
            S.barrier()

    def rank(self, c, R):
        A = R[:, 128:160]
        self.V("dve", "tensor_scalar", [self.RW], [R], out=A, in0=self.RW[:, c, :], scalar1=0.0, scalar2=1.0, op0=ALU.is_gt, op1=ALU.mult)
        if c == 0:
            self.V("dve", "memset", [], [self.Asum], self.Asum[:], 0.0)
        ps = self.psb()
        self.mm(ps[:, 0:32], self.ltri[:], A, True, False, [self.ltri, R], [ps])
        self.mm(ps[:, 0:32], self.cst[:, 128:256], self.Asum[:], False, True, [self.cst, self.Asum], [ps])
        self.act(self.RK[:, c, :], ps[:, 0:32], AF.Copy, [ps], [self.RK])
        self.V("dve", "tensor_tensor", [self.Asum, R], [self.Asum], out=self.Asum[:], in0=self.Asum[:], in1=A, op=ALU.add)

    def phase_moe_sparse(self, l):
        S = self.S
        i = self.i
        onesf = self.cst[:, 128:256]
        rows_t = Tl(None, self.ROWSt)
        acc_t = Tl(None, self.ACC2t)
        h2_t = Tl(None, self.H2t)
        with contextlib.ExitStack() as es:
            G = self.sb(es, "spG", [128, 1024], F32)
            widx = self.sb(es, "spwidx", [128, 2, NB], U32)
            init = self.sb(es, "spinit", [128, 128, 4], F32)
            self.V("pool", "memset", [], [init], init[:], 0.0)
            self.V("pool", "memset", [init], [init], init[:, :, 0:1], float(T))
            self.V("pool", "memset", [init], [init], init[:, :, 2:4], 1.0e6)
            S.dma("sp", self.ROWS.rearrange("(j p) c -> j (p c)", p=128), init[0:NB, :, :].rearrange("j p c -> j (p c)"), [init], [rows_t])
            ps = self.psb()
            self.mm(ps[:, 0:32], onesf, self.Asum[:], True, True, [self.cst, self.Asum], [ps])
            cnt, pad, pend, pst = G[:, 0:32], G[:, 32:64], G[:, 64:96], G[:, 96:128]
            cmp = self.sb(es, "spcmp", [128, NB, 32], F32)
            self.V("dve", "tensor_copy", [ps], [G], out=cnt, in_=ps[:, 0:32])
            cmp2 = cmp[:].rearrange("p a b -> p (a b)")[:, 0:32 * 68].rearrange("p (e m) -> p e m", m=68)
            self.V("dve", "tensor_tensor", [G, self.cst], [cmp], out=cmp2, in0=cnt.unsqueeze(2).to_broadcast([128, 32, 68]),
                   in1=self.cst[:, 896:896 + 68].unsqueeze(1).to_broadcast([128, 32, 68]), op=ALU.is_gt)
            self.V("dve", "tensor_reduce", [cmp], [G], out=pad, in_=cmp2, axis=AX.X, op=ALU.add)
            self.V("dve", "tensor_scalar", [G], [G], out=pad, in0=pad, scalar1=128.0, scalar2=0.0, op0=ALU.mult, op1=ALU.add)
            self.V("dve", "tensor_tensor_scan", [G, self.cst], [G], out=pend, data0=onesf[:, 0:32], data1=pad, initial=0.0, op0=ALU.mult, op1=ALU.add)
            self.V("dve", "tensor_tensor", [G], [G], out=pst, in0=pend, in1=pad, op=ALU.subtract)
            self.V("dve", "tensor_tensor", [G, self.cst], [cmp], out=cmp[:], in0=pend.unsqueeze(1).to_broadcast([128, NB, 32]),
                   in1=self.cst[:, 896:896 + NB].unsqueeze(2).to_broadcast([128, NB, 32]), op=ALU.is_le)
            be = G[:, 128:128 + NB]
            self.V("dve", "tensor_reduce", [cmp], [G], out=be, in_=cmp[:], axis=AX.X, op=ALU.add)
            self.V("dve", "tensor_scalar", [G], [G], out=be, in0=be, scalar1=31.0, scalar2=128.0, op0=ALU.min, op1=ALU.mult)
            same = G[:, 384:384 + NB]
            self.V("dve", "memset", [G], [G], same, 0.0)
            self.V("dve", "tensor_tensor", [G], [G], out=G[:, 386:384 + NB], in0=G[:, 130:128 + NB], in1=G[:, 128:126 + NB], op=ALU.is_equal)
            wf = G[:, 256:256 + NB]
            self.V("dve", "tensor_scalar", [G, self.cst], [G], out=wf, in0=be, scalar1=self.cst[:, 1024:1025], scalar2=2.0, op0=ALU.add, op1=ALU.mult)
            self.V("dve", "tensor_scalar", [G], [G], out=wf, in0=wf, scalar1=float(l * 8192), scalar2=1.0, op0=ALU.add, op1=ALU.mult)
            self.V("dve", "scalar_tensor_tensor", [G], [G], out=wf, in0=same, scalar=1.0e8, in1=wf, op0=ALU.mult, op1=ALU.add)
            self.V("dve", "tensor_copy", [G], [widx], out=widx[:, 0, :], in_=wf)
            self.V("dve", "tensor_scalar", [G], [G], out=wf, in0=wf, scalar1=1.0, scalar2=1.0, op0=ALU.add, op1=ALU.mult)
            self.V("dve", "tensor_copy", [G], [widx], out=widx[:, 1, :], in_=wf)
            Q = [self.sb(es, "spQ%d" % k, [128, 160], F32) for k in range(2)]
            rec = [self.sb(es, "sprec%d" % k, [128, 2, 4], F32) for k in range(2)]
            didx = [self.sb(es, "spdidx%d" % k, [128, 2], U32) for k in range(2)]
            for c in range(NCH):
                q = Q[c % 2]
                r_ = rec[c % 2]
                di = didx[c % 2]
                A, dst, d1, m1 = q[:, 0:32], q[:, 32:64], q[:, 64:96], q[:, 96:128]
                rd = [self.RW, self.RK, G, q]
                self.V("dve", "tensor_scalar", rd, [q], out=A, in0=self.RW[:, c, :], scalar1=0.0, scalar2=1.0, op0=ALU.is_gt, op1=ALU.mult)
                self.V("dve", "tensor_tensor", rd, [q], out=dst, in0=self.RK[:, c, :], in1=pst, op=ALU.add)
                self.V("dve", "scalar_tensor_tensor", rd, [q], out=d1, in0=dst, scalar=1.0, in1=A, op0=ALU.add, op1=ALU.mult)
                self.V("dve", "tensor_reduce", rd, [q], out=q[:, 128:129], in_=d1, axis=AX.X, op=ALU.max)
                self.V("dve", "tensor_scalar", rd, [q], out=m1, in0=d1, scalar1=q[:, 128:129], scalar2=1.0, op0=ALU.is_equal, op1=ALU.mult)
                self.V("dve", "tensor_tensor", rd, [q], out=m1, in0=m1, in1=self.RW[:, c, :], op=ALU.mult)
                self.V("dve", "tensor_reduce", rd, [q], out=q[:, 129:130], in_=m1, axis=AX.X, op=ALU.add)
                self.V("dve", "tensor_reduce", rd, [q], out=q[:, 130:131], in_=self.RW[:, c, :], axis=AX.X, op=ALU.add)
                self.V("dve", "tensor_scalar", rd, [q], out=m1, in0=A, scalar1=-1.0e9, scalar2=1.0e9, op0=ALU.mult, op1=ALU.add)
                self.V("dve", "tensor_tensor", rd, [q], out=m1, in0=m1, in1=dst, op=ALU.add)
                self.V("dve", "tensor_reduce", rd, [q], out=q[:, 131:132], in_=m1, axis=AX.X, op=ALU.min)
                self.V("dve", "tensor_scalar", rd, [q], out=q[:, 132:133], in0=q[:, 128:129], scalar1=-1.0, scalar2=1.0, op0=ALU.add, op1=ALU.mult)
                self.V("dve", "memset", [], [r_], r_[:], 0.0)
                for k in range(2):
                    self.V("dve", "tensor_scalar", [self.cst, r_], [r_], out=r_[:, k, 0:1], in0=self.cst[:, 1024:1025], scalar1=float(c * 128), scalar2=1.0,
                           op0=ALU.add, op1=ALU.mult)
                    self.V("dve", "tensor_scalar", [self.cst, r_], [r_], out=r_[:, k, 2:3], in0=self.cst[:, 1024:1025], scalar1=float(c * 128 + k * T), scalar2=2.0,
                           op0=ALU.add, op1=ALU.mult)
                    self.V("dve", "tensor_scalar", [r_], [r_], out=r_[:, k, 3:4], in0=r_[:, k, 2:3], scalar1=1.0, scalar2=1.0, op0=ALU.add, op1=ALU.mult)
                self.V("dve", "tensor_tensor", [q, r_], [r_], out=r_[:, 0, 1:2], in0=q[:, 130:131], in1=q[:, 129:130], op=ALU.subtract)
                self.V("dve", "tensor_copy", [q, r_], [r_], out=r_[:, 1, 1:2], in_=q[:, 129:130])
                self.V("dve", "tensor_copy", [q], [di], out=di[:, 0:1], in_=q[:, 131:132])
                self.V("dve", "tensor_copy", [q], [di], out=di[:, 1:2], in_=q[:, 132:133])
                for k in range(2):
                    S.dma_fn("pool", (lambda e, r_=r_, di=di, k=k: e.indirect_dma_start(out=self.ROWS, out_offset=bass.IndirectOffsetOnAxis(ap=di[:, k:k + 1], axis=0),
                                                                                      in_=r_[:, k, :], in_offset=None)), [r_, di], [rows_t])
            wgv = i["moe_w_gate"].rearrange("l e (p j) f -> (l e p) (j f)", j=8).rearrange("r (h x) -> (r h) x", h=2)
            wuv = i["moe_w_up"].rearrange("l e (p j) f -> (l e p) (j f)", j=8).rearrange("r (h x) -> (r h) x", h=2)
            wdv = i["moe_w_down"].rearrange("l e (p j) n -> (l e p) (j n)", j=4).rearrange("r (h x) -> (r h) x", h=2)
            wts = [(self.sb(es, "spwg%d" % k, [128, 8, 512], BF), self.sb(es, "spwu%d" % k, [128, 8, 512], BF),
                    self.sb(es, "spwd%d" % k, [128, 4, D], BF)) for k in range(2)]
            NQ = 4
            recs = [self.sb(es, "sprc%d" % k, [128, 4], F32) for k in range(NQ)]
            recu = [self.sb(es, "spru%d" % k, [128, 4], U32) for k in range(NQ)]
            hbs = [self.sb(es, "sphb%d" % k, [128, D], BF) for k in range(NQ)]
            hTs = [self.sb(es, "sphT%d" % k, [128, 8, 128], BF) for k in range(NQ)]
            sGs = [self.sb(es, "spsg%d" % k, [128, 512], F32) for k in range(2)]
            Hhs = [self.sb(es, "spHh%d" % k, [128, 512], BF) for k in range(2)]
            HhTs = [self.sb(es, "spHhT%d" % k, [128, 4, 128], BF) for k in range(2)]
            ys = [self.sb(es, "spy%d" % k, [128, D], F32) for k in range(2)]
            def gather(dst_ap, src, idx_ap, r, w, skip=False):
                if skip:
                    S.dma_fn("pool", (lambda e: e.indirect_dma_start(out=dst_ap, out_offset=None, in_=src, in_offset=bass.IndirectOffsetOnAxis(ap=idx_ap, axis=0),
                                                                     bounds_check=self._wbound_reg(e), oob_is_err=False)), r, w)
                else:
                    S.dma_fn("pool", (lambda e: e.indirect_dma_start(out=dst_ap, out_offset=None, in_=src, in_offset=bass.IndirectOffsetOnAxis(ap=idx_ap, axis=0))), r, w)

            def proA(j):
                rc, ru, hb = recs[j % NQ], recu[j % NQ], hbs[j % NQ]
                S.dma("sp", rc[:], self.ROWS[j * 128:(j + 1) * 128, :], [rows_t], [rc])
                self.V("dve", "tensor_copy", [rc], [ru], out=ru[:], in_=rc[:])
                gather(hb[:], self.H2, ru[:, 0:1], [ru, h2_t], [hb])

            def proB(j):
                hb, hT = hbs[j % NQ], hTs[j % NQ]
                pt = self.psb()
                ptv = pt[:].bitcast(BF).rearrange("p (k n) -> p k n", k=8)
                hbv = hb[:].rearrange("t (p j) -> t p j", j=8)
                for jx in range(8):
                    self.tr(ptv[:, jx, :], hbv[:, :, jx], self.identb[:], [hb, self.identb], [pt])
                self.act(hT[:], ptv, AF.Copy, [pt], [hT])

            def wload(j):
                wg, wu, wd = wts[j % 2]
                for h_ in range(2):
                    gather(wg[:, h_ * 4:(h_ + 1) * 4, :].rearrange("p j f -> p (j f)"), wgv, widx[:, h_, j:j + 1], [widx], [wg], skip=True)
                    gather(wu[:, h_ * 4:(h_ + 1) * 4, :].rearrange("p j f -> p (j f)"), wuv, widx[:, h_, j:j + 1], [widx], [wu], skip=True)
                    gather(wd[:, h_ * 2:(h_ + 1) * 2, :].rearrange("p j f -> p (j f)"), wdv, widx[:, h_, j:j + 1], [widx], [wd], skip=True)

            proA(0)
            proA(1)
            wload(0)
            proB(0)
            for j in range(NB):
                z = j % 2
                rc, ru, hT = recs[j % NQ], recu[j % NQ], hTs[j % NQ]
                sg, Hh, HhT, y = sGs[z], Hhs[z], HhTs[z], ys[z]
                wg, wu, wd = wts[z]
                if j + 2 < NB:
                    proA(j + 2)
                if j + 1 < NB:
                    wload(j + 1)
                pG, pU = self.psb(), self.psb()
                for jx in range(8):
                    self.mm(pG[:, :], hT[:, jx, :], wg[:, jx, :], jx == 0, jx == 7, [hT, wg], [pG])
                for jx in range(8):
                    self.mm(pU[:, :], hT[:, jx, :], wu[:, jx, :], jx == 0, jx == 7, [hT, wu], [pU])
                self.act(sg[:], pG[:, :], AF.Silu, [pG], [sg])
                self.V("dve", "scalar_tensor_tensor", [pU, rc, sg], [Hh], out=Hh[:], in0=pU[:, :], scalar=rc[:, 1:2], in1=sg[:], op0=ALU.mult, op1=ALU.mult)
                if j + 1 < NB:
                    proB(j + 1)
                pt2 = self.psb()
                pt2v = pt2[:].bitcast(BF)[:, 0:512].rearrange("p (k n) -> p k n", k=4)
                Hhv = Hh[:].rearrange("t (p j) -> t p j", j=4)
                for jx in range(4):
                    self.tr(pt2v[:, jx, :], Hhv[:, :, jx], self.identb[:], [Hh, self.identb], [pt2])
                self.act(HhT[:], pt2v, AF.Copy, [pt2], [HhT])
                for half in range(2):
                    pD = self.psb()
                    for jx in range(4):
                        self.mm(pD[:, :], HhT[:, jx, :], wd[:, jx, half * 512:(half + 1) * 512], jx == 0, jx == 3, [HhT, wd], [pD])
                    if half == 0:
                        self.act(y[:, 0:512], pD[:, :], AF.Copy, [pD], [y])
                    else:
                        self.V("dve", "tensor_copy", [pD], [y], out=y[:, 512:1024], in_=pD[:, :])
                for h_ in range(2):
                    S.dma_fn("pool", (lambda e, y=y, ru=ru, h_=h_: e.indirect_dma_start(out=self.ACC2.rearrange("r (h x) -> (r h) x", h=2),
                                                                                        out_offset=bass.IndirectOffsetOnAxis(ap=ru[:, 2 + h_:3 + h_], axis=0),
                                                                                        in_=y[:, h_ * 512:(h_ + 1) * 512], in_offset=None,
                                                                                        bounds_check=self._bound_reg(e), oob_is_err=False)), [y, ru], [acc_t])
            self.residual_setup(es, 5)
            a0 = [self.sb(es, "spa0%d" % k, [128, D], F32) for k in range(2)]
            a1 = [self.sb(es, "spa1%d" % k, [128, D], F32) for k in range(2)]
            for c in range(NCH):
                u0, u1 = a0[c % 2], a1[c % 2]
                S.dma("sp", u0[:], self.ACC2[c * 128:(c + 1) * 128, :], [acc_t], [u0])
                S.dma("sp", u1[:], self.ACC2[T + c * 128:T + (c + 1) * 128, :], [acc_t], [u1])
                self.V("pool", "tensor_tensor", [u0, u1], [u0], out=u0[:], in0=u0[:], in1=u1[:], op=ALU.add)
                self.residual(c, lambda half, u0=u0: u0[:, half * 512:(half + 1) * 512], [u0, u0])
            S.barrier()

    def _wbound_reg(self, e):
        if getattr(self, "_wbreg", None) is None:
            self._wbreg = e.to_reg(self.n_layers * 32 * 128 * 2 - 1)
        return self._wbreg

    def _bound_reg(self, e):
        if getattr(self, "_breg", None) is None:
            self._breg = e.to_reg(4 * T - 1)
        return self._breg

    def final_norm(self):
        S = self.S
        with contextlib.ExitStack() as es:
            g = self.sb(es, "fng", [128, D], F32)
            S.dma("sp", g[:], self.i["final_g"].partition_broadcast(128), [], [g])
            xs = [self.sb(es, "fnx%d" % k, [128, D], F32) for k in range(3)]
            sq = self.sb(es, "fnsq", [128, D], F32)
            st = [self.sb(es, "fnst%d" % k, [128, 4], F32) for k in range(2)]
            ot = [self.sb(es, "fno%d" % k, [128, D], F32) for k in range(2)]
            for c in range(2, NCH):
                x = xs[c % 3]
                S.dma("sp", x[:], self.X[c * 128:(c + 1) * 128, :], [Tl(None, self.Xt[c])], [x])
                s_ = st[c % 2]
                self.act(sq[:], x[:], AF.Square, [x], [sq, s_], accum_out=s_[:, 0:1])
                self.V("dve", "tensor_scalar", [s_], [s_], out=s_[:, 1:2], in0=s_[:, 0:1], scalar1=1.0 / D, scalar2=EPS, op0=ALU.mult, op1=ALU.add)
                self.V("dve", "reciprocal", [s_], [s_], out=s_[:, 2:3], in_=s_[:, 1:2])
                self.act(s_[:, 3:4], s_[:, 2:3], AF.Sqrt, [s_], [s_])
                o = ot[c % 2]
                self.V("dve", "scalar_tensor_tensor", [x, s_, g], [o], out=o[:], in0=x[:], scalar=s_[:, 3:4], in1=g[:], op0=ALU.mult, op1=ALU.mult)
                S.dma("sp", self.out[(c - 2) * 128:(c - 1) * 128, :], o[:], [o], [Tl(None, self.outt)])


def make_consts():
    cst = np.zeros((128, 1056), np.float32)
    cst[:, 0:128] = np.eye(128, dtype=np.float32)
    cst[:, 128:256] = 1.0
    R = np.zeros((128, 128), np.float32)
    for blk in range(2):
        o = blk * 64
        for q in range(16):
            R[o + 16 + q, o + q] = -1.0
            R[o + q, o + 16 + q] = 1.0
            R[o + 48 + q, o + 32 + q] = -1.0
            R[o + 32 + q, o + 48 + q] = 1.0
    cst[:, 256:384] = R
    s = np.arange(128)[:, None]
    t = np.arange(128)[None, :]
    same = (s // 64) == (t // 64)
    same32 = (s // 32) == (t // 32)
    cst[:, 384:512] = (same32 & (t >= s)).astype(np.float32)
    cst[:, 512:640] = (same32 & (t <= s)).astype(np.float32)
    cst[:, 640:768] = (same & (s % 64 < 32) & (t % 64 >= 32)).astype(np.float32)
    cst[:, 768:896] = (same & (s % 64 >= 32) & (t % 64 < 32)).astype(np.float32)
    cst[:, 896:1024] = 128.0 * np.arange(128, dtype=np.float32)[None, :]
    cst[:, 1024] = np.arange(128, dtype=np.float32)
    cst[:, 1025:1057 - 1 + 0] = 0.0
    inv_freq = (10000.0 ** (-np.arange(0, 32, 2, dtype=np.float32) / 32)).astype(np.float32)
    pos = np.arange(NLAT)
    row = (pos // 64).astype(np.float32)
    col = (pos % 64).astype(np.float32)
    ang_r = row[:, None] * inv_freq
    ang_c = col[:, None] * inv_freq
    ang = np.concatenate([ang_r, ang_r, ang_c, ang_c], axis=-1).astype(np.float32)
    rope = np.zeros((2, 128, T), np.float32)
    rope[0, :, :NCTX] = 1.0
    rope[0, 0:64, NCTX:] = np.cos(ang).T
    rope[0, 64:128, NCTX:] = np.cos(ang).T
    rope[1, 0:64, NCTX:] = np.sin(ang).T
    rope[1, 64:128, NCTX:] = np.sin(ang).T
    return cst, rope


def make_in_maps(inputs, cores, L=DEPTH, nexp=32):
    f = lambda a: np.ascontiguousarray(np.asarray(a, dtype=np.float32))
    cst, rope = make_consts()
    shared = {
        "c_ctx": f(inputs["c_ctx"]).reshape(1, D),
        "ada_w": f(inputs["ada_w"][:L]), "ada_b": f(inputs["ada_b"]),
        "norm1_g": f(inputs["norm1_g"]), "norm2_g": f(inputs["norm2_g"]),
        "w_in": f(inputs["w_in"][:L]), "w_branch": f(inputs["w_branch"][:L]), "w_out": f(inputs["w_out"][:L]),
        "hg_lb": f(inputs["hg_lb_logits"]), "hg_norm_g": f(inputs["hg_norm_g"]),
        "da_lambda": f(inputs["da_lambda"]).reshape(DEPTH, 256), "da_norm_g": f(inputs["da_norm_g"]),
        "cv_dw_w": f(inputs["cv_dw_w"]), "cv_dw_b": f(inputs["cv_dw_b"]),
        "cv_ln_g": f(inputs["cv_ln_g"]), "cv_ln_b": f(inputs["cv_ln_b"]),
        "sc_w": f(inputs["sc_w"]),
        "moe_w_r": np.ascontiguousarray(np.concatenate([f(inputs["moe_w_grp"]), f(inputs["moe_w_exp"])], axis=-1)),
        "moe_b_r": np.ascontiguousarray(np.concatenate([f(inputs["moe_b_grp"]), f(inputs["moe_b_exp"])], axis=-1)),
        "moe_w_gate": f(inputs["moe_w_gate"][:L, :nexp]), "moe_w_up": f(inputs["moe_w_up"][:L, :nexp]), "moe_w_down": f(inputs["moe_w_down"][:L, :nexp]),
        "final_g": f(inputs["final_g"]).reshape(1, D),
        "cst": cst, "rope": rope, "ltri": np.triu(np.ones((128, 128), np.float32), 1),
    }
    maps = []
    for cid in cores:
        b = cid % 4
        m = dict(shared)
        m["x"] = f(inputs["x"][b])
        m["c"] = f(inputs["c"][b]).reshape(1, D)
        m["ctx"] = f(inputs["ctx"][b])
        maps.append(m)
    return maps


def kernel(**inputs):
    nc = bass.Bass("TRN2", target_bir_lowering=False)
    Prog(nc).build()
    maps = make_in_maps(inputs, list(range(4)))
    res = run_bass_kernel_spmd(nc, maps, core_ids=list(range(4)))
    return np.stack([np.asarray(res.results[b]["out"], dtype=np.float32) for b in range(4)], axis=0)
```

```python
import contextlib
import math
import numpy as np
import concourse.bass as bass
import concourse.mybir as mybir
from concourse.bass_utils import run_bass_kernel_spmd

F32 = mybir.dt.float32
BF = mybir.dt.bfloat16
U32 = mybir.dt.uint32
AF = mybir.ActivationFunctionType
ALU = mybir.AluOpType
AX = mybir.AxisListType

D = 1024
NCTX = 256
NLAT = 4096
T = NCTX + NLAT
NCH = T // 128
NB = 2 * T // 128 + 32
SPARSE = True
DEPTH = 4
W = 512
INW = 10752
EPS = 1e-6
TILES = [(0, 256)] + [(256 + 512 * i, 512) for i in range(8)]
C_HQ, C_HI, C_HFF, C_HFB, C_HG = 0, 512, 1024, 1536, 2048
C_DQ, C_DK, C_DV = 2560, 3072, 3584
C_CVA, C_CVG = 4096, 4608
C_SB, C_SC, C_SX = 5120, 5632, 6144
C_GATE = 6656


class Trk:
    __slots__ = ("w", "r")

    def __init__(self):
        self.w = None
        self.r = {}


class Tl:
    def __init__(self, h, trk=None):
        self.h = h
        self.t = trk or Trk()

    def __getitem__(self, k):
        return self.h[k]


class Stream:
    def __init__(self, name):
        self.name = name
        self.ops = []
        self.seen = {}
        self.sem = None
        self.cnt = 0
        self.dslots = []
        self.dnext = 0


class Sch:
    SEM_MAX = 30000

    def __init__(self, nc, es):
        self.nc = nc
        self.es = es
        self.st = {k: Stream(k) for k in ("pe", "act", "dve", "pool", "sp")}
        self.nsem = 0
        for k, s in self.st.items():
            self._newsem(s)
        for k, n in (("sp", 24), ("pool", 12), ("act", 6)):
            s = self.st[k]
            for i in range(n):
                s.dslots.append([self._sem(), 0])

    def _sem(self):
        self.nsem += 1
        return self.es.enter_context(self.nc.semaphore("s%d" % self.nsem))

    def _newsem(self, s):
        s.sem = self._sem()
        s.cnt = 0

    def _need(self, s, tok, waits):
        if tok is None:
            return
        sem, val, src = tok
        if src == "pe" and s.name == "pe":
            return
        if s.seen.get(id(sem), 0) >= val:
            return
        k = id(sem)
        if k not in waits or waits[k][1] < val:
            waits[k] = (sem, val)

    def _deps(self, s, reads, writes):
        waits = {}
        for b in reads:
            self._need(s, b.t.w, waits)
        for b in writes:
            self._need(s, b.t.w, waits)
            for tok in b.t.r.values():
                self._need(s, tok, waits)
        for k, (sem, val) in waits.items():
            s.seen[k] = val
        return list(waits.values())

    def _mark(self, tok, reads, writes):
        for b in reads:
            b.t.r[id(tok[0])] = tok
        for b in writes:
            b.t.w = tok
            b.t.r = {}

    def op(self, eng, fn, reads=(), writes=()):
        s = self.st[eng]
        if s.cnt >= self.SEM_MAX:
            self._newsem(s)
        waits = self._deps(s, reads, writes)
        s.cnt += 1
        tok = (s.sem, s.cnt, eng)
        s.ops.append((waits, fn, (s.sem, 1)))
        self._mark(tok, reads, writes)
        return tok

    def dma(self, q, out, in_, reads=(), writes=(), **kw):
        return self.dma_fn(q, (lambda e: e.dma_start(out=out, in_=in_, **kw)), reads, writes)

    def dma_fn(self, q, fn, reads=(), writes=()):
        s = self.st[q]
        slot = s.dslots[s.dnext % len(s.dslots)]
        s.dnext += 1
        waits = self._deps(s, reads, writes)
        if slot[1] > 0 and s.seen.get(id(slot[0]), 0) < slot[1]:
            waits.append((slot[0], slot[1]))
            s.seen[id(slot[0])] = slot[1]
        slot[1] += 16
        tok = (slot[0], slot[1], "dma")
        s.ops.append((waits, fn, (slot[0], 16)))
        self._mark(tok, reads, writes)
        return tok

    def barrier(self):
        toks = []
        for s in self.st.values():
            if s.cnt > 0:
                toks.append((s.sem, s.cnt))
            for sl in s.dslots:
                if sl[1] > 0:
                    toks.append((sl[0], sl[1]))
        for s in self.st.values():
            waits = []
            for sem, val in toks:
                if sem is s.sem:
                    continue
                if s.seen.get(id(sem), 0) < val:
                    waits.append((sem, val))
                    s.seen[id(sem)] = val
            if waits:
                s.ops.append((waits, None, None))

    def emit(self):
        nc = self.nc
        self.barrier()
        with nc.Block() as block:
            def run(s):
                def f(e):
                    for waits, fn, inc in s.ops:
                        for sem, val in waits:
                            e.wait_ge(sem, val)
                        if fn is not None:
                            fn(e).then_inc(inc[0], inc[1])
                return f
            block.tensor(run(self.st["pe"]))
            block.scalar(run(self.st["act"]))
            block.vector(run(self.st["dve"]))
            block.gpsimd(run(self.st["pool"]))
            block.sync(run(self.st["sp"]))


class Prog:
    def __init__(self, nc, n_layers=DEPTH, dbg=None, nexp=32):
        self.nc = nc
        self.nexp = nexp
        self.n_layers = n_layers
        self.dbg = dbg or {}

    def sb(self, es, name, shape, dt):
        self.uid = getattr(self, "uid", 0) + 1
        return Tl(es.enter_context(self.nc.sbuf_tensor("%s_u%d" % (name, self.uid), list(shape), dt)))

    def dram(self, name, shape, dt, kind="Internal"):
        return self.nc.dram_tensor(name, list(shape), dt, kind=kind).ap()

    def mm(self, out, lhsT, rhs, start, stop, r, w):
        self.S.op("pe", lambda e: e.matmul(out, lhsT=lhsT, rhs=rhs, start=start, stop=stop), r, w)

    def tr(self, out, in_, ident, r, w):
        self.S.op("pe", lambda e: e.transpose(out=out, in_=in_, identity=ident), r, w)

    def act(self, out, in_, func, r, w, **kw):
        self.S.op("act", lambda e: e.activation(out=out, in_=in_, func=func, **kw), r, w)

    def V(self, eng, name, r, w, *a, **kw):
        self.S.op(eng, lambda e: getattr(e, name)(*a, **kw), r, w)

    def psb(self):
        b = self.banks[self.bi % 8]
        self.bi += 1
        return b

    def build(self):
        nc = self.nc
        L = self.n_layers
        i = {}
        def inp(name, shape, dt=F32):
            i[name] = self.dram(name, shape, dt, kind="ExternalInput")
        inp("x", [NLAT, D]); inp("c", [1, D]); inp("ctx", [NCTX, D]); inp("c_ctx", [1, D])
        inp("ada_w", [L, D, 6 * D]); inp("ada_b", [DEPTH, 6 * D])
        inp("norm1_g", [DEPTH, D]); inp("norm2_g", [DEPTH, D])
        inp("w_in", [L, D, INW]); inp("w_branch", [L, 4, W, D]); inp("w_out", [L, D, D])
        inp("hg_lb", [DEPTH, 2, W]); inp("hg_norm_g", [DEPTH, W])
        inp("da_lambda", [DEPTH, 256]); inp("da_norm_g", [DEPTH, 128])
        inp("cv_dw_w", [DEPTH, 31, W]); inp("cv_dw_b", [DEPTH, W]); inp("cv_ln_g", [DEPTH, W]); inp("cv_ln_b", [DEPTH, W])
        inp("sc_w", [DEPTH, 3, W])
        inp("moe_w_r", [DEPTH, D, 36]); inp("moe_b_r", [DEPTH, 36])
        inp("moe_w_gate", [L, self.nexp, D, W]); inp("moe_w_up", [L, self.nexp, D, W]); inp("moe_w_down", [L, self.nexp, W, D])
        inp("final_g", [1, D]); inp("ltri", [128, 128])
        inp("cst", [128, 1056]); inp("rope", [2, 128, T])
        self.i = i
        self.out = self.dram("out", [NLAT, D], F32, kind="ExternalOutput")
        self.X = self.dram("Xs", [T, D], F32)
        self.HT = self.dram("HTs", [8, 128, T], BF)
        self.Y = self.dram("Ys", [4, 4, 128, T], BF)
        self.MT = self.dram("MTs", [8, 128, T], BF)
        self.MODR = self.dram("MODs", [2, 128, 6 * D], F32)
        self.H2 = self.dram("H2s", [T + 1, D], BF)
        self.ROWS = self.dram("ROWSs", [NB * 128, 4], F32)
        self.ACC2 = self.dram("ACC2s", [2 * T, D], F32)
        self.H2t = Trk(); self.ROWSt = Trk(); self.ACC2t = Trk()
        self.Xt = [Trk() for _ in range(NCH)]
        self.HTt = [Trk() for _ in range(9)]
        self.Yt = [[Trk() for _ in range(9)] for _ in range(4)]
        self.MTt = [Trk() for _ in range(9)]
        self.MODt = Trk()
        self.outt = Trk()
        dbg_out = {}
        for k, shp in self.dbg.items():
            if not isinstance(shp, tuple):
                continue
            dbg_out[k] = self.dram("dbg_" + k, shp[0], shp[1], kind="ExternalOutput")
        self.dbg_out = dbg_out

        with contextlib.ExitStack() as es:
            self.S = S = Sch(nc, es)
            self.banks = [Tl(es.enter_context(nc.psum_tensor("pb%d" % k, [128, 512], F32))) for k in range(8)]
            self.bi = 0
            self.cst = self.sb(es, "cst_sb", [128, 1056], F32)
            S.dma("sp", self.cst[:], i["cst"], [], [self.cst])
            self.identb = self.sb(es, "identb", [128, 128], BF)
            self.onesb = self.sb(es, "onesb", [128, 128], BF)
            self.Rb = self.sb(es, "Rb", [128, 128], BF)
            self.identf = Tl(self.cst.h, self.cst.t)
            self.V("dve", "tensor_copy", [self.cst], [self.identb], out=self.identb[:], in_=self.cst[:, 0:128])
            self.V("dve", "tensor_copy", [self.cst], [self.onesb], out=self.onesb[:], in_=self.cst[:, 128:256])
            self.V("dve", "tensor_copy", [self.cst], [self.Rb], out=self.Rb[:], in_=self.cst[:, 256:384])
            self.sT = []
            for which, src in enumerate((i["c"], i["c_ctx"])):
                cT = self.sb(es, "cT%d" % which, [128, 8], F32)
                S.dma("sp", cT[:], src.rearrange("o (kc p) -> p (o kc)", p=128), [], [cT], allow_slow_non_contiguous=True)
                sg = self.sb(es, "cS%d" % which, [128, 8], F32)
                self.act(sg[:], cT[:], AF.Silu, [cT], [sg])
                rep = self.sb(es, "sT%d" % which, [128, 8, 128], F32)
                self.V("dve", "tensor_copy", [sg], [rep], out=rep[:], in_=sg[:].unsqueeze(2).to_broadcast([128, 8, 128]))
                self.sT.append(rep)
            self.P = self.sb(es, "Pparams", [128, 600], F32)
            self.RW = self.sb(es, "RW", [128, NCH, 32], F32)
            self.RK = self.sb(es, "RK", [128, NCH, 32], F32)
            self.Asum = self.sb(es, "Asum", [128, 32], F32)
            self.ltri = self.sb(es, "ltri_sb", [128, 128], F32)
            S.dma("sp", self.ltri[:], i["ltri"], [], [self.ltri])
            zr = self.sb(es, "zrow", [1, D], BF)
            self.V("dve", "memset", [], [zr], zr[:], 0.0)
            S.dma("sp", self.H2[T:T + 1, :], zr[:], [zr], [Tl(None, self.H2t)])
            self.LB = self.sb(es, "LB", [128, DEPTH, 8], F32)
            self.OML = self.sb(es, "OML", [128, DEPTH, 8], F32)
            lbe = self.sb(es, "lbe", [128, DEPTH, 8], F32)
            lbt = self.sb(es, "lbt", [128, 16], F32)
            for l_ in range(DEPTH):
                for d_ in range(2):
                    for j_ in range(4):
                        S.dma("sp", lbe[:, l_, d_ * 4 + j_:d_ * 4 + j_ + 1], i["hg_lb"][l_, d_:d_ + 1, j_ * 128:(j_ + 1) * 128].rearrange("o p -> p o"),
                              [], [lbe], allow_slow_non_contiguous=True)
            self.act(lbe[:], lbe[:], AF.Exp, [lbe], [lbe])
            self.V("dve", "tensor_tensor", [lbe], [lbt], out=lbt[:, 0:8], in0=lbe[:, 0, :], in1=lbe[:, 1, :], op=ALU.add)
            self.V("dve", "tensor_tensor", [lbe, lbt], [lbt], out=lbt[:, 0:8], in0=lbt[:, 0:8], in1=lbe[:, 2, :], op=ALU.add)
            self.V("dve", "tensor_tensor", [lbe, lbt], [lbt], out=lbt[:, 0:8], in0=lbt[:, 0:8], in1=lbe[:, 3, :], op=ALU.add)
            self.V("dve", "reciprocal", [lbt], [lbt], out=lbt[:, 8:16], in_=lbt[:, 0:8])
            self.V("dve", "memset", [], [self.LB], self.LB[:], 0.0)
            for l_ in range(1, DEPTH):
                self.V("dve", "tensor_tensor", [lbe, lbt], [lbe], out=lbe[:, l_, :], in0=lbe[:, l_, :], in1=lbt[:, 8:16], op=ALU.mult)
                self.V("dve", "tensor_tensor", [lbe, self.LB], [self.LB], out=self.LB[:, l_, :], in0=self.LB[:, l_ - 1, :], in1=lbe[:, l_, :], op=ALU.add)
            self.V("dve", "tensor_scalar", [self.LB], [self.OML], out=self.OML[:], in0=self.LB[:], scalar1=-1.0, scalar2=1.0, op0=ALU.mult, op1=ALU.add)
            self.seg = self.sb(es, "seg", [128, 512], F32)
            self.V("dve", "memset", [], [self.seg], self.seg[:], 1.0)
            self.V("dve", "memset", [self.seg], [self.seg], self.seg[:].rearrange("p (c s) -> p c s", s=64)[:, :, 0:1], 0.0)
            S.barrier()
            S.dma("sp", self.X[0:NCTX, :], i["ctx"], [], self._xt(0, 2))
            for q in range(4):
                S.dma("sp", self.X[NCTX + q * 1024: NCTX + (q + 1) * 1024, :], i["x"][q * 1024:(q + 1) * 1024, :], [],
                      self._xt(2 + q * 8, 8))
            for l in range(L):
                self.layer(l)
            self.final_norm()
            S.emit()

    def _xt(self, c0, n):
        return [Tl(None, t) for t in self.Xt[c0:c0 + n]]

    def layer(self, l):
        stop = self.dbg.get("stop")
        self.phase_mod(l)
        self.load_params(l)
        self.phase_norm(l, 1)
        if stop == "norm1":
            return
        self.phase_sconv(l)
        self.phase_conv(l)
        if stop == "convs":
            return self.dump_dbg()
        self.phase_attn(l)
        if stop == "attn":
            return self.dump_dbg()
        self.phase_hgrn(l)
        if stop == "hgrn":
            return self.dump_dbg()
        self.phase_merge(l)
        if stop == "merge":
            return self.dump_dbg()
        self.phase_norm(l, 2)
        if SPARSE:
            self.phase_moe_sparse(l)
        else:
            self.phase_moe(l)
        if stop == "moe":
            return self.dump_dbg()

    def dump_dbg(self):
        S = self.S
        if "y" in self.dbg_out:
            S.dma("sp", self.dbg_out["y"], self.Y, [Tl(None, t) for k in range(4) for t in self.Yt[k]], [Tl(None, Trk())])
        if "x" in self.dbg_out:
            S.dma("sp", self.dbg_out["x"], self.X, [Tl(None, t) for t in self.Xt], [Tl(None, Trk())])

    def load_w(self, es, name, l, c0, ncols):
        w = self.sb(es, name, [128, 8, ncols], BF)
        self.S.dma("pool", w[:], self.i["w_in"][l, :, c0:c0 + ncols].rearrange("(kc p) n -> p kc n", p=128), [], [w])
        return w

    def load_ht(self, buf, ti):
        t0, N = TILES[ti]
        self.S.dma("sp", buf[:, :, 0:N], self.HT[:, :, t0:t0 + N].rearrange("k p n -> p k n"), [Tl(None, self.HTt[ti])], [buf])

    def proj(self, ps, w, j0, ht, N, width=128):
        for kc in range(8):
            self.mm(ps[0:width, 0:N], w[:, kc, j0:j0 + width], ht[:, kc, 0:N], kc == 0, kc == 7, [w, ht], [ps])

    def rstd_from(self, src_ap, scale, out_t, tmp_t, N, r):
        self.V("dve", "tensor_scalar", r, [tmp_t], out=tmp_t[:, 0:N], in0=src_ap, scalar1=scale, scalar2=EPS, op0=ALU.mult, op1=ALU.add)
        self.V("dve", "reciprocal", [tmp_t], [tmp_t], out=tmp_t[:, 0:N], in_=tmp_t[:, 0:N])
        self.act(out_t[:, 0:N], tmp_t[:, 0:N], AF.Sqrt, [tmp_t], [out_t])

    def load_params(self, l):
        S = self.S
        i = self.i
        P = self.P
        nc = True
        def ld(dst, src):
            S.dma("sp", dst, src, [], [P], allow_slow_non_contiguous=True)
        for j in range(4):
            sl = slice(j * 128, (j + 1) * 128)
            ld(P[:, j * 3:j * 3 + 3], i["sc_w"][l][:, sl].rearrange("k p -> p k"))
            ld(P[:, 12 + j * 31:12 + (j + 1) * 31], i["cv_dw_w"][l][:, sl].rearrange("k p -> p k"))
            ld(P[:, 136 + j:137 + j], i["cv_dw_b"][l:l + 1, sl].rearrange("o p -> p o"))
            ld(P[:, 140 + j:141 + j], i["cv_ln_g"][l:l + 1, sl].rearrange("o p -> p o"))
            ld(P[:, 144 + j:145 + j], i["cv_ln_b"][l:l + 1, sl].rearrange("o p -> p o"))
            ld(P[:, 148 + j:149 + j], i["hg_norm_g"][l:l + 1, sl].rearrange("o p -> p o"))
        ld(P[:, 152:153], i["da_norm_g"][l:l + 1, :].rearrange("o p -> p o"))
        S.dma("sp", P[:, 160:416], i["da_lambda"][l:l + 1, :].partition_broadcast(128), [], [P])
        S.dma("sp", P[:, 416:452], i["moe_b_r"][l:l + 1, :].partition_broadcast(128), [], [P])
        lam_init = 0.8 - 0.6 * math.exp(-0.3 * l)
        self.V("dve", "tensor_tensor", [P], [P], out=P[:, 460:524], in0=P[:, 160:224], in1=P[:, 224:288], op=ALU.mult)
        self.V("dve", "tensor_tensor", [P], [P], out=P[:, 524:588], in0=P[:, 288:352], in1=P[:, 352:416], op=ALU.mult)
        self.V("dve", "tensor_reduce", [P], [P], out=P[:, 155:157], in_=P[:, 460:588].rearrange("p (a b) -> p a b", a=2), axis=AX.X, op=ALU.add)
        self.act(P[:, 157:159], P[:, 155:157], AF.Exp, [P], [P])
        self.V("dve", "tensor_tensor", [P], [P], out=P[:, 159:160], in0=P[:, 158:159], in1=P[:, 157:158], op=ALU.subtract)
        self.V("dve", "tensor_scalar", [P], [P], out=P[:, 153:154], in0=P[:, 159:160], scalar1=-lam_init, scalar2=1.0, op0=ALU.add, op1=ALU.mult)
        self.V("dve", "tensor_scalar", [P], [P], out=P[:, 154:155], in0=P[:, 152:153], scalar1=1.0 - lam_init, scalar2=0.0, op0=ALU.mult, op1=ALU.add)

    def phase_mod(self, l):
        S = self.S
        i = self.i
        modt = Tl(None, self.MODt)
        with contextlib.ExitStack() as es:
            wt = [self.sb(es, "adaw%d" % k, [128, 8, 512], F32) for k in range(2)]
            bt = [self.sb(es, "adab%d" % k, [1, 512], F32) for k in range(2)]
            ot = [self.sb(es, "adao%d" % k, [128, 512], F32) for k in range(2)]
            n = 0
            for blk in range(12):
                w = wt[blk % 2]
                b = bt[blk % 2]
                S.dma("sp", w[:], i["ada_w"][l, :, blk * 512:(blk + 1) * 512].rearrange("(kc p) n -> p kc n", p=128), [], [w])
                S.dma("sp", b[:], i["ada_b"][l:l + 1, blk * 512:(blk + 1) * 512], [], [b])
                for which in range(2):
                    ps = self.psb()
                    for kc in range(8):
                        self.mm(ps[:], self.sT[which][:, kc, :], w[:, kc, :], kc == 0, False, [self.sT[which], w], [ps])
                    self.mm(ps[:], self.cst[0:1, 128:256], b[0:1, :], False, True, [self.cst, b], [ps])
                    o = ot[n % 2]
                    n += 1
                    self.V("dve", "tensor_copy", [ps], [o], out=o[:], in_=ps[:])
                    S.dma("sp", self.MODR[which, :, blk * 512:(blk + 1) * 512], o[:], [o], [modt])
            S.barrier()

    def phase_norm(self, l, which):
        S = self.S
        i = self.i
        gsrc = i["norm1_g"] if which == 1 else i["norm2_g"]
        sh, sc = (0, 1) if which == 1 else (3, 4)
        modt = Tl(None, self.MODt)
        with contextlib.ExitStack() as es:
            A = [self.sb(es, "nA%d" % k, [128, D], F32) for k in range(2)]
            B = [self.sb(es, "nB%d" % k, [128, D], F32) for k in range(2)]
            g = self.sb(es, "ng", [128, D], F32)
            S.dma("sp", g[:], gsrc[l:l + 1, :].partition_broadcast(128), [], [g])
            for k in range(2):
                S.dma("sp", A[k][:], self.MODR[k, :, sc * D:(sc + 1) * D], [modt], [A[k]])
                S.dma("sp", B[k][:], self.MODR[k, :, sh * D:(sh + 1) * D], [modt], [B[k]])
                self.V("dve", "scalar_tensor_tensor", [A[k], g], [A[k]], out=A[k][:], in0=A[k][:], scalar=1.0, in1=g[:],
                       op0=ALU.add, op1=ALU.mult)
            xs = [self.sb(es, "nx%d" % k, [128, D], F32) for k in range(3)]
            sq = self.sb(es, "nsq", [128, D], F32)
            t1 = [self.sb(es, "nt%d" % k, [128, D], F32) for k in range(2)]
            hb = [self.sb(es, "nhb%d" % k, [128, D], BF) for k in range(2)]
            st = [self.sb(es, "nst%d" % k, [128, 4], F32) for k in range(2)]
            hT = [self.sb(es, "nhT%d" % k, [128, 8, 512], BF) for k in range(2)]
            if which == 2:
                wr = self.sb(es, "nwr", [128, 8, 36], F32)
                S.dma("sp", wr[:], i["moe_w_r"][l].rearrange("(kc p) n -> p kc n", p=128), [], [wr])
                h32 = [self.sb(es, "nh32%d" % k, [128, D], F32) for k in range(2)]
                h32T = [self.sb(es, "nh32T%d" % k, [128, 8, 128], F32) for k in range(2)]
                Rt = [self.sb(es, "nR%d" % k, [128, 160], F32) for k in range(2)]
            st3 = st + [self.sb(es, "nst2", [128, 4], F32)]
            sq2 = [sq, self.sb(es, "nsq2", [128, D], F32)]
            chunks = [(ti, t0, N, cc) for ti, (t0, N) in enumerate(TILES) for cc in range(N // 128)]

            def stageA(c):
                x = xs[c % 3]
                s_ = st3[c % 3]
                S.dma("sp", x[:], self.X[c * 128:(c + 1) * 128, :], [Tl(None, self.Xt[c])], [x])
                self.act(sq2[c % 2][:], x[:], AF.Square, [x], [sq2[c % 2], s_], accum_out=s_[:, 0:1])
                self.V("dve", "tensor_scalar", [s_], [s_], out=s_[:, 1:2], in0=s_[:, 0:1], scalar1=1.0 / D, scalar2=EPS,
                       op0=ALU.mult, op1=ALU.add)
                self.V("dve", "reciprocal", [s_], [s_], out=s_[:, 2:3], in_=s_[:, 1:2])
                self.act(s_[:, 3:4], s_[:, 2:3], AF.Sqrt, [s_], [s_])

            def stageB(ti, t0, N, cc):
                c = t0 // 128 + cc
                lat = 0 if c >= 2 else 1
                ht = hT[ti % 2]
                x = xs[c % 3]
                s_ = st3[c % 3]
                t = t1[c % 2]
                self.V("dve", "scalar_tensor_tensor", [x, s_, A[lat]], [t], out=t[:], in0=x[:], scalar=s_[:, 3:4],
                       in1=A[lat][:], op0=ALU.mult, op1=ALU.mult)
                h = hb[c % 2]
                self.V("pool", "tensor_tensor", [t, B[lat]], [h], out=h[:], in0=t[:], in1=B[lat][:], op=ALU.add)
                ps = self.psb()
                pv = ps[:].bitcast(BF).rearrange("p (k n) -> p k n", k=8)
                for k in range(8):
                    self.tr(pv[:, k, :], h[:, k * 128:(k + 1) * 128], self.identb[:], [h, self.identb], [ps])
                self.V("dve", "tensor_copy", [ps], [ht], out=ht[:, :, cc * 128:(cc + 1) * 128], in_=pv)
                if which == 2:
                    self.route(c, t, B[lat], h32[c % 2], h32T[c % 2], wr, Rt[c % 2])
                    if SPARSE:
                        S.dma("sp", self.H2[c * 128:(c + 1) * 128, :], h[:], [h], [Tl(None, self.H2t)])
                        self.rank(c, Rt[c % 2])
                if cc == N // 128 - 1:
                    S.dma("sp", self.HT[:, :, t0:t0 + N].rearrange("k p n -> p k n"), ht[:, :, 0:N], [ht], [Tl(None, self.HTt[ti])])

            stageA(0)
            for idx, (ti, t0, N, cc) in enumerate(chunks):
                if idx + 1 < len(chunks):
                    stageA(idx + 1)
                stageB(ti, t0, N, cc)
            S.barrier()
        if "h1" in self.dbg_out and l == self.dbg.get("layer", 0) and which == 1:
            S.dma("sp", self.dbg_out["h1"], self.HT, [Tl(None, t) for t in self.HTt], [Tl(None, Trk())])


    def phase_sconv(self, l):
        S = self.S
        P = self.P
        with contextlib.ExitStack() as es:
            wb = self.load_w(es, "scwb", l, C_SB, 512)
            wc = self.load_w(es, "scwc", l, C_SC, 512)
            wx = self.load_w(es, "scwx", l, C_SX, 512)
            ub = self.sb(es, "scu", [128, T + 3], F32)
            bb = self.sb(es, "scb", [128, T], F32)
            hts = [self.sb(es, "scht%d" % k, [128, 8, 512], BF) for k in range(2)]
            cs = [self.sb(es, "sccs%d" % k, [128, 512], F32) for k in range(2)]
            acc = [self.sb(es, "scacc%d" % k, [128, 512], F32) for k in range(2)]
            ys = [self.sb(es, "scy%d" % k, [128, 512], BF) for k in range(2)]
            self.V("pool", "memset", [], [ub], ub[:], 0.0)
            n = 0
            for j in range(4):
                for ti, (t0, N) in enumerate(TILES):
                    ht = hts[n % 2]
                    c_ = cs[n % 2]
                    n += 1
                    self.load_ht(ht, ti)
                    base = t0 + 1 if ti == 0 else t0 + 2
                    pb, pc, px = self.psb(), self.psb(), self.psb()
                    self.proj(pb, wb, j * 128, ht, N)
                    self.proj(pc, wc, j * 128, ht, N)
                    self.proj(px, wx, j * 128, ht, N)
                    self.act(c_[:, 0:N], pc[:, 0:N], AF.Copy, [pc], [c_])
                    self.V("dve", "tensor_tensor", [c_, px], [ub], out=ub[:, base:base + N], in0=c_[:, 0:N], in1=px[:, 0:N], op=ALU.mult)
                    self.act(bb[:, t0:t0 + N], pb[:, 0:N], AF.Copy, [pb], [bb])
                for ti, (t0, N) in enumerate(TILES):
                    base = t0 + 1 if ti == 0 else t0 + 2
                    a = acc[ti % 2]
                    y = ys[ti % 2]
                    self.V("dve", "tensor_scalar", [ub, P], [a], out=a[:, 0:N], in0=ub[:, base - 1:base - 1 + N], scalar1=P[:, j * 3:j * 3 + 1],
                           scalar2=0.0, op0=ALU.mult, op1=ALU.add)
                    for k in (1, 2):
                        self.V("dve", "scalar_tensor_tensor", [ub, P, a], [a], out=a[:, 0:N], in0=ub[:, base - 1 + k:base - 1 + k + N],
                               scalar=P[:, j * 3 + k:j * 3 + k + 1], in1=a[:, 0:N], op0=ALU.mult, op1=ALU.add)
                    self.V("pool", "tensor_tensor", [a, bb], [y], out=y[:, 0:N], in0=a[:, 0:N], in1=bb[:, t0:t0 + N], op=ALU.mult)
                    S.dma("sp", self.Y[3, j, :, t0:t0 + N], y[:, 0:N], [y], [Tl(None, self.Yt[3][ti])])
            S.barrier()

    def phase_conv(self, l):
        S = self.S
        P = self.P
        onesf = self.cst[:, 128:256]
        with contextlib.ExitStack() as es:
            wa = self.load_w(es, "cvwa", l, C_CVA, 512)
            wg = self.load_w(es, "cvwg", l, C_CVG, 512)
            vb = self.sb(es, "cvv", [128, 4, T + 45], BF)
            hts = [self.sb(es, "cvht%d" % k, [128, 8, 512], BF) for k in range(2)]
            sg = [self.sb(es, "cvsg%d" % k, [128, 512], F32) for k in range(2)]
            self.V("pool", "memset", [], [vb], vb[:], 0.0)
            n = 0
            for ti, (t0, N) in enumerate(TILES):
                ht = hts[ti % 2]
                self.load_ht(ht, ti)
                base = t0 + 15 if ti == 0 else t0 + 30
                for j in range(4):
                    pa, pg = self.psb(), self.psb()
                    self.proj(pa, wa, j * 128, ht, N)
                    self.proj(pg, wg, j * 128, ht, N)
                    s_ = sg[n % 2]
                    n += 1
                    self.act(s_[:, 0:N], pg[:, 0:N], AF.Sigmoid, [pg], [s_])
                    self.V("dve", "tensor_tensor", [s_, pa], [vb], out=vb[:, j, base:base + N], in0=pa[:, 0:N], in1=s_[:, 0:N], op=ALU.mult)
            DG = self.sb(es, "cvDG", [128, 124, 128], BF)
            for jk in range(124):
                self.V("pool", "tensor_scalar", [self.identb, P], [DG], out=DG[:, jk, :], in0=self.identb[:], scalar1=P[:, 12 + jk:13 + jk], scalar2=0.0,
                       op0=ALU.mult, op1=ALU.add)
            ca = [self.sb(es, "cvca%d" % k, [128, 4, 512], F32) for k in range(2)]
            cp = self.sb(es, "cvcp", [128, 512], F32)
            sq = self.sb(es, "cvsq", [128, 4, 512], F32)
            mean = self.sb(es, "cvmean", [128, 512], F32)
            tmp = self.sb(es, "cvtmp", [128, 512], F32)
            rstd = self.sb(es, "cvrstd", [128, 512], F32)
            dd = [self.sb(es, "cvd%d" % k, [128, 512], F32) for k in range(2)]
            ys = [self.sb(es, "cvy%d" % k, [128, 4, 512], BF) for k in range(2)]
            for ti, (t0, N) in enumerate(TILES):
                base = t0 + 15 if ti == 0 else t0 + 30
                a = ca[ti % 2]
                for j in range(4):
                    pc = self.psb()
                    for k in range(31):
                        self.mm(pc[:, 0:N], DG[:, j * 31 + k, :], vb[:, j, base - 15 + k:base - 15 + k + N], k == 0, k == 30, [DG, vb], [pc])
                    self.act(a[:, j, 0:N], pc[:, 0:N], AF.Identity, [pc, P], [a], bias=P[:, 136 + j:137 + j])
                    self.act(sq[:, j, 0:N], a[:, j, 0:N], AF.Square, [a], [sq])
                p1, p2 = self.psb(), self.psb()
                for j in range(4):
                    self.mm(p1[:, 0:N], onesf, a[:, j, 0:N], j == 0, j == 3, [self.cst, a], [p1])
                for j in range(4):
                    self.mm(p2[:, 0:N], onesf, sq[:, j, 0:N], j == 0, j == 3, [self.cst, sq], [p2])
                self.act(mean[:, 0:N], p1[:, 0:N], AF.Copy, [p1], [mean], scale=1.0 / W)
                self.V("pool", "tensor_tensor", [mean], [tmp], out=tmp[:, 0:N], in0=mean[:, 0:N], in1=mean[:, 0:N], op=ALU.mult)
                self.V("dve", "scalar_tensor_tensor", [p2, tmp], [tmp], out=tmp[:, 0:N], in0=p2[:, 0:N], scalar=1.0 / W, in1=tmp[:, 0:N],
                       op0=ALU.mult, op1=ALU.subtract)
                self.rstd_from(tmp[:, 0:N], 1.0, rstd, tmp, N, [tmp])
                y = ys[ti % 2]
                for j in range(4):
                    d = dd[j % 2]
                    self.V("dve", "tensor_tensor", [a, mean], [d], out=d[:, 0:N], in0=a[:, j, 0:N], in1=mean[:, 0:N], op=ALU.subtract)
                    self.V("pool", "tensor_tensor", [d, rstd], [d], out=d[:, 0:N], in0=d[:, 0:N], in1=rstd[:, 0:N], op=ALU.mult)
                    self.V("dve", "tensor_scalar", [d, P], [d], out=d[:, 0:N], in0=d[:, 0:N], scalar1=P[:, 140 + j:141 + j], scalar2=P[:, 144 + j:145 + j],
                           op0=ALU.mult, op1=ALU.add)
                    self.act(y[:, j, 0:N], d[:, 0:N], AF.Silu, [d], [y])
                S.dma("sp", self.Y[2, :, :, t0:t0 + N].rearrange("j p n -> p j n"), y[:, :, 0:N], [y], [Tl(None, self.Yt[2][ti])])
            S.barrier()

    def rope(self, ps, raw, cos, sin, t1, t2, dst_ap, dst_t, N, rot_ps):
        self.act(raw[:, 0:N], ps[:, 0:N], AF.Copy, [ps], [raw])
        self.mm(rot_ps[:, 0:N], self.Rb[:], raw[:, 0:N], True, True, [self.Rb, raw], [rot_ps])
        self.V("pool", "tensor_tensor", [raw, cos], [t1], out=t1[:, 0:N], in0=raw[:, 0:N], in1=cos[:, 0:N], op=ALU.mult)
        self.V("dve", "tensor_tensor", [rot_ps, sin], [t2], out=t2[:, 0:N], in0=rot_ps[:, 0:N], in1=sin[:, 0:N], op=ALU.mult)
        self.V("pool", "tensor_tensor", [t1, t2], [dst_t], out=dst_ap, in0=t1[:, 0:N], in1=t2[:, 0:N], op=ALU.add)

    def phase_attn(self, l):
        S = self.S
        P = self.P
        i = self.i
        onesf = self.cst[:, 128:256]
        B = self.banks
        with contextlib.ExitStack() as es:
            wq = self.load_w(es, "dawq", l, C_DQ, 512)
            wk = self.load_w(es, "dawk", l, C_DK, 512)
            wv = self.load_w(es, "dawv", l, C_DV, 512)
            KT = self.sb(es, "daKT", [128, 4, T], BF)
            Vt = self.sb(es, "daV", [128, NCH, 512], BF)
            hts = [self.sb(es, "daht%d" % k, [128, 8, 512], BF) for k in range(2)]
            cos = [self.sb(es, "dacos%d" % k, [128, 512], F32) for k in range(2)]
            sin = [self.sb(es, "dasin%d" % k, [128, 512], F32) for k in range(2)]
            raw = [self.sb(es, "daraw%d" % k, [128, 512], BF) for k in range(2)]
            t1 = [self.sb(es, "dat1%d" % k, [128, 512], F32) for k in range(2)]
            t2 = [self.sb(es, "dat2%d" % k, [128, 512], F32) for k in range(2)]
            n = 0
            for ti, (t0, N) in enumerate(TILES):
                ht = hts[ti % 2]
                self.load_ht(ht, ti)
                S.dma("sp", cos[ti % 2][:, 0:N], i["rope"][0, :, t0:t0 + N], [], [cos[ti % 2]])
                S.dma("sp", sin[ti % 2][:, 0:N], i["rope"][1, :, t0:t0 + N], [], [sin[ti % 2]])
                for h in range(4):
                    ps, rp = self.psb(), self.psb()
                    self.proj(ps, wk, h * 128, ht, N)
                    self.rope(ps, raw[n % 2], cos[ti % 2], sin[ti % 2], t1[n % 2], t2[n % 2], KT[:, h, t0:t0 + N], KT, N, rp)
                    n += 1
                for cc in range(N // 128):
                    ps = self.psb()
                    for kc in range(8):
                        self.mm(ps[:, :], ht[:, kc, cc * 128:(cc + 1) * 128], wv[:, kc, :], kc == 0, kc == 7, [ht, wv], [ps])
                    self.act(Vt[:, t0 // 128 + cc, :], ps[:, :], AF.Copy, [ps], [Vt])
            QT = [self.sb(es, "daQT%d" % k, [128, 512], BF) for k in range(2)]
            QZ = [[self.sb(es, "daQZ%d_%d" % (k, c), [128, 512], BF) for c in range(2)] for k in range(2)]
            for k in range(2):
                for c in range(2):
                    self.V("pool", "memset", [], [QZ[k][c]], QZ[k][c][:], 0.0)
            pT = [self.sb(es, "dapT%d" % k, [128, 512], BF) for k in range(4)]
            rd = [self.sb(es, "dard%d" % k, [128, 512], F32) for k in range(2)]
            rp_ = [self.sb(es, "darp%d" % k, [128, 512], F32) for k in range(2)]
            rinv = self.sb(es, "darinv", [128, 512], F32)
            Oc = [self.sb(es, "daOc%d" % k, [128, 512], F32) for k in range(2)]
            o = self.sb(es, "dao", [128, 512], F32)
            sq = self.sb(es, "dasq", [128, 512], F32)
            tmp = self.sb(es, "datmp", [128, 512], F32)
            rstd = self.sb(es, "darstd", [128, 512], F32)
            ys = [self.sb(es, "day%d" % k, [128, 4, 512], BF) for k in range(2)]
            it = 0
            for ti, (t0, N) in enumerate(TILES):
                ht = hts[ti % 2]
                self.load_ht(ht, ti)
                S.dma("sp", cos[ti % 2][:, 0:N], i["rope"][0, :, t0:t0 + N], [], [cos[ti % 2]])
                S.dma("sp", sin[ti % 2][:, 0:N], i["rope"][1, :, t0:t0 + N], [], [sin[ti % 2]])
                nk = 2 if ti == 0 else NCH
                y = ys[ti % 2]
                for h in range(4):
                    qt = QT[h % 2]
                    self.proj(B[7], wq, h * 128, ht, N)
                    self.rope(B[7], raw[n % 2], cos[ti % 2], sin[ti % 2], t1[n % 2], t2[n % 2], qt[:, 0:N], qt, N, B[2])
                    n += 1
                    qz = QZ[h % 2]
                    self.V("pool", "tensor_copy", [qt], [qz[0]], out=qz[0][0:64, 0:N], in_=qt[0:64, 0:N])
                    self.V("dve", "tensor_copy", [qt], [qz[1]], out=qz[1][64:128, 0:N], in_=qt[64:128, 0:N])
                    its = [(c, kc) for c in range(2) for kc in range(nk)]
                    slots = {}
                    def qk(j):
                        c, kc = its[j]
                        sps = B[2 + it_base[0] % 3]
                        p = pT[it_base[0] % 4]
                        it_base[0] += 1
                        slots[j] = (sps, p)
                        p0 = c * 64
                        self.mm(sps[:, 0:N], KT[:, h, kc * 128:(kc + 1) * 128], qz[c][:, 0:N], True, True, [KT, qz[c]], [sps])
                    it_base = [it]
                    qk(0)
                    if len(its) > 1:
                        qk(1)
                    for j, (c, kc) in enumerate(its):
                        sps, p = slots.pop(j)
                        oacc = B[c]
                        self.act(p[:, 0:N], sps[:, 0:N], AF.Exp, [sps], [p], scale=0.125)
                        if j + 2 < len(its):
                            qk(j + 2)
                        self.mm(oacc[:, 0:N], Vt[:, kc, h * 128:(h + 1) * 128], p[:, 0:N], kc == 0, kc == nk - 1, [Vt, p], [oacc])
                        rsb = B[5 + c]
                        self.mm(rsb[:, 0:N], self.onesb[:], p[:, 0:N], kc == 0, kc == nk - 1, [self.onesb, p], [rsb])
                        if kc == nk - 1:
                            self.V("dve", "reciprocal", [rsb], [rinv], out=rinv[:, 0:N], in_=rsb[:, 0:N])
                            self.V("dve", "tensor_tensor", [oacc, rinv], [Oc[c]], out=Oc[c][:, 0:N], in0=oacc[:, 0:N], in1=rinv[:, 0:N], op=ALU.mult)
                    it = it_base[0]
                    self.V("dve", "scalar_tensor_tensor", [Oc[0], Oc[1], P], [o], out=o[:, 0:N], in0=Oc[1][:, 0:N], scalar=P[:, 153:154], in1=Oc[0][:, 0:N],
                           op0=ALU.mult, op1=ALU.add)
                    self.act(sq[:, 0:N], o[:, 0:N], AF.Square, [o], [sq])
                    self.mm(B[7][:, 0:N], onesf, sq[:, 0:N], True, True, [self.cst, sq], [B[7]])
                    self.rstd_from(B[7][:, 0:N], 1.0 / 128, rstd, tmp, N, [B[7]])
                    self.V("dve", "scalar_tensor_tensor", [o, rstd, P], [y], out=y[:, h, 0:N], in0=o[:, 0:N], scalar=P[:, 154:155], in1=rstd[:, 0:N],
                           op0=ALU.mult, op1=ALU.mult)
                S.dma("sp", self.Y[1, :, :, t0:t0 + N].rearrange("j p n -> p j n"), y[:, :, 0:N], [y], [Tl(None, self.Yt[1][ti])])
            S.barrier()

    def phase_hgrn(self, l):
        S = self.S
        B = self.banks
        with contextlib.ExitStack() as es:
            rot = [2]
            def rb():
                b = B[rot[0]]
                rot[0] = 2 + (rot[0] - 1) % 6
                return b
            sh = {k: self.sb(es, "hgs_" + k, [128, 512], F32) for k in ("osum", "sq", "tmp", "rstd", "sgg", "y1")}
            sh["ys"] = [self.sb(es, "hgys%d" % k, [128, 512], BF) for k in range(2)]
            sh["n"] = 0
            chains = []
            for k in range(2):
                c = {"k": k}
                c["OF"] = self.sb(es, "hgOF%d" % k, [128, T], F32)
                c["S32"] = self.sb(es, "hgS%d" % k, [128, 128], F32)
                c["Sbf"] = [self.sb(es, "hgSb%d_%d" % (k, j), [128, 128], BF) for j in range(2)]
                c["ht"] = self.sb(es, "hght%d" % k, [128, 8, 512], BF)
                c["w"] = [self.sb(es, "hgw%d_%d" % (k, j), [128, 8, 128], BF) for j in range(4)]
                for nm in ("q32", "ee", "ff", "lf", "kk", "pre", "bb", "d3", "d2", "d3a"):
                    c[nm] = self.sb(es, "hg%s%d" % (nm, k), [128, 512], F32)
                for nm in ("qE1", "qE3", "kE4", "kE2", "qE3b", "kE4b"):
                    c[nm] = self.sb(es, "hg%s%d" % (nm, k), [128, 512], BF)
                c["iT"] = self.sb(es, "hgiT%d" % k, [128, 4, 128], BF)
                c["kT"] = self.sb(es, "hgkT%d" % k, [128, 4, 128], BF)
                c["AM"] = [self.sb(es, "hgAM%d_%d" % (k, j), [128, 128], BF) for j in range(2)]
                c["a1"] = [self.sb(es, "hga1%d_%d" % (k, j), [128, 128], F32) for j in range(2)]
                c["a2"] = [self.sb(es, "hga2%d_%d" % (k, j), [128, 128], F32) for j in range(2)]
                c["ops"] = B[k]
                chains.append(c)
            for pair in ((0, 1), (2, 3)):
                for d in range(2):
                    gens = [self.hgrn_chain(l, h, d, chains[k], sh, rb) for k, h in enumerate(pair)]
                    live = list(gens)
                    while live:
                        for g in list(live):
                            try:
                                next(g)
                            except StopIteration:
                                live.remove(g)
            S.barrier()

    def hgrn_chain(self, l, h, d, c, sh, rb):
        S = self.S
        P = self.P
        onesf = self.cst[:, 128:256]
        w = c["w"]
        OF, S32, Sbf, ht, ops = c["OF"], c["S32"], c["Sbf"], c["ht"], c["ops"]
        q32, ee, ff, lf, kk, pre, bb, d3, d2, d3a = (c[n] for n in ("q32", "ee", "ff", "lf", "kk", "pre", "bb", "d3", "d2", "d3a"))
        qE1, qE3, kE4, kE2, qE3b, kE4b = (c[n] for n in ("qE1", "qE3", "kE4", "kE2", "qE3b", "kE4b"))
        iT, kT = c["iT"], c["kT"]
        E1, E3, E3b, E4b, E2, E4 = ee, ff, lf, d3, d2, d3a
        cols = (C_HQ, C_HFF if d == 0 else C_HFB, C_HI, C_HG)
        for k in range(4):
            S.dma("pool", w[k][:], self.i["w_in"][l, :, cols[k] + h * 128:cols[k] + (h + 1) * 128].rearrange("(kc p) n -> p kc n", p=128), [], [w[k]])
        self.V("pool", "memset", [], [S32], S32[:], 0.0)
        self.V("pool", "memset", [], [Sbf[0]], Sbf[0][:], 0.0)
        cur = 0
        am_i = 0
        lbc = self.LB[:, l, d * 4 + h:d * 4 + h + 1]
        omc = self.OML[:, l, d * 4 + h:d * 4 + h + 1]
        order = list(range(9)) if d == 0 else [0] + list(range(8, 0, -1))
        mask = self.cst[:, 384:512] if d == 0 else self.cst[:, 512:640]
        masko = self.cst[:, 640:768] if d == 0 else self.cst[:, 768:896]
        for ti in order:
            t0, N = TILES[ti]
            nb = N // 128
            nch = N // 64
            self.load_ht(ht, ti)
            pq, pf = rb(), rb()
            self.proj(pq, w[0], 0, ht, N)
            self.proj(pf, w[1], 0, ht, N)
            self.act(q32[:, 0:N], pq[:, 0:N], AF.Copy, [pq], [q32])
            self.act(ee[:, 0:N], pf[:, 0:N], AF.Exp, [pf], [ee], scale=-1.0)
            self.V("dve", "tensor_scalar", [ee], [ee], out=ee[:, 0:N], in0=ee[:, 0:N], scalar1=1.0, scalar2=1.0, op0=ALU.add, op1=ALU.mult)
            self.V("dve", "reciprocal", [ee], [ee], out=ee[:, 0:N], in_=ee[:, 0:N])
            self.V("dve", "tensor_scalar", [ee, self.LB, self.OML], [ff], out=ff[:, 0:N], in0=ee[:, 0:N], scalar1=omc, scalar2=lbc, op0=ALU.mult, op1=ALU.add)
            self.act(lf[:, 0:N], ff[:, 0:N], AF.Ln, [ff], [lf])
            self.V("pool", "tensor_scalar", [ff], [kk], out=kk[:, 0:N], in0=ff[:, 0:N], scalar1=-1.0, scalar2=1.0, op0=ALU.mult, op1=ALU.add)
            self.V("dve", "tensor_tensor_scan", [self.seg, lf], [pre], out=pre[:, 0:N], data0=self.seg[:, 0:N], data1=lf[:, 0:N], initial=0.0, op0=ALU.mult, op1=ALU.add)
            v3 = lambda t_: t_[:, 0:N].rearrange("p (c s) -> p c s", s=64)
            v32 = lambda t_: t_[:, 0:N].rearrange("p (c s) -> p c s", s=32)
            bc = lambda t_, col: v3(t_)[:, :, col:col + 1].to_broadcast([128, nch, 64])
            if d == 0:
                b_ = pre
                cend = 63
            else:
                b_ = bb
                cend = 0
                self.V("dve", "tensor_tensor", [lf, pre], [bb], out=bb[:, 0:N], in0=lf[:, 0:N], in1=pre[:, 0:N], op=ALU.subtract)
                self.V("dve", "tensor_tensor", [bb, pre], [bb], out=v3(bb), in0=v3(bb), in1=bc(pre, 63), op=ALU.add)
            yield
            self.V("dve", "tensor_tensor", [b_], [d3], out=v3(d3), in0=v3(b_), in1=bc(b_, 32), op=ALU.subtract)
            self.V("pool", "tensor_tensor", [b_], [d2], out=v3(d2), in0=v3(b_), in1=bc(b_, cend), op=ALU.subtract)
            self.V("dve", "tensor_tensor", [b_], [d3a], out=v32(d3a), in0=v32(b_), in1=v32(b_)[:, :, 16:17].to_broadcast([128, 2 * nch, 32]), op=ALU.subtract)
            self.act(E1[:, 0:N], b_[:, 0:N], AF.Exp, [b_], [E1])
            self.act(E2[:, 0:N], d2[:, 0:N], AF.Exp, [d2], [E2], scale=-1.0)
            self.act(E3[:, 0:N], d3a[:, 0:N], AF.Exp, [d3a], [E3])
            self.act(E4[:, 0:N], d3a[:, 0:N], AF.Exp, [d3a], [E4], scale=-1.0)
            self.act(E3b[:, 0:N], d3[:, 0:N], AF.Exp, [d3], [E3b])
            self.act(E4b[:, 0:N], d3[:, 0:N], AF.Exp, [d3], [E4b], scale=-1.0)
            self.V("dve", "tensor_tensor", [q32, E1], [qE1], out=qE1[:, 0:N], in0=q32[:, 0:N], in1=E1[:, 0:N], op=ALU.mult)
            self.V("pool", "tensor_tensor", [q32, E3], [qE3], out=qE3[:, 0:N], in0=q32[:, 0:N], in1=E3[:, 0:N], op=ALU.mult)
            self.V("dve", "tensor_tensor", [kk, E4], [kE4], out=kE4[:, 0:N], in0=kk[:, 0:N], in1=E4[:, 0:N], op=ALU.mult)
            self.V("pool", "tensor_tensor", [kk, E2], [kE2], out=kE2[:, 0:N], in0=kk[:, 0:N], in1=E2[:, 0:N], op=ALU.mult)
            self.V("dve", "tensor_tensor", [q32, E3b], [qE3b], out=qE3b[:, 0:N], in0=q32[:, 0:N], in1=E3b[:, 0:N], op=ALU.mult)
            self.V("pool", "tensor_tensor", [kk, E4b], [kE4b], out=kE4b[:, 0:N], in0=kk[:, 0:N], in1=E4b[:, 0:N], op=ALU.mult)
            qz, kz = (slice(0, 32), slice(32, 64)) if d == 0 else (slice(32, 64), slice(0, 32))
            self.V("dve", "memset", [qE3b], [qE3b], v3(qE3b)[:, :, qz], 0.0)
            self.V("pool", "memset", [kE4b], [kE4b], v3(kE4b)[:, :, kz], 0.0)
            yield
            for cc in range(nb):
                pi = rb()
                for kc in range(8):
                    self.mm(pi[:, 0:128], ht[:, kc, cc * 128:(cc + 1) * 128], w[2][:, kc, :], kc == 0, kc == 7, [ht, w[2]], [pi])
                self.act(iT[:, cc, :], pi[:, 0:128], AF.Copy, [pi], [iT])
                pt = rb()
                ptv = pt[:].bitcast(BF)
                self.tr(ptv[:, 0:128], kE2[:, cc * 128:(cc + 1) * 128], self.identb[:], [kE2, self.identb], [pt])
                self.act(kT[:, cc, :], ptv[:, 0:128], AF.Copy, [pt], [kT])
            yield
            blks = list(range(nb)) if d == 0 else list(range(nb - 1, -1, -1))
            for blk in blks:
                pa = rb()
                bs = slice(blk * 128, (blk + 1) * 128)
                self.mm(pa[:, 0:128], kE4[:, bs], qE3[:, bs], True, True, [kE4, qE3], [pa])
                pb_ = rb()
                self.mm(pb_[:, 0:128], kE4b[:, bs], qE3b[:, bs], True, True, [kE4b, qE3b], [pb_])
                am = c["AM"][am_i % 2]
                a1 = c["a1"][am_i % 2]
                a2 = c["a2"][am_i % 2]
                am_i += 1
                self.V("dve", "tensor_tensor", [pa, self.cst], [a1], out=a1[:], in0=pa[:, 0:128], in1=mask, op=ALU.mult)
                self.V("dve", "tensor_tensor", [pb_, self.cst], [a2], out=a2[:], in0=pb_[:, 0:128], in1=masko, op=ALU.mult)
                self.V("pool", "tensor_tensor", [a1, a2], [am], out=am[:], in0=a1[:], in1=a2[:], op=ALU.add)
                for ch in ((0, 1) if d == 0 else (1, 0)):
                    p0 = ch * 64
                    c0 = blk * 128 + p0
                    self.mm(ops[:, c0:c0 + 64], Sbf[cur][:], qE1[:, c0:c0 + 64], True, False, [Sbf[cur], qE1], [ops])
                    self.mm(ops[:, c0:c0 + 64], iT[p0:p0 + 64, blk, :], am[p0:p0 + 64, p0:p0 + 64], False, True, [iT, am], [ops])
                    pS = rb()
                    self.mm(pS[:, 0:128], kT[p0:p0 + 64, blk, :], iT[p0:p0 + 64, blk, :], True, True, [kT, iT], [pS])
                    ce = c0 + cend
                    self.V("dve", "scalar_tensor_tensor", [S32, E1, pS], [S32], out=S32[:], in0=S32[:], scalar=E1[:, ce:ce + 1], in1=pS[:, 0:128],
                           op0=ALU.mult, op1=ALU.add)
                    cur = 1 - cur
                    self.act(Sbf[cur][:], S32[:], AF.Copy, [S32], [Sbf[cur]])
                    yield
            if d == 0:
                self.act(OF[:, t0:t0 + N], ops[:, 0:N], AF.Copy, [ops], [OF])
            else:
                osum, sq, tmp, rstd, sgg, y1 = (sh[n_] for n_ in ("osum", "sq", "tmp", "rstd", "sgg", "y1"))
                self.V("dve", "tensor_tensor", [OF, ops], [osum], out=osum[:, 0:N], in0=OF[:, t0:t0 + N], in1=ops[:, 0:N], op=ALU.add)
                self.act(sq[:, 0:N], osum[:, 0:N], AF.Square, [osum], [sq])
                pss = rb()
                self.mm(pss[:, 0:N], onesf, sq[:, 0:N], True, True, [self.cst, sq], [pss])
                self.rstd_from(pss[:, 0:N], 1.0 / 128, rstd, tmp, N, [pss])
                pg = rb()
                self.proj(pg, w[3], 0, ht, N)
                self.act(sgg[:, 0:N], pg[:, 0:N], AF.Sigmoid, [pg], [sgg])
                self.V("dve", "scalar_tensor_tensor", [osum, rstd, P], [y1], out=y1[:, 0:N], in0=osum[:, 0:N], scalar=P[:, 148 + h:149 + h], in1=rstd[:, 0:N],
                       op0=ALU.mult, op1=ALU.mult)
                y = sh["ys"][sh["n"] % 2]
                sh["n"] += 1
                self.V("pool", "tensor_tensor", [y1, sgg], [y], out=y[:, 0:N], in0=y1[:, 0:N], in1=sgg[:, 0:N], op=ALU.mult)
                S.dma("sp", self.Y[0, h, :, t0:t0 + N], y[:, 0:N], [y], [Tl(None, self.Yt[0][ti])])
            yield

    def phase_merge(self, l):
        S = self.S
        i = self.i
        with contextlib.ExitStack() as es:
            wg = self.sb(es, "mgwg", [128, 8, 4096], BF)
            for k in range(4):
                S.dma("pool", wg[:, :, k * 1024:(k + 1) * 1024], i["w_in"][l, :, C_GATE + k * 1024:C_GATE + (k + 1) * 1024].rearrange("(kc p) n -> p kc n", p=128), [], [wg])
            wb = self.sb(es, "mgwb", [128, 4, 4, 1024], BF)
            for k in range(4):
                S.dma("pool", wb[:, k, :, :], i["w_branch"][l, k].rearrange("(cc p) n -> p cc n", p=128), [], [wb])
            ht = self.sb(es, "mght", [128, 8, 512], BF)
            Yk = [self.sb(es, "mgY%d" % k, [128, 4, 512], BF) for k in range(4)]
            sg = [self.sb(es, "mgsg%d" % k, [128, 512], F32) for k in range(2)]
            tmp = [self.sb(es, "mgtmp%d" % k, [128, 512], F32) for k in range(2)]
            macc = [self.sb(es, "mgacc%d" % k, [128, 512], F32) for k in range(2)]
            mT = [self.sb(es, "mgmT%d" % k, [128, 8, 512], BF) for k in range(2)]
            n = 0
            for ti, (t0, N) in enumerate(TILES):
                self.load_ht(ht, ti)
                for k in range(4):
                    S.dma("sp", Yk[k][:, :, 0:N], self.Y[k, :, :, t0:t0 + N].rearrange("j p n -> p j n"), [Tl(None, self.Yt[k][ti])], [Yk[k]])
                m = mT[ti % 2]
                for nch in range(8):
                    ma = macc[nch % 2]
                    for k in range(4):
                        pg, pp = self.psb(), self.psb()
                        self.proj(pg, wg, k * 1024 + nch * 128, ht, N)
                        for cc in range(4):
                            self.mm(pp[:, 0:N], wb[:, k, cc, nch * 128:(nch + 1) * 128], Yk[k][:, cc, 0:N], cc == 0, cc == 3, [wb, Yk[k]], [pp])
                        s_ = sg[n % 2]
                        t_ = tmp[n % 2]
                        n += 1
                        self.act(s_[:, 0:N], pg[:, 0:N], AF.Sigmoid, [pg], [s_])
                        if k == 0:
                            self.V("dve", "tensor_tensor", [pp, s_], [ma], out=ma[:, 0:N], in0=pp[:, 0:N], in1=s_[:, 0:N], op=ALU.mult)
                        else:
                            self.V("dve", "tensor_tensor", [pp, s_], [t_], out=t_[:, 0:N], in0=pp[:, 0:N], in1=s_[:, 0:N], op=ALU.mult)
                            self.V("pool", "tensor_tensor", [ma, t_], [ma], out=ma[:, 0:N], in0=ma[:, 0:N], in1=t_[:, 0:N], op=ALU.add)
                    self.V("pool", "tensor_copy", [ma], [m], out=m[:, nch, 0:N], in_=ma[:, 0:N])
                S.dma("sp", self.MT[:, :, t0:t0 + N].rearrange("k p n -> p k n"), m[:, :, 0:N], [m], [Tl(None, self.MTt[ti])])
            S.barrier()
        with contextlib.ExitStack() as es:
            wo = self.sb(es, "mgwo", [128, 8, 1024], BF)
            S.dma("pool", wo[:], i["w_out"][l].rearrange("(kc p) n -> p kc n", p=128), [], [wo])
            mts = [self.sb(es, "mgmt%d" % k, [128, 8, 512], BF) for k in range(2)]
            self.residual_setup(es, 2)
            for ti, (t0, N) in enumerate(TILES):
                mt = mts[ti % 2]
                S.dma("sp", mt[:, :, 0:N], self.MT[:, :, t0:t0 + N].rearrange("k p n -> p k n"), [Tl(None, self.MTt[ti])], [mt])
                for cc in range(N // 128):
                    c = t0 // 128 + cc
                    halves = []
                    for half in range(2):
                        po = self.psb()
                        for kc in range(8):
                            self.mm(po[:, :], mt[:, kc, cc * 128:(cc + 1) * 128], wo[:, kc, half * 512:(half + 1) * 512], kc == 0, kc == 7, [mt, wo], [po])
                        halves.append(po)
                    self.residual(c, lambda half: halves[half][:, :], halves)
            S.barrier()

    def residual_setup(self, es, modidx):
        S = self.S
        self.rmod = [self.sb(es, "rsmod%d" % k, [128, D], F32) for k in range(2)]
        for k in range(2):
            S.dma("sp", self.rmod[k][:], self.MODR[k, :, modidx * D:(modidx + 1) * D], [Tl(None, self.MODt)], [self.rmod[k]])
        self.rx = [self.sb(es, "rsx%d" % k, [128, D], F32) for k in range(3)]
        self.rtmp = [self.sb(es, "rstmp%d" % k, [128, D], F32) for k in range(2)]

    def residual(self, c, delta_ap, delta_tiles):
        S = self.S
        lat = 0 if c >= 2 else 1
        x = self.rx[c % 3]
        t = self.rtmp[c % 2]
        xt = Tl(None, self.Xt[c])
        S.dma("sp", x[:], self.X[c * 128:(c + 1) * 128, :], [xt], [x])
        for half in range(2):
            hs = slice(half * 512, (half + 1) * 512)
            self.V("dve", "tensor_tensor", [delta_tiles[half], self.rmod[lat]], [t], out=t[:, hs], in0=delta_ap(half), in1=self.rmod[lat][:, hs], op=ALU.mult)
        self.V("dve", "tensor_tensor", [x, t], [x], out=x[:], in0=x[:], in1=t[:], op=ALU.add)
        S.dma("act", self.X[c * 128:(c + 1) * 128, :], x[:], [x], [xt])

    def route(self, c, t, Bm, h32, h32T, wr, R):
        P = self.P
        self.V("pool", "tensor_tensor", [t, Bm], [h32], out=h32[:], in0=t[:], in1=Bm[:], op=ALU.add)
        for g in range(2):
            ps = self.psb()
            for k in range(4):
                kk = g * 4 + k
                self.tr(ps[:, k * 128:(k + 1) * 128], h32[:, kk * 128:(kk + 1) * 128], self.cst[:, 0:128], [h32, self.cst], [ps])
            self.act(h32T[:, g * 4:(g + 1) * 4, :], ps[:].rearrange("p (k n) -> p k n", k=4), AF.Copy, [ps], [h32T])
        pl = self.psb()
        for kc in range(8):
            self.mm(pl[:, 0:36], h32T[:, kc, :], wr[:, kc, :], kc == 0, kc == 7, [h32T, wr], [pl])
        dv = lambda name, w_, **kw: self.V("dve", name, [R, P] + w_[1:], [w_[0]], **kw)
        self.V("dve", "tensor_tensor", [pl, P], [R], out=R[:, 0:36], in0=pl[:, 0:36], in1=P[:, 416:452], op=ALU.add)
        RR = [R]
        dv("tensor_reduce", RR, out=R[:, 36:37], in_=R[:, 0:4], axis=AX.X, op=ALU.max)
        dv("tensor_scalar", RR, out=R[:, 37:38], in0=R[:, 36:37], scalar1=-1.0, scalar2=0.0, op0=ALU.mult, op1=ALU.add)
        self.act(R[:, 44:48], R[:, 0:4], AF.Exp, [R], [R], bias=R[:, 37:38], accum_out=R[:, 38:39])
        dv("reciprocal", RR, out=R[:, 39:40], in_=R[:, 38:39])
        dv("tensor_scalar", RR, out=R[:, 40:44], in0=R[:, 0:4], scalar1=R[:, 36:37], scalar2=1.0, op0=ALU.is_equal, op1=ALU.mult)
        dv("tensor_tensor", RR, out=R[:, 48:80].rearrange("p (g e) -> p g e", g=4), in0=R[:, 4:36].rearrange("p (g e) -> p g e", g=4),
           in1=R[:, 40:44].unsqueeze(2).to_broadcast([128, 4, 8]), op=ALU.mult)
        dv("tensor_reduce", RR, out=R[:, 80:88], in_=R[:, 48:80].rearrange("p (g e) -> p e g", g=4), axis=AX.X, op=ALU.add)
        dv("tensor_reduce", RR, out=R[:, 88:89], in_=R[:, 80:88], axis=AX.X, op=ALU.max)
        dv("tensor_scalar", RR, out=R[:, 89:97], in0=R[:, 80:88], scalar1=R[:, 88:89], scalar2=1.0, op0=ALU.is_equal, op1=ALU.mult)
        dv("scalar_tensor_tensor", RR, out=R[:, 97:105], in0=R[:, 89:97], scalar=-1e30, in1=R[:, 80:88], op0=ALU.mult, op1=ALU.add)
        dv("tensor_reduce", RR, out=R[:, 105:106], in_=R[:, 97:105], axis=AX.X, op=ALU.max)
        dv("tensor_scalar", RR, out=R[:, 106:114], in0=R[:, 97:105], scalar1=R[:, 105:106], scalar2=1.0, op0=ALU.is_equal, op1=ALU.mult)
        dv("tensor_tensor", RR, out=R[:, 114:115], in0=R[:, 105:106], in1=R[:, 88:89], op=ALU.subtract)
        self.act(R[:, 115:116], R[:, 114:115], AF.Exp, [R], [R])
        dv("tensor_scalar", RR, out=R[:, 116:117], in0=R[:, 115:116], scalar1=1.0, scalar2=1.0, op0=ALU.add, op1=ALU.mult)
        dv("reciprocal", RR, out=R[:, 116:117], in_=R[:, 116:117])
        dv("tensor_tensor", RR, out=R[:, 117:118], in0=R[:, 116:117], in1=R[:, 39:40], op=ALU.mult)
        dv("tensor_tensor", RR, out=R[:, 118:119], in0=R[:, 39:40], in1=R[:, 117:118], op=ALU.subtract)
        dv("tensor_scalar", RR, out=R[:, 119:127], in0=R[:, 89:97], scalar1=R[:, 117:118], scalar2=0.0, op0=ALU.mult, op1=ALU.add)
        dv("scalar_tensor_tensor", RR, out=R[:, 119:127], in0=R[:, 106:114], scalar=R[:, 118:119], in1=R[:, 119:127], op0=ALU.mult, op1=ALU.add)
        self.V("dve", "tensor_tensor", [R], [self.RW], out=self.RW[:, c, :].rearrange("p (g e) -> p g e", g=4),
               in0=R[:, 40:44].unsqueeze(2).to_broadcast([128, 4, 8]), in1=R[:, 119:127].unsqueeze(1).to_broadcast([128, 4, 8]), op=ALU.mult)

    def phase_moe(self, l):
        S = self.S
        i = self.i
        with contextlib.ExitStack() as es:
            acc = self.sb(es, "moacc", [128, 10, D], F32)
            hTb = self.sb(es, "mohT", [128, 8, 1280], BF)
            wts = [(self.sb(es, "mowg%d" % k, [128, 8, 512], BF), self.sb(es, "mowu%d" % k, [128, 8, 512], BF),
                    self.sb(es, "mowd%d" % k, [128, 4, D], BF)) for k in range(2)]
            sG = [self.sb(es, "mosg%d" % k, [128, 512], F32) for k in range(2)]
            Hh = [self.sb(es, "moHh%d" % k, [128, 4, 512], BF) for k in range(2)]
            self.residual_setup(es, 5)
            n = 0
            m = 0
            for blk in ((0, 1, 2), (3, 4), (5, 6), (7, 8)):
                col = 0
                tcs = []
                for ti in blk:
                    t0, N = TILES[ti]
                    S.dma("sp", hTb[:, :, col:col + N], self.HT[:, :, t0:t0 + N].rearrange("k p n -> p k n"), [Tl(None, self.HTt[ti])], [hTb])
                    tcs.append((col, N, t0))
                    col += N
                for e in range(self.nexp):
                    wg, wu, wd = wts[e % 2]
                    S.dma("pool", wg[:], i["moe_w_gate"][l, e].rearrange("(kc p) f -> p kc f", p=128), [], [wg])
                    S.dma("pool", wu[:], i["moe_w_up"][l, e].rearrange("(kc p) f -> p kc f", p=128), [], [wu])
                    S.dma("pool", wd[:], i["moe_w_down"][l, e].rearrange("(fc p) n -> p fc n", p=128), [], [wd])
                    for (col, N, t0) in tcs:
                        hh = Hh[m % 2]
                        m += 1
                        for fc in range(4):
                            pG, pU = self.psb(), self.psb()
                            for kc in range(8):
                                self.mm(pG[:, 0:N], wg[:, kc, fc * 128:(fc + 1) * 128], hTb[:, kc, col:col + N], kc == 0, kc == 7, [wg, hTb], [pG])
                            for kc in range(8):
                                self.mm(pU[:, 0:N], wu[:, kc, fc * 128:(fc + 1) * 128], hTb[:, kc, col:col + N], kc == 0, kc == 7, [wu, hTb], [pU])
                            sg = sG[n % 2]
                            n += 1
                            self.act(sg[:, 0:N], pG[:, 0:N], AF.Silu, [pG], [sg])
                            self.V("dve", "tensor_tensor", [sg, pU], [hh], out=hh[:, fc, 0:N], in0=sg[:, 0:N], in1=pU[:, 0:N], op=ALU.mult)
                        for cc in range(N // 128):
                            ci = col // 128 + cc
                            c = t0 // 128 + cc
                            for half in range(2):
                                hs = slice(half * 512, (half + 1) * 512)
                                pD = self.psb()
                                for fc in range(4):
                                    self.mm(pD[:, :], hh[:, fc, cc * 128:(cc + 1) * 128], wd[:, fc, hs], fc == 0, fc == 3, [hh, wd], [pD])
                                if e == 0:
                                    self.V("dve", "tensor_scalar", [pD, self.RW], [acc], out=acc[:, ci, hs], in0=pD[:, :], scalar1=self.RW[:, c, e:e + 1], scalar2=0.0,
                                           op0=ALU.mult, op1=ALU.add)
                                else:
                                    self.V("dve", "scalar_tensor_tensor", [pD, self.RW, acc], [acc], out=acc[:, ci, hs], in0=pD[:, :], scalar=self.RW[:, c, e:e + 1],
                                           in1=acc[:, ci, hs], op0=ALU.mult, op1=ALU.add)
                for (col, N, t0) in tcs:
                    for cc in range(N // 128):
                        ci = col // 128 + cc
                        c = t0 // 128 + cc
                        self.residual(c, lambda half, ci=ci: acc[:, ci, half * 512:(half + 1) * 512], [acc, acc])
            S.barrier()

    def rank(self, c, R):
        A = R[:, 128:160]
        self.V("dve", "tensor_scalar", [self.RW], [R], out=A, in0=self.RW[:, c, :], scalar1=0.0, scalar2=1.0, op0=ALU.is_gt, op1=ALU.mult)
        if c == 0:
            self.V("dve", "memset", [], [self.Asum], self.Asum[:], 0.0)
        ps = self.psb()
        self.mm(ps[:, 0:32], self.ltri[:], A, True, False, [self.ltri, R], [ps])
        self.mm(ps[:, 0:32], self.cst[:, 128:256], self.Asum[:], False, True, [self.cst, self.Asum], [ps])
        self.act(self.RK[:, c, :], ps[:, 0:32], AF.Copy, [ps], [self.RK])
        self.V("dve", "tensor_tensor", [self.Asum, R], [self.Asum], out=self.Asum[:], in0=self.Asum[:], in1=A, op=ALU.add)

    def phase_moe_sparse(self, l):
        S = self.S
        i = self.i
        onesf = self.cst[:, 128:256]
        rows_t = Tl(None, self.ROWSt)
        acc_t = Tl(None, self.ACC2t)
        h2_t = Tl(None, self.H2t)
        with contextlib.ExitStack() as es:
            G = self.sb(es, "spG", [128, 1024], F32)
            widx = self.sb(es, "spwidx", [128, 2, NB], U32)
            init = self.sb(es, "spinit", [128, 128, 4], F32)
            self.V("pool", "memset", [], [init], init[:], 0.0)
            self.V("pool", "memset", [init], [init], init[:, :, 0:1], float(T))
            self.V("pool", "memset", [init], [init], init[:, :, 2:4], 1.0e6)
            S.dma("sp", self.ROWS.rearrange("(j p) c -> j (p c)", p=128), init[0:NB, :, :].rearrange("j p c -> j (p c)"), [init], [rows_t])
            ps = self.psb()
            self.mm(ps[:, 0:32], onesf, self.Asum[:], True, True, [self.cst, self.Asum], [ps])
            cnt, pad, pend, pst = G[:, 0:32], G[:, 32:64], G[:, 64:96], G[:, 96:128]
            cmp = self.sb(es, "spcmp", [128, NB, 32], F32)
            self.V("dve", "tensor_copy", [ps], [G], out=cnt, in_=ps[:, 0:32])
            cmp2 = cmp[:].rearrange("p a b -> p (a b)")[:, 0:32 * 68].rearrange("p (e m) -> p e m", m=68)
            self.V("dve", "tensor_tensor", [G, self.cst], [cmp], out=cmp2, in0=cnt.unsqueeze(2).to_broadcast([128, 32, 68]),
                   in1=self.cst[:, 896:896 + 68].unsqueeze(1).to_broadcast([128, 32, 68]), op=ALU.is_gt)
            self.V("dve", "tensor_reduce", [cmp], [G], out=pad, in_=cmp2, axis=AX.X, op=ALU.add)
            self.V("dve", "tensor_scalar", [G], [G], out=pad, in0=pad, scalar1=128.0, scalar2=0.0, op0=ALU.mult, op1=ALU.add)
            self.V("dve", "tensor_tensor_scan", [G, self.cst], [G], out=pend, data0=onesf[:, 0:32], data1=pad, initial=0.0, op0=ALU.mult, op1=ALU.add)
            self.V("dve", "tensor_tensor", [G], [G], out=pst, in0=pend, in1=pad, op=ALU.subtract)
            self.V("dve", "tensor_tensor", [G, self.cst], [cmp], out=cmp[:], in0=pend.unsqueeze(1).to_broadcast([128, NB, 32]),
                   in1=self.cst[:, 896:896 + NB].unsqueeze(2).to_broadcast([128, NB, 32]), op=ALU.is_le)
            be = G[:, 128:128 + NB]
            self.V("dve", "tensor_reduce", [cmp], [G], out=be, in_=cmp[:], axis=AX.X, op=ALU.add)
            self.V("dve", "tensor_scalar", [G], [G], out=be, in0=be, scalar1=31.0, scalar2=128.0, op0=ALU.min, op1=ALU.mult)
            same = G[:, 384:384 + NB]
            self.V("dve", "memset", [G], [G], same, 0.0)
            self.V("dve", "tensor_tensor", [G], [G], out=G[:, 386:384 + NB], in0=G[:, 130:128 + NB], in1=G[:, 128:126 + NB], op=ALU.is_equal)
            wf = G[:, 256:256 + NB]
            self.V("dve", "tensor_scalar", [G, self.cst], [G], out=wf, in0=be, scalar1=self.cst[:, 1024:1025], scalar2=2.0, op0=ALU.add, op1=ALU.mult)
            self.V("dve", "tensor_scalar", [G], [G], out=wf, in0=wf, scalar1=float(l * 8192), scalar2=1.0, op0=ALU.add, op1=ALU.mult)
            self.V("dve", "scalar_tensor_tensor", [G], [G], out=wf, in0=same, scalar=1.0e8, in1=wf, op0=ALU.mult, op1=ALU.add)
            self.V("dve", "tensor_copy", [G], [widx], out=widx[:, 0, :], in_=wf)
            self.V("dve", "tensor_scalar", [G], [G], out=wf, in0=wf, scalar1=1.0, scalar2=1.0, op0=ALU.add, op1=ALU.mult)
            self.V("dve", "tensor_copy", [G], [widx], out=widx[:, 1, :], in_=wf)
            Q = [self.sb(es, "spQ%d" % k, [128, 160], F32) for k in range(2)]
            rec = [self.sb(es, "sprec%d" % k, [128, 2, 4], F32) for k in range(2)]
            didx = [self.sb(es, "spdidx%d" % k, [128, 2], U32) for k in range(2)]
            for c in range(NCH):
                q = Q[c % 2]
                r_ = rec[c % 2]
                di = didx[c % 2]
                A, dst, d1, m1 = q[:, 0:32], q[:, 32:64], q[:, 64:96], q[:, 96:128]
                rd = [self.RW, self.RK, G, q]
                self.V("dve", "tensor_scalar", rd, [q], out=A, in0=self.RW[:, c, :], scalar1=0.0, scalar2=1.0, op0=ALU.is_gt, op1=ALU.mult)
                self.V("dve", "tensor_tensor", rd, [q], out=dst, in0=self.RK[:, c, :], in1=pst, op=ALU.add)
                self.V("dve", "scalar_tensor_tensor", rd, [q], out=d1, in0=dst, scalar=1.0, in1=A, op0=ALU.add, op1=ALU.mult)
                self.V("dve", "tensor_reduce", rd, [q], out=q[:, 128:129], in_=d1, axis=AX.X, op=ALU.max)
                self.V("dve", "tensor_scalar", rd, [q], out=m1, in0=d1, scalar1=q[:, 128:129], scalar2=1.0, op0=ALU.is_equal, op1=ALU.mult)
                self.V("dve", "tensor_tensor", rd, [q], out=m1, in0=m1, in1=self.RW[:, c, :], op=ALU.mult)
                self.V("dve", "tensor_reduce", rd, [q], out=q[:, 129:130], in_=m1, axis=AX.X, op=ALU.add)
                self.V("dve", "tensor_reduce", rd, [q], out=q[:, 130:131], in_=self.RW[:, c, :], axis=AX.X, op=ALU.add)
                self.V("dve", "tensor_scalar", rd, [q], out=m1, in0=A, scalar1=-1.0e9, scalar2=1.0e9, op0=ALU.mult, op1=ALU.add)
                self.V("dve", "tensor_tensor", rd, [q], out=m1, in0=m1, in1=dst, op=ALU.add)
                self.V("dve", "tensor_reduce", rd, [q], out=q[:, 131:132], in_=m1, axis=AX.X, op=ALU.min)
                self.V("dve", "tensor_scalar", rd, [q], out=q[:, 132:133], in0=q[:, 128:129], scalar1=-1.0, scalar2=1.0, op0=ALU.add, op1=ALU.mult)
                self.V("dve", "memset", [], [r_], r_[:], 0.0)
                for k in range(2):
                    self.V("dve", "tensor_scalar", [self.cst, r_], [r_], out=r_[:, k, 0:1], in0=self.cst[:, 1024:1025], scalar1=float(c * 128), scalar2=1.0,
                           op0=ALU.add, op1=ALU.mult)
                    self.V("dve", "tensor_scalar", [self.cst, r_], [r_], out=r_[:, k, 2:3], in0=self.cst[:, 1024:1025], scalar1=float(c * 128 + k * T), scalar2=2.0,
                           op0=ALU.add, op1=ALU.mult)
                    self.V("dve", "tensor_scalar", [r_], [r_], out=r_[:, k, 3:4], in0=r_[:, k, 2:3], scalar1=1.0, scalar2=1.0, op0=ALU.add, op1=ALU.mult)
                self.V("dve", "tensor_tensor", [q, r_], [r_], out=r_[:, 0, 1:2], in0=q[:, 130:131], in1=q[:, 129:130], op=ALU.subtract)
                self.V("dve", "tensor_copy", [q, r_], [r_], out=r_[:, 1, 1:2], in_=q[:, 129:130])
                self.V("dve", "tensor_copy", [q], [di], out=di[:, 0:1], in_=q[:, 131:132])
                self.V("dve", "tensor_copy", [q], [di], out=di[:, 1:2], in_=q[:, 132:133])
                for k in range(2):
                    S.dma_fn("pool", (lambda e, r_=r_, di=di, k=k: e.indirect_dma_start(out=self.ROWS, out_offset=bass.IndirectOffsetOnAxis(ap=di[:, k:k + 1], axis=0),
                                                                                      in_=r_[:, k, :], in_offset=None)), [r_, di], [rows_t])
            wgv = i["moe_w_gate"].rearrange("l e (p j) f -> (l e p) (j f)", j=8).rearrange("r (h x) -> (r h) x", h=2)
            wuv = i["moe_w_up"].rearrange("l e (p j) f -> (l e p) (j f)", j=8).rearrange("r (h x) -> (r h) x", h=2)
            wdv = i["moe_w_down"].rearrange("l e (p j) n -> (l e p) (j n)", j=4).rearrange("r (h x) -> (r h) x", h=2)
            wts = [(self.sb(es, "spwg%d" % k, [128, 8, 512], BF), self.sb(es, "spwu%d" % k, [128, 8, 512], BF),
                    self.sb(es, "spwd%d" % k, [128, 4, D], BF)) for k in range(2)]
            NQ = 4
            recs = [self.sb(es, "sprc%d" % k, [128, 4], F32) for k in range(NQ)]
            recu = [self.sb(es, "spru%d" % k, [128, 4], U32) for k in range(NQ)]
            hbs = [self.sb(es, "sphb%d" % k, [128, D], BF) for k in range(NQ)]
            hTs = [self.sb(es, "sphT%d" % k, [128, 8, 128], BF) for k in range(NQ)]
            sGs = [self.sb(es, "spsg%d" % k, [128, 512], F32) for k in range(2)]
            Hhs = [self.sb(es, "spHh%d" % k, [128, 512], BF) for k in range(2)]
            HhTs = [self.sb(es, "spHhT%d" % k, [128, 4, 128], BF) for k in range(2)]
            ys = [self.sb(es, "spy%d" % k, [128, D], F32) for k in range(2)]
            def gather(dst_ap, src, idx_ap, r, w, skip=False):
                if skip:
                    S.dma_fn("pool", (lambda e: e.indirect_dma_start(out=dst_ap, out_offset=None, in_=src, in_offset=bass.IndirectOffsetOnAxis(ap=idx_ap, axis=0),
                                                                     bounds_check=self._wbound_reg(e), oob_is_err=False)), r, w)
                else:
                    S.dma_fn("pool", (lambda e: e.indirect_dma_start(out=dst_ap, out_offset=None, in_=src, in_offset=bass.IndirectOffsetOnAxis(ap=idx_ap, axis=0))), r, w)

            def proA(j):
                rc, ru, hb = recs[j % NQ], recu[j % NQ], hbs[j % NQ]
                S.dma("sp", rc[:], self.ROWS[j * 128:(j + 1) * 128, :], [rows_t], [rc])
                self.V("dve", "tensor_copy", [rc], [ru], out=ru[:], in_=rc[:])
                gather(hb[:], self.H2, ru[:, 0:1], [ru, h2_t], [hb])

            def proB(j):
                hb, hT = hbs[j % NQ], hTs[j % NQ]
                pt = self.psb()
                ptv = pt[:].bitcast(BF).rearrange("p (k n) -> p k n", k=8)
                hbv = hb[:].rearrange("t (p j) -> t p j", j=8)
                for jx in range(8):
                    self.tr(ptv[:, jx, :], hbv[:, :, jx], self.identb[:], [hb, self.identb], [pt])
                self.act(hT[:], ptv, AF.Copy, [pt], [hT])

            def wload(j):
                wg, wu, wd = wts[j % 2]
                for h_ in range(2):
                    gather(wg[:, h_ * 4:(h_ + 1) * 4, :].rearrange("p j f -> p (j f)"), wgv, widx[:, h_, j:j + 1], [widx], [wg], skip=True)
                    gather(wu[:, h_ * 4:(h_ + 1) * 4, :].rearrange("p j f -> p (j f)"), wuv, widx[:, h_, j:j + 1], [widx], [wu], skip=True)
                    gather(wd[:, h_ * 2:(h_ + 1) * 2, :].rearrange("p j f -> p (j f)"), wdv, widx[:, h_, j:j + 1], [widx], [wd], skip=True)

            proA(0)
            proA(1)
            wload(0)
            proB(0)
            for j in range(NB):
                z = j % 2
                rc, ru, hT = recs[j % NQ], recu[j % NQ], hTs[j % NQ]
                sg, Hh, HhT, y = sGs[z], Hhs[z], HhTs[z], ys[z]
                wg, wu, wd = wts[z]
                if j + 2 < NB:
                    proA(j + 2)
                if j + 1 < NB:
                    wload(j + 1)
                pG, pU = self.psb(), self.psb()
                for jx in range(8):
                    self.mm(pG[:, :], hT[:, jx, :], wg[:, jx, :], jx == 0, jx == 7, [hT, wg], [pG])
                for jx in range(8):
                    self.mm(pU[:, :], hT[:, jx, :], wu[:, jx, :], jx == 0, jx == 7, [hT, wu], [pU])
                self.act(sg[:], pG[:, :], AF.Silu, [pG], [sg])
                self.V("dve", "scalar_tensor_tensor", [pU, rc, sg], [Hh], out=Hh[:], in0=pU[:, :], scalar=rc[:, 1:2], in1=sg[:], op0=ALU.mult, op1=ALU.mult)
                if j + 1 < NB:
                    proB(j + 1)
                pt2 = self.psb()
                pt2v = pt2[:].bitcast(BF)[:, 0:512].rearrange("p (k n) -> p k n", k=4)
                Hhv = Hh[:].rearrange("t (p j) -> t p j", j=4)
                for jx in range(4):
                    self.tr(pt2v[:, jx, :], Hhv[:, :, jx], self.identb[:], [Hh, self.identb], [pt2])
                self.act(HhT[:], pt2v, AF.Copy, [pt2], [HhT])
                for half in range(2):
                    pD = self.psb()
                    for jx in range(4):
                        self.mm(pD[:, :], HhT[:, jx, :], wd[:, jx, half * 512:(half + 1) * 512], jx == 0, jx == 3, [HhT, wd], [pD])
                    if half == 0:
                        self.act(y[:, 0:512], pD[:, :], AF.Copy, [pD], [y])
                    else:
                        self.V("dve", "tensor_copy", [pD], [y], out=y[:, 512:1024], in_=pD[:, :])
                for h_ in range(2):
                    S.dma_fn("pool", (lambda e, y=y, ru=ru, h_=h_: e.indirect_dma_start(out=self.ACC2.rearrange("r (h x) -> (r h) x", h=2),
                                                                                        out_offset=bass.IndirectOffsetOnAxis(ap=ru[:, 2 + h_:3 + h_], axis=0),
                                                                                        in_=y[:, h_ * 512:(h_ + 1) * 512], in_offset=None,
                                                                                        bounds_check=self._bound_reg(e), oob_is_err=False)), [y, ru], [acc_t])
            self.residual_setup(es, 5)
            a0 = [self.sb(es, "spa0%d" % k, [128, D], F32) for k in range(3)]
            a1 = [self.sb(es, "spa1%d" % k, [128, D], F32) for k in range(3)]
            for c in range(NCH):
                u0, u1 = a0[c % 3], a1[c % 3]
                S.dma("sp", u0[:], self.ACC2[c * 128:(c + 1) * 128, :], [acc_t], [u0])
                S.dma("sp", u1[:], self.ACC2[T + c * 128:T + (c + 1) * 128, :], [acc_t], [u1])
                self.V("dve", "tensor_tensor", [u0, u1], [u0], out=u0[:], in0=u0[:], in1=u1[:], op=ALU.add)
                self.residual(c, lambda half, u0=u0: u0[:, half * 512:(half + 1) * 512], [u0, u0])
            S.barrier()

    def _wbound_reg(self, e):
        if getattr(self, "_wbreg", None) is None:
            self._wbreg = e.to_reg(self.n_layers * 32 * 128 * 2 - 1)
        return self._wbreg

    def _bound_reg(self, e):
        if getattr(self, "_breg", None) is None:
            self._breg = e.to_reg(4 * T - 1)
        return self._breg

    def final_norm(self):
        S = self.S
        with contextlib.ExitStack() as es:
            g = self.sb(es, "fng", [128, D], F32)
            S.dma("sp", g[:], self.i["final_g"].partition_broadcast(128), [], [g])
            xs = [self.sb(es, "fnx%d" % k, [128, D], F32) for k in range(3)]
            sq = self.sb(es, "fnsq", [128, D], F32)
            st = [self.sb(es, "fnst%d" % k, [128, 4], F32) for k in range(2)]
            ot = [self.sb(es, "fno%d" % k, [128, D], F32) for k in range(2)]
            for c in range(2, NCH):
                x = xs[c % 3]
                S.dma("sp", x[:], self.X[c * 128:(c + 1) * 128, :], [Tl(None, self.Xt[c])], [x])
                s_ = st[c % 2]
                self.act(sq[:], x[:], AF.Square, [x], [sq, s_], accum_out=s_[:, 0:1])
                self.V("dve", "tensor_scalar", [s_], [s_], out=s_[:, 1:2], in0=s_[:, 0:1], scalar1=1.0 / D, scalar2=EPS, op0=ALU.mult, op1=ALU.add)
                self.V("dve", "reciprocal", [s_], [s_], out=s_[:, 2:3], in_=s_[:, 1:2])
                self.act(s_[:, 3:4], s_[:, 2:3], AF.Sqrt, [s_], [s_])
                o = ot[c % 2]
                self.V("dve", "scalar_tensor_tensor", [x, s_, g], [o], out=o[:], in0=x[:], scalar=s_[:, 3:4], in1=g[:], op0=ALU.mult, op1=ALU.mult)
                S.dma("sp", self.out[(c - 2) * 128:(c - 1) * 128, :], o[:], [o], [Tl(None, self.outt)])


def make_consts():
    cst = np.zeros((128, 1056), np.float32)
    cst[:, 0:128] = np.eye(128, dtype=np.float32)
    cst[:, 128:256] = 1.0
    R = np.zeros((128, 128), np.float32)
    for blk in range(2):
        o = blk * 64
        for q in range(16):
            R[o + 16 + q, o + q] = -1.0
            R[o + q, o + 16 + q] = 1.0
            R[o + 48 + q, o + 32 + q] = -1.0
            R[o + 32 + q, o + 48 + q] = 1.0
    cst[:, 256:384] = R
    s = np.arange(128)[:, None]
    t = np.arange(128)[None, :]
    same = (s // 64) == (t // 64)
    same32 = (s // 32) == (t // 32)
    cst[:, 384:512] = (same32 & (t >= s)).astype(np.float32)
    cst[:, 512:640] = (same32 & (t <= s)).astype(np.float32)
    cst[:, 640:768] = (same & (s % 64 < 32) & (t % 64 >= 32)).astype(np.float32)
    cst[:, 768:896] = (same & (s % 64 >= 32) & (t % 64 < 32)).astype(np.float32)
    cst[:, 896:1024] = 128.0 * np.arange(128, dtype=np.float32)[None, :]
    cst[:, 1024] = np.arange(128, dtype=np.float32)
    cst[:, 1025:1057 - 1 + 0] = 0.0
    inv_freq = (10000.0 ** (-np.arange(0, 32, 2, dtype=np.float32) / 32)).astype(np.float32)
    pos = np.arange(NLAT)
    row = (pos // 64).astype(np.float32)
    col = (pos % 64).astype(np.float32)
    ang_r = row[:, None] * inv_freq
    ang_c = col[:, None] * inv_freq
    ang = np.concatenate([ang_r, ang_r, ang_c, ang_c], axis=-1).astype(np.float32)
    rope = np.zeros((2, 128, T), np.float32)
    rope[0, :, :NCTX] = 1.0
    rope[0, 0:64, NCTX:] = np.cos(ang).T
    rope[0, 64:128, NCTX:] = np.cos(ang).T
    rope[1, 0:64, NCTX:] = np.sin(ang).T
    rope[1, 64:128, NCTX:] = np.sin(ang).T
    return cst, rope


def make_in_maps(inputs, cores, L=DEPTH, nexp=32):
    f = lambda a: np.ascontiguousarray(np.asarray(a, dtype=np.float32))
    cst, rope = make_consts()
    shared = {
        "c_ctx": f(inputs["c_ctx"]).reshape(1, D),
        "ada_w": f(inputs["ada_w"][:L]), "ada_b": f(inputs["ada_b"]),
        "norm1_g": f(inputs["norm1_g"]), "norm2_g": f(inputs["norm2_g"]),
        "w_in": f(inputs["w_in"][:L]), "w_branch": f(inputs["w_branch"][:L]), "w_out": f(inputs["w_out"][:L]),
        "hg_lb": f(inputs["hg_lb_logits"]), "hg_norm_g": f(inputs["hg_norm_g"]),
        "da_lambda": f(inputs["da_lambda"]).reshape(DEPTH, 256), "da_norm_g": f(inputs["da_norm_g"]),
        "cv_dw_w": f(inputs["cv_dw_w"]), "cv_dw_b": f(inputs["cv_dw_b"]),
        "cv_ln_g": f(inputs["cv_ln_g"]), "cv_ln_b": f(inputs["cv_ln_b"]),
        "sc_w": f(inputs["sc_w"]),
        "moe_w_r": np.ascontiguousarray(np.concatenate([f(inputs["moe_w_grp"]), f(inputs["moe_w_exp"])], axis=-1)),
        "moe_b_r": np.ascontiguousarray(np.concatenate([f(inputs["moe_b_grp"]), f(inputs["moe_b_exp"])], axis=-1)),
        "moe_w_gate": f(inputs["moe_w_gate"][:L, :nexp]), "moe_w_up": f(inputs["moe_w_up"][:L, :nexp]), "moe_w_down": f(inputs["moe_w_down"][:L, :nexp]),
        "final_g": f(inputs["final_g"]).reshape(1, D),
        "cst": cst, "rope": rope, "ltri": np.triu(np.ones((128, 128), np.float32), 1),
    }
    maps = []
    for cid in cores:
        b = cid % 4
        m = dict(shared)
        m["x"] = f(inputs["x"][b])
        m["c"] = f(inputs["c"][b]).reshape(1, D)
        m["ctx"] = f(inputs["ctx"][b])
        maps.append(m)
    return maps


def kernel(**inputs):
    nc = bass.Bass("TRN2", target_bir_lowering=False)
    Prog(nc).build()
    maps = make_in_maps(inputs, list(range(4)))
    res = run_bass_kernel_spmd(nc, maps, core_ids=list(range(4)))
    return np.stack([np.asarray(res.results[b]["out"], dtype=np.float32) for b in range(4)], axis=0)
```

```python
import contextlib
import math
import numpy as np
import concourse.bass as bass
import concourse.mybir as mybir
from concourse.bass_utils import run_bass_kernel_spmd

F32 = mybir.dt.float32
BF = mybir.dt.bfloat16
U32 = mybir.dt.uint32
AF = mybir.ActivationFunctionType
ALU = mybir.AluOpType
AX = mybir.AxisListType

D = 1024
NCTX = 256
NLAT = 4096
T = NCTX + NLAT
NCH = T // 128
NB = 2 * T // 128 + 32
SPARSE = True
DEPTH = 4
W = 512
INW = 10752
EPS = 1e-6
TILES = [(0, 256)] + [(256 + 512 * i, 512) for i in range(8)]
C_HQ, C_HI, C_HFF, C_HFB, C_HG = 0, 512, 1024, 1536, 2048
C_DQ, C_DK, C_DV = 2560, 3072, 3584
C_CVA, C_CVG = 4096, 4608
C_SB, C_SC, C_SX = 5120, 5632, 6144
C_GATE = 6656


class Trk:
    __slots__ = ("w", "r")

    def __init__(self):
        self.w = None
        self.r = {}


class Tl:
    def __init__(self, h, trk=None):
        self.h = h
        self.t = trk or Trk()

    def __getitem__(self, k):
        return self.h[k]


class Stream:
    def __init__(self, name):
        self.name = name
        self.ops = []
        self.seen = {}
        self.sem = None
        self.cnt = 0
        self.dslots = []
        self.dnext = 0


class Sch:
    SEM_MAX = 30000

    def __init__(self, nc, es):
        self.nc = nc
        self.es = es
        self.st = {k: Stream(k) for k in ("pe", "act", "dve", "pool", "sp")}
        self.nsem = 0
        for k, s in self.st.items():
            self._newsem(s)
        for k, n in (("sp", 24), ("pool", 12), ("act", 6)):
            s = self.st[k]
            for i in range(n):
                s.dslots.append([self._sem(), 0])

    def _sem(self):
        self.nsem += 1
        return self.es.enter_context(self.nc.semaphore("s%d" % self.nsem))

    def _newsem(self, s):
        s.sem = self._sem()
        s.cnt = 0

    def _need(self, s, tok, waits):
        if tok is None:
            return
        sem, val, src = tok
        if src == "pe" and s.name == "pe":
            return
        if s.seen.get(id(sem), 0) >= val:
            return
        k = id(sem)
        if k not in waits or waits[k][1] < val:
            waits[k] = (sem, val)

    def _deps(self, s, reads, writes):
        waits = {}
        for b in reads:
            self._need(s, b.t.w, waits)
        for b in writes:
            self._need(s, b.t.w, waits)
            for tok in b.t.r.values():
                self._need(s, tok, waits)
        for k, (sem, val) in waits.items():
            s.seen[k] = val
        return list(waits.values())

    def _mark(self, tok, reads, writes):
        for b in reads:
            b.t.r[id(tok[0])] = tok
        for b in writes:
            b.t.w = tok
            b.t.r = {}

    def op(self, eng, fn, reads=(), writes=()):
        s = self.st[eng]
        if s.cnt >= self.SEM_MAX:
            self._newsem(s)
        waits = self._deps(s, reads, writes)
        s.cnt += 1
        tok = (s.sem, s.cnt, eng)
        s.ops.append((waits, fn, (s.sem, 1)))
        self._mark(tok, reads, writes)
        return tok

    def dma(self, q, out, in_, reads=(), writes=(), **kw):
        return self.dma_fn(q, (lambda e: e.dma_start(out=out, in_=in_, **kw)), reads, writes)

    def dma_fn(self, q, fn, reads=(), writes=()):
        s = self.st[q]
        slot = s.dslots[s.dnext % len(s.dslots)]
        s.dnext += 1
        waits = self._deps(s, reads, writes)
        if slot[1] > 0 and s.seen.get(id(slot[0]), 0) < slot[1]:
            waits.append((slot[0], slot[1]))
            s.seen[id(slot[0])] = slot[1]
        slot[1] += 16
        tok = (slot[0], slot[1], "dma")
        s.ops.append((waits, fn, (slot[0], 16)))
        self._mark(tok, reads, writes)
        return tok

    def barrier(self):
        toks = []
        for s in self.st.values():
            if s.cnt > 0:
                toks.append((s.sem, s.cnt))
            for sl in s.dslots:
                if sl[1] > 0:
                    toks.append((sl[0], sl[1]))
        for s in self.st.values():
            waits = []
            for sem, val in toks:
                if sem is s.sem:
                    continue
                if s.seen.get(id(sem), 0) < val:
                    waits.append((sem, val))
                    s.seen[id(sem)] = val
            if waits:
                s.ops.append((waits, None, None))

    def emit(self):
        nc = self.nc
        self.barrier()
        with nc.Block() as block:
            def run(s):
                def f(e):
                    for waits, fn, inc in s.ops:
                        for sem, val in waits:
                            e.wait_ge(sem, val)
                        if fn is not None:
                            fn(e).then_inc(inc[0], inc[1])
                return f
            block.tensor(run(self.st["pe"]))
            block.scalar(run(self.st["act"]))
            block.vector(run(self.st["dve"]))
            block.gpsimd(run(self.st["pool"]))
            block.sync(run(self.st["sp"]))


class Prog:
    def __init__(self, nc, n_layers=DEPTH, dbg=None, nexp=32):
        self.nc = nc
        self.nexp = nexp
        self.n_layers = n_layers
        self.dbg = dbg or {}

    def sb(self, es, name, shape, dt):
        self.uid = getattr(self, "uid", 0) + 1
        return Tl(es.enter_context(self.nc.sbuf_tensor("%s_u%d" % (name, self.uid), list(shape), dt)))

    def dram(self, name, shape, dt, kind="Internal"):
        return self.nc.dram_tensor(name, list(shape), dt, kind=kind).ap()

    def mm(self, out, lhsT, rhs, start, stop, r, w):
        self.S.op("pe", lambda e: e.matmul(out, lhsT=lhsT, rhs=rhs, start=start, stop=stop), r, w)

    def tr(self, out, in_, ident, r, w):
        self.S.op("pe", lambda e: e.transpose(out=out, in_=in_, identity=ident), r, w)

    def act(self, out, in_, func, r, w, **kw):
        self.S.op("act", lambda e: e.activation(out=out, in_=in_, func=func, **kw), r, w)

    def V(self, eng, name, r, w, *a, **kw):
        self.S.op(eng, lambda e: getattr(e, name)(*a, **kw), r, w)

    def psb(self):
        b = self.banks[self.bi % 8]
        self.bi += 1
        return b

    def build(self):
        nc = self.nc
        L = self.n_layers
        i = {}
        def inp(name, shape, dt=F32):
            i[name] = self.dram(name, shape, dt, kind="ExternalInput")
        inp("x", [NLAT, D]); inp("c", [1, D]); inp("ctx", [NCTX, D]); inp("c_ctx", [1, D])
        inp("ada_w", [L, D, 6 * D]); inp("ada_b", [DEPTH, 6 * D])
        inp("norm1_g", [DEPTH, D]); inp("norm2_g", [DEPTH, D])
        inp("w_in", [L, D, INW]); inp("w_branch", [L, 4, W, D]); inp("w_out", [L, D, D])
        inp("hg_lb", [DEPTH, 2, W]); inp("hg_norm_g", [DEPTH, W])
        inp("da_lambda", [DEPTH, 256]); inp("da_norm_g", [DEPTH, 128])
        inp("cv_dw_w", [DEPTH, 31, W]); inp("cv_dw_b", [DEPTH, W]); inp("cv_ln_g", [DEPTH, W]); inp("cv_ln_b", [DEPTH, W])
        inp("sc_w", [DEPTH, 3, W])
        inp("moe_w_r", [DEPTH, D, 36]); inp("moe_b_r", [DEPTH, 36])
        inp("moe_w_gate", [L, self.nexp, D, W]); inp("moe_w_up", [L, self.nexp, D, W]); inp("moe_w_down", [L, self.nexp, W, D])
        inp("final_g", [1, D]); inp("ltri", [128, 128])
        inp("cst", [128, 1056]); inp("rope", [2, 128, T])
        self.i = i
        self.out = self.dram("out", [NLAT, D], F32, kind="ExternalOutput")
        self.X = self.dram("Xs", [T, D], F32)
        self.HT = self.dram("HTs", [8, 128, T], BF)
        self.Y = self.dram("Ys", [4, 4, 128, T], BF)
        self.MT = self.dram("MTs", [8, 128, T], BF)
        self.MODR = self.dram("MODs", [2, 128, 6 * D], F32)
        self.H2 = self.dram("H2s", [T + 1, D], BF)
        self.ROWS = self.dram("ROWSs", [NB * 128, 4], F32)
        self.ACC2 = self.dram("ACC2s", [2 * T, D], F32)
        self.H2t = Trk(); self.ROWSt = Trk(); self.ACC2t = Trk()
        self.Xt = [Trk() for _ in range(NCH)]
        self.HTt = [Trk() for _ in range(9)]
        self.Yt = [[Trk() for _ in range(9)] for _ in range(4)]
        self.MTt = [Trk() for _ in range(9)]
        self.MODt = Trk()
        self.outt = Trk()
        dbg_out = {}
        for k, shp in self.dbg.items():
            if not isinstance(shp, tuple):
                continue
            dbg_out[k] = self.dram("dbg_" + k, shp[0], shp[1], kind="ExternalOutput")
        self.dbg_out = dbg_out

        with contextlib.ExitStack() as es:
            self.S = S = Sch(nc, es)
            self.banks = [Tl(es.enter_context(nc.psum_tensor("pb%d" % k, [128, 512], F32))) for k in range(8)]
            self.bi = 0
            self.cst = self.sb(es, "cst_sb", [128, 1056], F32)
            S.dma("sp", self.cst[:], i["cst"], [], [self.cst])
            self.identb = self.sb(es, "identb", [128, 128], BF)
            self.onesb = self.sb(es, "onesb", [128, 128], BF)
            self.Rb = self.sb(es, "Rb", [128, 128], BF)
            self.identf = Tl(self.cst.h, self.cst.t)
            self.V("dve", "tensor_copy", [self.cst], [self.identb], out=self.identb[:], in_=self.cst[:, 0:128])
            self.V("dve", "tensor_copy", [self.cst], [self.onesb], out=self.onesb[:], in_=self.cst[:, 128:256])
            self.V("dve", "tensor_copy", [self.cst], [self.Rb], out=self.Rb[:], in_=self.cst[:, 256:384])
            self.sT = []
            for which, src in enumerate((i["c"], i["c_ctx"])):
                cT = self.sb(es, "cT%d" % which, [128, 8], F32)
                S.dma("sp", cT[:], src.rearrange("o (kc p) -> p (o kc)", p=128), [], [cT], allow_slow_non_contiguous=True)
                sg = self.sb(es, "cS%d" % which, [128, 8], F32)
                self.act(sg[:], cT[:], AF.Silu, [cT], [sg])
                rep = self.sb(es, "sT%d" % which, [128, 8, 128], F32)
                self.V("dve", "tensor_copy", [sg], [rep], out=rep[:], in_=sg[:].unsqueeze(2).to_broadcast([128, 8, 128]))
                self.sT.append(rep)
            self.P = self.sb(es, "Pparams", [128, 600], F32)
            self.RW = self.sb(es, "RW", [128, NCH, 32], F32)
            self.RK = self.sb(es, "RK", [128, NCH, 32], F32)
            self.Asum = self.sb(es, "Asum", [128, 32], F32)
            self.ltri = self.sb(es, "ltri_sb", [128, 128], F32)
            S.dma("sp", self.ltri[:], i["ltri"], [], [self.ltri])
            zr = self.sb(es, "zrow", [1, D], BF)
            self.V("dve", "memset", [], [zr], zr[:], 0.0)
            S.dma("sp", self.H2[T:T + 1, :], zr[:], [zr], [Tl(None, self.H2t)])
            self.LB = self.sb(es, "LB", [128, DEPTH, 8], F32)
            self.OML = self.sb(es, "OML", [128, DEPTH, 8], F32)
            lbe = self.sb(es, "lbe", [128, DEPTH, 8], F32)
            lbt = self.sb(es, "lbt", [128, 16], F32)
            for l_ in range(DEPTH):
                for d_ in range(2):
                    for j_ in range(4):
                        S.dma("sp", lbe[:, l_, d_ * 4 + j_:d_ * 4 + j_ + 1], i["hg_lb"][l_, d_:d_ + 1, j_ * 128:(j_ + 1) * 128].rearrange("o p -> p o"),
                              [], [lbe], allow_slow_non_contiguous=True)
            self.act(lbe[:], lbe[:], AF.Exp, [lbe], [lbe])
            self.V("dve", "tensor_tensor", [lbe], [lbt], out=lbt[:, 0:8], in0=lbe[:, 0, :], in1=lbe[:, 1, :], op=ALU.add)
            self.V("dve", "tensor_tensor", [lbe, lbt], [lbt], out=lbt[:, 0:8], in0=lbt[:, 0:8], in1=lbe[:, 2, :], op=ALU.add)
            self.V("dve", "tensor_tensor", [lbe, lbt], [lbt], out=lbt[:, 0:8], in0=lbt[:, 0:8], in1=lbe[:, 3, :], op=ALU.add)
            self.V("dve", "reciprocal", [lbt], [lbt], out=lbt[:, 8:16], in_=lbt[:, 0:8])
            self.V("dve", "memset", [], [self.LB], self.LB[:], 0.0)
            for l_ in range(1, DEPTH):
                self.V("dve", "tensor_tensor", [lbe, lbt], [lbe], out=lbe[:, l_, :], in0=lbe[:, l_, :], in1=lbt[:, 8:16], op=ALU.mult)
                self.V("dve", "tensor_tensor", [lbe, self.LB], [self.LB], out=self.LB[:, l_, :], in0=self.LB[:, l_ - 1, :], in1=lbe[:, l_, :], op=ALU.add)
            self.V("dve", "tensor_scalar", [self.LB], [self.OML], out=self.OML[:], in0=self.LB[:], scalar1=-1.0, scalar2=1.0, op0=ALU.mult, op1=ALU.add)
            self.seg = self.sb(es, "seg", [128, 512], F32)
            self.V("dve", "memset", [], [self.seg], self.seg[:], 1.0)
            self.V("dve", "memset", [self.seg], [self.seg], self.seg[:].rearrange("p (c s) -> p c s", s=64)[:, :, 0:1], 0.0)
            S.barrier()
            S.dma("sp", self.X[0:NCTX, :], i["ctx"], [], self._xt(0, 2))
            for q in range(4):
                S.dma("sp", self.X[NCTX + q * 1024: NCTX + (q + 1) * 1024, :], i["x"][q * 1024:(q + 1) * 1024, :], [],
                      self._xt(2 + q * 8, 8))
            for l in range(L):
                self.layer(l)
            self.final_norm()
            S.emit()

    def _xt(self, c0, n):
        return [Tl(None, t) for t in self.Xt[c0:c0 + n]]

    def layer(self, l):
        stop = self.dbg.get("stop")
        self.phase_mod(l)
        self.load_params(l)
        self.phase_norm(l, 1)
        if stop == "norm1":
            return
        self.phase_sconv(l)
        self.phase_conv(l)
        if stop == "convs":
            return self.dump_dbg()
        self.phase_attn(l)
        if stop == "attn":
            return self.dump_dbg()
        self.phase_hgrn(l)
        if stop == "hgrn":
            return self.dump_dbg()
        self.phase_merge(l)
        if stop == "merge":
            return self.dump_dbg()
        self.phase_norm(l, 2)
        if SPARSE:
            self.phase_moe_sparse(l)
        else:
            self.phase_moe(l)
        if stop == "moe":
            return self.dump_dbg()

    def dump_dbg(self):
        S = self.S
        if "y" in self.dbg_out:
            S.dma("sp", self.dbg_out["y"], self.Y, [Tl(None, t) for k in range(4) for t in self.Yt[k]], [Tl(None, Trk())])
        if "x" in self.dbg_out:
            S.dma("sp", self.dbg_out["x"], self.X, [Tl(None, t) for t in self.Xt], [Tl(None, Trk())])

    def load_w(self, es, name, l, c0, ncols):
        w = self.sb(es, name, [128, 8, ncols], BF)
        self.S.dma("pool", w[:], self.i["w_in"][l, :, c0:c0 + ncols].rearrange("(kc p) n -> p kc n", p=128), [], [w])
        return w

    def load_ht(self, buf, ti):
        t0, N = TILES[ti]
        self.S.dma("sp", buf[:, :, 0:N], self.HT[:, :, t0:t0 + N].rearrange("k p n -> p k n"), [Tl(None, self.HTt[ti])], [buf])

    def proj(self, ps, w, j0, ht, N, width=128):
        for kc in range(8):
            self.mm(ps[0:width, 0:N], w[:, kc, j0:j0 + width], ht[:, kc, 0:N], kc == 0, kc == 7, [w, ht], [ps])

    def rstd_from(self, src_ap, scale, out_t, tmp_t, N, r):
        self.V("dve", "tensor_scalar", r, [tmp_t], out=tmp_t[:, 0:N], in0=src_ap, scalar1=scale, scalar2=EPS, op0=ALU.mult, op1=ALU.add)
        self.V("dve", "reciprocal", [tmp_t], [tmp_t], out=tmp_t[:, 0:N], in_=tmp_t[:, 0:N])
        self.act(out_t[:, 0:N], tmp_t[:, 0:N], AF.Sqrt, [tmp_t], [out_t])

    def load_params(self, l):
        S = self.S
        i = self.i
        P = self.P
        nc = True
        def ld(dst, src):
            S.dma("sp", dst, src, [], [P], allow_slow_non_contiguous=True)
        for j in range(4):
            sl = slice(j * 128, (j + 1) * 128)
            ld(P[:, j * 3:j * 3 + 3], i["sc_w"][l][:, sl].rearrange("k p -> p k"))
            ld(P[:, 12 + j * 31:12 + (j + 1) * 31], i["cv_dw_w"][l][:, sl].rearrange("k p -> p k"))
            ld(P[:, 136 + j:137 + j], i["cv_dw_b"][l:l + 1, sl].rearrange("o p -> p o"))
            ld(P[:, 140 + j:141 + j], i["cv_ln_g"][l:l + 1, sl].rearrange("o p -> p o"))
            ld(P[:, 144 + j:145 + j], i["cv_ln_b"][l:l + 1, sl].rearrange("o p -> p o"))
            ld(P[:, 148 + j:149 + j], i["hg_norm_g"][l:l + 1, sl].rearrange("o p -> p o"))
        ld(P[:, 152:153], i["da_norm_g"][l:l + 1, :].rearrange("o p -> p o"))
        S.dma("sp", P[:, 160:416], i["da_lambda"][l:l + 1, :].partition_broadcast(128), [], [P])
        S.dma("sp", P[:, 416:452], i["moe_b_r"][l:l + 1, :].partition_broadcast(128), [], [P])
        lam_init = 0.8 - 0.6 * math.exp(-0.3 * l)
        self.V("dve", "tensor_tensor", [P], [P], out=P[:, 460:524], in0=P[:, 160:224], in1=P[:, 224:288], op=ALU.mult)
        self.V("dve", "tensor_tensor", [P], [P], out=P[:, 524:588], in0=P[:, 288:352], in1=P[:, 352:416], op=ALU.mult)
        self.V("dve", "tensor_reduce", [P], [P], out=P[:, 155:157], in_=P[:, 460:588].rearrange("p (a b) -> p a b", a=2), axis=AX.X, op=ALU.add)
        self.act(P[:, 157:159], P[:, 155:157], AF.Exp, [P], [P])
        self.V("dve", "tensor_tensor", [P], [P], out=P[:, 159:160], in0=P[:, 158:159], in1=P[:, 157:158], op=ALU.subtract)
        self.V("dve", "tensor_scalar", [P], [P], out=P[:, 153:154], in0=P[:, 159:160], scalar1=-lam_init, scalar2=1.0, op0=ALU.add, op1=ALU.mult)
        self.V("dve", "tensor_scalar", [P], [P], out=P[:, 154:155], in0=P[:, 152:153], scalar1=1.0 - lam_init, scalar2=0.0, op0=ALU.mult, op1=ALU.add)

    def phase_mod(self, l):
        S = self.S
        i = self.i
        modt = Tl(None, self.MODt)
        with contextlib.ExitStack() as es:
            wt = [self.sb(es, "adaw%d" % k, [128, 8, 512], F32) for k in range(2)]
            bt = [self.sb(es, "adab%d" % k, [1, 512], F32) for k in range(2)]
            ot = [self.sb(es, "adao%d" % k, [128, 512], F32) for k in range(2)]
            n = 0
            for blk in range(12):
                w = wt[blk % 2]
                b = bt[blk % 2]
                S.dma("sp", w[:], i["ada_w"][l, :, blk * 512:(blk + 1) * 512].rearrange("(kc p) n -> p kc n", p=128), [], [w])
                S.dma("sp", b[:], i["ada_b"][l:l + 1, blk * 512:(blk + 1) * 512], [], [b])
                for which in range(2):
                    ps = self.psb()
                    for kc in range(8):
                        self.mm(ps[:], self.sT[which][:, kc, :], w[:, kc, :], kc == 0, False, [self.sT[which], w], [ps])
                    self.mm(ps[:], self.cst[0:1, 128:256], b[0:1, :], False, True, [self.cst, b], [ps])
                    o = ot[n % 2]
                    n += 1
                    self.V("dve", "tensor_copy", [ps], [o], out=o[:], in_=ps[:])
                    S.dma("sp", self.MODR[which, :, blk * 512:(blk + 1) * 512], o[:], [o], [modt])
            S.barrier()

    def phase_norm(self, l, which):
        S = self.S
        i = self.i
        gsrc = i["norm1_g"] if which == 1 else i["norm2_g"]
        sh, sc = (0, 1) if which == 1 else (3, 4)
        modt = Tl(None, self.MODt)
        with contextlib.ExitStack() as es:
            A = [self.sb(es, "nA%d" % k, [128, D], F32) for k in range(2)]
            B = [self.sb(es, "nB%d" % k, [128, D], F32) for k in range(2)]
            g = self.sb(es, "ng", [128, D], F32)
            S.dma("sp", g[:], gsrc[l:l + 1, :].partition_broadcast(128), [], [g])
            for k in range(2):
                S.dma("sp", A[k][:], self.MODR[k, :, sc * D:(sc + 1) * D], [modt], [A[k]])
                S.dma("sp", B[k][:], self.MODR[k, :, sh * D:(sh + 1) * D], [modt], [B[k]])
                self.V("dve", "scalar_tensor_tensor", [A[k], g], [A[k]], out=A[k][:], in0=A[k][:], scalar=1.0, in1=g[:],
                       op0=ALU.add, op1=ALU.mult)
            xs = [self.sb(es, "nx%d" % k, [128, D], F32) for k in range(3)]
            sq = self.sb(es, "nsq", [128, D], F32)
            t1 = [self.sb(es, "nt%d" % k, [128, D], F32) for k in range(2)]
            hb = [self.sb(es, "nhb%d" % k, [128, D], BF) for k in range(2)]
            st = [self.sb(es, "nst%d" % k, [128, 4], F32) for k in range(2)]
            hT = [self.sb(es, "nhT%d" % k, [128, 8, 512], BF) for k in range(2)]
            if which == 2:
                wr = self.sb(es, "nwr", [128, 8, 36], F32)
                S.dma("sp", wr[:], i["moe_w_r"][l].rearrange("(kc p) n -> p kc n", p=128), [], [wr])
                h32 = [self.sb(es, "nh32%d" % k, [128, D], F32) for k in range(2)]
                h32T = [self.sb(es, "nh32T%d" % k, [128, 8, 128], F32) for k in range(2)]
                Rt = [self.sb(es, "nR%d" % k, [128, 160], F32) for k in range(2)]
            st3 = st + [self.sb(es, "nst2", [128, 4], F32)]
            sq2 = [sq, self.sb(es, "nsq2", [128, D], F32)]
            chunks = [(ti, t0, N, cc) for ti, (t0, N) in enumerate(TILES) for cc in range(N // 128)]

            def stageA(c):
                x = xs[c % 3]
                s_ = st3[c % 3]
                S.dma("sp", x[:], self.X[c * 128:(c + 1) * 128, :], [Tl(None, self.Xt[c])], [x])
                self.act(sq2[c % 2][:], x[:], AF.Square, [x], [sq2[c % 2], s_], accum_out=s_[:, 0:1])
                self.V("dve", "tensor_scalar", [s_], [s_], out=s_[:, 1:2], in0=s_[:, 0:1], scalar1=1.0 / D, scalar2=EPS,
                       op0=ALU.mult, op1=ALU.add)
                self.V("dve", "reciprocal", [s_], [s_], out=s_[:, 2:3], in_=s_[:, 1:2])
                self.act(s_[:, 3:4], s_[:, 2:3], AF.Sqrt, [s_], [s_])

            def stageB(ti, t0, N, cc):
                c = t0 // 128 + cc
                lat = 0 if c >= 2 else 1
                ht = hT[ti % 2]
                x = xs[c % 3]
                s_ = st3[c % 3]
                t = t1[c % 2]
                self.V("dve", "scalar_tensor_tensor", [x, s_, A[lat]], [t], out=t[:], in0=x[:], scalar=s_[:, 3:4],
                       in1=A[lat][:], op0=ALU.mult, op1=ALU.mult)
                h = hb[c % 2]
                self.V("pool", "tensor_tensor", [t, B[lat]], [h], out=h[:], in0=t[:], in1=B[lat][:], op=ALU.add)
                ps = self.psb()
                pv = ps[:].bitcast(BF).rearrange("p (k n) -> p k n", k=8)
                for k in range(8):
                    self.tr(pv[:, k, :], h[:, k * 128:(k + 1) * 128], self.identb[:], [h, self.identb], [ps])
                self.V("dve", "tensor_copy", [ps], [ht], out=ht[:, :, cc * 128:(cc + 1) * 128], in_=pv)
                if which == 2:
                    self.route(c, t, B[lat], h32[c % 2], h32T[c % 2], wr, Rt[c % 2])
                    if SPARSE:
                        S.dma("sp", self.H2[c * 128:(c + 1) * 128, :], h[:], [h], [Tl(None, self.H2t)])
                        self.rank(c, Rt[c % 2])
                if cc == N // 128 - 1:
                    S.dma("sp", self.HT[:, :, t0:t0 + N].rearrange("k p n -> p k n"), ht[:, :, 0:N], [ht], [Tl(None, self.HTt[ti])])

            stageA(0)
            for idx, (ti, t0, N, cc) in enumerate(chunks):
                if idx + 1 < len(chunks):
                    stageA(idx + 1)
                stageB(ti, t0, N, cc)
            S.barrier()
        if "h1" in self.dbg_out and l == self.dbg.get("layer", 0) and which == 1:
            S.dma("sp", self.dbg_out["h1"], self.HT, [Tl(None, t) for t in self.HTt], [Tl(None, Trk())])


    def phase_sconv(self, l):
        S = self.S
        P = self.P
        with contextlib.ExitStack() as es:
            wb = self.load_w(es, "scwb", l, C_SB, 512)
            wc = self.load_w(es, "scwc", l, C_SC, 512)
            wx = self.load_w(es, "scwx", l, C_SX, 512)
            ub = self.sb(es, "scu", [128, T + 3], F32)
            bb = self.sb(es, "scb", [128, T], F32)
            hts = [self.sb(es, "scht%d" % k, [128, 8, 512], BF) for k in range(2)]
            cs = [self.sb(es, "sccs%d" % k, [128, 512], F32) for k in range(2)]
            acc = [self.sb(es, "scacc%d" % k, [128, 512], F32) for k in range(2)]
            ys = [self.sb(es, "scy%d" % k, [128, 512], BF) for k in range(2)]
            self.V("pool", "memset", [], [ub], ub[:], 0.0)
            n = 0
            for j in range(4):
                for ti, (t0, N) in enumerate(TILES):
                    ht = hts[n % 2]
                    c_ = cs[n % 2]
                    n += 1
                    self.load_ht(ht, ti)
                    base = t0 + 1 if ti == 0 else t0 + 2
                    pb, pc, px = self.psb(), self.psb(), self.psb()
                    self.proj(pb, wb, j * 128, ht, N)
                    self.proj(pc, wc, j * 128, ht, N)
                    self.proj(px, wx, j * 128, ht, N)
                    self.act(c_[:, 0:N], pc[:, 0:N], AF.Copy, [pc], [c_])
                    self.V("dve", "tensor_tensor", [c_, px], [ub], out=ub[:, base:base + N], in0=c_[:, 0:N], in1=px[:, 0:N], op=ALU.mult)
                    self.act(bb[:, t0:t0 + N], pb[:, 0:N], AF.Copy, [pb], [bb])
                for ti, (t0, N) in enumerate(TILES):
                    base = t0 + 1 if ti == 0 else t0 + 2
                    a = acc[ti % 2]
                    y = ys[ti % 2]
                    self.V("dve", "tensor_scalar", [ub, P], [a], out=a[:, 0:N], in0=ub[:, base - 1:base - 1 + N], scalar1=P[:, j * 3:j * 3 + 1],
                           scalar2=0.0, op0=ALU.mult, op1=ALU.add)
                    for k in (1, 2):
                        self.V("dve", "scalar_tensor_tensor", [ub, P, a], [a], out=a[:, 0:N], in0=ub[:, base - 1 + k:base - 1 + k + N],
                               scalar=P[:, j * 3 + k:j * 3 + k + 1], in1=a[:, 0:N], op0=ALU.mult, op1=ALU.add)
                    self.V("pool", "tensor_tensor", [a, bb], [y], out=y[:, 0:N], in0=a[:, 0:N], in1=bb[:, t0:t0 + N], op=ALU.mult)
                    S.dma("sp", self.Y[3, j, :, t0:t0 + N], y[:, 0:N], [y], [Tl(None, self.Yt[3][ti])])
            S.barrier()

    def phase_conv(self, l):
        S = self.S
        P = self.P
        onesf = self.cst[:, 128:256]
        with contextlib.ExitStack() as es:
            wa = self.load_w(es, "cvwa", l, C_CVA, 512)
            wg = self.load_w(es, "cvwg", l, C_CVG, 512)
            vb = self.sb(es, "cvv", [128, 4, T + 45], BF)
            hts = [self.sb(es, "cvht%d" % k, [128, 8, 512], BF) for k in range(2)]
            sg = [self.sb(es, "cvsg%d" % k, [128, 512], F32) for k in range(2)]
            self.V("pool", "memset", [], [vb], vb[:], 0.0)
            n = 0
            for ti, (t0, N) in enumerate(TILES):
                ht = hts[ti % 2]
                self.load_ht(ht, ti)
                base = t0 + 15 if ti == 0 else t0 + 30
                for j in range(4):
                    pa, pg = self.psb(), self.psb()
                    self.proj(pa, wa, j * 128, ht, N)
                    self.proj(pg, wg, j * 128, ht, N)
                    s_ = sg[n % 2]
                    n += 1
                    self.act(s_[:, 0:N], pg[:, 0:N], AF.Sigmoid, [pg], [s_])
                    self.V("dve", "tensor_tensor", [s_, pa], [vb], out=vb[:, j, base:base + N], in0=pa[:, 0:N], in1=s_[:, 0:N], op=ALU.mult)
            DG = self.sb(es, "cvDG", [128, 124, 128], BF)
            for jk in range(124):
                self.V("pool", "tensor_scalar", [self.identb, P], [DG], out=DG[:, jk, :], in0=self.identb[:], scalar1=P[:, 12 + jk:13 + jk], scalar2=0.0,
                       op0=ALU.mult, op1=ALU.add)
            ca = [self.sb(es, "cvca%d" % k, [128, 4, 512], F32) for k in range(2)]
            cp = self.sb(es, "cvcp", [128, 512], F32)
            sq = self.sb(es, "cvsq", [128, 4, 512], F32)
            mean = self.sb(es, "cvmean", [128, 512], F32)
            tmp = self.sb(es, "cvtmp", [128, 512], F32)
            rstd = self.sb(es, "cvrstd", [128, 512], F32)
            dd = [self.sb(es, "cvd%d" % k, [128, 512], F32) for k in range(2)]
            ys = [self.sb(es, "cvy%d" % k, [128, 4, 512], BF) for k in range(2)]
            for ti, (t0, N) in enumerate(TILES):
                base = t0 + 15 if ti == 0 else t0 + 30
                a = ca[ti % 2]
                for j in range(4):
                    pc = self.psb()
                    for k in range(31):
                        self.mm(pc[:, 0:N], DG[:, j * 31 + k, :], vb[:, j, base - 15 + k:base - 15 + k + N], k == 0, k == 30, [DG, vb], [pc])
                    self.act(a[:, j, 0:N], pc[:, 0:N], AF.Identity, [pc, P], [a], bias=P[:, 136 + j:137 + j])
                    self.act(sq[:, j, 0:N], a[:, j, 0:N], AF.Square, [a], [sq])
                p1, p2 = self.psb(), self.psb()
                for j in range(4):
                    self.mm(p1[:, 0:N], onesf, a[:, j, 0:N], j == 0, j == 3, [self.cst, a], [p1])
                for j in range(4):
                    self.mm(p2[:, 0:N], onesf, sq[:, j, 0:N], j == 0, j == 3, [self.cst, sq], [p2])
                self.act(mean[:, 0:N], p1[:, 0:N], AF.Copy, [p1], [mean], scale=1.0 / W)
                self.V("pool", "tensor_tensor", [mean], [tmp], out=tmp[:, 0:N], in0=mean[:, 0:N], in1=mean[:, 0:N], op=ALU.mult)
                self.V("dve", "scalar_tensor_tensor", [p2, tmp], [tmp], out=tmp[:, 0:N], in0=p2[:, 0:N], scalar=1.0 / W, in1=tmp[:, 0:N],
                       op0=ALU.mult, op1=ALU.subtract)
                self.rstd_from(tmp[:, 0:N], 1.0, rstd, tmp, N, [tmp])
                y = ys[ti % 2]
                for j in range(4):
                    d = dd[j % 2]
                    self.V("dve", "tensor_tensor", [a, mean], [d], out=d[:, 0:N], in0=a[:, j, 0:N], in1=mean[:, 0:N], op=ALU.subtract)
                    self.V("pool", "tensor_tensor", [d, rstd], [d], out=d[:, 0:N], in0=d[:, 0:N], in1=rstd[:, 0:N], op=ALU.mult)
                    self.V("dve", "tensor_scalar", [d, P], [d], out=d[:, 0:N], in0=d[:, 0:N], scalar1=P[:, 140 + j:141 + j], scalar2=P[:, 144 + j:145 + j],
                           op0=ALU.mult, op1=ALU.add)
                    self.act(y[:, j, 0:N], d[:, 0:N], AF.Silu, [d], [y])
                S.dma("sp", self.Y[2, :, :, t0:t0 + N].rearrange("j p n -> p j n"), y[:, :, 0:N], [y], [Tl(None, self.Yt[2][ti])])
            S.barrier()

    def rope(self, ps, raw, cos, sin, t1, t2, dst_ap, dst_t, N, rot_ps):
        self.act(raw[:, 0:N], ps[:, 0:N], AF.Copy, [ps], [raw])
        self.mm(rot_ps[:, 0:N], self.Rb[:], raw[:, 0:N], True, True, [self.Rb, raw], [rot_ps])
        self.V("pool", "tensor_tensor", [raw, cos], [t1], out=t1[:, 0:N], in0=raw[:, 0:N], in1=cos[:, 0:N], op=ALU.mult)
        self.V("dve", "tensor_tensor", [rot_ps, sin], [t2], out=t2[:, 0:N], in0=rot_ps[:, 0:N], in1=sin[:, 0:N], op=ALU.mult)
        self.V("pool", "tensor_tensor", [t1, t2], [dst_t], out=dst_ap, in0=t1[:, 0:N], in1=t2[:, 0:N], op=ALU.add)

    def phase_attn(self, l):
        S = self.S
        P = self.P
        i = self.i
        onesf = self.cst[:, 128:256]
        B = self.banks
        with contextlib.ExitStack() as es:
            wq = self.load_w(es, "dawq", l, C_DQ, 512)
            wk = self.load_w(es, "dawk", l, C_DK, 512)
            wv = self.load_w(es, "dawv", l, C_DV, 512)
            KT = self.sb(es, "daKT", [128, 4, T], BF)
            Vt = self.sb(es, "daV", [128, NCH, 512], BF)
            hts = [self.sb(es, "daht%d" % k, [128, 8, 512], BF) for k in range(2)]
            cos = [self.sb(es, "dacos%d" % k, [128, 512], F32) for k in range(2)]
            sin = [self.sb(es, "dasin%d" % k, [128, 512], F32) for k in range(2)]
            raw = [self.sb(es, "daraw%d" % k, [128, 512], BF) for k in range(2)]
            t1 = [self.sb(es, "dat1%d" % k, [128, 512], F32) for k in range(2)]
            t2 = [self.sb(es, "dat2%d" % k, [128, 512], F32) for k in range(2)]
            n = 0
            for ti, (t0, N) in enumerate(TILES):
                ht = hts[ti % 2]
                self.load_ht(ht, ti)
                S.dma("sp", cos[ti % 2][:, 0:N], i["rope"][0, :, t0:t0 + N], [], [cos[ti % 2]])
                S.dma("sp", sin[ti % 2][:, 0:N], i["rope"][1, :, t0:t0 + N], [], [sin[ti % 2]])
                for h in range(4):
                    ps, rp = self.psb(), self.psb()
                    self.proj(ps, wk, h * 128, ht, N)
                    self.rope(ps, raw[n % 2], cos[ti % 2], sin[ti % 2], t1[n % 2], t2[n % 2], KT[:, h, t0:t0 + N], KT, N, rp)
                    n += 1
                for cc in range(N // 128):
                    ps = self.psb()
                    for kc in range(8):
                        self.mm(ps[:, :], ht[:, kc, cc * 128:(cc + 1) * 128], wv[:, kc, :], kc == 0, kc == 7, [ht, wv], [ps])
                    self.act(Vt[:, t0 // 128 + cc, :], ps[:, :], AF.Copy, [ps], [Vt])
            QT = [self.sb(es, "daQT%d" % k, [128, 512], BF) for k in range(2)]
            QZ = [[self.sb(es, "daQZ%d_%d" % (k, c), [128, 512], BF) for c in range(2)] for k in range(2)]
            for k in range(2):
                for c in range(2):
                    self.V("pool", "memset", [], [QZ[k][c]], QZ[k][c][:], 0.0)
            pT = [self.sb(es, "dapT%d" % k, [128, 512], BF) for k in range(4)]
            rd = [self.sb(es, "dard%d" % k, [128, 512], F32) for k in range(2)]
            rp_ = [self.sb(es, "darp%d" % k, [128, 512], F32) for k in range(2)]
            rinv = self.sb(es, "darinv", [128, 512], F32)
            Oc = [self.sb(es, "daOc%d" % k, [128, 512], F32) for k in range(2)]
            o = self.sb(es, "dao", [128, 512], F32)
            sq = self.sb(es, "dasq", [128, 512], F32)
            tmp = self.sb(es, "datmp", [128, 512], F32)
            rstd = self.sb(es, "darstd", [128, 512], F32)
            ys = [self.sb(es, "day%d" % k, [128, 4, 512], BF) for k in range(2)]
            it = 0
            for ti, (t0, N) in enumerate(TILES):
                ht = hts[ti % 2]
                self.load_ht(ht, ti)
                S.dma("sp", cos[ti % 2][:, 0:N], i["rope"][0, :, t0:t0 + N], [], [cos[ti % 2]])
                S.dma("sp", sin[ti % 2][:, 0:N], i["rope"][1, :, t0:t0 + N], [], [sin[ti % 2]])
                nk = 2 if ti == 0 else NCH
                y = ys[ti % 2]
                def qprep(h_, n_):
                    qt_ = QT[h_ % 2]
                    self.proj(B[7], wq, h_ * 128, ht, N)
                    self.rope(B[7], raw[n_ % 2], cos[ti % 2], sin[ti % 2], t1[n_ % 2], t2[n_ % 2], qt_[:, 0:N], qt_, N, B[7])
                    qz_ = QZ[h_ % 2]
                    self.V("pool", "tensor_copy", [qt_], [qz_[0]], out=qz_[0][0:64, 0:N], in_=qt_[0:64, 0:N])
                    self.V("dve", "tensor_copy", [qt_], [qz_[1]], out=qz_[1][64:128, 0:N], in_=qt_[64:128, 0:N])
                qprep(0, n)
                n += 1
                for h in range(4):
                    qt = QT[h % 2]
                    qz = QZ[h % 2]
                    if h + 1 < 4:
                        qprep(h + 1, n)
                        n += 1
                    its = [(c, kc) for c in range(2) for kc in range(nk)]
                    slots = {}
                    def qk(j):
                        c, kc = its[j]
                        sps = B[2 + it_base[0] % 3]
                        p = pT[it_base[0] % 4]
                        it_base[0] += 1
                        slots[j] = (sps, p)
                        p0 = c * 64
                        self.mm(sps[:, 0:N], KT[:, h, kc * 128:(kc + 1) * 128], qz[c][:, 0:N], True, True, [KT, qz[c]], [sps])
                    it_base = [it]
                    qk(0)
                    if len(its) > 1:
                        qk(1)
                    for j, (c, kc) in enumerate(its):
                        sps, p = slots.pop(j)
                        oacc = B[c]
                        self.act(p[:, 0:N], sps[:, 0:N], AF.Exp, [sps], [p], scale=0.125)
                        if j + 2 < len(its):
                            qk(j + 2)
                        self.mm(oacc[:, 0:N], Vt[:, kc, h * 128:(h + 1) * 128], p[:, 0:N], kc == 0, kc == nk - 1, [Vt, p], [oacc])
                        rsb = B[5 + c]
                        self.mm(rsb[:, 0:N], self.onesb[:], p[:, 0:N], kc == 0, kc == nk - 1, [self.onesb, p], [rsb])
                        if kc == nk - 1:
                            self.V("dve", "reciprocal", [rsb], [rinv], out=rinv[:, 0:N], in_=rsb[:, 0:N])
                            self.V("dve", "tensor_tensor", [oacc, rinv], [Oc[c]], out=Oc[c][:, 0:N], in0=oacc[:, 0:N], in1=rinv[:, 0:N], op=ALU.mult)
                    it = it_base[0]
                    self.V("dve", "scalar_tensor_tensor", [Oc[0], Oc[1], P], [o], out=o[:, 0:N], in0=Oc[1][:, 0:N], scalar=P[:, 153:154], in1=Oc[0][:, 0:N],
                           op0=ALU.mult, op1=ALU.add)
                    self.act(sq[:, 0:N], o[:, 0:N], AF.Square, [o], [sq])
                    self.mm(B[7][:, 0:N], onesf, sq[:, 0:N], True, True, [self.cst, sq], [B[7]])
                    self.rstd_from(B[7][:, 0:N], 1.0 / 128, rstd, tmp, N, [B[7]])
                    self.V("dve", "scalar_tensor_tensor", [o, rstd, P], [y], out=y[:, h, 0:N], in0=o[:, 0:N], scalar=P[:, 154:155], in1=rstd[:, 0:N],
                           op0=ALU.mult, op1=ALU.mult)
                S.dma("sp", self.Y[1, :, :, t0:t0 + N].rearrange("j p n -> p j n"), y[:, :, 0:N], [y], [Tl(None, self.Yt[1][ti])])
            S.barrier()

    def phase_hgrn(self, l):
        S = self.S
        B = self.banks
        with contextlib.ExitStack() as es:
            rot = [2]
            def rb():
                b = B[rot[0]]
                rot[0] = 2 + (rot[0] - 1) % 6
                return b
            sh = {k: self.sb(es, "hgs_" + k, [128, 512], F32) for k in ("osum", "sq", "tmp", "rstd", "sgg", "y1")}
            sh["ys"] = [self.sb(es, "hgys%d" % k, [128, 512], BF) for k in range(2)]
            sh["n"] = 0
            chains = []
            for k in range(2):
                c = {"k": k}
                c["OF"] = self.sb(es, "hgOF%d" % k, [128, T], F32)
                c["S32"] = self.sb(es, "hgS%d" % k, [128, 128], F32)
                c["Sbf"] = [self.sb(es, "hgSb%d_%d" % (k, j), [128, 128], BF) for j in range(2)]
                c["ht"] = self.sb(es, "hght%d" % k, [128, 8, 512], BF)
                c["w"] = [self.sb(es, "hgw%d_%d" % (k, j), [128, 8, 128], BF) for j in range(4)]
                for nm in ("q32", "ee", "ff", "lf", "kk", "pre", "bb", "d3", "d2", "d3a"):
                    c[nm] = self.sb(es, "hg%s%d" % (nm, k), [128, 512], F32)
                for nm in ("qE1", "qE3", "kE4", "kE2", "qE3b", "kE4b"):
                    c[nm] = self.sb(es, "hg%s%d" % (nm, k), [128, 512], BF)
                c["iT"] = self.sb(es, "hgiT%d" % k, [128, 4, 128], BF)
                c["kT"] = self.sb(es, "hgkT%d" % k, [128, 4, 128], BF)
                c["AM"] = [self.sb(es, "hgAM%d_%d" % (k, j), [128, 128], BF) for j in range(2)]
                c["a1"] = [self.sb(es, "hga1%d_%d" % (k, j), [128, 128], F32) for j in range(2)]
                c["a2"] = [self.sb(es, "hga2%d_%d" % (k, j), [128, 128], F32) for j in range(2)]
                c["ops"] = B[k]
                chains.append(c)
            for pair in ((0, 1), (2, 3)):
                for d in range(2):
                    gens = [self.hgrn_chain(l, h, d, chains[k], sh, rb) for k, h in enumerate(pair)]
                    live = list(gens)
                    while live:
                        for g in list(live):
                            try:
                                next(g)
                            except StopIteration:
                                live.remove(g)
            S.barrier()

    def hgrn_chain(self, l, h, d, c, sh, rb):
        S = self.S
        P = self.P
        onesf = self.cst[:, 128:256]
        w = c["w"]
        OF, S32, Sbf, ht, ops = c["OF"], c["S32"], c["Sbf"], c["ht"], c["ops"]
        q32, ee, ff, lf, kk, pre, bb, d3, d2, d3a = (c[n] for n in ("q32", "ee", "ff", "lf", "kk", "pre", "bb", "d3", "d2", "d3a"))
        qE1, qE3, kE4, kE2, qE3b, kE4b = (c[n] for n in ("qE1", "qE3", "kE4", "kE2", "qE3b", "kE4b"))
        iT, kT = c["iT"], c["kT"]
        E1, E3, E3b, E4b, E2, E4 = ee, ff, lf, d3, d2, d3a
        cols = (C_HQ, C_HFF if d == 0 else C_HFB, C_HI, C_HG)
        for k in range(4):
            S.dma("pool", w[k][:], self.i["w_in"][l, :, cols[k] + h * 128:cols[k] + (h + 1) * 128].rearrange("(kc p) n -> p kc n", p=128), [], [w[k]])
        self.V("pool", "memset", [], [S32], S32[:], 0.0)
        self.V("pool", "memset", [], [Sbf[0]], Sbf[0][:], 0.0)
        cur = 0
        am_i = 0
        lbc = self.LB[:, l, d * 4 + h:d * 4 + h + 1]
        omc = self.OML[:, l, d * 4 + h:d * 4 + h + 1]
        order = list(range(9)) if d == 0 else [0] + list(range(8, 0, -1))
        mask = self.cst[:, 384:512] if d == 0 else self.cst[:, 512:640]
        masko = self.cst[:, 640:768] if d == 0 else self.cst[:, 768:896]
        for ti in order:
            t0, N = TILES[ti]
            nb = N // 128
            nch = N // 64
            self.load_ht(ht, ti)
            pq, pf = rb(), rb()
            self.proj(pq, w[0], 0, ht, N)
            self.proj(pf, w[1], 0, ht, N)
            self.act(q32[:, 0:N], pq[:, 0:N], AF.Copy, [pq], [q32])
            self.act(ee[:, 0:N], pf[:, 0:N], AF.Exp, [pf], [ee], scale=-1.0)
            self.V("dve", "tensor_scalar", [ee], [ee], out=ee[:, 0:N], in0=ee[:, 0:N], scalar1=1.0, scalar2=1.0, op0=ALU.add, op1=ALU.mult)
            self.V("dve", "reciprocal", [ee], [ee], out=ee[:, 0:N], in_=ee[:, 0:N])
            self.V("dve", "tensor_scalar", [ee, self.LB, self.OML], [ff], out=ff[:, 0:N], in0=ee[:, 0:N], scalar1=omc, scalar2=lbc, op0=ALU.mult, op1=ALU.add)
            self.act(lf[:, 0:N], ff[:, 0:N], AF.Ln, [ff], [lf])
            self.V("pool", "tensor_scalar", [ff], [kk], out=kk[:, 0:N], in0=ff[:, 0:N], scalar1=-1.0, scalar2=1.0, op0=ALU.mult, op1=ALU.add)
            self.V("dve", "tensor_tensor_scan", [self.seg, lf], [pre], out=pre[:, 0:N], data0=self.seg[:, 0:N], data1=lf[:, 0:N], initial=0.0, op0=ALU.mult, op1=ALU.add)
            v3 = lambda t_: t_[:, 0:N].rearrange("p (c s) -> p c s", s=64)
            v32 = lambda t_: t_[:, 0:N].rearrange("p (c s) -> p c s", s=32)
            bc = lambda t_, col: v3(t_)[:, :, col:col + 1].to_broadcast([128, nch, 64])
            if d == 0:
                b_ = pre
                cend = 63
            else:
                b_ = bb
                cend = 0
                self.V("dve", "tensor_tensor", [lf, pre], [bb], out=bb[:, 0:N], in0=lf[:, 0:N], in1=pre[:, 0:N], op=ALU.subtract)
                self.V("dve", "tensor_tensor", [bb, pre], [bb], out=v3(bb), in0=v3(bb), in1=bc(pre, 63), op=ALU.add)
            yield
            self.V("dve", "tensor_tensor", [b_], [d3], out=v3(d3), in0=v3(b_), in1=bc(b_, 32), op=ALU.subtract)
            self.V("pool", "tensor_tensor", [b_], [d2], out=v3(d2), in0=v3(b_), in1=bc(b_, cend), op=ALU.subtract)
            self.V("dve", "tensor_tensor", [b_], [d3a], out=v32(d3a), in0=v32(b_), in1=v32(b_)[:, :, 16:17].to_broadcast([128, 2 * nch, 32]), op=ALU.subtract)
            self.act(E1[:, 0:N], b_[:, 0:N], AF.Exp, [b_], [E1])
            self.act(E2[:, 0:N], d2[:, 0:N], AF.Exp, [d2], [E2], scale=-1.0)
            self.act(E3[:, 0:N], d3a[:, 0:N], AF.Exp, [d3a], [E3])
            self.act(E4[:, 0:N], d3a[:, 0:N], AF.Exp, [d3a], [E4], scale=-1.0)
            self.act(E3b[:, 0:N], d3[:, 0:N], AF.Exp, [d3], [E3b])
            self.act(E4b[:, 0:N], d3[:, 0:N], AF.Exp, [d3], [E4b], scale=-1.0)
            self.V("dve", "tensor_tensor", [q32, E1], [qE1], out=qE1[:, 0:N], in0=q32[:, 0:N], in1=E1[:, 0:N], op=ALU.mult)
            self.V("pool", "tensor_tensor", [q32, E3], [qE3], out=qE3[:, 0:N], in0=q32[:, 0:N], in1=E3[:, 0:N], op=ALU.mult)
            self.V("dve", "tensor_tensor", [kk, E4], [kE4], out=kE4[:, 0:N], in0=kk[:, 0:N], in1=E4[:, 0:N], op=ALU.mult)
            self.V("pool", "tensor_tensor", [kk, E2], [kE2], out=kE2[:, 0:N], in0=kk[:, 0:N], in1=E2[:, 0:N], op=ALU.mult)
            self.V("dve", "tensor_tensor", [q32, E3b], [qE3b], out=qE3b[:, 0:N], in0=q32[:, 0:N], in1=E3b[:, 0:N], op=ALU.mult)
            self.V("pool", "tensor_tensor", [kk, E4b], [kE4b], out=kE4b[:, 0:N], in0=kk[:, 0:N], in1=E4b[:, 0:N], op=ALU.mult)
            qz, kz = (slice(0, 32), slice(32, 64)) if d == 0 else (slice(32, 64), slice(0, 32))
            self.V("dve", "memset", [qE3b], [qE3b], v3(qE3b)[:, :, qz], 0.0)
            self.V("pool", "memset", [kE4b], [kE4b], v3(kE4b)[:, :, kz], 0.0)
            yield
            for cc in range(nb):
                pi = rb()
                for kc in range(8):
                    self.mm(pi[:, 0:128], ht[:, kc, cc * 128:(cc + 1) * 128], w[2][:, kc, :], kc == 0, kc == 7, [ht, w[2]], [pi])
                self.act(iT[:, cc, :], pi[:, 0:128], AF.Copy, [pi], [iT])
                pt = rb()
                ptv = pt[:].bitcast(BF)
                self.tr(ptv[:, 0:128], kE2[:, cc * 128:(cc + 1) * 128], self.identb[:], [kE2, self.identb], [pt])
                self.act(kT[:, cc, :], ptv[:, 0:128], AF.Copy, [pt], [kT])
            yield
            blks = list(range(nb)) if d == 0 else list(range(nb - 1, -1, -1))
            for blk in blks:
                pa = rb()
                bs = slice(blk * 128, (blk + 1) * 128)
                self.mm(pa[:, 0:128], kE4[:, bs], qE3[:, bs], True, True, [kE4, qE3], [pa])
                pb_ = rb()
                self.mm(pb_[:, 0:128], kE4b[:, bs], qE3b[:, bs], True, True, [kE4b, qE3b], [pb_])
                am = c["AM"][am_i % 2]
                a1 = c["a1"][am_i % 2]
                a2 = c["a2"][am_i % 2]
                am_i += 1
                self.V("dve", "tensor_tensor", [pa, self.cst], [a1], out=a1[:], in0=pa[:, 0:128], in1=mask, op=ALU.mult)
                self.V("dve", "tensor_tensor", [pb_, self.cst], [a2], out=a2[:], in0=pb_[:, 0:128], in1=masko, op=ALU.mult)
                self.V("pool", "tensor_tensor", [a1, a2], [am], out=am[:], in0=a1[:], in1=a2[:], op=ALU.add)
                for ch in ((0, 1) if d == 0 else (1, 0)):
                    p0 = ch * 64
                    c0 = blk * 128 + p0
                    self.mm(ops[:, c0:c0 + 64], Sbf[cur][:], qE1[:, c0:c0 + 64], True, False, [Sbf[cur], qE1], [ops])
                    self.mm(ops[:, c0:c0 + 64], iT[p0:p0 + 64, blk, :], am[p0:p0 + 64, p0:p0 + 64], False, True, [iT, am], [ops])
                    pS = rb()
                    self.mm(pS[:, 0:128], kT[p0:p0 + 64, blk, :], iT[p0:p0 + 64, blk, :], True, True, [kT, iT], [pS])
                    ce = c0 + cend
                    self.V("dve", "scalar_tensor_tensor", [S32, E1, pS], [S32], out=S32[:], in0=S32[:], scalar=E1[:, ce:ce + 1], in1=pS[:, 0:128],
                           op0=ALU.mult, op1=ALU.add)
                    cur = 1 - cur
                    self.act(Sbf[cur][:], S32[:], AF.Copy, [S32], [Sbf[cur]])
                    yield
            if d == 0:
                self.act(OF[:, t0:t0 + N], ops[:, 0:N], AF.Copy, [ops], [OF])
            else:
                osum, sq, tmp, rstd, sgg, y1 = (sh[n_] for n_ in ("osum", "sq", "tmp", "rstd", "sgg", "y1"))
                self.V("dve", "tensor_tensor", [OF, ops], [osum], out=osum[:, 0:N], in0=OF[:, t0:t0 + N], in1=ops[:, 0:N], op=ALU.add)
                self.act(sq[:, 0:N], osum[:, 0:N], AF.Square, [osum], [sq])
                pss = rb()
                self.mm(pss[:, 0:N], onesf, sq[:, 0:N], True, True, [self.cst, sq], [pss])
                self.rstd_from(pss[:, 0:N], 1.0 / 128, rstd, tmp, N, [pss])
                pg = rb()
                self.proj(pg, w[3], 0, ht, N)
                self.act(sgg[:, 0:N], pg[:, 0:N], AF.Sigmoid, [pg], [sgg])
                self.V("dve", "scalar_tensor_tensor", [osum, rstd, P], [y1], out=y1[:, 0:N], in0=osum[:, 0:N], scalar=P[:, 148 + h:149 + h], in1=rstd[:, 0:N],
                       op0=ALU.mult, op1=ALU.mult)
                y = sh["ys"][sh["n"] % 2]
                sh["n"] += 1
                self.V("pool", "tensor_tensor", [y1, sgg], [y], out=y[:, 0:N], in0=y1[:, 0:N], in1=sgg[:, 0:N], op=ALU.mult)
                S.dma("sp", self.Y[0, h, :, t0:t0 + N], y[:, 0:N], [y], [Tl(None, self.Yt[0][ti])])
            yield

    def phase_merge(self, l):
        S = self.S
        i = self.i
        with contextlib.ExitStack() as es:
            wg = self.sb(es, "mgwg", [128, 8, 4096], BF)
            for k in range(4):
                S.dma("pool", wg[:, :, k * 1024:(k + 1) * 1024], i["w_in"][l, :, C_GATE + k * 1024:C_GATE + (k + 1) * 1024].rearrange("(kc p) n -> p kc n", p=128), [], [wg])
            wb = self.sb(es, "mgwb", [128, 4, 4, 1024], BF)
            for k in range(4):
                S.dma("pool", wb[:, k, :, :], i["w_branch"][l, k].rearrange("(cc p) n -> p cc n", p=128), [], [wb])
            ht = self.sb(es, "mght", [128, 8, 512], BF)
            Yk = [self.sb(es, "mgY%d" % k, [128, 4, 512], BF) for k in range(4)]
            sg = [self.sb(es, "mgsg%d" % k, [128, 512], F32) for k in range(2)]
            tmp = [self.sb(es, "mgtmp%d" % k, [128, 512], F32) for k in range(2)]
            macc = [self.sb(es, "mgacc%d" % k, [128, 512], F32) for k in range(2)]
            mT = [self.sb(es, "mgmT%d" % k, [128, 8, 512], BF) for k in range(2)]
            n = 0
            for ti, (t0, N) in enumerate(TILES):
                self.load_ht(ht, ti)
                for k in range(4):
                    S.dma("sp", Yk[k][:, :, 0:N], self.Y[k, :, :, t0:t0 + N].rearrange("j p n -> p j n"), [Tl(None, self.Yt[k][ti])], [Yk[k]])
                m = mT[ti % 2]
                for nch in range(8):
                    ma = macc[nch % 2]
                    for k in range(4):
                        pg, pp = self.psb(), self.psb()
                        self.proj(pg, wg, k * 1024 + nch * 128, ht, N)
                        for cc in range(4):
                            self.mm(pp[:, 0:N], wb[:, k, cc, nch * 128:(nch + 1) * 128], Yk[k][:, cc, 0:N], cc == 0, cc == 3, [wb, Yk[k]], [pp])
                        s_ = sg[n % 2]
                        t_ = tmp[n % 2]
                        n += 1
                        self.act(s_[:, 0:N], pg[:, 0:N], AF.Sigmoid, [pg], [s_])
                        if k == 0:
                            self.V("dve", "tensor_tensor", [pp, s_], [ma], out=ma[:, 0:N], in0=pp[:, 0:N], in1=s_[:, 0:N], op=ALU.mult)
                        else:
                            self.V("dve", "tensor_tensor", [pp, s_], [t_], out=t_[:, 0:N], in0=pp[:, 0:N], in1=s_[:, 0:N], op=ALU.mult)
                            self.V("pool", "tensor_tensor", [ma, t_], [ma], out=ma[:, 0:N], in0=ma[:, 0:N], in1=t_[:, 0:N], op=ALU.add)
                    self.V("pool", "tensor_copy", [ma], [m], out=m[:, nch, 0:N], in_=ma[:, 0:N])
                S.dma("sp", self.MT[:, :, t0:t0 + N].rearrange("k p n -> p k n"), m[:, :, 0:N], [m], [Tl(None, self.MTt[ti])])
            S.barrier()
        with contextlib.ExitStack() as es:
            wo = self.sb(es, "mgwo", [128, 8, 1024], BF)
            S.dma("pool", wo[:], i["w_out"][l].rearrange("(kc p) n -> p kc n", p=128), [], [wo])
            mts = [self.sb(es, "mgmt%d" % k, [128, 8, 512], BF) for k in range(2)]
            self.residual_setup(es, 2)
            for ti, (t0, N) in enumerate(TILES):
                mt = mts[ti % 2]
                S.dma("sp", mt[:, :, 0:N], self.MT[:, :, t0:t0 + N].rearrange("k p n -> p k n"), [Tl(None, self.MTt[ti])], [mt])
                for cc in range(N // 128):
                    c = t0 // 128 + cc
                    halves = []
                    for half in range(2):
                        po = self.psb()
                        for kc in range(8):
                            self.mm(po[:, :], mt[:, kc, cc * 128:(cc + 1) * 128], wo[:, kc, half * 512:(half + 1) * 512], kc == 0, kc == 7, [mt, wo], [po])
                        halves.append(po)
                    self.residual(c, lambda half: halves[half][:, :], halves)
            S.barrier()

    def residual_setup(self, es, modidx):
        S = self.S
        self.rmod = [self.sb(es, "rsmod%d" % k, [128, D], F32) for k in range(2)]
        for k in range(2):
            S.dma("sp", self.rmod[k][:], self.MODR[k, :, modidx * D:(modidx + 1) * D], [Tl(None, self.MODt)], [self.rmod[k]])
        self.rx = [self.sb(es, "rsx%d" % k, [128, D], F32) for k in range(3)]
        self.rtmp = [self.sb(es, "rstmp%d" % k, [128, D], F32) for k in range(2)]

    def residual(self, c, delta_ap, delta_tiles):
        S = self.S
        lat = 0 if c >= 2 else 1
        x = self.rx[c % 3]
        t = self.rtmp[c % 2]
        xt = Tl(None, self.Xt[c])
        S.dma("sp", x[:], self.X[c * 128:(c + 1) * 128, :], [xt], [x])
        for half in range(2):
            hs = slice(half * 512, (half + 1) * 512)
            self.V("dve", "tensor_tensor", [delta_tiles[half], self.rmod[lat]], [t], out=t[:, hs], in0=delta_ap(half), in1=self.rmod[lat][:, hs], op=ALU.mult)
        self.V("dve", "tensor_tensor", [x, t], [x], out=x[:], in0=x[:], in1=t[:], op=ALU.add)
        S.dma("act", self.X[c * 128:(c + 1) * 128, :], x[:], [x], [xt])

    def route(self, c, t, Bm, h32, h32T, wr, R):
        P = self.P
        self.V("pool", "tensor_tensor", [t, Bm], [h32], out=h32[:], in0=t[:], in1=Bm[:], op=ALU.add)
        for g in range(2):
            ps = self.psb()
            for k in range(4):
                kk = g * 4 + k
                self.tr(ps[:, k * 128:(k + 1) * 128], h32[:, kk * 128:(kk + 1) * 128], self.cst[:, 0:128], [h32, self.cst], [ps])
            self.act(h32T[:, g * 4:(g + 1) * 4, :], ps[:].rearrange("p (k n) -> p k n", k=4), AF.Copy, [ps], [h32T])
        pl = self.psb()
        for kc in range(8):
            self.mm(pl[:, 0:36], h32T[:, kc, :], wr[:, kc, :], kc == 0, kc == 7, [h32T, wr], [pl])
        dv = lambda name, w_, **kw: self.V("dve", name, [R, P] + w_[1:], [w_[0]], **kw)
        self.V("dve", "tensor_tensor", [pl, P], [R], out=R[:, 0:36], in0=pl[:, 0:36], in1=P[:, 416:452], op=ALU.add)
        RR = [R]
        dv("tensor_reduce", RR, out=R[:, 36:37], in_=R[:, 0:4], axis=AX.X, op=ALU.max)
        dv("tensor_scalar", RR, out=R[:, 37:38], in0=R[:, 36:37], scalar1=-1.0, scalar2=0.0, op0=ALU.mult, op1=ALU.add)
        self.act(R[:, 44:48], R[:, 0:4], AF.Exp, [R], [R], bias=R[:, 37:38], accum_out=R[:, 38:39])
        dv("reciprocal", RR, out=R[:, 39:40], in_=R[:, 38:39])
        dv("tensor_scalar", RR, out=R[:, 40:44], in0=R[:, 0:4], scalar1=R[:, 36:37], scalar2=1.0, op0=ALU.is_equal, op1=ALU.mult)
        dv("tensor_tensor", RR, out=R[:, 48:80].rearrange("p (g e) -> p g e", g=4), in0=R[:, 4:36].rearrange("p (g e) -> p g e", g=4),
           in1=R[:, 40:44].unsqueeze(2).to_broadcast([128, 4, 8]), op=ALU.mult)
        dv("tensor_reduce", RR, out=R[:, 80:88], in_=R[:, 48:80].rearrange("p (g e) -> p e g", g=4), axis=AX.X, op=ALU.add)
        dv("tensor_reduce", RR, out=R[:, 88:89], in_=R[:, 80:88], axis=AX.X, op=ALU.max)
        dv("tensor_scalar", RR, out=R[:, 89:97], in0=R[:, 80:88], scalar1=R[:, 88:89], scalar2=1.0, op0=ALU.is_equal, op1=ALU.mult)
        dv("scalar_tensor_tensor", RR, out=R[:, 97:105], in0=R[:, 89:97], scalar=-1e30, in1=R[:, 80:88], op0=ALU.mult, op1=ALU.add)
        dv("tensor_reduce", RR, out=R[:, 105:106], in_=R[:, 97:105], axis=AX.X, op=ALU.max)
        dv("tensor_scalar", RR, out=R[:, 106:114], in0=R[:, 97:105], scalar1=R[:, 105:106], scalar2=1.0, op0=ALU.is_equal, op1=ALU.mult)
        dv("tensor_tensor", RR, out=R[:, 114:115], in0=R[:, 105:106], in1=R[:, 88:89], op=ALU.subtract)
        self.act(R[:, 115:116], R[:, 114:115], AF.Exp, [R], [R])
        dv("tensor_scalar", RR, out=R[:, 116:117], in0=R[:, 115:116], scalar1=1.0, scalar2=1.0, op0=ALU.add, op1=ALU.mult)
        dv("reciprocal", RR, out=R[:, 116:117], in_=R[:, 116:117])
        dv("tensor_tensor", RR, out=R[:, 117:118], in0=R[:, 116:117], in1=R[:, 39:40], op=ALU.mult)
        dv("tensor_tensor", RR, out=R[:, 118:119], in0=R[:, 39:40], in1=R[:, 117:118], op=ALU.subtract)
        dv("tensor_scalar", RR, out=R[:, 119:127], in0=R[:, 89:97], scalar1=R[:, 117:118], scalar2=0.0, op0=ALU.mult, op1=ALU.add)
        dv("scalar_tensor_tensor", RR, out=R[:, 119:127], in0=R[:, 106:114], scalar=R[:, 118:119], in1=R[:, 119:127], op0=ALU.mult, op1=ALU.add)
        self.V("dve", "tensor_tensor", [R], [self.RW], out=self.RW[:, c, :].rearrange("p (g e) -> p g e", g=4),
               in0=R[:, 40:44].unsqueeze(2).to_broadcast([128, 4, 8]), in1=R[:, 119:127].unsqueeze(1).to_broadcast([128, 4, 8]), op=ALU.mult)

    def phase_moe(self, l):
        S = self.S
        i = self.i
        with contextlib.ExitStack() as es:
            acc = self.sb(es, "moacc", [128, 10, D], F32)
            hTb = self.sb(es, "mohT", [128, 8, 1280], BF)
            wts = [(self.sb(es, "mowg%d" % k, [128, 8, 512], BF), self.sb(es, "mowu%d" % k, [128, 8, 512], BF),
                    self.sb(es, "mowd%d" % k, [128, 4, D], BF)) for k in range(2)]
            sG = [self.sb(es, "mosg%d" % k, [128, 512], F32) for k in range(2)]
            Hh = [self.sb(es, "moHh%d" % k, [128, 4, 512], BF) for k in range(2)]
            self.residual_setup(es, 5)
            n = 0
            m = 0
            for blk in ((0, 1, 2), (3, 4), (5, 6), (7, 8)):
                col = 0
                tcs = []
                for ti in blk:
                    t0, N = TILES[ti]
                    S.dma("sp", hTb[:, :, col:col + N], self.HT[:, :, t0:t0 + N].rearrange("k p n -> p k n"), [Tl(None, self.HTt[ti])], [hTb])
                    tcs.append((col, N, t0))
                    col += N
                for e in range(self.nexp):
                    wg, wu, wd = wts[e % 2]
                    S.dma("pool", wg[:], i["moe_w_gate"][l, e].rearrange("(kc p) f -> p kc f", p=128), [], [wg])
                    S.dma("pool", wu[:], i["moe_w_up"][l, e].rearrange("(kc p) f -> p kc f", p=128), [], [wu])
                    S.dma("pool", wd[:], i["moe_w_down"][l, e].rearrange("(fc p) n -> p fc n", p=128), [], [wd])
                    for (col, N, t0) in tcs:
                        hh = Hh[m % 2]
                        m += 1
                        for fc in range(4):
                            pG, pU = self.psb(), self.psb()
                            for kc in range(8):
                                self.mm(pG[:, 0:N], wg[:, kc, fc * 128:(fc + 1) * 128], hTb[:, kc, col:col + N], kc == 0, kc == 7, [wg, hTb], [pG])
                            for kc in range(8):
                                self.mm(pU[:, 0:N], wu[:, kc, fc * 128:(fc + 1) * 128], hTb[:, kc, col:col + N], kc == 0, kc == 7, [wu, hTb], [pU])
                            sg = sG[n % 2]
                            n += 1
                            self.act(sg[:, 0:N], pG[:, 0:N], AF.Silu, [pG], [sg])
                            self.V("dve", "tensor_tensor", [sg, pU], [hh], out=hh[:, fc, 0:N], in0=sg[:, 0:N], in1=pU[:, 0:N], op=ALU.mult)
                        for cc in range(N // 128):
                            ci = col // 128 + cc
                            c = t0 // 128 + cc
                            for half in range(2):
                                hs = slice(half * 512, (half + 1) * 512)
                                pD = self.psb()
                                for fc in range(4):
                                    self.mm(pD[:, :], hh[:, fc, cc * 128:(cc + 1) * 128], wd[:, fc, hs], fc == 0, fc == 3, [hh, wd], [pD])
                                if e == 0:
                                    self.V("dve", "tensor_scalar", [pD, self.RW], [acc], out=acc[:, ci, hs], in0=pD[:, :], scalar1=self.RW[:, c, e:e + 1], scalar2=0.0,
                                           op0=ALU.mult, op1=ALU.add)
                                else:
                                    self.V("dve", "scalar_tensor_tensor", [pD, self.RW, acc], [acc], out=acc[:, ci, hs], in0=pD[:, :], scalar=self.RW[:, c, e:e + 1],
                                           in1=acc[:, ci, hs], op0=ALU.mult, op1=ALU.add)
                for (col, N, t0) in tcs:
                    for cc in range(N // 128):
                        ci = col // 128 + cc
                        c = t0 // 128 + cc
                        self.residual(c, lambda half, ci=ci: acc[:, ci, half * 512:(half + 1) * 512], [acc, acc])
            S.barrier()

    def rank(self, c, R):
        A = R[:, 128:160]
        self.V("dve", "tensor_scalar", [self.RW], [R], out=A, in0=self.RW[:, c, :], scalar1=0.0, scalar2=1.0, op0=ALU.is_gt, op1=ALU.mult)
        if c == 0:
            self.V("dve", "memset", [], [self.Asum], self.Asum[:], 0.0)
        ps = self.psb()
        self.mm(ps[:, 0:32], self.ltri[:], A, True, False, [self.ltri, R], [ps])
        self.mm(ps[:, 0:32], self.cst[:, 128:256], self.Asum[:], False, True, [self.cst, self.Asum], [ps])
        self.act(self.RK[:, c, :], ps[:, 0:32], AF.Copy, [ps], [self.RK])
        self.V("dve", "tensor_tensor", [self.Asum, R], [self.Asum], out=self.Asum[:], in0=self.Asum[:], in1=A, op=ALU.add)

    def phase_moe_sparse(self, l):
        S = self.S
        i = self.i
        onesf = self.cst[:, 128:256]
        rows_t = Tl(None, self.ROWSt)
        acc_t = Tl(None, self.ACC2t)
        h2_t = Tl(None, self.H2t)
        with contextlib.ExitStack() as es:
            G = self.sb(es, "spG", [128, 1024], F32)
            widx = self.sb(es, "spwidx", [128, 2, NB], U32)
            init = self.sb(es, "spinit", [128, 128, 4], F32)
            self.V("pool", "memset", [], [init], init[:], 0.0)
            self.V("pool", "memset", [init], [init], init[:, :, 0:1], float(T))
            self.V("pool", "memset", [init], [init], init[:, :, 2:4], 1.0e6)
            S.dma("sp", self.ROWS.rearrange("(j p) c -> j (p c)", p=128), init[0:NB, :, :].rearrange("j p c -> j (p c)"), [init], [rows_t])
            ps = self.psb()
            self.mm(ps[:, 0:32], onesf, self.Asum[:], True, True, [self.cst, self.Asum], [ps])
            cnt, pad, pend, pst = G[:, 0:32], G[:, 32:64], G[:, 64:96], G[:, 96:128]
            cmp = self.sb(es, "spcmp", [128, NB, 32], F32)
            self.V("dve", "tensor_copy", [ps], [G], out=cnt, in_=ps[:, 0:32])
            cmp2 = cmp[:].rearrange("p a b -> p (a b)")[:, 0:32 * 68].rearrange("p (e m) -> p e m", m=68)
            self.V("dve", "tensor_tensor", [G, self.cst], [cmp], out=cmp2, in0=cnt.unsqueeze(2).to_broadcast([128, 32, 68]),
                   in1=self.cst[:, 896:896 + 68].unsqueeze(1).to_broadcast([128, 32, 68]), op=ALU.is_gt)
            self.V("dve", "tensor_reduce", [cmp], [G], out=pad, in_=cmp2, axis=AX.X, op=ALU.add)
            self.V("dve", "tensor_scalar", [G], [G], out=pad, in0=pad, scalar1=128.0, scalar2=0.0, op0=ALU.mult, op1=ALU.add)
            self.V("dve", "tensor_tensor_scan", [G, self.cst], [G], out=pend, data0=onesf[:, 0:32], data1=pad, initial=0.0, op0=ALU.mult, op1=ALU.add)
            self.V("dve", "tensor_tensor", [G], [G], out=pst, in0=pend, in1=pad, op=ALU.subtract)
            self.V("dve", "tensor_tensor", [G, self.cst], [cmp], out=cmp[:], in0=pend.unsqueeze(1).to_broadcast([128, NB, 32]),
                   in1=self.cst[:, 896:896 + NB].unsqueeze(2).to_broadcast([128, NB, 32]), op=ALU.is_le)
            be = G[:, 128:128 + NB]
            self.V("dve", "tensor_reduce", [cmp], [G], out=be, in_=cmp[:], axis=AX.X, op=ALU.add)
            self.V("dve", "tensor_scalar", [G], [G], out=be, in0=be, scalar1=31.0, scalar2=128.0, op0=ALU.min, op1=ALU.mult)
            same = G[:, 384:384 + NB]
            self.V("dve", "memset", [G], [G], same, 0.0)
            self.V("dve", "tensor_tensor", [G], [G], out=G[:, 386:384 + NB], in0=G[:, 130:128 + NB], in1=G[:, 128:126 + NB], op=ALU.is_equal)
            wf = G[:, 256:256 + NB]
            self.V("dve", "tensor_scalar", [G, self.cst], [G], out=wf, in0=be, scalar1=self.cst[:, 1024:1025], scalar2=2.0, op0=ALU.add, op1=ALU.mult)
            self.V("dve", "tensor_scalar", [G], [G], out=wf, in0=wf, scalar1=float(l * 8192), scalar2=1.0, op0=ALU.add, op1=ALU.mult)
            self.V("dve", "scalar_tensor_tensor", [G], [G], out=wf, in0=same, scalar=1.0e8, in1=wf, op0=ALU.mult, op1=ALU.add)
            self.V("dve", "tensor_copy", [G], [widx], out=widx[:, 0, :], in_=wf)
            self.V("dve", "tensor_scalar", [G], [G], out=wf, in0=wf, scalar1=1.0, scalar2=1.0, op0=ALU.add, op1=ALU.mult)
            self.V("dve", "tensor_copy", [G], [widx], out=widx[:, 1, :], in_=wf)
            Q = [self.sb(es, "spQ%d" % k, [128, 160], F32) for k in range(2)]
            rec = [self.sb(es, "sprec%d" % k, [128, 2, 4], F32) for k in range(2)]
            didx = [self.sb(es, "spdidx%d" % k, [128, 2], U32) for k in range(2)]
            for c in range(NCH):
                q = Q[c % 2]
                r_ = rec[c % 2]
                di = didx[c % 2]
                A, dst, d1, m1 = q[:, 0:32], q[:, 32:64], q[:, 64:96], q[:, 96:128]
                rd = [self.RW, self.RK, G, q]
                self.V("dve", "tensor_scalar", rd, [q], out=A, in0=self.RW[:, c, :], scalar1=0.0, scalar2=1.0, op0=ALU.is_gt, op1=ALU.mult)
                self.V("dve", "tensor_tensor", rd, [q], out=dst, in0=self.RK[:, c, :], in1=pst, op=ALU.add)
                self.V("dve", "scalar_tensor_tensor", rd, [q], out=d1, in0=dst, scalar=1.0, in1=A, op0=ALU.add, op1=ALU.mult)
                self.V("dve", "tensor_reduce", rd, [q], out=q[:, 128:129], in_=d1, axis=AX.X, op=ALU.max)
                self.V("dve", "tensor_scalar", rd, [q], out=m1, in0=d1, scalar1=q[:, 128:129], scalar2=1.0, op0=ALU.is_equal, op1=ALU.mult)
                self.V("dve", "tensor_tensor", rd, [q], out=m1, in0=m1, in1=self.RW[:, c, :], op=ALU.mult)
                self.V("dve", "tensor_reduce", rd, [q], out=q[:, 129:130], in_=m1, axis=AX.X, op=ALU.add)
                self.V("dve", "tensor_reduce", rd, [q], out=q[:, 130:131], in_=self.RW[:, c, :], axis=AX.X, op=ALU.add)
                self.V("dve", "tensor_scalar", rd, [q], out=m1, in0=A, scalar1=-1.0e9, scalar2=1.0e9, op0=ALU.mult, op1=ALU.add)
                self.V("dve", "tensor_tensor", rd, [q], out=m1, in0=m1, in1=dst, op=ALU.add)
                self.V("dve", "tensor_reduce", rd, [q], out=q[:, 131:132], in_=m1, axis=AX.X, op=ALU.min)
                self.V("dve", "tensor_scalar", rd, [q], out=q[:, 132:133], in0=q[:, 128:129], scalar1=-1.0, scalar2=1.0, op0=ALU.add, op1=ALU.mult)
                self.V("dve", "memset", [], [r_], r_[:], 0.0)
                for k in range(2):
                    self.V("dve", "tensor_scalar", [self.cst, r_], [r_], out=r_[:, k, 0:1], in0=self.cst[:, 1024:1025], scalar1=float(c * 128), scalar2=1.0,
                           op0=ALU.add, op1=ALU.mult)
                    self.V("dve", "tensor_scalar", [self.cst, r_], [r_], out=r_[:, k, 2:3], in0=self.cst[:, 1024:1025], scalar1=float(c * 128 + k * T), scalar2=2.0,
                           op0=ALU.add, op1=ALU.mult)
                    self.V("dve", "tensor_scalar", [r_], [r_], out=r_[:, k, 3:4], in0=r_[:, k, 2:3], scalar1=1.0, scalar2=1.0, op0=ALU.add, op1=ALU.mult)
                self.V("dve", "tensor_tensor", [q, r_], [r_], out=r_[:, 0, 1:2], in0=q[:, 130:131], in1=q[:, 129:130], op=ALU.subtract)
                self.V("dve", "tensor_copy", [q, r_], [r_], out=r_[:, 1, 1:2], in_=q[:, 129:130])
                self.V("dve", "tensor_copy", [q], [di], out=di[:, 0:1], in_=q[:, 131:132])
                self.V("dve", "tensor_copy", [q], [di], out=di[:, 1:2], in_=q[:, 132:133])
                for k in range(2):
                    S.dma_fn("pool", (lambda e, r_=r_, di=di, k=k: e.indirect_dma_start(out=self.ROWS, out_offset=bass.IndirectOffsetOnAxis(ap=di[:, k:k + 1], axis=0),
                                                                                      in_=r_[:, k, :], in_offset=None)), [r_, di], [rows_t])
            wgv = i["moe_w_gate"].rearrange("l e (p j) f -> (l e p) (j f)", j=8).rearrange("r (h x) -> (r h) x", h=2)
            wuv = i["moe_w_up"].rearrange("l e (p j) f -> (l e p) (j f)", j=8).rearrange("r (h x) -> (r h) x", h=2)
            wdv = i["moe_w_down"].rearrange("l e (p j) n -> (l e p) (j n)", j=4).rearrange("r (h x) -> (r h) x", h=2)
            wts = [(self.sb(es, "spwg%d" % k, [128, 8, 512], BF), self.sb(es, "spwu%d" % k, [128, 8, 512], BF),
                    self.sb(es, "spwd%d" % k, [128, 4, D], BF)) for k in range(2)]
            NQ = 4
            recs = [self.sb(es, "sprc%d" % k, [128, 4], F32) for k in range(NQ)]
            recu = [self.sb(es, "spru%d" % k, [128, 4], U32) for k in range(NQ)]
            hbs = [self.sb(es, "sphb%d" % k, [128, D], BF) for k in range(NQ)]
            hTs = [self.sb(es, "sphT%d" % k, [128, 8, 128], BF) for k in range(NQ)]
            sGs = [self.sb(es, "spsg%d" % k, [128, 512], F32) for k in range(2)]
            Hhs = [self.sb(es, "spHh%d" % k, [128, 512], BF) for k in range(2)]
            HhTs = [self.sb(es, "spHhT%d" % k, [128, 4, 128], BF) for k in range(2)]
            ys = [self.sb(es, "spy%d" % k, [128, D], F32) for k in range(2)]
            def gather(dst_ap, src, idx_ap, r, w, skip=False):
                if skip:
                    S.dma_fn("pool", (lambda e: e.indirect_dma_start(out=dst_ap, out_offset=None, in_=src, in_offset=bass.IndirectOffsetOnAxis(ap=idx_ap, axis=0),
                                                                     bounds_check=self._wbound_reg(e), oob_is_err=False)), r, w)
                else:
                    S.dma_fn("pool", (lambda e: e.indirect_dma_start(out=dst_ap, out_offset=None, in_=src, in_offset=bass.IndirectOffsetOnAxis(ap=idx_ap, axis=0))), r, w)

            def proA(j):
                rc, ru, hb = recs[j % NQ], recu[j % NQ], hbs[j % NQ]
                S.dma("sp", rc[:], self.ROWS[j * 128:(j + 1) * 128, :], [rows_t], [rc])
                self.V("dve", "tensor_copy", [rc], [ru], out=ru[:], in_=rc[:])
                gather(hb[:], self.H2, ru[:, 0:1], [ru, h2_t], [hb])

            def proB(j):
                hb, hT = hbs[j % NQ], hTs[j % NQ]
                pt = self.psb()
                ptv = pt[:].bitcast(BF).rearrange("p (k n) -> p k n", k=8)
                hbv = hb[:].rearrange("t (p j) -> t p j", j=8)
                for jx in range(8):
                    self.tr(ptv[:, jx, :], hbv[:, :, jx], self.identb[:], [hb, self.identb], [pt])
                self.act(hT[:], ptv, AF.Copy, [pt], [hT])

            def wload(j):
                wg, wu, wd = wts[j % 2]
                for h_ in range(2):
                    gather(wg[:, h_ * 4:(h_ + 1) * 4, :].rearrange("p j f -> p (j f)"), wgv, widx[:, h_, j:j + 1], [widx], [wg], skip=True)
                    gather(wu[:, h_ * 4:(h_ + 1) * 4, :].rearrange("p j f -> p (j f)"), wuv, widx[:, h_, j:j + 1], [widx], [wu], skip=True)
                    gather(wd[:, h_ * 2:(h_ + 1) * 2, :].rearrange("p j f -> p (j f)"), wdv, widx[:, h_, j:j + 1], [widx], [wd], skip=True)

            proA(0)
            proA(1)
            wload(0)
            proB(0)
            for j in range(NB):
                z = j % 2
                rc, ru, hT = recs[j % NQ], recu[j % NQ], hTs[j % NQ]
                sg, Hh, HhT, y = sGs[z], Hhs[z], HhTs[z], ys[z]
                wg, wu, wd = wts[z]
                if j + 2 < NB:
                    proA(j + 2)
                if j + 1 < NB:
                    wload(j + 1)
                pG, pU = self.psb(), self.psb()
                for jx in range(8):
                    self.mm(pG[:, :], hT[:, jx, :], wg[:, jx, :], jx == 0, jx == 7, [hT, wg], [pG])
                for jx in range(8):
                    self.mm(pU[:, :], hT[:, jx, :], wu[:, jx, :], jx == 0, jx == 7, [hT, wu], [pU])
                self.act(sg[:], pG[:, :], AF.Silu, [pG], [sg])
                self.V("dve", "scalar_tensor_tensor", [pU, rc, sg], [Hh], out=Hh[:], in0=pU[:, :], scalar=rc[:, 1:2], in1=sg[:], op0=ALU.mult, op1=ALU.mult)
                if j + 1 < NB:
                    proB(j + 1)
                pt2 = self.psb()
                pt2v = pt2[:].bitcast(BF)[:, 0:512].rearrange("p (k n) -> p k n", k=4)
                Hhv = Hh[:].rearrange("t (p j) -> t p j", j=4)
                for jx in range(4):
                    self.tr(pt2v[:, jx, :], Hhv[:, :, jx], self.identb[:], [Hh, self.identb], [pt2])
                self.act(HhT[:], pt2v, AF.Copy, [pt2], [HhT])
                for half in range(2):
                    pD = self.psb()
                    for jx in range(4):
                        self.mm(pD[:, :], HhT[:, jx, :], wd[:, jx, half * 512:(half + 1) * 512], jx == 0, jx == 3, [HhT, wd], [pD])
                    if half == 0:
                        self.act(y[:, 0:512], pD[:, :], AF.Copy, [pD], [y])
                    else:
                        self.V("dve", "tensor_copy", [pD], [y], out=y[:, 512:1024], in_=pD[:, :])
                for h_ in range(2):
                    S.dma_fn("pool", (lambda e, y=y, ru=ru, h_=h_: e.indirect_dma_start(out=self.ACC2.rearrange("r (h x) -> (r h) x", h=2),
                                                                                        out_offset=bass.IndirectOffsetOnAxis(ap=ru[:, 2 + h_:3 + h_], axis=0),
                                                                                        in_=y[:, h_ * 512:(h_ + 1) * 512], in_offset=None,
                                                                                        bounds_check=self._bound_reg(e), oob_is_err=False)), [y, ru], [acc_t])
            self.residual_setup(es, 5)
            a0 = [self.sb(es, "spa0%d" % k, [128, D], F32) for k in range(3)]
            a1 = [self.sb(es, "spa1%d" % k, [128, D], F32) for k in range(3)]
            for c in range(NCH):
                u0, u1 = a0[c % 3], a1[c % 3]
                S.dma("sp", u0[:], self.ACC2[c * 128:(c + 1) * 128, :], [acc_t], [u0])
                S.dma("sp", u1[:], self.ACC2[T + c * 128:T + (c + 1) * 128, :], [acc_t], [u1])
                self.V("dve", "tensor_tensor", [u0, u1], [u0], out=u0[:], in0=u0[:], in1=u1[:], op=ALU.add)
                self.residual(c, lambda half, u0=u0: u0[:, half * 512:(half + 1) * 512], [u0, u0])
            S.barrier()

    def _wbound_reg(self, e):
        if getattr(self, "_wbreg", None) is None:
            self._wbreg = e.to_reg(self.n_layers * 32 * 128 * 2 - 1)
        return self._wbreg

    def _bound_reg(self, e):
        if getattr(self, "_breg", None) is None:
            self._breg = e.to_reg(4 * T - 1)
        return self._breg

    def final_norm(self):
        S = self.S
        with contextlib.ExitStack() as es:
            g = self.sb(es, "fng", [128, D], F32)
            S.dma("sp", g[:], self.i["final_g"].partition_broadcast(128), [], [g])
            xs = [self.sb(es, "fnx%d" % k, [128, D], F32) for k in range(3)]
            sq = self.sb(es, "fnsq", [128, D], F32)
            st = [self.sb(es, "fnst%d" % k, [128, 4], F32) for k in range(2)]
            ot = [self.sb(es, "fno%d" % k, [128, D], F32) for k in range(2)]
            for c in range(2, NCH):
                x = xs[c % 3]
                S.dma("sp", x[:], self.X[c * 128:(c + 1) * 128, :], [Tl(None, self.Xt[c])], [x])
                s_ = st[c % 2]
                self.act(sq[:], x[:], AF.Square, [x], [sq, s_], accum_out=s_[:, 0:1])
                self.V("dve", "tensor_scalar", [s_], [s_], out=s_[:, 1:2], in0=s_[:, 0:1], scalar1=1.0 / D, scalar2=EPS, op0=ALU.mult, op1=ALU.add)
                self.V("dve", "reciprocal", [s_], [s_], out=s_[:, 2:3], in_=s_[:, 1:2])
                self.act(s_[:, 3:4], s_[:, 2:3], AF.Sqrt, [s_], [s_])
                o = ot[c % 2]
                self.V("dve", "scalar_tensor_tensor", [x, s_, g], [o], out=o[:], in0=x[:], scalar=s_[:, 3:4], in1=g[:], op0=ALU.mult, op1=ALU.mult)
                S.dma("sp", self.out[(c - 2) * 128:(c - 1) * 128, :], o[:], [o], [Tl(None, self.outt)])


def make_consts():
    cst = np.zeros((128, 1056), np.float32)
    cst[:, 0:128] = np.eye(128, dtype=np.float32)
    cst[:, 128:256] = 1.0
    R = np.zeros((128, 128), np.float32)
    for blk in range(2):
        o = blk * 64
        for q in range(16):
            R[o + 16 + q, o + q] = -1.0
            R[o + q, o + 16 + q] = 1.0
            R[o + 48 + q, o + 32 + q] = -1.0
            R[o + 32 + q, o + 48 + q] = 1.0
    cst[:, 256:384] = R
    s = np.arange(128)[:, None]
    t = np.arange(128)[None, :]
    same = (s // 64) == (t // 64)
    same32 = (s // 32) == (t // 32)
    cst[:, 384:512] = (same32 & (t >= s)).astype(np.float32)
    cst[:, 512:640] = (same32 & (t <= s)).astype(np.float32)
    cst[:, 640:768] = (same & (s % 64 < 32) & (t % 64 >= 32)).astype(np.float32)
    cst[:, 768:896] = (same & (s % 64 >= 32) & (t % 64 < 32)).astype(np.float32)
    cst[:, 896:1024] = 128.0 * np.arange(128, dtype=np.float32)[None, :]
    cst[:, 1024] = np.arange(128, dtype=np.float32)
    cst[:, 1025:1057 - 1 + 0] = 0.0
    inv_freq = (10000.0 ** (-np.arange(0, 32, 2, dtype=np.float32) / 32)).astype(np.float32)
    pos = np.arange(NLAT)
    row = (pos // 64).astype(np.float32)
    col = (pos % 64).astype(np.float32)
    ang_r = row[:, None] * inv_freq
    ang_c = col[:, None] * inv_freq
    ang = np.concatenate([ang_r, ang_r, ang_c, ang_c], axis=-1).astype(np.float32)
    rope = np.zeros((2, 128, T), np.float32)
    rope[0, :, :NCTX] = 1.0
    rope[0, 0:64, NCTX:] = np.cos(ang).T
    rope[0, 64:128, NCTX:] = np.cos(ang).T
    rope[1, 0:64, NCTX:] = np.sin(ang).T
    rope[1, 64:128, NCTX:] = np.sin(ang).T
    return cst, rope


def make_in_maps(inputs, cores, L=DEPTH, nexp=32):
    f = lambda a: np.ascontiguousarray(np.asarray(a, dtype=np.float32))
    cst, rope = make_consts()
    shared = {
        "c_ctx": f(inputs["c_ctx"]).reshape(1, D),
        "ada_w": f(inputs["ada_w"][:L]), "ada_b": f(inputs["ada_b"]),
        "norm1_g": f(inputs["norm1_g"]), "norm2_g": f(inputs["norm2_g"]),
        "w_in": f(inputs["w_in"][:L]), "w_branch": f(inputs["w_branch"][:L]), "w_out": f(inputs["w_out"][:L]),
        "hg_lb": f(inputs["hg_lb_logits"]), "hg_norm_g": f(inputs["hg_norm_g"]),
        "da_lambda": f(inputs["da_lambda"]).reshape(DEPTH, 256), "da_norm_g": f(inputs["da_norm_g"]),
        "cv_dw_w": f(inputs["cv_dw_w"]), "cv_dw_b": f(inputs["cv_dw_b"]),
        "cv_ln_g": f(inputs["cv_ln_g"]), "cv_ln_b": f(inputs["cv_ln_b"]),
        "sc_w": f(inputs["sc_w"]),
        "moe_w_r": np.ascontiguousarray(np.concatenate([f(inputs["moe_w_grp"]), f(inputs["moe_w_exp"])], axis=-1)),
        "moe_b_r": np.ascontiguousarray(np.concatenate([f(inputs["moe_b_grp"]), f(inputs["moe_b_exp"])], axis=-1)),
        "moe_w_gate": f(inputs["moe_w_gate"][:L, :nexp]), "moe_w_up": f(inputs["moe_w_up"][:L, :nexp]), "moe_w_down": f(inputs["moe_w_down"][:L, :nexp]),
        "final_g": f(inputs["final_g"]).reshape(1, D),
        "cst": cst, "rope": rope, "ltri": np.triu(np.ones((128, 128), np.float32), 1),
    }
    maps = []
    for cid in cores:
        b = cid % 4
        m = dict(shared)
        m["x"] = f(inputs["x"][b])
        m["c"] = f(inputs["c"][b]).reshape(1, D)
        m["ctx"] = f(inputs["ctx"][b])
        maps.append(m)
    return maps


def kernel(**inputs):
    nc = bass.Bass("TRN2", target_bir_lowering=False)
    Prog(nc).build()
    maps = make_in_maps(inputs, list(range(4)))
    res = run_bass_kernel_spmd(nc, maps, core_ids=list(range(4)))
    return np.stack([np.asarray(res.results[b]["out"], dtype=np.float32) for b in range(4)], axis=0)
```

```python
import contextlib
import math
import numpy as np
import concourse.bass as bass
import concourse.mybir as mybir
from concourse.bass_utils import run_bass_kernel_spmd

F32 = mybir.dt.float32
BF = mybir.dt.bfloat16
U32 = mybir.dt.uint32
AF = mybir.ActivationFunctionType
ALU = mybir.AluOpType
AX = mybir.AxisListType

D = 1024
NCTX = 256
NLAT = 4096
T = NCTX + NLAT
NCH = T // 128
NB = 2 * T // 128 + 32
SPARSE = True
DEPTH = 4
W = 512
INW = 10752
EPS = 1e-6
TILES = [(0, 256)] + [(256 + 512 * i, 512) for i in range(8)]
C_HQ, C_HI, C_HFF, C_HFB, C_HG = 0, 512, 1024, 1536, 2048
C_DQ, C_DK, C_DV = 2560, 3072, 3584
C_CVA, C_CVG = 4096, 4608
C_SB, C_SC, C_SX = 5120, 5632, 6144
C_GATE = 6656


class Trk:
    __slots__ = ("w", "r")

    def __init__(self):
        self.w = None
        self.r = {}


class Tl:
    def __init__(self, h, trk=None):
        self.h = h
        self.t = trk or Trk()

    def __getitem__(self, k):
        return self.h[k]


class Stream:
    def __init__(self, name):
        self.name = name
        self.ops = []
        self.seen = {}
        self.sem = None
        self.cnt = 0
        self.dslots = []
        self.dnext = 0


class Sch:
    SEM_MAX = 30000

    def __init__(self, nc, es):
        self.nc = nc
        self.es = es
        self.st = {k: Stream(k) for k in ("pe", "act", "dve", "pool", "sp")}
        self.nsem = 0
        for k, s in self.st.items():
            self._newsem(s)
        for k, n in (("sp", 24), ("pool", 12), ("act", 6)):
            s = self.st[k]
            for i in range(n):
                s.dslots.append([self._sem(), 0])

    def _sem(self):
        self.nsem += 1
        return self.es.enter_context(self.nc.semaphore("s%d" % self.nsem))

    def _newsem(self, s):
        s.sem = self._sem()
        s.cnt = 0

    def _need(self, s, tok, waits):
        if tok is None:
            return
        sem, val, src = tok
        if src == "pe" and s.name == "pe":
            return
        if s.seen.get(id(sem), 0) >= val:
            return
        k = id(sem)
        if k not in waits or waits[k][1] < val:
            waits[k] = (sem, val)

    def _deps(self, s, reads, writes):
        waits = {}
        for b in reads:
            self._need(s, b.t.w, waits)
        for b in writes:
            self._need(s, b.t.w, waits)
            for tok in b.t.r.values():
                self._need(s, tok, waits)
        for k, (sem, val) in waits.items():
            s.seen[k] = val
        return list(waits.values())

    def _mark(self, tok, reads, writes):
        for b in reads:
            b.t.r[id(tok[0])] = tok
        for b in writes:
            b.t.w = tok
            b.t.r = {}

    def op(self, eng, fn, reads=(), writes=()):
        s = self.st[eng]
        if s.cnt >= self.SEM_MAX:
            self._newsem(s)
        waits = self._deps(s, reads, writes)
        s.cnt += 1
        tok = (s.sem, s.cnt, eng)
        s.ops.append((waits, fn, (s.sem, 1)))
        self._mark(tok, reads, writes)
        return tok

    def dma(self, q, out, in_, reads=(), writes=(), **kw):
        return self.dma_fn(q, (lambda e: e.dma_start(out=out, in_=in_, **kw)), reads, writes)

    def dma_fn(self, q, fn, reads=(), writes=()):
        s = self.st[q]
        slot = s.dslots[s.dnext % len(s.dslots)]
        s.dnext += 1
        waits = self._deps(s, reads, writes)
        if slot[1] > 0 and s.seen.get(id(slot[0]), 0) < slot[1]:
            waits.append((slot[0], slot[1]))
            s.seen[id(slot[0])] = slot[1]
        slot[1] += 16
        tok = (slot[0], slot[1], "dma")
        s.ops.append((waits, fn, (slot[0], 16)))
        self._mark(tok, reads, writes)
        return tok

    def barrier(self):
        toks = []
        for s in self.st.values():
            if s.cnt > 0:
                toks.append((s.sem, s.cnt))
            for sl in s.dslots:
                if sl[1] > 0:
                    toks.append((sl[0], sl[1]))
        for s in self.st.values():
            waits = []
            for sem, val in toks:
                if sem is s.sem:
                    continue
                if s.seen.get(id(sem), 0) < val:
                    waits.append((sem, val))
                    s.seen[id(sem)] = val
            if waits:
                s.ops.append((waits, None, None))

    def emit(self):
        nc = self.nc
        self.barrier()
        with nc.Block() as block:
            def run(s):
                def f(e):
                    for waits, fn, inc in s.ops:
                        for sem, val in waits:
                            e.wait_ge(sem, val)
                        if fn is not None:
                            fn(e).then_inc(inc[0], inc[1])
                return f
            block.tensor(run(self.st["pe"]))
            block.scalar(run(self.st["act"]))
            block.vector(run(self.st["dve"]))
            block.gpsimd(run(self.st["pool"]))
            block.sync(run(self.st["sp"]))


class Prog:
    def __init__(self, nc, n_layers=DEPTH, dbg=None, nexp=32):
        self.nc = nc
        self.nexp = nexp
        self.n_layers = n_layers
        self.dbg = dbg or {}

    def sb(self, es, name, shape, dt):
        self.uid = getattr(self, "uid", 0) + 1
        return Tl(es.enter_context(self.nc.sbuf_tensor("%s_u%d" % (name, self.uid), list(shape), dt)))

    def dram(self, name, shape, dt, kind="Internal"):
        return self.nc.dram_tensor(name, list(shape), dt, kind=kind).ap()

    def mm(self, out, lhsT, rhs, start, stop, r, w):
        self.S.op("pe", lambda e: e.matmul(out, lhsT=lhsT, rhs=rhs, start=start, stop=stop), r, w)

    def tr(self, out, in_, ident, r, w):
        self.S.op("pe", lambda e: e.transpose(out=out, in_=in_, identity=ident), r, w)

    def act(self, out, in_, func, r, w, **kw):
        self.S.op("act", lambda e: e.activation(out=out, in_=in_, func=func, **kw), r, w)

    def V(self, eng, name, r, w, *a, **kw):
        self.S.op(eng, lambda e: getattr(e, name)(*a, **kw), r, w)

    def psb(self):
        b = self.banks[self.bi % 8]
        self.bi += 1
        return b

    def build(self):
        nc = self.nc
        L = self.n_layers
        i = {}
        def inp(name, shape, dt=F32):
            i[name] = self.dram(name, shape, dt, kind="ExternalInput")
        inp("x", [NLAT, D]); inp("c", [1, D]); inp("ctx", [NCTX, D]); inp("c_ctx", [1, D])
        inp("ada_w", [L, D, 6 * D]); inp("ada_b", [DEPTH, 6 * D])
        inp("norm1_g", [DEPTH, D]); inp("norm2_g", [DEPTH, D])
        inp("w_in", [L, D, INW]); inp("w_branch", [L, 4, W, D]); inp("w_out", [L, D, D])
        inp("hg_lb", [DEPTH, 2, W]); inp("hg_norm_g", [DEPTH, W])
        inp("da_lambda", [DEPTH, 256]); inp("da_norm_g", [DEPTH, 128])
        inp("cv_dw_w", [DEPTH, 31, W]); inp("cv_dw_b", [DEPTH, W]); inp("cv_ln_g", [DEPTH, W]); inp("cv_ln_b", [DEPTH, W])
        inp("sc_w", [DEPTH, 3, W])
        inp("moe_w_r", [DEPTH, D, 36]); inp("moe_b_r", [DEPTH, 36])
        inp("moe_w_gate", [L, self.nexp, D, W]); inp("moe_w_up", [L, self.nexp, D, W]); inp("moe_w_down", [L, self.nexp, W, D])
        inp("final_g", [1, D]); inp("ltri", [128, 128])
        inp("cst", [128, 1056]); inp("rope", [2, 128, T])
        self.i = i
        self.out = self.dram("out", [NLAT, D], F32, kind="ExternalOutput")
        self.X = self.dram("Xs", [T, D], F32)
        self.HT = self.dram("HTs", [8, 128, T], BF)
        self.Y = self.dram("Ys", [4, 4, 128, T], BF)
        self.MT = self.dram("MTs", [8, 128, T], BF)
        self.MODR = self.dram("MODs", [2, 128, 6 * D], F32)
        self.H2 = self.dram("H2s", [T + 1, D], BF)
        self.ROWS = self.dram("ROWSs", [NB * 128, 4], F32)
        self.ACC2 = self.dram("ACC2s", [2 * T, D], F32)
        self.H2t = Trk(); self.ROWSt = Trk(); self.ACC2t = Trk()
        self.Xt = [Trk() for _ in range(NCH)]
        self.HTt = [Trk() for _ in range(9)]
        self.Yt = [[Trk() for _ in range(9)] for _ in range(4)]
        self.MTt = [Trk() for _ in range(9)]
        self.MODt = Trk()
        self.outt = Trk()
        dbg_out = {}
        for k, shp in self.dbg.items():
            if not isinstance(shp, tuple):
                continue
            dbg_out[k] = self.dram("dbg_" + k, shp[0], shp[1], kind="ExternalOutput")
        self.dbg_out = dbg_out

        with contextlib.ExitStack() as es:
            self.S = S = Sch(nc, es)
            self.banks = [Tl(es.enter_context(nc.psum_tensor("pb%d" % k, [128, 512], F32))) for k in range(8)]
            self.bi = 0
            self.cst = self.sb(es, "cst_sb", [128, 1056], F32)
            S.dma("sp", self.cst[:], i["cst"], [], [self.cst])
            self.identb = self.sb(es, "identb", [128, 128], BF)
            self.onesb = self.sb(es, "onesb", [128, 128], BF)
            self.Rb = self.sb(es, "Rb", [128, 128], BF)
            self.identf = Tl(self.cst.h, self.cst.t)
            self.V("dve", "tensor_copy", [self.cst], [self.identb], out=self.identb[:], in_=self.cst[:, 0:128])
            self.V("dve", "tensor_copy", [self.cst], [self.onesb], out=self.onesb[:], in_=self.cst[:, 128:256])
            self.V("dve", "tensor_copy", [self.cst], [self.Rb], out=self.Rb[:], in_=self.cst[:, 256:384])
            self.sT = []
            for which, src in enumerate((i["c"], i["c_ctx"])):
                cT = self.sb(es, "cT%d" % which, [128, 8], F32)
                S.dma("sp", cT[:], src.rearrange("o (kc p) -> p (o kc)", p=128), [], [cT], allow_slow_non_contiguous=True)
                sg = self.sb(es, "cS%d" % which, [128, 8], F32)
                self.act(sg[:], cT[:], AF.Silu, [cT], [sg])
                rep = self.sb(es, "sT%d" % which, [128, 8, 128], F32)
                self.V("dve", "tensor_copy", [sg], [rep], out=rep[:], in_=sg[:].unsqueeze(2).to_broadcast([128, 8, 128]))
                self.sT.append(rep)
            self.P = self.sb(es, "Pparams", [128, 600], F32)
            self.RW = self.sb(es, "RW", [128, NCH, 32], F32)
            self.RK = self.sb(es, "RK", [128, NCH, 32], F32)
            self.Asum = self.sb(es, "Asum", [128, 32], F32)
            self.ltri = self.sb(es, "ltri_sb", [128, 128], F32)
            S.dma("sp", self.ltri[:], i["ltri"], [], [self.ltri])
            zr = self.sb(es, "zrow", [1, D], BF)
            self.V("dve", "memset", [], [zr], zr[:], 0.0)
            S.dma("sp", self.H2[T:T + 1, :], zr[:], [zr], [Tl(None, self.H2t)])
            self.LB = self.sb(es, "LB", [128, DEPTH, 8], F32)
            self.OML = self.sb(es, "OML", [128, DEPTH, 8], F32)
            lbe = self.sb(es, "lbe", [128, DEPTH, 8], F32)
            lbt = self.sb(es, "lbt", [128, 16], F32)
            for l_ in range(DEPTH):
                for d_ in range(2):
                    for j_ in range(4):
                        S.dma("sp", lbe[:, l_, d_ * 4 + j_:d_ * 4 + j_ + 1], i["hg_lb"][l_, d_:d_ + 1, j_ * 128:(j_ + 1) * 128].rearrange("o p -> p o"),
                              [], [lbe], allow_slow_non_contiguous=True)
            self.act(lbe[:], lbe[:], AF.Exp, [lbe], [lbe])
            self.V("dve", "tensor_tensor", [lbe], [lbt], out=lbt[:, 0:8], in0=lbe[:, 0, :], in1=lbe[:, 1, :], op=ALU.add)
            self.V("dve", "tensor_tensor", [lbe, lbt], [lbt], out=lbt[:, 0:8], in0=lbt[:, 0:8], in1=lbe[:, 2, :], op=ALU.add)
            self.V("dve", "tensor_tensor", [lbe, lbt], [lbt], out=lbt[:, 0:8], in0=lbt[:, 0:8], in1=lbe[:, 3, :], op=ALU.add)
            self.V("dve", "reciprocal", [lbt], [lbt], out=lbt[:, 8:16], in_=lbt[:, 0:8])
            self.V("dve", "memset", [], [self.LB], self.LB[:], 0.0)
            for l_ in range(1, DEPTH):
                self.V("dve", "tensor_tensor", [lbe, lbt], [lbe], out=lbe[:, l_, :], in0=lbe[:, l_, :], in1=lbt[:, 8:16], op=ALU.mult)
                self.V("dve", "tensor_tensor", [lbe, self.LB], [self.LB], out=self.LB[:, l_, :], in0=self.LB[:, l_ - 1, :], in1=lbe[:, l_, :], op=ALU.add)
            self.V("dve", "tensor_scalar", [self.LB], [self.OML], out=self.OML[:], in0=self.LB[:], scalar1=-1.0, scalar2=1.0, op0=ALU.mult, op1=ALU.add)
            self.seg = self.sb(es, "seg", [128, 512], F32)
            self.V("dve", "memset", [], [self.seg], self.seg[:], 1.0)
            self.V("dve", "memset", [self.seg], [self.seg], self.seg[:].rearrange("p (c s) -> p c s", s=64)[:, :, 0:1], 0.0)
            S.barrier()
            S.dma("sp", self.X[0:NCTX, :], i["ctx"], [], self._xt(0, 2))
            for q in range(4):
                S.dma("sp", self.X[NCTX + q * 1024: NCTX + (q + 1) * 1024, :], i["x"][q * 1024:(q + 1) * 1024, :], [],
                      self._xt(2 + q * 8, 8))
            for l in range(L):
                self.layer(l)
            self.final_norm()
            S.emit()

    def _xt(self, c0, n):
        return [Tl(None, t) for t in self.Xt[c0:c0 + n]]

    def layer(self, l):
        stop = self.dbg.get("stop")
        self.phase_mod(l)
        self.load_params(l)
        self.phase_norm(l, 1)
        if stop == "norm1":
            return
        self.phase_sconv(l)
        self.phase_conv(l)
        if stop == "convs":
            return self.dump_dbg()
        self.phase_attn(l)
        if stop == "attn":
            return self.dump_dbg()
        self.phase_hgrn(l)
        if stop == "hgrn":
            return self.dump_dbg()
        self.phase_merge(l)
        if stop == "merge":
            return self.dump_dbg()
        self.phase_norm(l, 2)
        if SPARSE:
            self.phase_moe_sparse(l)
        else:
            self.phase_moe(l)
        if stop == "moe":
            return self.dump_dbg()

    def dump_dbg(self):
        S = self.S
        if "y" in self.dbg_out:
            S.dma("sp", self.dbg_out["y"], self.Y, [Tl(None, t) for k in range(4) for t in self.Yt[k]], [Tl(None, Trk())])
        if "x" in self.dbg_out:
            S.dma("sp", self.dbg_out["x"], self.X, [Tl(None, t) for t in self.Xt], [Tl(None, Trk())])

    def load_w(self, es, name, l, c0, ncols):
        w = self.sb(es, name, [128, 8, ncols], BF)
        self.S.dma("pool", w[:], self.i["w_in"][l, :, c0:c0 + ncols].rearrange("(kc p) n -> p kc n", p=128), [], [w])
        return w

    def load_ht(self, buf, ti):
        t0, N = TILES[ti]
        self.S.dma("sp", buf[:, :, 0:N], self.HT[:, :, t0:t0 + N].rearrange("k p n -> p k n"), [Tl(None, self.HTt[ti])], [buf])

    def proj(self, ps, w, j0, ht, N, width=128):
        for kc in range(8):
            self.mm(ps[0:width, 0:N], w[:, kc, j0:j0 + width], ht[:, kc, 0:N], kc == 0, kc == 7, [w, ht], [ps])

    def rstd_from(self, src_ap, scale, out_t, tmp_t, N, r):
        self.V("dve", "tensor_scalar", r, [tmp_t], out=tmp_t[:, 0:N], in0=src_ap, scalar1=scale, scalar2=EPS, op0=ALU.mult, op1=ALU.add)
        self.V("dve", "reciprocal", [tmp_t], [tmp_t], out=tmp_t[:, 0:N], in_=tmp_t[:, 0:N])
        self.act(out_t[:, 0:N], tmp_t[:, 0:N], AF.Sqrt, [tmp_t], [out_t])

    def load_params(self, l):
        S = self.S
        i = self.i
        P = self.P
        nc = True
        def ld(dst, src):
            S.dma("sp", dst, src, [], [P], allow_slow_non_contiguous=True)
        for j in range(4):
            sl = slice(j * 128, (j + 1) * 128)
            ld(P[:, j * 3:j * 3 + 3], i["sc_w"][l][:, sl].rearrange("k p -> p k"))
            ld(P[:, 12 + j * 31:12 + (j + 1) * 31], i["cv_dw_w"][l][:, sl].rearrange("k p -> p k"))
            ld(P[:, 136 + j:137 + j], i["cv_dw_b"][l:l + 1, sl].rearrange("o p -> p o"))
            ld(P[:, 140 + j:141 + j], i["cv_ln_g"][l:l + 1, sl].rearrange("o p -> p o"))
            ld(P[:, 144 + j:145 + j], i["cv_ln_b"][l:l + 1, sl].rearrange("o p -> p o"))
            ld(P[:, 148 + j:149 + j], i["hg_norm_g"][l:l + 1, sl].rearrange("o p -> p o"))
        ld(P[:, 152:153], i["da_norm_g"][l:l + 1, :].rearrange("o p -> p o"))
        S.dma("sp", P[:, 160:416], i["da_lambda"][l:l + 1, :].partition_broadcast(128), [], [P])
        S.dma("sp", P[:, 416:452], i["moe_b_r"][l:l + 1, :].partition_broadcast(128), [], [P])
        lam_init = 0.8 - 0.6 * math.exp(-0.3 * l)
        self.V("dve", "tensor_tensor", [P], [P], out=P[:, 460:524], in0=P[:, 160:224], in1=P[:, 224:288], op=ALU.mult)
        self.V("dve", "tensor_tensor", [P], [P], out=P[:, 524:588], in0=P[:, 288:352], in1=P[:, 352:416], op=ALU.mult)
        self.V("dve", "tensor_reduce", [P], [P], out=P[:, 155:157], in_=P[:, 460:588].rearrange("p (a b) -> p a b", a=2), axis=AX.X, op=ALU.add)
        self.act(P[:, 157:159], P[:, 155:157], AF.Exp, [P], [P])
        self.V("dve", "tensor_tensor", [P], [P], out=P[:, 159:160], in0=P[:, 158:159], in1=P[:, 157:158], op=ALU.subtract)
        self.V("dve", "tensor_scalar", [P], [P], out=P[:, 153:154], in0=P[:, 159:160], scalar1=-lam_init, scalar2=1.0, op0=ALU.add, op1=ALU.mult)
        self.V("dve", "tensor_scalar", [P], [P], out=P[:, 154:155], in0=P[:, 152:153], scalar1=1.0 - lam_init, scalar2=0.0, op0=ALU.mult, op1=ALU.add)

    def phase_mod(self, l):
        S = self.S
        i = self.i
        modt = Tl(None, self.MODt)
        with contextlib.ExitStack() as es:
            wt = [self.sb(es, "adaw%d" % k, [128, 8, 512], F32) for k in range(2)]
            bt = [self.sb(es, "adab%d" % k, [1, 512], F32) for k in range(2)]
            ot = [self.sb(es, "adao%d" % k, [128, 512], F32) for k in range(2)]
            n = 0
            for blk in range(12):
                w = wt[blk % 2]
                b = bt[blk % 2]
                S.dma("sp", w[:], i["ada_w"][l, :, blk * 512:(blk + 1) * 512].rearrange("(kc p) n -> p kc n", p=128), [], [w])
                S.dma("sp", b[:], i["ada_b"][l:l + 1, blk * 512:(blk + 1) * 512], [], [b])
                for which in range(2):
                    ps = self.psb()
                    for kc in range(8):
                        self.mm(ps[:], self.sT[which][:, kc, :], w[:, kc, :], kc == 0, False, [self.sT[which], w], [ps])
                    self.mm(ps[:], self.cst[0:1, 128:256], b[0:1, :], False, True, [self.cst, b], [ps])
                    o = ot[n % 2]
                    n += 1
                    self.V("dve", "tensor_copy", [ps], [o], out=o[:], in_=ps[:])
                    S.dma("sp", self.MODR[which, :, blk * 512:(blk + 1) * 512], o[:], [o], [modt])
            S.barrier()

    def phase_norm(self, l, which):
        S = self.S
        i = self.i
        gsrc = i["norm1_g"] if which == 1 else i["norm2_g"]
        sh, sc = (0, 1) if which == 1 else (3, 4)
        modt = Tl(None, self.MODt)
        with contextlib.ExitStack() as es:
            A = [self.sb(es, "nA%d" % k, [128, D], F32) for k in range(2)]
            B = [self.sb(es, "nB%d" % k, [128, D], F32) for k in range(2)]
            g = self.sb(es, "ng", [128, D], F32)
            S.dma("sp", g[:], gsrc[l:l + 1, :].partition_broadcast(128), [], [g])
            for k in range(2):
                S.dma("sp", A[k][:], self.MODR[k, :, sc * D:(sc + 1) * D], [modt], [A[k]])
                S.dma("sp", B[k][:], self.MODR[k, :, sh * D:(sh + 1) * D], [modt], [B[k]])
                self.V("dve", "scalar_tensor_tensor", [A[k], g], [A[k]], out=A[k][:], in0=A[k][:], scalar=1.0, in1=g[:],
                       op0=ALU.add, op1=ALU.mult)
            xs = [self.sb(es, "nx%d" % k, [128, D], F32) for k in range(3)]
            sq = self.sb(es, "nsq", [128, D], F32)
            t1 = [self.sb(es, "nt%d" % k, [128, D], F32) for k in range(2)]
            hb = [self.sb(es, "nhb%d" % k, [128, D], BF) for k in range(2)]
            st = [self.sb(es, "nst%d" % k, [128, 4], F32) for k in range(2)]
            hT = [self.sb(es, "nhT%d" % k, [128, 8, 512], BF) for k in range(2)]
            if which == 2:
                wr = self.sb(es, "nwr", [128, 8, 36], F32)
                S.dma("sp", wr[:], i["moe_w_r"][l].rearrange("(kc p) n -> p kc n", p=128), [], [wr])
                h32 = [self.sb(es, "nh32%d" % k, [128, D], F32) for k in range(2)]
                h32T = [self.sb(es, "nh32T%d" % k, [128, 8, 128], F32) for k in range(2)]
                Rt = [self.sb(es, "nR%d" % k, [128, 160], F32) for k in range(2)]
            st3 = st + [self.sb(es, "nst2", [128, 4], F32)]
            sq2 = [sq, self.sb(es, "nsq2", [128, D], F32)]
            chunks = [(ti, t0, N, cc) for ti, (t0, N) in enumerate(TILES) for cc in range(N // 128)]

            def stageA(c):
                x = xs[c % 3]
                s_ = st3[c % 3]
                S.dma("sp", x[:], self.X[c * 128:(c + 1) * 128, :], [Tl(None, self.Xt[c])], [x])
                self.act(sq2[c % 2][:], x[:], AF.Square, [x], [sq2[c % 2], s_], accum_out=s_[:, 0:1])
                self.V("dve", "tensor_scalar", [s_], [s_], out=s_[:, 1:2], in0=s_[:, 0:1], scalar1=1.0 / D, scalar2=EPS,
                       op0=ALU.mult, op1=ALU.add)
                self.V("dve", "reciprocal", [s_], [s_], out=s_[:, 2:3], in_=s_[:, 1:2])
                self.act(s_[:, 3:4], s_[:, 2:3], AF.Sqrt, [s_], [s_])

            def stageB(ti, t0, N, cc):
                c = t0 // 128 + cc
                lat = 0 if c >= 2 else 1
                ht = hT[ti % 2]
                x = xs[c % 3]
                s_ = st3[c % 3]
                t = t1[c % 2]
                self.V("dve", "scalar_tensor_tensor", [x, s_, A[lat]], [t], out=t[:], in0=x[:], scalar=s_[:, 3:4],
                       in1=A[lat][:], op0=ALU.mult, op1=ALU.mult)
                h = hb[c % 2]
                self.V("pool", "tensor_tensor", [t, B[lat]], [h], out=h[:], in0=t[:], in1=B[lat][:], op=ALU.add)
                ps = self.psb()
                pv = ps[:].bitcast(BF).rearrange("p (k n) -> p k n", k=8)
                for k in range(8):
                    self.tr(pv[:, k, :], h[:, k * 128:(k + 1) * 128], self.identb[:], [h, self.identb], [ps])
                self.V("dve", "tensor_copy", [ps], [ht], out=ht[:, :, cc * 128:(cc + 1) * 128], in_=pv)
                if which == 2:
                    self.route(c, t, B[lat], h32[c % 2], h32T[c % 2], wr, Rt[c % 2])
                    if SPARSE:
                        S.dma("sp", self.H2[c * 128:(c + 1) * 128, :], h[:], [h], [Tl(None, self.H2t)])
                        self.rank(c, Rt[c % 2])
                if cc == N // 128 - 1:
                    S.dma("sp", self.HT[:, :, t0:t0 + N].rearrange("k p n -> p k n"), ht[:, :, 0:N], [ht], [Tl(None, self.HTt[ti])])

            stageA(0)
            for idx, (ti, t0, N, cc) in enumerate(chunks):
                if idx + 1 < len(chunks):
                    stageA(idx + 1)
                stageB(ti, t0, N, cc)
            S.barrier()
        if "h1" in self.dbg_out and l == self.dbg.get("layer", 0) and which == 1:
            S.dma("sp", self.dbg_out["h1"], self.HT, [Tl(None, t) for t in self.HTt], [Tl(None, Trk())])


    def phase_sconv(self, l):
        S = self.S
        P = self.P
        with contextlib.ExitStack() as es:
            wb = self.load_w(es, "scwb", l, C_SB, 512)
            wc = self.load_w(es, "scwc", l, C_SC, 512)
            wx = self.load_w(es, "scwx", l, C_SX, 512)
            ub = self.sb(es, "scu", [128, T + 3], F32)
            bb = self.sb(es, "scb", [128, T], F32)
            hts = [self.sb(es, "scht%d" % k, [128, 8, 512], BF) for k in range(2)]
            cs = [self.sb(es, "sccs%d" % k, [128, 512], F32) for k in range(2)]
            acc = [self.sb(es, "scacc%d" % k, [128, 512], F32) for k in range(2)]
            ys = [self.sb(es, "scy%d" % k, [128, 512], BF) for k in range(2)]
            self.V("pool", "memset", [], [ub], ub[:], 0.0)
            n = 0
            for j in range(4):
                for ti, (t0, N) in enumerate(TILES):
                    ht = hts[n % 2]
                    c_ = cs[n % 2]
                    n += 1
                    self.load_ht(ht, ti)
                    base = t0 + 1 if ti == 0 else t0 + 2
                    pb, pc, px = self.psb(), self.psb(), self.psb()
                    self.proj(pb, wb, j * 128, ht, N)
                    self.proj(pc, wc, j * 128, ht, N)
                    self.proj(px, wx, j * 128, ht, N)
                    self.act(c_[:, 0:N], pc[:, 0:N], AF.Copy, [pc], [c_])
                    self.V("dve", "tensor_tensor", [c_, px], [ub], out=ub[:, base:base + N], in0=c_[:, 0:N], in1=px[:, 0:N], op=ALU.mult)
                    self.act(bb[:, t0:t0 + N], pb[:, 0:N], AF.Copy, [pb], [bb])
                for ti, (t0, N) in enumerate(TILES):
                    base = t0 + 1 if ti == 0 else t0 + 2
                    a = acc[ti % 2]
                    y = ys[ti % 2]
                    self.V("dve", "tensor_scalar", [ub, P], [a], out=a[:, 0:N], in0=ub[:, base - 1:base - 1 + N], scalar1=P[:, j * 3:j * 3 + 1],
                           scalar2=0.0, op0=ALU.mult, op1=ALU.add)
                    for k in (1, 2):
                        self.V("dve", "scalar_tensor_tensor", [ub, P, a], [a], out=a[:, 0:N], in0=ub[:, base - 1 + k:base - 1 + k + N],
                               scalar=P[:, j * 3 + k:j * 3 + k + 1], in1=a[:, 0:N], op0=ALU.mult, op1=ALU.add)
                    self.V("pool", "tensor_tensor", [a, bb], [y], out=y[:, 0:N], in0=a[:, 0:N], in1=bb[:, t0:t0 + N], op=ALU.mult)
                    S.dma("sp", self.Y[3, j, :, t0:t0 + N], y[:, 0:N], [y], [Tl(None, self.Yt[3][ti])])
            S.barrier()

    def phase_conv(self, l):
        S = self.S
        P = self.P
        onesf = self.cst[:, 128:256]
        with contextlib.ExitStack() as es:
            wa = self.load_w(es, "cvwa", l, C_CVA, 512)
            wg = self.load_w(es, "cvwg", l, C_CVG, 512)
            vb = self.sb(es, "cvv", [128, 4, T + 45], BF)
            hts = [self.sb(es, "cvht%d" % k, [128, 8, 512], BF) for k in range(2)]
            sg = [self.sb(es, "cvsg%d" % k, [128, 512], F32) for k in range(2)]
            self.V("pool", "memset", [], [vb], vb[:], 0.0)
            n = 0
            for ti, (t0, N) in enumerate(TILES):
                ht = hts[ti % 2]
                self.load_ht(ht, ti)
                base = t0 + 15 if ti == 0 else t0 + 30
                for j in range(4):
                    pa, pg = self.psb(), self.psb()
                    self.proj(pa, wa, j * 128, ht, N)
                    self.proj(pg, wg, j * 128, ht, N)
                    s_ = sg[n % 2]
                    n += 1
                    self.act(s_[:, 0:N], pg[:, 0:N], AF.Sigmoid, [pg], [s_])
                    self.V("dve", "tensor_tensor", [s_, pa], [vb], out=vb[:, j, base:base + N], in0=pa[:, 0:N], in1=s_[:, 0:N], op=ALU.mult)
            DG = self.sb(es, "cvDG", [128, 124, 128], BF)
            for jk in range(124):
                self.V("pool", "tensor_scalar", [self.identb, P], [DG], out=DG[:, jk, :], in0=self.identb[:], scalar1=P[:, 12 + jk:13 + jk], scalar2=0.0,
                       op0=ALU.mult, op1=ALU.add)
            ca = [self.sb(es, "cvca%d" % k, [128, 4, 512], F32) for k in range(2)]
            cp = self.sb(es, "cvcp", [128, 512], F32)
            sq = self.sb(es, "cvsq", [128, 4, 512], F32)
            mean = self.sb(es, "cvmean", [128, 512], F32)
            tmp = self.sb(es, "cvtmp", [128, 512], F32)
            rstd = self.sb(es, "cvrstd", [128, 512], F32)
            dd = [self.sb(es, "cvd%d" % k, [128, 512], F32) for k in range(2)]
            ys = [self.sb(es, "cvy%d" % k, [128, 4, 512], BF) for k in range(2)]
            for ti, (t0, N) in enumerate(TILES):
                base = t0 + 15 if ti == 0 else t0 + 30
                a = ca[ti % 2]
                for j in range(4):
                    pc = self.psb()
                    for k in range(31):
                        self.mm(pc[:, 0:N], DG[:, j * 31 + k, :], vb[:, j, base - 15 + k:base - 15 + k + N], k == 0, k == 30, [DG, vb], [pc])
                    self.act(a[:, j, 0:N], pc[:, 0:N], AF.Identity, [pc, P], [a], bias=P[:, 136 + j:137 + j])
                    self.act(sq[:, j, 0:N], a[:, j, 0:N], AF.Square, [a], [sq])
                p1, p2 = self.psb(), self.psb()
                for j in range(4):
                    self.mm(p1[:, 0:N], onesf, a[:, j, 0:N], j == 0, j == 3, [self.cst, a], [p1])
                for j in range(4):
                    self.mm(p2[:, 0:N], onesf, sq[:, j, 0:N], j == 0, j == 3, [self.cst, sq], [p2])
                self.act(mean[:, 0:N], p1[:, 0:N], AF.Copy, [p1], [mean], scale=1.0 / W)
                self.V("pool", "tensor_tensor", [mean], [tmp], out=tmp[:, 0:N], in0=mean[:, 0:N], in1=mean[:, 0:N], op=ALU.mult)
                self.V("dve", "scalar_tensor_tensor", [p2, tmp], [tmp], out=tmp[:, 0:N], in0=p2[:, 0:N], scalar=1.0 / W, in1=tmp[:, 0:N],
                       op0=ALU.mult, op1=ALU.subtract)
                self.rstd_from(tmp[:, 0:N], 1.0, rstd, tmp, N, [tmp])
                y = ys[ti % 2]
                for j in range(4):
                    d = dd[j % 2]
                    self.V("dve", "tensor_tensor", [a, mean], [d], out=d[:, 0:N], in0=a[:, j, 0:N], in1=mean[:, 0:N], op=ALU.subtract)
                    self.V("pool", "tensor_tensor", [d, rstd], [d], out=d[:, 0:N], in0=d[:, 0:N], in1=rstd[:, 0:N], op=ALU.mult)
                    self.V("dve", "tensor_scalar", [d, P], [d], out=d[:, 0:N], in0=d[:, 0:N], scalar1=P[:, 140 + j:141 + j], scalar2=P[:, 144 + j:145 + j],
                           op0=ALU.mult, op1=ALU.add)
                    self.act(y[:, j, 0:N], d[:, 0:N], AF.Silu, [d], [y])
                S.dma("sp", self.Y[2, :, :, t0:t0 + N].rearrange("j p n -> p j n"), y[:, :, 0:N], [y], [Tl(None, self.Yt[2][ti])])
            S.barrier()

    def rope(self, ps, raw, cos, sin, t1, t2, dst_ap, dst_t, N, rot_ps):
        self.act(raw[:, 0:N], ps[:, 0:N], AF.Copy, [ps], [raw])
        self.mm(rot_ps[:, 0:N], self.Rb[:], raw[:, 0:N], True, True, [self.Rb, raw], [rot_ps])
        self.V("pool", "tensor_tensor", [raw, cos], [t1], out=t1[:, 0:N], in0=raw[:, 0:N], in1=cos[:, 0:N], op=ALU.mult)
        self.V("dve", "tensor_tensor", [rot_ps, sin], [t2], out=t2[:, 0:N], in0=rot_ps[:, 0:N], in1=sin[:, 0:N], op=ALU.mult)
        self.V("pool", "tensor_tensor", [t1, t2], [dst_t], out=dst_ap, in0=t1[:, 0:N], in1=t2[:, 0:N], op=ALU.add)

    def phase_attn(self, l):
        S = self.S
        P = self.P
        i = self.i
        onesf = self.cst[:, 128:256]
        B = self.banks
        with contextlib.ExitStack() as es:
            wq = self.load_w(es, "dawq", l, C_DQ, 512)
            wk = self.load_w(es, "dawk", l, C_DK, 512)
            wv = self.load_w(es, "dawv", l, C_DV, 512)
            KT = self.sb(es, "daKT", [128, 4, T], BF)
            Vt = self.sb(es, "daV", [128, NCH, 512], BF)
            hts = [self.sb(es, "daht%d" % k, [128, 8, 512], BF) for k in range(2)]
            cos = [self.sb(es, "dacos%d" % k, [128, 512], F32) for k in range(2)]
            sin = [self.sb(es, "dasin%d" % k, [128, 512], F32) for k in range(2)]
            raw = [self.sb(es, "daraw%d" % k, [128, 512], BF) for k in range(2)]
            t1 = [self.sb(es, "dat1%d" % k, [128, 512], F32) for k in range(2)]
            t2 = [self.sb(es, "dat2%d" % k, [128, 512], F32) for k in range(2)]
            n = 0
            for ti, (t0, N) in enumerate(TILES):
                ht = hts[ti % 2]
                self.load_ht(ht, ti)
                S.dma("sp", cos[ti % 2][:, 0:N], i["rope"][0, :, t0:t0 + N], [], [cos[ti % 2]])
                S.dma("sp", sin[ti % 2][:, 0:N], i["rope"][1, :, t0:t0 + N], [], [sin[ti % 2]])
                for h in range(4):
                    ps, rp = self.psb(), self.psb()
                    self.proj(ps, wk, h * 128, ht, N)
                    self.rope(ps, raw[n % 2], cos[ti % 2], sin[ti % 2], t1[n % 2], t2[n % 2], KT[:, h, t0:t0 + N], KT, N, rp)
                    n += 1
                for cc in range(N // 128):
                    ps = self.psb()
                    for kc in range(8):
                        self.mm(ps[:, :], ht[:, kc, cc * 128:(cc + 1) * 128], wv[:, kc, :], kc == 0, kc == 7, [ht, wv], [ps])
                    self.act(Vt[:, t0 // 128 + cc, :], ps[:, :], AF.Copy, [ps], [Vt])
            QT = [self.sb(es, "daQT%d" % k, [128, 512], BF) for k in range(2)]
            QZ = [[self.sb(es, "daQZ%d_%d" % (k, c), [128, 512], BF) for c in range(2)] for k in range(2)]
            for k in range(2):
                for c in range(2):
                    self.V("pool", "memset", [], [QZ[k][c]], QZ[k][c][:], 0.0)
            pT = [self.sb(es, "dapT%d" % k, [128, 512], BF) for k in range(4)]
            rd = [self.sb(es, "dard%d" % k, [128, 512], F32) for k in range(2)]
            rp_ = [self.sb(es, "darp%d" % k, [128, 512], F32) for k in range(2)]
            rinv = self.sb(es, "darinv", [128, 512], F32)
            Oc = [self.sb(es, "daOc%d" % k, [128, 512], F32) for k in range(2)]
            o = self.sb(es, "dao", [128, 512], F32)
            sq = self.sb(es, "dasq", [128, 512], F32)
            tmp = self.sb(es, "datmp", [128, 512], F32)
            rstd = self.sb(es, "darstd", [128, 512], F32)
            ys = [self.sb(es, "day%d" % k, [128, 4, 512], BF) for k in range(2)]
            it = 0
            for ti, (t0, N) in enumerate(TILES):
                ht = hts[ti % 2]
                self.load_ht(ht, ti)
                S.dma("sp", cos[ti % 2][:, 0:N], i["rope"][0, :, t0:t0 + N], [], [cos[ti % 2]])
                S.dma("sp", sin[ti % 2][:, 0:N], i["rope"][1, :, t0:t0 + N], [], [sin[ti % 2]])
                nk = 2 if ti == 0 else NCH
                y = ys[ti % 2]
                def qprep(h_, n_):
                    qt_ = QT[h_ % 2]
                    self.proj(B[7], wq, h_ * 128, ht, N)
                    self.rope(B[7], raw[n_ % 2], cos[ti % 2], sin[ti % 2], t1[n_ % 2], t2[n_ % 2], qt_[:, 0:N], qt_, N, B[7])
                    qz_ = QZ[h_ % 2]
                    self.V("pool", "tensor_copy", [qt_], [qz_[0]], out=qz_[0][0:64, 0:N], in_=qt_[0:64, 0:N])
                    self.V("dve", "tensor_copy", [qt_], [qz_[1]], out=qz_[1][64:128, 0:N], in_=qt_[64:128, 0:N])
                qprep(0, n)
                n += 1
                for h in range(4):
                    qt = QT[h % 2]
                    qz = QZ[h % 2]
                    if h + 1 < 4:
                        qprep(h + 1, n)
                        n += 1
                    its = [(c, kc) for c in range(2) for kc in range(nk)]
                    slots = {}
                    def qk(j):
                        c, kc = its[j]
                        sps = B[2 + it_base[0] % 3]
                        p = pT[it_base[0] % 4]
                        it_base[0] += 1
                        slots[j] = (sps, p)
                        p0 = c * 64
                        self.mm(sps[:, 0:N], KT[:, h, kc * 128:(kc + 1) * 128], qz[c][:, 0:N], True, True, [KT, qz[c]], [sps])
                    it_base = [it]
                    qk(0)
                    if len(its) > 1:
                        qk(1)
                    for j, (c, kc) in enumerate(its):
                        sps, p = slots.pop(j)
                        oacc = B[c]
                        self.act(p[:, 0:N], sps[:, 0:N], AF.Exp, [sps], [p], scale=0.125)
                        if j + 2 < len(its):
                            qk(j + 2)
                        self.mm(oacc[:, 0:N], Vt[:, kc, h * 128:(h + 1) * 128], p[:, 0:N], kc == 0, kc == nk - 1, [Vt, p], [oacc])
                        rsb = B[5 + c]
                        self.mm(rsb[:, 0:N], self.onesb[:], p[:, 0:N], kc == 0, kc == nk - 1, [self.onesb, p], [rsb])
                        if kc == nk - 1:
                            self.V("dve", "reciprocal", [rsb], [rinv], out=rinv[:, 0:N], in_=rsb[:, 0:N])
                            self.V("dve", "tensor_tensor", [oacc, rinv], [Oc[c]], out=Oc[c][:, 0:N], in0=oacc[:, 0:N], in1=rinv[:, 0:N], op=ALU.mult)
                    it = it_base[0]
                    self.V("dve", "scalar_tensor_tensor", [Oc[0], Oc[1], P], [o], out=o[:, 0:N], in0=Oc[1][:, 0:N], scalar=P[:, 153:154], in1=Oc[0][:, 0:N],
                           op0=ALU.mult, op1=ALU.add)
                    self.act(sq[:, 0:N], o[:, 0:N], AF.Square, [o], [sq])
                    self.mm(B[7][:, 0:N], onesf, sq[:, 0:N], True, True, [self.cst, sq], [B[7]])
                    self.rstd_from(B[7][:, 0:N], 1.0 / 128, rstd, tmp, N, [B[7]])
                    self.V("dve", "scalar_tensor_tensor", [o, rstd, P], [y], out=y[:, h, 0:N], in0=o[:, 0:N], scalar=P[:, 154:155], in1=rstd[:, 0:N],
                           op0=ALU.mult, op1=ALU.mult)
                S.dma("pool", self.Y[1, :, :, t0:t0 + N].rearrange("j p n -> p j n"), y[:, :, 0:N], [y], [Tl(None, self.Yt[1][ti])])
            S.barrier()

    def phase_hgrn(self, l):
        S = self.S
        B = self.banks
        with contextlib.ExitStack() as es:
            rot = [2]
            def rb():
                b = B[rot[0]]
                rot[0] = 2 + (rot[0] - 1) % 6
                return b
            sh = {k: self.sb(es, "hgs_" + k, [128, 512], F32) for k in ("osum", "sq", "tmp", "rstd", "sgg", "y1")}
            sh["ys"] = [self.sb(es, "hgys%d" % k, [128, 512], BF) for k in range(2)]
            sh["n"] = 0
            chains = []
            for k in range(2):
                c = {"k": k}
                c["OF"] = self.sb(es, "hgOF%d" % k, [128, T], F32)
                c["S32"] = self.sb(es, "hgS%d" % k, [128, 128], F32)
                c["Sbf"] = [self.sb(es, "hgSb%d_%d" % (k, j), [128, 128], BF) for j in range(2)]
                c["ht"] = self.sb(es, "hght%d" % k, [128, 8, 512], BF)
                c["w"] = [self.sb(es, "hgw%d_%d" % (k, j), [128, 8, 128], BF) for j in range(4)]
                for nm in ("q32", "ee", "ff", "lf", "kk", "pre", "bb", "d3", "d2", "d3a"):
                    c[nm] = self.sb(es, "hg%s%d" % (nm, k), [128, 512], F32)
                for nm in ("qE1", "qE3", "kE4", "kE2", "qE3b", "kE4b"):
                    c[nm] = self.sb(es, "hg%s%d" % (nm, k), [128, 512], BF)
                c["iT"] = self.sb(es, "hgiT%d" % k, [128, 4, 128], BF)
                c["kT"] = self.sb(es, "hgkT%d" % k, [128, 4, 128], BF)
                c["AM"] = [self.sb(es, "hgAM%d_%d" % (k, j), [128, 128], BF) for j in range(2)]
                c["a1"] = [self.sb(es, "hga1%d_%d" % (k, j), [128, 128], F32) for j in range(2)]
                c["a2"] = [self.sb(es, "hga2%d_%d" % (k, j), [128, 128], F32) for j in range(2)]
                c["ops"] = B[k]
                chains.append(c)
            for pair in ((0, 1), (2, 3)):
                for d in range(2):
                    gens = [self.hgrn_chain(l, h, d, chains[k], sh, rb) for k, h in enumerate(pair)]
                    live = list(gens)
                    while live:
                        for g in list(live):
                            try:
                                next(g)
                            except StopIteration:
                                live.remove(g)
            S.barrier()

    def hgrn_chain(self, l, h, d, c, sh, rb):
        S = self.S
        P = self.P
        onesf = self.cst[:, 128:256]
        w = c["w"]
        OF, S32, Sbf, ht, ops = c["OF"], c["S32"], c["Sbf"], c["ht"], c["ops"]
        q32, ee, ff, lf, kk, pre, bb, d3, d2, d3a = (c[n] for n in ("q32", "ee", "ff", "lf", "kk", "pre", "bb", "d3", "d2", "d3a"))
        qE1, qE3, kE4, kE2, qE3b, kE4b = (c[n] for n in ("qE1", "qE3", "kE4", "kE2", "qE3b", "kE4b"))
        iT, kT = c["iT"], c["kT"]
        E1, E3, E3b, E4b, E2, E4 = ee, ff, lf, d3, d2, d3a
        cols = (C_HQ, C_HFF if d == 0 else C_HFB, C_HI, C_HG)
        for k in range(4):
            S.dma("pool", w[k][:], self.i["w_in"][l, :, cols[k] + h * 128:cols[k] + (h + 1) * 128].rearrange("(kc p) n -> p kc n", p=128), [], [w[k]])
        self.V("pool", "memset", [], [S32], S32[:], 0.0)
        self.V("pool", "memset", [], [Sbf[0]], Sbf[0][:], 0.0)
        cur = 0
        am_i = 0
        lbc = self.LB[:, l, d * 4 + h:d * 4 + h + 1]
        omc = self.OML[:, l, d * 4 + h:d * 4 + h + 1]
        order = list(range(9)) if d == 0 else [0] + list(range(8, 0, -1))
        mask = self.cst[:, 384:512] if d == 0 else self.cst[:, 512:640]
        masko = self.cst[:, 640:768] if d == 0 else self.cst[:, 768:896]
        for ti in order:
            t0, N = TILES[ti]
            nb = N // 128
            nch = N // 64
            self.load_ht(ht, ti)
            pq, pf = rb(), rb()
            self.proj(pq, w[0], 0, ht, N)
            self.proj(pf, w[1], 0, ht, N)
            self.act(q32[:, 0:N], pq[:, 0:N], AF.Copy, [pq], [q32])
            self.act(ee[:, 0:N], pf[:, 0:N], AF.Exp, [pf], [ee], scale=-1.0)
            self.V("dve", "tensor_scalar", [ee], [ee], out=ee[:, 0:N], in0=ee[:, 0:N], scalar1=1.0, scalar2=1.0, op0=ALU.add, op1=ALU.mult)
            self.V("dve", "reciprocal", [ee], [ee], out=ee[:, 0:N], in_=ee[:, 0:N])
            self.V("dve", "tensor_scalar", [ee, self.LB, self.OML], [ff], out=ff[:, 0:N], in0=ee[:, 0:N], scalar1=omc, scalar2=lbc, op0=ALU.mult, op1=ALU.add)
            self.act(lf[:, 0:N], ff[:, 0:N], AF.Ln, [ff], [lf])
            self.V("pool", "tensor_scalar", [ff], [kk], out=kk[:, 0:N], in0=ff[:, 0:N], scalar1=-1.0, scalar2=1.0, op0=ALU.mult, op1=ALU.add)
            self.V("dve", "tensor_tensor_scan", [self.seg, lf], [pre], out=pre[:, 0:N], data0=self.seg[:, 0:N], data1=lf[:, 0:N], initial=0.0, op0=ALU.mult, op1=ALU.add)
            v3 = lambda t_: t_[:, 0:N].rearrange("p (c s) -> p c s", s=64)
            v32 = lambda t_: t_[:, 0:N].rearrange("p (c s) -> p c s", s=32)
            bc = lambda t_, col: v3(t_)[:, :, col:col + 1].to_broadcast([128, nch, 64])
            if d == 0:
                b_ = pre
                cend = 63
            else:
                b_ = bb
                cend = 0
                self.V("dve", "tensor_tensor", [lf, pre], [bb], out=bb[:, 0:N], in0=lf[:, 0:N], in1=pre[:, 0:N], op=ALU.subtract)
                self.V("dve", "tensor_tensor", [bb, pre], [bb], out=v3(bb), in0=v3(bb), in1=bc(pre, 63), op=ALU.add)
            yield
            self.V("dve", "tensor_tensor", [b_], [d3], out=v3(d3), in0=v3(b_), in1=bc(b_, 32), op=ALU.subtract)
            self.V("pool", "tensor_tensor", [b_], [d2], out=v3(d2), in0=v3(b_), in1=bc(b_, cend), op=ALU.subtract)
            self.V("dve", "tensor_tensor", [b_], [d3a], out=v32(d3a), in0=v32(b_), in1=v32(b_)[:, :, 16:17].to_broadcast([128, 2 * nch, 32]), op=ALU.subtract)
            self.act(E1[:, 0:N], b_[:, 0:N], AF.Exp, [b_], [E1])
            self.act(E2[:, 0:N], d2[:, 0:N], AF.Exp, [d2], [E2], scale=-1.0)
            self.act(E3[:, 0:N], d3a[:, 0:N], AF.Exp, [d3a], [E3])
            self.act(E4[:, 0:N], d3a[:, 0:N], AF.Exp, [d3a], [E4], scale=-1.0)
            self.act(E3b[:, 0:N], d3[:, 0:N], AF.Exp, [d3], [E3b])
            self.act(E4b[:, 0:N], d3[:, 0:N], AF.Exp, [d3], [E4b], scale=-1.0)
            self.V("dve", "tensor_tensor", [q32, E1], [qE1], out=qE1[:, 0:N], in0=q32[:, 0:N], in1=E1[:, 0:N], op=ALU.mult)
            self.V("pool", "tensor_tensor", [q32, E3], [qE3], out=qE3[:, 0:N], in0=q32[:, 0:N], in1=E3[:, 0:N], op=ALU.mult)
            self.V("dve", "tensor_tensor", [kk, E4], [kE4], out=kE4[:, 0:N], in0=kk[:, 0:N], in1=E4[:, 0:N], op=ALU.mult)
            self.V("pool", "tensor_tensor", [kk, E2], [kE2], out=kE2[:, 0:N], in0=kk[:, 0:N], in1=E2[:, 0:N], op=ALU.mult)
            self.V("dve", "tensor_tensor", [q32, E3b], [qE3b], out=qE3b[:, 0:N], in0=q32[:, 0:N], in1=E3b[:, 0:N], op=ALU.mult)
            self.V("pool", "tensor_tensor", [kk, E4b], [kE4b], out=kE4b[:, 0:N], in0=kk[:, 0:N], in1=E4b[:, 0:N], op=ALU.mult)
            qz, kz = (slice(0, 32), slice(32, 64)) if d == 0 else (slice(32, 64), slice(0, 32))
            self.V("dve", "memset", [qE3b], [qE3b], v3(qE3b)[:, :, qz], 0.0)
            self.V("pool", "memset", [kE4b], [kE4b], v3(kE4b)[:, :, kz], 0.0)
            yield
            for cc in range(nb):
                pi = rb()
                for kc in range(8):
                    self.mm(pi[:, 0:128], ht[:, kc, cc * 128:(cc + 1) * 128], w[2][:, kc, :], kc == 0, kc == 7, [ht, w[2]], [pi])
                self.act(iT[:, cc, :], pi[:, 0:128], AF.Copy, [pi], [iT])
                pt = rb()
                ptv = pt[:].bitcast(BF)
                self.tr(ptv[:, 0:128], kE2[:, cc * 128:(cc + 1) * 128], self.identb[:], [kE2, self.identb], [pt])
                self.act(kT[:, cc, :], ptv[:, 0:128], AF.Copy, [pt], [kT])
            yield
            blks = list(range(nb)) if d == 0 else list(range(nb - 1, -1, -1))
            for blk in blks:
                pa = rb()
                bs = slice(blk * 128, (blk + 1) * 128)
                self.mm(pa[:, 0:128], kE4[:, bs], qE3[:, bs], True, True, [kE4, qE3], [pa])
                pb_ = rb()
                self.mm(pb_[:, 0:128], kE4b[:, bs], qE3b[:, bs], True, True, [kE4b, qE3b], [pb_])
                am = c["AM"][am_i % 2]
                a1 = c["a1"][am_i % 2]
                a2 = c["a2"][am_i % 2]
                am_i += 1
                self.V("dve", "tensor_tensor", [pa, self.cst], [a1], out=a1[:], in0=pa[:, 0:128], in1=mask, op=ALU.mult)
                self.V("dve", "tensor_tensor", [pb_, self.cst], [a2], out=a2[:], in0=pb_[:, 0:128], in1=masko, op=ALU.mult)
                self.V("pool", "tensor_tensor", [a1, a2], [am], out=am[:], in0=a1[:], in1=a2[:], op=ALU.add)
                for ch in ((0, 1) if d == 0 else (1, 0)):
                    p0 = ch * 64
                    c0 = blk * 128 + p0
                    self.mm(ops[:, c0:c0 + 64], Sbf[cur][:], qE1[:, c0:c0 + 64], True, False, [Sbf[cur], qE1], [ops])
                    self.mm(ops[:, c0:c0 + 64], iT[p0:p0 + 64, blk, :], am[p0:p0 + 64, p0:p0 + 64], False, True, [iT, am], [ops])
                    pS = rb()
                    self.mm(pS[:, 0:128], kT[p0:p0 + 64, blk, :], iT[p0:p0 + 64, blk, :], True, True, [kT, iT], [pS])
                    ce = c0 + cend
                    self.V("dve", "scalar_tensor_tensor", [S32, E1, pS], [S32], out=S32[:], in0=S32[:], scalar=E1[:, ce:ce + 1], in1=pS[:, 0:128],
                           op0=ALU.mult, op1=ALU.add)
                    cur = 1 - cur
                    self.act(Sbf[cur][:], S32[:], AF.Copy, [S32], [Sbf[cur]])
                    yield
            if d == 0:
                self.act(OF[:, t0:t0 + N], ops[:, 0:N], AF.Copy, [ops], [OF])
            else:
                osum, sq, tmp, rstd, sgg, y1 = (sh[n_] for n_ in ("osum", "sq", "tmp", "rstd", "sgg", "y1"))
                self.V("dve", "tensor_tensor", [OF, ops], [osum], out=osum[:, 0:N], in0=OF[:, t0:t0 + N], in1=ops[:, 0:N], op=ALU.add)
                self.act(sq[:, 0:N], osum[:, 0:N], AF.Square, [osum], [sq])
                pss = rb()
                self.mm(pss[:, 0:N], onesf, sq[:, 0:N], True, True, [self.cst, sq], [pss])
                self.rstd_from(pss[:, 0:N], 1.0 / 128, rstd, tmp, N, [pss])
                pg = rb()
                self.proj(pg, w[3], 0, ht, N)
                self.act(sgg[:, 0:N], pg[:, 0:N], AF.Sigmoid, [pg], [sgg])
                self.V("dve", "scalar_tensor_tensor", [osum, rstd, P], [y1], out=y1[:, 0:N], in0=osum[:, 0:N], scalar=P[:, 148 + h:149 + h], in1=rstd[:, 0:N],
                       op0=ALU.mult, op1=ALU.mult)
                y = sh["ys"][sh["n"] % 2]
                sh["n"] += 1
                self.V("pool", "tensor_tensor", [y1, sgg], [y], out=y[:, 0:N], in0=y1[:, 0:N], in1=sgg[:, 0:N], op=ALU.mult)
                S.dma("pool", self.Y[0, h, :, t0:t0 + N], y[:, 0:N], [y], [Tl(None, self.Yt[0][ti])])
            yield

    def phase_merge(self, l):
        S = self.S
        i = self.i
        with contextlib.ExitStack() as es:
            wg = self.sb(es, "mgwg", [128, 8, 4096], BF)
            for k in range(4):
                S.dma("pool", wg[:, :, k * 1024:(k + 1) * 1024], i["w_in"][l, :, C_GATE + k * 1024:C_GATE + (k + 1) * 1024].rearrange("(kc p) n -> p kc n", p=128), [], [wg])
            wb = self.sb(es, "mgwb", [128, 4, 4, 1024], BF)
            for k in range(4):
                S.dma("pool", wb[:, k, :, :], i["w_branch"][l, k].rearrange("(cc p) n -> p cc n", p=128), [], [wb])
            ht = self.sb(es, "mght", [128, 8, 512], BF)
            Yk = [self.sb(es, "mgY%d" % k, [128, 4, 512], BF) for k in range(4)]
            sg = [self.sb(es, "mgsg%d" % k, [128, 512], F32) for k in range(2)]
            tmp = [self.sb(es, "mgtmp%d" % k, [128, 512], F32) for k in range(2)]
            macc = [self.sb(es, "mgacc%d" % k, [128, 512], F32) for k in range(2)]
            mT = [self.sb(es, "mgmT%d" % k, [128, 8, 512], BF) for k in range(2)]
            n = 0
            for ti, (t0, N) in enumerate(TILES):
                self.load_ht(ht, ti)
                for k in range(4):
                    S.dma("sp", Yk[k][:, :, 0:N], self.Y[k, :, :, t0:t0 + N].rearrange("j p n -> p j n"), [Tl(None, self.Yt[k][ti])], [Yk[k]])
                m = mT[ti % 2]
                for nch in range(8):
                    ma = macc[nch % 2]
                    for k in range(4):
                        pg, pp = self.psb(), self.psb()
                        self.proj(pg, wg, k * 1024 + nch * 128, ht, N)
                        for cc in range(4):
                            self.mm(pp[:, 0:N], wb[:, k, cc, nch * 128:(nch + 1) * 128], Yk[k][:, cc, 0:N], cc == 0, cc == 3, [wb, Yk[k]], [pp])
                        s_ = sg[n % 2]
                        t_ = tmp[n % 2]
                        n += 1
                        self.act(s_[:, 0:N], pg[:, 0:N], AF.Sigmoid, [pg], [s_])
                        if k == 0:
                            self.V("dve", "tensor_tensor", [pp, s_], [ma], out=ma[:, 0:N], in0=pp[:, 0:N], in1=s_[:, 0:N], op=ALU.mult)
                        else:
                            self.V("dve", "tensor_tensor", [pp, s_], [t_], out=t_[:, 0:N], in0=pp[:, 0:N], in1=s_[:, 0:N], op=ALU.mult)
                            self.V("pool", "tensor_tensor", [ma, t_], [ma], out=ma[:, 0:N], in0=ma[:, 0:N], in1=t_[:, 0:N], op=ALU.add)
                    self.V("pool", "tensor_copy", [ma], [m], out=m[:, nch, 0:N], in_=ma[:, 0:N])
                S.dma("sp", self.MT[:, :, t0:t0 + N].rearrange("k p n -> p k n"), m[:, :, 0:N], [m], [Tl(None, self.MTt[ti])])
            S.barrier()
        with contextlib.ExitStack() as es:
            wo = self.sb(es, "mgwo", [128, 8, 1024], BF)
            S.dma("pool", wo[:], i["w_out"][l].rearrange("(kc p) n -> p kc n", p=128), [], [wo])
            mts = [self.sb(es, "mgmt%d" % k, [128, 8, 512], BF) for k in range(2)]
            self.residual_setup(es, 2)
            for ti, (t0, N) in enumerate(TILES):
                mt = mts[ti % 2]
                S.dma("sp", mt[:, :, 0:N], self.MT[:, :, t0:t0 + N].rearrange("k p n -> p k n"), [Tl(None, self.MTt[ti])], [mt])
                for cc in range(N // 128):
                    c = t0 // 128 + cc
                    halves = []
                    for half in range(2):
                        po = self.psb()
                        for kc in range(8):
                            self.mm(po[:, :], mt[:, kc, cc * 128:(cc + 1) * 128], wo[:, kc, half * 512:(half + 1) * 512], kc == 0, kc == 7, [mt, wo], [po])
                        halves.append(po)
                    self.residual(c, lambda half: halves[half][:, :], halves)
            S.barrier()

    def residual_setup(self, es, modidx):
        S = self.S
        self.rmod = [self.sb(es, "rsmod%d" % k, [128, D], F32) for k in range(2)]
        for k in range(2):
            S.dma("sp", self.rmod[k][:], self.MODR[k, :, modidx * D:(modidx + 1) * D], [Tl(None, self.MODt)], [self.rmod[k]])
        self.rx = [self.sb(es, "rsx%d" % k, [128, D], F32) for k in range(3)]
        self.rtmp = [self.sb(es, "rstmp%d" % k, [128, D], F32) for k in range(2)]

    def residual(self, c, delta_ap, delta_tiles):
        S = self.S
        lat = 0 if c >= 2 else 1
        x = self.rx[c % 3]
        t = self.rtmp[c % 2]
        xt = Tl(None, self.Xt[c])
        S.dma("sp", x[:], self.X[c * 128:(c + 1) * 128, :], [xt], [x])
        for half in range(2):
            hs = slice(half * 512, (half + 1) * 512)
            self.V("dve", "tensor_tensor", [delta_tiles[half], self.rmod[lat]], [t], out=t[:, hs], in0=delta_ap(half), in1=self.rmod[lat][:, hs], op=ALU.mult)
        self.V("dve", "tensor_tensor", [x, t], [x], out=x[:], in0=x[:], in1=t[:], op=ALU.add)
        S.dma("act", self.X[c * 128:(c + 1) * 128, :], x[:], [x], [xt])

    def route(self, c, t, Bm, h32, h32T, wr, R):
        P = self.P
        self.V("pool", "tensor_tensor", [t, Bm], [h32], out=h32[:], in0=t[:], in1=Bm[:], op=ALU.add)
        for g in range(2):
            ps = self.psb()
            for k in range(4):
                kk = g * 4 + k
                self.tr(ps[:, k * 128:(k + 1) * 128], h32[:, kk * 128:(kk + 1) * 128], self.cst[:, 0:128], [h32, self.cst], [ps])
            self.act(h32T[:, g * 4:(g + 1) * 4, :], ps[:].rearrange("p (k n) -> p k n", k=4), AF.Copy, [ps], [h32T])
        pl = self.psb()
        for kc in range(8):
            self.mm(pl[:, 0:36], h32T[:, kc, :], wr[:, kc, :], kc == 0, kc == 7, [h32T, wr], [pl])
        dv = lambda name, w_, **kw: self.V("dve", name, [R, P] + w_[1:], [w_[0]], **kw)
        self.V("dve", "tensor_tensor", [pl, P], [R], out=R[:, 0:36], in0=pl[:, 0:36], in1=P[:, 416:452], op=ALU.add)
        RR = [R]
        dv("tensor_reduce", RR, out=R[:, 36:37], in_=R[:, 0:4], axis=AX.X, op=ALU.max)
        dv("tensor_scalar", RR, out=R[:, 37:38], in0=R[:, 36:37], scalar1=-1.0, scalar2=0.0, op0=ALU.mult, op1=ALU.add)
        self.act(R[:, 44:48], R[:, 0:4], AF.Exp, [R], [R], bias=R[:, 37:38], accum_out=R[:, 38:39])
        dv("reciprocal", RR, out=R[:, 39:40], in_=R[:, 38:39])
        dv("tensor_scalar", RR, out=R[:, 40:44], in0=R[:, 0:4], scalar1=R[:, 36:37], scalar2=1.0, op0=ALU.is_equal, op1=ALU.mult)
        dv("tensor_tensor", RR, out=R[:, 48:80].rearrange("p (g e) -> p g e", g=4), in0=R[:, 4:36].rearrange("p (g e) -> p g e", g=4),
           in1=R[:, 40:44].unsqueeze(2).to_broadcast([128, 4, 8]), op=ALU.mult)
        dv("tensor_reduce", RR, out=R[:, 80:88], in_=R[:, 48:80].rearrange("p (g e) -> p e g", g=4), axis=AX.X, op=ALU.add)
        dv("tensor_reduce", RR, out=R[:, 88:89], in_=R[:, 80:88], axis=AX.X, op=ALU.max)
        dv("tensor_scalar", RR, out=R[:, 89:97], in0=R[:, 80:88], scalar1=R[:, 88:89], scalar2=1.0, op0=ALU.is_equal, op1=ALU.mult)
        dv("scalar_tensor_tensor", RR, out=R[:, 97:105], in0=R[:, 89:97], scalar=-1e30, in1=R[:, 80:88], op0=ALU.mult, op1=ALU.add)
        dv("tensor_reduce", RR, out=R[:, 105:106], in_=R[:, 97:105], axis=AX.X, op=ALU.max)
        dv("tensor_scalar", RR, out=R[:, 106:114], in0=R[:, 97:105], scalar1=R[:, 105:106], scalar2=1.0, op0=ALU.is_equal, op1=ALU.mult)
        dv("tensor_tensor", RR, out=R[:, 114:115], in0=R[:, 105:106], in1=R[:, 88:89], op=ALU.subtract)
        self.act(R[:, 115:116], R[:, 114:115], AF.Exp, [R], [R])
        dv("tensor_scalar", RR, out=R[:, 116:117], in0=R[:, 115:116], scalar1=1.0, scalar2=1.0, op0=ALU.add, op1=ALU.mult)
        dv("reciprocal", RR, out=R[:, 116:117], in_=R[:, 116:117])
        dv("tensor_tensor", RR, out=R[:, 117:118], in0=R[:, 116:117], in1=R[:, 39:40], op=ALU.mult)
        dv("tensor_tensor", RR, out=R[:, 118:119], in0=R[:, 39:40], in1=R[:, 117:118], op=ALU.subtract)
        dv("tensor_scalar", RR, out=R[:, 119:127], in0=R[:, 89:97], scalar1=R[:, 117:118], scalar2=0.0, op0=ALU.mult, op1=ALU.add)
        dv("scalar_tensor_tensor", RR, out=R[:, 119:127], in0=R[:, 106:114], scalar=R[:, 118:119], in1=R[:, 119:127], op0=ALU.mult, op1=ALU.add)
        self.V("dve", "tensor_tensor", [R], [self.RW], out=self.RW[:, c, :].rearrange("p (g e) -> p g e", g=4),
               in0=R[:, 40:44].unsqueeze(2).to_broadcast([128, 4, 8]), in1=R[:, 119:127].unsqueeze(1).to_broadcast([128, 4, 8]), op=ALU.mult)

    def phase_moe(self, l):
        S = self.S
        i = self.i
        with contextlib.ExitStack() as es:
            acc = self.sb(es, "moacc", [128, 10, D], F32)
            hTb = self.sb(es, "mohT", [128, 8, 1280], BF)
            wts = [(self.sb(es, "mowg%d" % k, [128, 8, 512], BF), self.sb(es, "mowu%d" % k, [128, 8, 512], BF),
                    self.sb(es, "mowd%d" % k, [128, 4, D], BF)) for k in range(2)]
            sG = [self.sb(es, "mosg%d" % k, [128, 512], F32) for k in range(2)]
            Hh = [self.sb(es, "moHh%d" % k, [128, 4, 512], BF) for k in range(2)]
            self.residual_setup(es, 5)
            n = 0
            m = 0
            for blk in ((0, 1, 2), (3, 4), (5, 6), (7, 8)):
                col = 0
                tcs = []
                for ti in blk:
                    t0, N = TILES[ti]
                    S.dma("sp", hTb[:, :, col:col + N], self.HT[:, :, t0:t0 + N].rearrange("k p n -> p k n"), [Tl(None, self.HTt[ti])], [hTb])
                    tcs.append((col, N, t0))
                    col += N
                for e in range(self.nexp):
                    wg, wu, wd = wts[e % 2]
                    S.dma("pool", wg[:], i["moe_w_gate"][l, e].rearrange("(kc p) f -> p kc f", p=128), [], [wg])
                    S.dma("pool", wu[:], i["moe_w_up"][l, e].rearrange("(kc p) f -> p kc f", p=128), [], [wu])
                    S.dma("pool", wd[:], i["moe_w_down"][l, e].rearrange("(fc p) n -> p fc n", p=128), [], [wd])
                    for (col, N, t0) in tcs:
                        hh = Hh[m % 2]
                        m += 1
                        for fc in range(4):
                            pG, pU = self.psb(), self.psb()
                            for kc in range(8):
                                self.mm(pG[:, 0:N], wg[:, kc, fc * 128:(fc + 1) * 128], hTb[:, kc, col:col + N], kc == 0, kc == 7, [wg, hTb], [pG])
                            for kc in range(8):
                                self.mm(pU[:, 0:N], wu[:, kc, fc * 128:(fc + 1) * 128], hTb[:, kc, col:col + N], kc == 0, kc == 7, [wu, hTb], [pU])
                            sg = sG[n % 2]
                            n += 1
                            self.act(sg[:, 0:N], pG[:, 0:N], AF.Silu, [pG], [sg])
                            self.V("dve", "tensor_tensor", [sg, pU], [hh], out=hh[:, fc, 0:N], in0=sg[:, 0:N], in1=pU[:, 0:N], op=ALU.mult)
                        for cc in range(N // 128):
                            ci = col // 128 + cc
                            c = t0 // 128 + cc
                            for half in range(2):
                                hs = slice(half * 512, (half + 1) * 512)
                                pD = self.psb()
                                for fc in range(4):
                                    self.mm(pD[:, :], hh[:, fc, cc * 128:(cc + 1) * 128], wd[:, fc, hs], fc == 0, fc == 3, [hh, wd], [pD])
                                if e == 0:
                                    self.V("dve", "tensor_scalar", [pD, self.RW], [acc], out=acc[:, ci, hs], in0=pD[:, :], scalar1=self.RW[:, c, e:e + 1], scalar2=0.0,
                                           op0=ALU.mult, op1=ALU.add)
                                else:
                                    self.V("dve", "scalar_tensor_tensor", [pD, self.RW, acc], [acc], out=acc[:, ci, hs], in0=pD[:, :], scalar=self.RW[:, c, e:e + 1],
                                           in1=acc[:, ci, hs], op0=ALU.mult, op1=ALU.add)
                for (col, N, t0) in tcs:
                    for cc in range(N // 128):
                        ci = col // 128 + cc
                        c = t0 // 128 + cc
                        self.residual(c, lambda half, ci=ci: acc[:, ci, half * 512:(half + 1) * 512], [acc, acc])
            S.barrier()

    def rank(self, c, R):
        A = R[:, 128:160]
        self.V("dve", "tensor_scalar", [self.RW], [R], out=A, in0=self.RW[:, c, :], scalar1=0.0, scalar2=1.0, op0=ALU.is_gt, op1=ALU.mult)
        if c == 0:
            self.V("dve", "memset", [], [self.Asum], self.Asum[:], 0.0)
        ps = self.psb()
        self.mm(ps[:, 0:32], self.ltri[:], A, True, False, [self.ltri, R], [ps])
        self.mm(ps[:, 0:32], self.cst[:, 128:256], self.Asum[:], False, True, [self.cst, self.Asum], [ps])
        self.act(self.RK[:, c, :], ps[:, 0:32], AF.Copy, [ps], [self.RK])
        self.V("dve", "tensor_tensor", [self.Asum, R], [self.Asum], out=self.Asum[:], in0=self.Asum[:], in1=A, op=ALU.add)

    def phase_moe_sparse(self, l):
        S = self.S
        i = self.i
        onesf = self.cst[:, 128:256]
        rows_t = Tl(None, self.ROWSt)
        acc_t = Tl(None, self.ACC2t)
        h2_t = Tl(None, self.H2t)
        with contextlib.ExitStack() as es:
            G = self.sb(es, "spG", [128, 1024], F32)
            widx = self.sb(es, "spwidx", [128, 2, NB], U32)
            init = self.sb(es, "spinit", [128, 128, 4], F32)
            self.V("pool", "memset", [], [init], init[:], 0.0)
            self.V("pool", "memset", [init], [init], init[:, :, 0:1], float(T))
            self.V("pool", "memset", [init], [init], init[:, :, 2:4], 1.0e6)
            S.dma("sp", self.ROWS.rearrange("(j p) c -> j (p c)", p=128), init[0:NB, :, :].rearrange("j p c -> j (p c)"), [init], [rows_t])
            ps = self.psb()
            self.mm(ps[:, 0:32], onesf, self.Asum[:], True, True, [self.cst, self.Asum], [ps])
            cnt, pad, pend, pst = G[:, 0:32], G[:, 32:64], G[:, 64:96], G[:, 96:128]
            cmp = self.sb(es, "spcmp", [128, NB, 32], F32)
            self.V("dve", "tensor_copy", [ps], [G], out=cnt, in_=ps[:, 0:32])
            cmp2 = cmp[:].rearrange("p a b -> p (a b)")[:, 0:32 * 68].rearrange("p (e m) -> p e m", m=68)
            self.V("dve", "tensor_tensor", [G, self.cst], [cmp], out=cmp2, in0=cnt.unsqueeze(2).to_broadcast([128, 32, 68]),
                   in1=self.cst[:, 896:896 + 68].unsqueeze(1).to_broadcast([128, 32, 68]), op=ALU.is_gt)
            self.V("dve", "tensor_reduce", [cmp], [G], out=pad, in_=cmp2, axis=AX.X, op=ALU.add)
            self.V("dve", "tensor_scalar", [G], [G], out=pad, in0=pad, scalar1=128.0, scalar2=0.0, op0=ALU.mult, op1=ALU.add)
            self.V("dve", "tensor_tensor_scan", [G, self.cst], [G], out=pend, data0=onesf[:, 0:32], data1=pad, initial=0.0, op0=ALU.mult, op1=ALU.add)
            self.V("dve", "tensor_tensor", [G], [G], out=pst, in0=pend, in1=pad, op=ALU.subtract)
            self.V("dve", "tensor_tensor", [G, self.cst], [cmp], out=cmp[:], in0=pend.unsqueeze(1).to_broadcast([128, NB, 32]),
                   in1=self.cst[:, 896:896 + NB].unsqueeze(2).to_broadcast([128, NB, 32]), op=ALU.is_le)
            be = G[:, 128:128 + NB]
            self.V("dve", "tensor_reduce", [cmp], [G], out=be, in_=cmp[:], axis=AX.X, op=ALU.add)
            self.V("dve", "tensor_scalar", [G], [G], out=be, in0=be, scalar1=31.0, scalar2=128.0, op0=ALU.min, op1=ALU.mult)
            same = G[:, 384:384 + NB]
            self.V("dve", "memset", [G], [G], same, 0.0)
            self.V("dve", "tensor_tensor", [G], [G], out=G[:, 386:384 + NB], in0=G[:, 130:128 + NB], in1=G[:, 128:126 + NB], op=ALU.is_equal)
            wf = G[:, 256:256 + NB]
            self.V("dve", "tensor_scalar", [G, self.cst], [G], out=wf, in0=be, scalar1=self.cst[:, 1024:1025], scalar2=2.0, op0=ALU.add, op1=ALU.mult)
            self.V("dve", "tensor_scalar", [G], [G], out=wf, in0=wf, scalar1=float(l * 8192), scalar2=1.0, op0=ALU.add, op1=ALU.mult)
            self.V("dve", "scalar_tensor_tensor", [G], [G], out=wf, in0=same, scalar=1.0e8, in1=wf, op0=ALU.mult, op1=ALU.add)
            self.V("dve", "tensor_copy", [G], [widx], out=widx[:, 0, :], in_=wf)
            self.V("dve", "tensor_scalar", [G], [G], out=wf, in0=wf, scalar1=1.0, scalar2=1.0, op0=ALU.add, op1=ALU.mult)
            self.V("dve", "tensor_copy", [G], [widx], out=widx[:, 1, :], in_=wf)
            Q = [self.sb(es, "spQ%d" % k, [128, 160], F32) for k in range(2)]
            rec = [self.sb(es, "sprec%d" % k, [128, 2, 4], F32) for k in range(2)]
            didx = [self.sb(es, "spdidx%d" % k, [128, 2], U32) for k in range(2)]
            for c in range(NCH):
                q = Q[c % 2]
                r_ = rec[c % 2]
                di = didx[c % 2]
                A, dst, d1, m1 = q[:, 0:32], q[:, 32:64], q[:, 64:96], q[:, 96:128]
                rd = [self.RW, self.RK, G, q]
                self.V("dve", "tensor_scalar", rd, [q], out=A, in0=self.RW[:, c, :], scalar1=0.0, scalar2=1.0, op0=ALU.is_gt, op1=ALU.mult)
                self.V("dve", "tensor_tensor", rd, [q], out=dst, in0=self.RK[:, c, :], in1=pst, op=ALU.add)
                self.V("dve", "scalar_tensor_tensor", rd, [q], out=d1, in0=dst, scalar=1.0, in1=A, op0=ALU.add, op1=ALU.mult)
                self.V("dve", "tensor_reduce", rd, [q], out=q[:, 128:129], in_=d1, axis=AX.X, op=ALU.max)
                self.V("dve", "tensor_scalar", rd, [q], out=m1, in0=d1, scalar1=q[:, 128:129], scalar2=1.0, op0=ALU.is_equal, op1=ALU.mult)
                self.V("dve", "tensor_tensor", rd, [q], out=m1, in0=m1, in1=self.RW[:, c, :], op=ALU.mult)
                self.V("dve", "tensor_reduce", rd, [q], out=q[:, 129:130], in_=m1, axis=AX.X, op=ALU.add)
                self.V("dve", "tensor_reduce", rd, [q], out=q[:, 130:131], in_=self.RW[:, c, :], axis=AX.X, op=ALU.add)
                self.V("dve", "tensor_scalar", rd, [q], out=m1, in0=A, scalar1=-1.0e9, scalar2=1.0e9, op0=ALU.mult, op1=ALU.add)
                self.V("dve", "tensor_tensor", rd, [q], out=m1, in0=m1, in1=dst, op=ALU.add)
                self.V("dve", "tensor_reduce", rd, [q], out=q[:, 131:132], in_=m1, axis=AX.X, op=ALU.min)
                self.V("dve", "tensor_scalar", rd, [q], out=q[:, 132:133], in0=q[:, 128:129], scalar1=-1.0, scalar2=1.0, op0=ALU.add, op1=ALU.mult)
                self.V("dve", "memset", [], [r_], r_[:], 0.0)
                for k in range(2):
                    self.V("dve", "tensor_scalar", [self.cst, r_], [r_], out=r_[:, k, 0:1], in0=self.cst[:, 1024:1025], scalar1=float(c * 128), scalar2=1.0,
                           op0=ALU.add, op1=ALU.mult)
                    self.V("dve", "tensor_scalar", [self.cst, r_], [r_], out=r_[:, k, 2:3], in0=self.cst[:, 1024:1025], scalar1=float(c * 128 + k * T), scalar2=2.0,
                           op0=ALU.add, op1=ALU.mult)
                    self.V("dve", "tensor_scalar", [r_], [r_], out=r_[:, k, 3:4], in0=r_[:, k, 2:3], scalar1=1.0, scalar2=1.0, op0=ALU.add, op1=ALU.mult)
                self.V("dve", "tensor_tensor", [q, r_], [r_], out=r_[:, 0, 1:2], in0=q[:, 130:131], in1=q[:, 129:130], op=ALU.subtract)
                self.V("dve", "tensor_copy", [q, r_], [r_], out=r_[:, 1, 1:2], in_=q[:, 129:130])
                self.V("dve", "tensor_copy", [q], [di], out=di[:, 0:1], in_=q[:, 131:132])
                self.V("dve", "tensor_copy", [q], [di], out=di[:, 1:2], in_=q[:, 132:133])
                for k in range(2):
                    S.dma_fn("pool", (lambda e, r_=r_, di=di, k=k: e.indirect_dma_start(out=self.ROWS, out_offset=bass.IndirectOffsetOnAxis(ap=di[:, k:k + 1], axis=0),
                                                                                      in_=r_[:, k, :], in_offset=None)), [r_, di], [rows_t])
            wgv = i["moe_w_gate"].rearrange("l e (p j) f -> (l e p) (j f)", j=8).rearrange("r (h x) -> (r h) x", h=2)
            wuv = i["moe_w_up"].rearrange("l e (p j) f -> (l e p) (j f)", j=8).rearrange("r (h x) -> (r h) x", h=2)
            wdv = i["moe_w_down"].rearrange("l e (p j) n -> (l e p) (j n)", j=4).rearrange("r (h x) -> (r h) x", h=2)
            wts = [(self.sb(es, "spwg%d" % k, [128, 8, 512], BF), self.sb(es, "spwu%d" % k, [128, 8, 512], BF),
                    self.sb(es, "spwd%d" % k, [128, 4, D], BF)) for k in range(2)]
            NQ = 4
            recs = [self.sb(es, "sprc%d" % k, [128, 4], F32) for k in range(NQ)]
            recu = [self.sb(es, "spru%d" % k, [128, 4], U32) for k in range(NQ)]
            hbs = [self.sb(es, "sphb%d" % k, [128, D], BF) for k in range(NQ)]
            hTs = [self.sb(es, "sphT%d" % k, [128, 8, 128], BF) for k in range(NQ)]
            sGs = [self.sb(es, "spsg%d" % k, [128, 512], F32) for k in range(2)]
            Hhs = [self.sb(es, "spHh%d" % k, [128, 512], BF) for k in range(2)]
            HhTs = [self.sb(es, "spHhT%d" % k, [128, 4, 128], BF) for k in range(2)]
            ys = [self.sb(es, "spy%d" % k, [128, D], F32) for k in range(2)]
            def gather(dst_ap, src, idx_ap, r, w, skip=False):
                if skip:
                    S.dma_fn("pool", (lambda e: e.indirect_dma_start(out=dst_ap, out_offset=None, in_=src, in_offset=bass.IndirectOffsetOnAxis(ap=idx_ap, axis=0),
                                                                     bounds_check=self._wbound_reg(e), oob_is_err=False)), r, w)
                else:
                    S.dma_fn("pool", (lambda e: e.indirect_dma_start(out=dst_ap, out_offset=None, in_=src, in_offset=bass.IndirectOffsetOnAxis(ap=idx_ap, axis=0))), r, w)

            def proA(j):
                rc, ru, hb = recs[j % NQ], recu[j % NQ], hbs[j % NQ]
                S.dma("sp", rc[:], self.ROWS[j * 128:(j + 1) * 128, :], [rows_t], [rc])
                self.V("dve", "tensor_copy", [rc], [ru], out=ru[:], in_=rc[:])
                gather(hb[:], self.H2, ru[:, 0:1], [ru, h2_t], [hb])

            def proB(j):
                hb, hT = hbs[j % NQ], hTs[j % NQ]
                pt = self.psb()
                ptv = pt[:].bitcast(BF).rearrange("p (k n) -> p k n", k=8)
                hbv = hb[:].rearrange("t (p j) -> t p j", j=8)
                for jx in range(8):
                    self.tr(ptv[:, jx, :], hbv[:, :, jx], self.identb[:], [hb, self.identb], [pt])
                self.act(hT[:], ptv, AF.Copy, [pt], [hT])

            def wload(j):
                wg, wu, wd = wts[j % 2]
                for h_ in range(2):
                    gather(wg[:, h_ * 4:(h_ + 1) * 4, :].rearrange("p j f -> p (j f)"), wgv, widx[:, h_, j:j + 1], [widx], [wg], skip=True)
                    gather(wu[:, h_ * 4:(h_ + 1) * 4, :].rearrange("p j f -> p (j f)"), wuv, widx[:, h_, j:j + 1], [widx], [wu], skip=True)
                    gather(wd[:, h_ * 2:(h_ + 1) * 2, :].rearrange("p j f -> p (j f)"), wdv, widx[:, h_, j:j + 1], [widx], [wd], skip=True)

            proA(0)
            proA(1)
            wload(0)
            proB(0)
            for j in range(NB):
                z = j % 2
                rc, ru, hT = recs[j % NQ], recu[j % NQ], hTs[j % NQ]
                sg, Hh, HhT, y = sGs[z], Hhs[z], HhTs[z], ys[z]
                wg, wu, wd = wts[z]
                if j + 2 < NB:
                    proA(j + 2)
                if j + 1 < NB:
                    wload(j + 1)
                pG, pU = self.psb(), self.psb()
                for jx in range(8):
                    self.mm(pG[:, :], hT[:, jx, :], wg[:, jx, :], jx == 0, jx == 7, [hT, wg], [pG])
                for jx in range(8):
                    self.mm(pU[:, :], hT[:, jx, :], wu[:, jx, :], jx == 0, jx == 7, [hT, wu], [pU])
                self.act(sg[:], pG[:, :], AF.Silu, [pG], [sg])
                self.V("dve", "scalar_tensor_tensor", [pU, rc, sg], [Hh], out=Hh[:], in0=pU[:, :], scalar=rc[:, 1:2], in1=sg[:], op0=ALU.mult, op1=ALU.mult)
                if j + 1 < NB:
                    proB(j + 1)
                pt2 = self.psb()
                pt2v = pt2[:].bitcast(BF)[:, 0:512].rearrange("p (k n) -> p k n", k=4)
                Hhv = Hh[:].rearrange("t (p j) -> t p j", j=4)
                for jx in range(4):
                    self.tr(pt2v[:, jx, :], Hhv[:, :, jx], self.identb[:], [Hh, self.identb], [pt2])
                self.act(HhT[:], pt2v, AF.Copy, [pt2], [HhT])
                for half in range(2):
                    pD = self.psb()
                    for jx in range(4):
                        self.mm(pD[:, :], HhT[:, jx, :], wd[:, jx, half * 512:(half + 1) * 512], jx == 0, jx == 3, [HhT, wd], [pD])
                    if half == 0:
                        self.act(y[:, 0:512], pD[:, :], AF.Copy, [pD], [y])
                    else:
                        self.V("dve", "tensor_copy", [pD], [y], out=y[:, 512:1024], in_=pD[:, :])
                for h_ in range(2):
                    S.dma_fn("pool", (lambda e, y=y, ru=ru, h_=h_: e.indirect_dma_start(out=self.ACC2.rearrange("r (h x) -> (r h) x", h=2),
                                                                                        out_offset=bass.IndirectOffsetOnAxis(ap=ru[:, 2 + h_:3 + h_], axis=0),
                                                                                        in_=y[:, h_ * 512:(h_ + 1) * 512], in_offset=None,
                                                                                        bounds_check=self._bound_reg(e), oob_is_err=False)), [y, ru], [acc_t])
            self.residual_setup(es, 5)
            a0 = [self.sb(es, "spa0%d" % k, [128, D], F32) for k in range(3)]
            a1 = [self.sb(es, "spa1%d" % k, [128, D], F32) for k in range(3)]
            for c in range(NCH):
                u0, u1 = a0[c % 3], a1[c % 3]
                S.dma("sp", u0[:], self.ACC2[c * 128:(c + 1) * 128, :], [acc_t], [u0])
                S.dma("sp", u1[:], self.ACC2[T + c * 128:T + (c + 1) * 128, :], [acc_t], [u1])
                self.V("dve", "tensor_tensor", [u0, u1], [u0], out=u0[:], in0=u0[:], in1=u1[:], op=ALU.add)
                self.residual(c, lambda half, u0=u0: u0[:, half * 512:(half + 1) * 512], [u0, u0])
            S.barrier()

    def _wbound_reg(self, e):
        if getattr(self, "_wbreg", None) is None:
            self._wbreg = e.to_reg(self.n_layers * 32 * 128 * 2 - 1)
        return self._wbreg

    def _bound_reg(self, e):
        if getattr(self, "_breg", None) is None:
            self._breg = e.to_reg(4 * T - 1)
        return self._breg

    def final_norm(self):
        S = self.S
        with contextlib.ExitStack() as es:
            g = self.sb(es, "fng", [128, D], F32)
            S.dma("sp", g[:], self.i["final_g"].partition_broadcast(128), [], [g])
            xs = [self.sb(es, "fnx%d" % k, [128, D], F32) for k in range(3)]
            sq = self.sb(es, "fnsq", [128, D], F32)
            st = [self.sb(es, "fnst%d" % k, [128, 4], F32) for k in range(2)]
            ot = [self.sb(es, "fno%d" % k, [128, D], F32) for k in range(2)]
            for c in range(2, NCH):
                x = xs[c % 3]
                S.dma("sp", x[:], self.X[c * 128:(c + 1) * 128, :], [Tl(None, self.Xt[c])], [x])
                s_ = st[c % 2]
                self.act(sq[:], x[:], AF.Square, [x], [sq, s_], accum_out=s_[:, 0:1])
                self.V("dve", "tensor_scalar", [s_], [s_], out=s_[:, 1:2], in0=s_[:, 0:1], scalar1=1.0 / D, scalar2=EPS, op0=ALU.mult, op1=ALU.add)
                self.V("dve", "reciprocal", [s_], [s_], out=s_[:, 2:3], in_=s_[:, 1:2])
                self.act(s_[:, 3:4], s_[:, 2:3], AF.Sqrt, [s_], [s_])
                o = ot[c % 2]
                self.V("dve", "scalar_tensor_tensor", [x, s_, g], [o], out=o[:], in0=x[:], scalar=s_[:, 3:4], in1=g[:], op0=ALU.mult, op1=ALU.mult)
                S.dma("sp", self.out[(c - 2) * 128:(c - 1) * 128, :], o[:], [o], [Tl(None, self.outt)])


def make_consts():
    cst = np.zeros((128, 1056), np.float32)
    cst[:, 0:128] = np.eye(128, dtype=np.float32)
    cst[:, 128:256] = 1.0
    R = np.zeros((128, 128), np.float32)
    for blk in range(2):
        o = blk * 64
        for q in range(16):
            R[o + 16 + q, o + q] = -1.0
            R[o + q, o + 16 + q] = 1.0
            R[o + 48 + q, o + 32 + q] = -1.0
            R[o + 32 + q, o + 48 + q] = 1.0
    cst[:, 256:384] = R
    s = np.arange(128)[:, None]
    t = np.arange(128)[None, :]
    same = (s // 64) == (t // 64)
    same32 = (s // 32) == (t // 32)
    cst[:, 384:512] = (same32 & (t >= s)).astype(np.float32)
    cst[:, 512:640] = (same32 & (t <= s)).astype(np.float32)
    cst[:, 640:768] = (same & (s % 64 < 32) & (t % 64 >= 32)).astype(np.float32)
    cst[:, 768:896] = (same & (s % 64 >= 32) & (t % 64 < 32)).astype(np.float32)
    cst[:, 896:1024] = 128.0 * np.arange(128, dtype=np.float32)[None, :]
    cst[:, 1024] = np.arange(128, dtype=np.float32)
    cst[:, 1025:1057 - 1 + 0] = 0.0
    inv_freq = (10000.0 ** (-np.arange(0, 32, 2, dtype=np.float32) / 32)).astype(np.float32)
    pos = np.arange(NLAT)
    row = (pos // 64).astype(np.float32)
    col = (pos % 64).astype(np.float32)
    ang_r = row[:, None] * inv_freq
    ang_c = col[:, None] * inv_freq
    ang = np.concatenate([ang_r, ang_r, ang_c, ang_c], axis=-1).astype(np.float32)
    rope = np.zeros((2, 128, T), np.float32)
    rope[0, :, :NCTX] = 1.0
    rope[0, 0:64, NCTX:] = np.cos(ang).T
    rope[0, 64:128, NCTX:] = np.cos(ang).T
    rope[1, 0:64, NCTX:] = np.sin(ang).T
    rope[1, 64:128, NCTX:] = np.sin(ang).T
    return cst, rope


def make_in_maps(inputs, cores, L=DEPTH, nexp=32):
    f = lambda a: np.ascontiguousarray(np.asarray(a, dtype=np.float32))
    cst, rope = make_consts()
    shared = {
        "c_ctx": f(inputs["c_ctx"]).reshape(1, D),
        "ada_w": f(inputs["ada_w"][:L]), "ada_b": f(inputs["ada_b"]),
        "norm1_g": f(inputs["norm1_g"]), "norm2_g": f(inputs["norm2_g"]),
        "w_in": f(inputs["w_in"][:L]), "w_branch": f(inputs["w_branch"][:L]), "w_out": f(inputs["w_out"][:L]),
        "hg_lb": f(inputs["hg_lb_logits"]), "hg_norm_g": f(inputs["hg_norm_g"]),
        "da_lambda": f(inputs["da_lambda"]).reshape(DEPTH, 256), "da_norm_g": f(inputs["da_norm_g"]),
        "cv_dw_w": f(inputs["cv_dw_w"]), "cv_dw_b": f(inputs["cv_dw_b"]),
        "cv_ln_g": f(inputs["cv_ln_g"]), "cv_ln_b": f(inputs["cv_ln_b"]),
        "sc_w": f(inputs["sc_w"]),
        "moe_w_r": np.ascontiguousarray(np.concatenate([f(inputs["moe_w_grp"]), f(inputs["moe_w_exp"])], axis=-1)),
        "moe_b_r": np.ascontiguousarray(np.concatenate([f(inputs["moe_b_grp"]), f(inputs["moe_b_exp"])], axis=-1)),
        "moe_w_gate": f(inputs["moe_w_gate"][:L, :nexp]), "moe_w_up": f(inputs["moe_w_up"][:L, :nexp]), "moe_w_down": f(inputs["moe_w_down"][:L, :nexp]),
        "final_g": f(inputs["final_g"]).reshape(1, D),
        "cst": cst, "rope": rope, "ltri": np.triu(np.ones((128, 128), np.float32), 1),
    }
    maps = []
    for cid in cores:
        b = cid % 4
        m = dict(shared)
        m["x"] = f(inputs["x"][b])
        m["c"] = f(inputs["c"][b]).reshape(1, D)
        m["ctx"] = f(inputs["ctx"][b])
        maps.append(m)
    return maps


def kernel(**inputs):
    nc = bass.Bass("TRN2", target_bir_lowering=False)
    Prog(nc).build()
    maps = make_in_maps(inputs, list(range(4)))
    res = run_bass_kernel_spmd(nc, maps, core_ids=list(range(4)))
    return np.stack([np.asarray(res.results[b]["out"], dtype=np.float32) for b in range(4)], axis=0)
```
